# Optimizing a Trainium2 kernel written in Bass

```python
import jax, jax.numpy as jnp
from jax import lax
import numpy as np

D_MODEL = 2048
BATCH = 16
SEQ = 2048
DEPTH = 2

CTX_LEN = 256
GRID_W = 64
MIX_WIDTH = D_MODEL // 2
HG_HEAD_DIM = 128
HG_HEADS = MIX_WIDTH // HG_HEAD_DIM
HG_CHUNK = 64
ATTN_HEAD_DIM = 64
ATTN_HEADS = MIX_WIDTH // ATTN_HEAD_DIM
ATTN_KV_HEADS = ATTN_HEADS // 8
ATTN_GROUP = ATTN_HEADS // ATTN_KV_HEADS
KV_WIDTH = ATTN_KV_HEADS * ATTN_HEAD_DIM
WINDOW = 128
ATTN_BLOCK = 128
ROPE_THETA = 10000.0
D_FF = ((8 * D_MODEL // 3 + 255) // 256) * 256
N_EXPERTS = 8
TOP_K = 2
D_EXPERT = 7 * D_MODEL // 2
EPS = 1e-6
IN_WIDTHS = (MIX_WIDTH, MIX_WIDTH, MIX_WIDTH, MIX_WIDTH, MIX_WIDTH, MIX_WIDTH, KV_WIDTH, KV_WIDTH, D_MODEL, D_MODEL)
N_IN = sum(IN_WIDTHS)

kernel_name = 'hybrid_hgrn2_swa_moe_flow_block'


def _rms(x):
    xf = x.astype(jnp.float32)
    return (xf * lax.rsqrt(jnp.mean(xf * xf, axis=-1, keepdims=True) + EPS)).astype(x.dtype)


def rmsnorm(x, g):
    return _rms(x) * g


def modulate(h, shift, scale):
    return h * (1.0 + scale) + shift


def heads(t, n):
    return t.reshape(*t.shape[:-1], n, t.shape[-1] // n)


def swiglu(h, wg, wu, wd):
    return (jax.nn.silu(h @ wg) * (h @ wu)) @ wd


def moe_swiglu(h, router, wg, wu, wd):
    logits = (h @ router).astype(jnp.float32)
    top_v, top_i = lax.top_k(logits, TOP_K)
    w = jax.nn.softmax(top_v, axis=-1)
    combine = jnp.sum(jax.nn.one_hot(top_i, N_EXPERTS, dtype=jnp.float32) * w[..., None], axis=-2).astype(h.dtype)
    out = jnp.zeros_like(h)
    for e in range(N_EXPERTS):
        out = out + combine[..., e:e + 1] * swiglu(h, wg[e], wu[e], wd[e])
    return out


def channel_mixer(h, layer, ffn_w_gate, ffn_w_up, ffn_w_down, moe_router, moe_w_gate, moe_w_up, moe_w_down):
    i = layer // 2
    if layer % 2 == 0:
        return swiglu(h, ffn_w_gate[i], ffn_w_up[i], ffn_w_down[i])
    return moe_swiglu(h, moe_router[i], moe_w_gate[i], moe_w_up[i], moe_w_down[i])


def rope_tables(length):
    rows = length // GRID_W
    row = jnp.repeat(jnp.arange(rows, dtype=jnp.float32), GRID_W)
    col = jnp.tile(jnp.arange(GRID_W, dtype=jnp.float32), rows)
    n_freq = ATTN_HEAD_DIM // 4
    inv = ROPE_THETA ** (-jnp.arange(n_freq, dtype=jnp.float32) / n_freq)
    ang = jnp.stack([row[:, None] * inv, col[:, None] * inv], axis=1)
    return jnp.cos(ang), jnp.sin(ang)


def apply_rope(t, cos, sin):
    tr = t.reshape(*t.shape[:-1], 2, 2, ATTN_HEAD_DIM // 4)
    x1, x2 = tr[..., 0, :], tr[..., 1, :]
    cs, sn = cos[None, :, None], sin[None, :, None]
    out = jnp.stack([x1 * cs - x2 * sn, x2 * cs + x1 * sn], axis=-2)
    return out.reshape(t.shape).astype(t.dtype)


def forget_gate(f_raw, lb):
    f = lb + (1.0 - lb) * jax.nn.sigmoid(f_raw.astype(jnp.float32))
    return heads(jnp.log(f), HG_HEADS), heads(1.0 - f, HG_HEADS)


def lower_bound(raw, layer):
    p = jax.nn.softmax(raw.astype(jnp.float32), axis=0)
    return (jnp.cumsum(p, axis=0) - p[0])[layer]


def hgrn2_chunk_scan(q, k, v, log_f, s0):
    b_, length, h, _ = q.shape
    n_chunks = length // HG_CHUNK

    def chunks(t):
        return t.reshape(b_, n_chunks, HG_CHUNK, h, t.shape[-1]).transpose(1, 0, 3, 2, 4)

    lower = jnp.tril(jnp.ones((HG_CHUNK, HG_CHUNK), dtype=bool))[:, :, None]

    def step(state, inp):
        qc, kc, vc, gc = inp
        b = jnp.cumsum(gc, axis=2)
        o_inter = jnp.einsum('bhtd,bhdv->bhtv', qc * jnp.exp(b), state)
        decay = jnp.exp(jnp.where(lower, b[:, :, :, None, :] - b[:, :, None, :, :], -jnp.inf))
        scores = jnp.einsum('bhtd,bhsd,bhtsd->bhts', qc, kc, decay)
        o = o_inter + jnp.einsum('bhts,bhsv->bhtv', scores, vc)
        b_last = b[:, :, -1:, :]
        state = jnp.exp(b_last[:, :, 0, :])[..., None] * state + jnp.einsum('bhsd,bhsv->bhdv', kc * jnp.exp(b_last - b), vc)
        return state, o

    s_fin, o = lax.scan(step, s0, (chunks(q), chunks(k), chunks(v), chunks(log_f)))
    o = o.transpose(1, 0, 3, 2, 4).reshape(b_, length, h, v.shape[-1])
    return o, s_fin


def _orient(ts, reverse):
    return tuple((jnp.flip(t, axis=1) if reverse else t).astype(jnp.float32) for t in ts)


def hgrn2_direction(ctx_in, lat_in, reverse):
    ctx_in, lat_in = _orient(ctx_in, reverse), _orient(lat_in, reverse)
    s0 = jnp.zeros((ctx_in[0].shape[0], HG_HEADS, HG_HEAD_DIM, HG_HEAD_DIM), jnp.float32)
    o_ctx, s_ctx = hgrn2_chunk_scan(*ctx_in, s0)
    o_lat, _ = hgrn2_chunk_scan(*lat_in, s_ctx)
    if reverse:
        o_ctx, o_lat = jnp.flip(o_ctx, axis=1), jnp.flip(o_lat, axis=1)
    return o_ctx, o_lat


def window_attention(q, k, v, k_ctx, v_ctx, sink):
    b_, length = q.shape[:2]
    nb = length // ATTN_BLOCK
    n_side = -(-WINDOW // ATTN_BLOCK)
    kw = (2 * n_side + 1) * ATTN_BLOCK
    qb = q.reshape(b_, nb, ATTN_BLOCK, ATTN_KV_HEADS, ATTN_GROUP, ATTN_HEAD_DIM)
    pad = ((0, 0), (n_side * ATTN_BLOCK, n_side * ATTN_BLOCK), (0, 0), (0, 0))

    def banded(t):
        tp = jnp.pad(t, pad).reshape(b_, nb + 2 * n_side, ATTN_BLOCK, ATTN_KV_HEADS, ATTN_HEAD_DIM)
        return jnp.concatenate([tp[:, s:s + nb] for s in range(2 * n_side + 1)], axis=2)

    kb, vb = banded(k), banded(v)
    start = jnp.arange(nb)[:, None] * ATTN_BLOCK
    q_pos = start + jnp.arange(ATTN_BLOCK)[None, :]
    k_pos = start - n_side * ATTN_BLOCK + jnp.arange(kw)[None, :]
    valid = ((jnp.abs(q_pos[:, :, None] - k_pos[:, None, :]) <= WINDOW)
             & (k_pos[:, None, :] >= 0) & (k_pos[:, None, :] < length))
    s_win = jnp.einsum('bnqgrd,bnkgd->bngrqk', qb, kb).astype(jnp.float32)
    s_win = jnp.where(valid[None, :, None, None], s_win, -jnp.inf)
    s_ctx = jnp.einsum('bnqgrd,bcgd->bngrqc', qb, k_ctx).astype(jnp.float32)
    s_sink = jnp.broadcast_to(sink.astype(jnp.float32).reshape(1, 1, ATTN_KV_HEADS, ATTN_GROUP, 1, 1),
                              s_ctx.shape[:-1] + (1,))
    p = jax.nn.softmax(jnp.concatenate([s_sink, s_ctx, s_win], axis=-1), axis=-1).astype(v.dtype)
    n_ctx = k_ctx.shape[1]
    out = (jnp.einsum('bngrqc,bcgd->bnqgrd', p[..., 1:1 + n_ctx], v_ctx)
           + jnp.einsum('bngrqk,bnkgd->bnqgrd', p[..., 1 + n_ctx:], vb))
    return out.reshape(b_, length, ATTN_HEADS * ATTN_HEAD_DIM)


def context_attention(q, k, v, sink):
    b_, n_ctx = q.shape[:2]
    qc = q.reshape(b_, n_ctx, ATTN_KV_HEADS, ATTN_GROUP, ATTN_HEAD_DIM)
    s = jnp.einsum('bqgrd,bkgd->bgrqk', qc, k).astype(jnp.float32)
    s_sink = jnp.broadcast_to(sink.astype(jnp.float32).reshape(1, ATTN_KV_HEADS, ATTN_GROUP, 1, 1), s.shape[:-1] + (1,))
    p = jax.nn.softmax(jnp.concatenate([s_sink, s], axis=-1), axis=-1).astype(v.dtype)
    out = jnp.einsum('bgrqk,bkgd->bqgrd', p[..., 1:], v)
    return out.reshape(b_, n_ctx, ATTN_HEADS * ATTN_HEAD_DIM)


def branch_merge(o_hg, g, o_attn, gate_a, gate_b, hg_gain, w_a, w_b, w_o):
    a = _rms(o_hg).reshape(g.shape) * hg_gain * jax.nn.silu(g)
    y = jax.nn.sigmoid(gate_a) * (a @ w_a) + jax.nn.sigmoid(gate_b) * (o_attn @ w_b)
    return y @ w_o


def token_mixer(h_ctx, h_lat, cos, sin, w_in, lb_fwd, lb_bwd, hg_gain, sink, w_a, w_b, w_o, need_ctx):
    splits = [sum(IN_WIDTHS[:i + 1]) for i in range(len(IN_WIDTHS) - 1)]
    pc = jnp.split(h_ctx @ w_in, splits, axis=-1)
    pl = jnp.split(h_lat @ w_in, splits, axis=-1)

    def hgrn_inputs(p):
        q = heads(p[0], HG_HEADS) * HG_HEAD_DIM ** -0.5
        v = heads(p[3], HG_HEADS)
        lf_f, k_f = forget_gate(p[1], lb_fwd)
        lf_b, k_b = forget_gate(p[2], lb_bwd)
        return (q, k_f, v, lf_f), (q, k_b, v, lf_b)

    c_fwd, c_bwd = hgrn_inputs(pc)
    l_fwd, l_bwd = hgrn_inputs(pl)
    oc_f, ol_f = hgrn2_direction(c_fwd, l_fwd, reverse=False)
    oc_b, ol_b = hgrn2_direction(c_bwd, l_bwd, reverse=True)

    scale = ATTN_HEAD_DIM ** -0.5
    k_ctx, v_ctx = heads(pc[6], ATTN_KV_HEADS), heads(pc[7], ATTN_KV_HEADS)
    q_lat = apply_rope(heads(pl[5], ATTN_HEADS), cos, sin) * scale
    k_lat = apply_rope(heads(pl[6], ATTN_KV_HEADS), cos, sin)
    a_lat = window_attention(q_lat, k_lat, heads(pl[7], ATTN_KV_HEADS), k_ctx, v_ctx, sink)
    y_lat = branch_merge((ol_f + ol_b).astype(h_lat.dtype), pl[4], a_lat, pl[8], pl[9], hg_gain, w_a, w_b, w_o)
    if not need_ctx:
        return y_lat, None
    a_ctx = context_attention(heads(pc[5], ATTN_HEADS) * scale, k_ctx, v_ctx, sink)
    y_ctx = branch_merge((oc_f + oc_b).astype(h_ctx.dtype), pc[4], a_ctx, pc[8], pc[9], hg_gain, w_a, w_b, w_o)
    return y_lat, y_ctx


def setup_inputs(seed: int = 0) -> dict:
    key = jax.random.key(seed)
    ks = jax.random.split(key, 24)
    n_dense = (DEPTH + 1) // 2
    n_moe = DEPTH // 2

    def nrm(k, shape, scale=1.0):
        return jax.random.normal(k, shape, jnp.float32) * scale

    return {
        'x': nrm(ks[0], (BATCH, SEQ, D_MODEL)),
        'c': nrm(ks[1], (BATCH, D_MODEL)),
        'ctx': nrm(ks[2], (BATCH, CTX_LEN, D_MODEL)),
        'c_ctx': nrm(ks[3], (D_MODEL,)),
        'w_mod': nrm(ks[4], (DEPTH, D_MODEL, 6 * D_MODEL), 0.5 * D_MODEL ** -0.5),
        'b_mod': nrm(ks[5], (DEPTH, 6 * D_MODEL), 0.02),
        'norm_mix': 1.0 + nrm(ks[6], (DEPTH, D_MODEL), 0.02),
        'norm_ffn': 1.0 + nrm(ks[7], (DEPTH, D_MODEL), 0.02),
        'w_in': nrm(ks[8], (DEPTH, D_MODEL, N_IN), D_MODEL ** -0.5),
        'hg_lb_fwd': nrm(ks[9], (DEPTH, MIX_WIDTH)),
        'hg_lb_bwd': nrm(ks[10], (DEPTH, MIX_WIDTH)),
        'hg_norm': 1.0 + nrm(ks[11], (DEPTH, MIX_WIDTH), 0.02),
        'attn_sink': nrm(ks[12], (DEPTH, ATTN_HEADS), 0.5),
        'w_branch_a': nrm(ks[13], (DEPTH, MIX_WIDTH, D_MODEL), MIX_WIDTH ** -0.5),
        'w_branch_b': nrm(ks[14], (DEPTH, MIX_WIDTH, D_MODEL), MIX_WIDTH ** -0.5),
        'w_out': nrm(ks[15], (DEPTH, D_MODEL, D_MODEL), D_MODEL ** -0.5),
        'ffn_w_gate': nrm(ks[16], (n_dense, D_MODEL, D_FF), D_MODEL ** -0.5),
        'ffn_w_up': nrm(ks[17], (n_dense, D_MODEL, D_FF), D_MODEL ** -0.5),
        'ffn_w_down': nrm(ks[18], (n_dense, D_FF, D_MODEL), D_FF ** -0.5),
        'moe_router': nrm(ks[19], (n_moe, D_MODEL, N_EXPERTS), D_MODEL ** -0.5),
        'moe_w_gate': nrm(ks[20], (n_moe, N_EXPERTS, D_MODEL, D_EXPERT), D_MODEL ** -0.5),
        'moe_w_up': nrm(ks[21], (n_moe, N_EXPERTS, D_MODEL, D_EXPERT), D_MODEL ** -0.5),
        'moe_w_down': nrm(ks[22], (n_moe, N_EXPERTS, D_EXPERT, D_MODEL), D_EXPERT ** -0.5),
        'final_norm': 1.0 + nrm(ks[23], (D_MODEL,), 0.02),
    }


def reference(x, c, ctx, c_ctx, w_mod, b_mod, norm_mix, norm_ffn, w_in, hg_lb_fwd, hg_lb_bwd, hg_norm,
              attn_sink, w_branch_a, w_branch_b, w_out, ffn_w_gate, ffn_w_up, ffn_w_down,
              moe_router, moe_w_gate, moe_w_up, moe_w_down, final_norm):
    cos, sin = rope_tables(x.shape[1])
    c_act, c_ctx_act = jax.nn.silu(c), jax.nn.silu(c_ctx)
    for layer in range(DEPTH):
        need_ctx = layer < DEPTH - 1
        mod_lat = jnp.split((c_act @ w_mod[layer] + b_mod[layer])[:, None, :], 6, axis=-1)
        mod_ctx = jnp.split(c_ctx_act @ w_mod[layer] + b_mod[layer], 6, axis=-1)
        h_lat = modulate(rmsnorm(x, norm_mix[layer]), mod_lat[0], mod_lat[1])
        h_ctx = modulate(rmsnorm(ctx, norm_mix[layer]), mod_ctx[0], mod_ctx[1])
        y_lat, y_ctx = token_mixer(h_ctx, h_lat, cos, sin, w_in[layer],
                                   lower_bound(hg_lb_fwd, layer), lower_bound(hg_lb_bwd, layer),
                                   hg_norm[layer], attn_sink[layer], w_branch_a[layer], w_branch_b[layer],
                                   w_out[layer], need_ctx)
        x = x + mod_lat[2] * y_lat
        h_lat = modulate(rmsnorm(x, norm_ffn[layer]), mod_lat[3], mod_lat[4])
        x = x + mod_lat[5] * channel_mixer(h_lat, layer, ffn_w_gate, ffn_w_up, ffn_w_down,
                                           moe_router, moe_w_gate, moe_w_up, moe_w_down)
        if need_ctx:
            ctx = ctx + mod_ctx[2] * y_ctx
            h_ctx = modulate(rmsnorm(ctx, norm_ffn[layer]), mod_ctx[3], mod_ctx[4])
            ctx = ctx + mod_ctx[5] * channel_mixer(h_ctx, layer, ffn_w_gate, ffn_w_up, ffn_w_down,
                                                   moe_router, moe_w_gate, moe_w_up, moe_w_down)
    return rmsnorm(x, final_norm)
```

```python
import numpy as np
from contextlib import ExitStack
import concourse.bass as bass
import concourse.mybir as mybir
from concourse.bass_utils import run_bass_kernel_spmd

F32 = mybir.dt.float32
BF16 = mybir.dt.bfloat16
I32 = mybir.dt.int32
AF = mybir.ActivationFunctionType
ALU = mybir.AluOpType
AX = mybir.AxisListType

EPS = 1e-6
NEG = -30000.0


class Res:
    __slots__ = ("name", "last_w", "readers")

    def __init__(self, name=""):
        self.name = name
        self.last_w = None
        self.readers = {}


class Sched:
    COMPUTE = ("pe", "act", "dve", "pool")
    NDMA = 8

    def __init__(self, nc, es):
        self.nc = nc
        self.sem = {}
        for e in self.COMPUTE:
            self.sem[e] = es.enter_context(nc.semaphore("sem_" + e))
        self.count = {e: 0 for e in self.COMPUTE}
        self.engs = ("pe", "act", "dve", "pool", "sp")
        self.seen = {e: {} for e in self.engs}
        self.q = {e: [] for e in self.engs}
        self.dma_uses = {}
        self.dma_i = {}
        for e in ("sp", "act", "pool"):
            for i in range(self.NDMA):
                self.sem[("d", e, i)] = es.enter_context(nc.semaphore("dsem_%s%d" % (e, i)))
            self.dma_uses[e] = [0] * self.NDMA
            self.dma_i[e] = 0
        self.ninstr = 0

    def _deps(self, reads, writes):
        deps = {}
        for r in reads:
            ev = r.last_w
            if ev is not None and deps.get(ev[0], 0) < ev[1]:
                deps[ev[0]] = ev[1]
        for w in writes:
            ev = w.last_w
            if ev is not None and deps.get(ev[0], 0) < ev[1]:
                deps[ev[0]] = ev[1]
            for k, v in w.readers.items():
                if deps.get(k, 0) < v:
                    deps[k] = v
        return deps

    def _waits(self, eng, deps, skip=None):
        waits = []
        seen = self.seen[eng]
        for k, v in deps.items():
            if k == skip or seen.get(k, 0) >= v:
                continue
            seen[k] = v
            waits.append((k, v))
        return waits

    def _commit(self, ev, reads, writes):
        k, v = ev
        for r in reads:
            if r.readers.get(k, 0) < v:
                r.readers[k] = v
        for w in writes:
            w.last_w = ev
            w.readers = {}

    def op(self, eng, fn, reads=(), writes=(), signal=True):
        deps = self._deps(reads, writes)
        waits = self._waits(eng, deps, skip=("pe" if eng == "pe" else None))
        sem = self.sem
        if signal:
            self.count[eng] += 1
            ev = (eng, self.count[eng])
            own = sem[eng]
        else:
            ev = (eng, self.count[eng] + 1)
            own = None

        def run(e, waits=waits, fn=fn, own=own):
            for k, v in waits:
                e.wait_ge(sem[k], v)
            ins = fn(e)
            if own is not None:
                ins.then_inc(own, 1)
        self.q[eng].append(run)
        self._commit(ev, reads, writes)
        self.ninstr += 1
        return ev

    def dma(self, eng, fn, reads=(), writes=()):
        i = self.dma_i[eng] % self.NDMA
        self.dma_i[eng] += 1
        key = ("d", eng, i)
        prev = 16 * self.dma_uses[eng][i]
        self.dma_uses[eng][i] += 1
        ev = (key, prev + 16)
        deps = self._deps(reads, writes)
        if prev > 0 and deps.get(key, 0) < prev:
            deps[key] = prev
        waits = self._waits(eng, deps)
        sem = self.sem
        own = sem[key]

        def run(e, waits=waits, fn=fn, own=own):
            for k, v in waits:
                e.wait_ge(sem[k], v)
            fn(e).then_inc(own, 16)
        self.q[eng].append(run)
        self._commit(ev, reads, writes)
        self.ninstr += 1
        return ev

    def barrier(self):
        targets = {}
        for e in self.COMPUTE:
            if self.count[e] > 0:
                targets[e] = self.count[e]
        for e in ("sp", "act", "pool"):
            for i in range(self.NDMA):
                if self.dma_uses[e][i] > 0:
                    targets[("d", e, i)] = 16 * self.dma_uses[e][i]
        sem = self.sem
        for eng in self.engs:
            waits = self._waits(eng, targets, skip=(eng if eng in self.COMPUTE else None))
            if waits:
                def run(e, waits=waits):
                    for k, v in waits:
                        e.wait_ge(sem[k], v)
                self.q[eng].append(run)

    def flush(self):
        nc = self.nc
        q = self.q
        with nc.Block() as block:
            if q["pe"]:
                @block.tensor
                def _(e):
                    for f in q["pe"]:
                        f(e)
            if q["act"]:
                @block.scalar
                def _(e):
                    for f in q["act"]:
                        f(e)
            if q["dve"]:
                @block.vector
                def _(e):
                    for f in q["dve"]:
                        f(e)
            if q["pool"]:
                @block.gpsimd
                def _(e):
                    for f in q["pool"]:
                        f(e)
            if q["sp"]:
                @block.sync
                def _(e):
                    for f in q["sp"]:
                        f(e)
        self.q = {e: [] for e in self.engs}


_uid = [0]


class TPool:
    def __init__(self, es, nc, name, shape, dtype, n, psum=False):
        self.tiles = []
        for i in range(n):
            _uid[0] += 1
            nm = "%s_%d" % (name, _uid[0])
            mk = nc.psum_tensor if psum else nc.sbuf_tensor
            t = es.enter_context(mk(nm, list(shape), dtype))
            self.tiles.append((t, Res(nm)))
        self.i = 0

    def next(self):
        t = self.tiles[self.i % len(self.tiles)]
        self.i += 1
        return t


def tile1(es, nc, name, shape, dtype, psum=False):
    return TPool(es, nc, name, shape, dtype, 1, psum).tiles[0]


class Cfg:
    def __init__(self, NSEQ=2, SEQ=2048, CTX=256, DFF=5632, DEXP=7168, MOE="dense", DEBUG=False, STOP=None):
        self.DEBUG = DEBUG
        self.STOP = STOP
        self.D = 2048
        self.KC = 16
        self.NSEQ = NSEQ
        self.NR = NSEQ + 1
        self.SEQ = SEQ
        self.CTX = CTX
        self.T = CTX + SEQ
        self.NT = self.T // 128
        self.NCT = CTX // 128
        self.DFF = DFF
        self.DEXP = DEXP
        self.NE = 8
        self.NIN = 10496
        self.NCH = self.T // 64
        self.NCC = CTX // 64
        self.MOE = MOE
        self.PUMP = 7
        self.NSLOT = (2 * NSEQ * SEQ) // 512 + 8
        self.tblocks = [(0, CTX)] + [(CTX + i * 512, 512) for i in range(SEQ // 512)]
        self.lat_blocks = self.tblocks[1:]
        self.groups = [list(range(0, self.NCC))] + [list(range(self.NCC + 8 * i, self.NCC + 8 * i + 8))
                                                    for i in range((SEQ // 64) // 8)]


COL = dict(hq=0, ff=1024, fb=2048, hv=3072, hg=4096, aq=5120, ak=6144, av=6272, ga=6400, gb=8448)


def host_consts(cfg):
    c = {}
    c["ident"] = np.eye(128, dtype=np.float32)
    i = np.arange(64)
    sb_, tb_ = i[:, None] // 16, i[None, :] // 16
    c["maskD_f"] = ((i[:, None] <= i[None, :]) & (sb_ == tb_)).astype(np.float32)
    c["maskO_f"] = (sb_ < tb_).astype(np.float32)
    c["maskD_b"] = ((i[:, None] >= i[None, :]) & (sb_ == tb_)).astype(np.float32)
    c["maskO_b"] = (sb_ > tb_).astype(np.float32)
    j = np.arange(128)
    c["mneg_prev"] = np.where(j[None, :] <= j[:, None], 0.0, NEG).astype(np.float32)
    c["mneg_next"] = np.where(j[:, None] <= j[None, :], 0.0, NEG).astype(np.float32)
    c["ustrict"] = (j[:, None] < j[None, :]).astype(np.float32)
    t = np.arange(cfg.SEQ)
    row = (t // 64).astype(np.float32)
    col = (t % 64).astype(np.float32)
    inv = (10000.0 ** (-np.arange(16, dtype=np.float32) / 16)).astype(np.float32)
    d = np.arange(64)
    axis = d // 32
    freq = d % 16
    second = (d % 32) >= 16
    pos = np.where(axis[:, None] == 0, row[None, :], col[None, :]).astype(np.float32)
    ang = (pos * inv[freq][:, None]).astype(np.float32)
    cos = np.cos(ang).astype(np.float32)
    sin = np.sin(ang).astype(np.float32)
    sin_s = np.where(second[:, None], sin, -sin).astype(np.float32)
    c["cosT"] = np.concatenate([cos, cos], 0)
    c["sinT"] = np.concatenate([sin_s, sin_s], 0)
    pm = np.zeros((128, 128), np.float32)
    for m in range(128):
        dd = m % 64
        partner = dd - 16 if (dd % 32) >= 16 else dd + 16
        pm[(m // 64) * 64 + partner, m] = 1.0
    c["pm"] = pm
    c["ones"] = np.ones((128, 128), np.float32)
    NG = cfg.DEXP // 512
    c["jgrid"] = np.broadcast_to(np.repeat(512.0 * np.arange(cfg.NSLOT, dtype=np.float32), 8)[None, :], (128, cfg.NSLOT * 8)).copy()
    KMAX = max(1, (cfg.NSEQ * cfg.SEQ) // 512)
    c["kgrid"] = np.broadcast_to(np.tile(512.0 * np.arange(KMAX, dtype=np.float32), 8)[None, :], (128, 8 * KMAX)).copy()
    c["gp"] = (np.arange(NG, dtype=np.float32)[None, :] * 128 + np.arange(128, dtype=np.float32)[:, None]).astype(np.float32)
    return c


CONST_SHAPES = lambda cfg: dict(ident=[128, 128], maskD_f=[64, 64], maskO_f=[64, 64], maskD_b=[64, 64], maskO_b=[64, 64], mneg_prev=[128, 128],
                                mneg_next=[128, 128], ustrict=[128, 128], cosT=[128, cfg.SEQ],
                                sinT=[128, cfg.SEQ], pm=[128, 128], ones=[128, 128], jgrid=[128, cfg.NSLOT * 8], gp=[128, cfg.DEXP // 512], kgrid=[128, 8 * max(1, (cfg.NSEQ * cfg.SEQ) // 512)])


class K:
    def __init__(self, cfg):
        self.cfg = cfg
        nc = self.nc = bass.Bass("TRN2", target_bir_lowering=False)
        D = cfg.D
        T = cfg.T
        def din(name, shape, dt=F32):
            return nc.dram_tensor(name, list(shape), dt, kind="ExternalInput").ap()
        def scr(name, shape, dt):
            return nc.dram_tensor(name, list(shape), dt, kind=("ExternalOutput" if cfg.DEBUG else "Internal")).ap()
        self.x_in = din("x_in", [cfg.NSEQ, cfg.SEQ, D])
        self.ctx_in = din("ctx_in", [cfg.NSEQ, cfg.CTX, D])
        self.cvec = din("cvec", [cfg.NR, D])
        self.w_mod = din("w_mod", [2, D, 6 * D])
        self.b_mod = din("b_mod", [2, 6 * D])
        self.norm_mix = din("norm_mix", [2, D])
        self.norm_ffn = din("norm_ffn", [2, D])
        self.w_in = din("w_in", [2, D, cfg.NIN])
        self.lb_f = din("hg_lb_fwd", [2, 1024])
        self.lb_b = din("hg_lb_bwd", [2, 1024])
        self.hg_norm = din("hg_norm", [2, 1024])
        self.sink = din("attn_sink", [2, 16])
        self.w_a = din("w_branch_a", [2, 1024, D])
        self.w_b = din("w_branch_b", [2, 1024, D])
        self.w_o = din("w_out", [2, D, D])
        self.ffn_wg = din("ffn_w_gate", [1, D, cfg.DFF])
        self.ffn_wu = din("ffn_w_up", [1, D, cfg.DFF])
        self.ffn_wd = din("ffn_w_down", [1, cfg.DFF, D])
        self.router = din("moe_router", [1, D, 8])
        self.moe_wg = din("moe_w_gate", [1, 8, D, cfg.DEXP])
        self.moe_wu = din("moe_w_up", [1, 8, D, cfg.DEXP])
        self.moe_wd = din("moe_w_down", [1, 8, cfg.DEXP, D])
        self.final_norm = din("final_norm", [D])
        self.cst_d = {k: din("c_" + k, s) for k, s in CONST_SHAPES(cfg).items()}
        self.out = nc.dram_tensor("out", [cfg.NSEQ, cfg.SEQ, D], F32, kind="ExternalOutput").ap()
        self.MOD = scr("MOD", [2, cfg.NR, 6 * D], F32)
        self.XR = scr("XR", [cfg.NSEQ, T, D], F32)
        self.QT = scr("QT", [8, 128, T], BF16)
        self.GT = scr("GT", [2, 8, 128, T], F32)
        self.KT = scr("KT", [2, 8, 128, T], BF16)
        self.SGT = scr("SGT", [8, 128, T], BF16)
        self.V = scr("V", [T, 1024], BF16)
        self.QA = scr("QA", [8, 128, T], BF16)
        self.KA = scr("KA", [2, 128, T], BF16)
        self.VA = scr("VA", [T, 128], BF16)
        self.GA = scr("GA", [16, 128, T], BF16)
        self.GB = scr("GB", [16, 128, T], BF16)
        self.AT = scr("AT", [8, 128, T], BF16)
        self.OAT = scr("OAT", [8, 128, T], BF16)
        NG = cfg.DEXP // 512
        NTg = cfg.NSEQ * cfg.SEQ // 128
        if cfg.MOE == "routed":
            self.H2 = scr("H2", [cfg.NSEQ * cfg.SEQ, D], BF16)
            self.HS = scr("HS", [cfg.NSLOT * 512, D], BF16)
            self.YP = scr("YP", [cfg.NSLOT * 512, D], F32)
            self.SELD = scr("SELD", [128, NTg, 8], F32)
            self.CBD = scr("CBD", [128, NTg, 8], F32)
            self.WGB = scr("WGB", [8 * NG * 128, 16 * 512], BF16)
            self.WUB = scr("WUB", [8 * NG * 128, 16 * 512], BF16)
            self.WDB = scr("WDB", [8 * NG * 128, 4 * D], BF16)
        self.r_H2 = Res(); self.r_HS = Res(); self.r_YP = Res(); self.r_SELD = Res(); self.r_WB = Res()
        self.pre_jobs = self.prepass_jobs() if cfg.MOE == "routed" else []
        self.dbg = {}
        self.r_MOD = Res('MOD')
        self.r_out = Res('out')
        self.r_QT = [Res() for _ in range(8)]
        self.r_SGT = [Res() for _ in range(8)]
        self.r_GA = [Res() for _ in range(16)]
        self.r_GB = [Res() for _ in range(16)]
        self.r_GT = [[Res() for _ in range(8)] for _ in range(2)]
        self.r_KT = [[Res() for _ in range(8)] for _ in range(2)]
        self.r_QA = [Res() for _ in range(8)]
        self.r_KA = [Res() for _ in range(2)]
        self.r_V = Res()
        self.r_VA = Res()
        self.r_AT = [Res() for _ in range(8)]
        self.r_OAT = [Res() for _ in range(8)]
        self.r_XR = [[Res('XR') for _ in range(cfg.NT)] for _ in range(cfg.NSEQ)]

    def xsrc(self, first, s, tile):
        cfg = self.cfg
        if first:
            if tile < cfg.NCT:
                return self.ctx_in[s, tile * 128:(tile + 1) * 128, :]
            tt = tile - cfg.NCT
            return self.x_in[s, tt * 128:(tt + 1) * 128, :]
        return self.XR[s, tile * 128:(tile + 1) * 128, :]

    def load_wblock(self, pool, w2d, n0, nn, nk=None, k0=0, eng="pool"):
        S = self.S
        nk = nk if nk is not None else w2d.shape[0] // 128
        t, r = pool.next()
        src = w2d[k0 * 128:(k0 + nk) * 128, n0:n0 + nn].rearrange("(kc p) n -> p kc n", p=128)
        S.dma(eng, lambda e: e.dma_start(out=t[:, 0:nk, 0:nn], in_=src), writes=[r])
        return t, r

    def bcast_row(self, t, r, row_ap, n, eng="sp"):
        self.S.dma(eng, lambda e: e.dma_start(out=t[:, 0:n], in_=row_ap.partition_broadcast(128)), writes=[r])

    def build(self):
        cfg = self.cfg
        nc = self.nc
        with ExitStack() as es:
            self.S = S = Sched(nc, es)
            es.enter_context(nc.allow_non_contiguous_dma(reason='small strided layout loads'))
            self.C = {}
            for k in ("ident", "mneg_prev", "mneg_next", "ustrict", "pm", "ones"):
                shp = CONST_SHAPES(cfg)[k]
                t, r = tile1(es, nc, "c_" + k, shp, BF16)
                S.dma("pool", lambda e, t=t, k=k: e.dma_start(out=t[:], in_=self.cst_d[k][:, :]), writes=[r])
                self.C[k] = (t, r)
            stop = cfg.STOP
            self.phase_mod()
            if stop == "mod":
                return nc
            for l in range(2):
                for s in range(cfg.NSEQ):
                    with ExitStack() as bs:
                        self.BIG = tile1(bs, nc, "BIG", [128, cfg.KC, cfg.T], BF16)
                        self.phase_norm(l, s, 1)
                        if stop == "norm":
                            self.dump("BIG", self.BIG[0], self.BIG[1], [128, cfg.KC, cfg.T], BF16)
                            self.end_phase()
                            return nc
                        self.phase_inproj(l, s)
                    if stop == "inproj":
                        return nc
                    self.phase_scan(l, s)
                    if stop in ("scan", "scanprep"):
                        return nc
                    self.phase_attn(l, s)
                    if stop == "attn":
                        return nc
                    with ExitStack() as bs:
                        self.BIG = tile1(bs, nc, "BIG", [128, cfg.KC, cfg.T], BF16)
                        self.phase_merge(l, s)
                        if stop == "merge":
                            return nc
                        self.phase_norm(l, s, 2)
                        if l == 1 and cfg.MOE == "routed":
                            self.route_local(s)
                        else:
                            self.phase_swiglu(l, s)
                    if stop == "ffn":
                        return nc
                if stop == "layer0":
                    return nc
            if cfg.MOE == "routed":
                self.phase_moe_routed()
            else:
                self.phase_final()
        return nc

    def dump(self, name, tile, r, shape, dt):
        if not self.cfg.DEBUG:
            return
        d = self.nc.dram_tensor("dbg_" + name, list(shape), dt, kind="ExternalOutput").ap()
        self.S.dma("sp", lambda e: e.dma_start(out=d, in_=tile[:]), reads=[r])

    def end_phase(self):
        self.S.barrier()
        self.S.flush()

    def phase_mod(self):
        cfg, nc, S = self.cfg, self.nc, self.S
        D, KC, NR = cfg.D, cfg.KC, cfg.NR
        with ExitStack() as ph:
            cT, r_cT = tile1(ph, nc, "cT", [128, KC, NR], F32)
            cTb, r_cTb = tile1(ph, nc, "cTb", [128, KC, NR], BF16)
            for r in range(NR):
                src = self.cvec[r:r + 1, :].rearrange("o (kc p) -> p kc o", p=128)
                S.dma("sp", lambda e, src=src, r=r: e.dma_start(out=cT[:, :, r:r + 1], in_=src), writes=[r_cT])
            S.op("act", lambda e: e.activation(out=cTb[:], in_=cT[:], func=AF.Silu), reads=[r_cT], writes=[r_cTb])
            wpool = TPool(ph, nc, "wmod", [128, KC, 512], BF16, 3)
            pspool = TPool(ph, nc, "psmod", [NR, 512], F32, 2, psum=True)
            bias, r_bias = tile1(ph, nc, "bmod", [NR, 6 * D], F32)
            res, r_res = tile1(ph, nc, "resmod", [NR, 6 * D], F32)
            for l in range(2):
                S.dma("sp", lambda e, l=l: e.dma_start(out=bias[:], in_=self.b_mod[l:l + 1, :].partition_broadcast(NR)), writes=[r_bias])
                nb = 6 * D // 512
                for b in range(nb):
                    w, r_w = self.load_wblock(wpool, self.w_mod[l], b * 512, 512)
                    ps, r_ps = pspool.next()
                    for kc in range(KC):
                        S.op("pe", lambda e, ps=ps, w=w, kc=kc: e.matmul(ps[:], lhsT=cTb[:, kc, :], rhs=w[:, kc, :], start=(kc == 0), stop=(kc == KC - 1)),
                             reads=[r_cTb, r_w], writes=[r_ps], signal=(kc == KC - 1))
                    S.op("dve", lambda e, ps=ps, b=b: e.tensor_tensor(out=res[:, b * 512:(b + 1) * 512], in0=ps[:], in1=bias[:, b * 512:(b + 1) * 512], op=ALU.add),
                         reads=[r_ps, r_bias], writes=[r_res])
                S.dma("sp", lambda e, l=l: e.dma_start(out=self.MOD[l], in_=res[:]), reads=[r_res], writes=[self.r_MOD])
            self.end_phase()

    def phase_norm(self, l, s, which):
        cfg, nc, S = self.cfg, self.nc, self.S
        D, KC = cfg.D, cfg.KC
        first = (l == 0 and which == 1)
        nw = self.norm_mix if which == 1 else self.norm_ffn
        base = 0 if which == 1 else 3
        BIG, r_BIG = self.BIG
        ident, r_ident = self.C["ident"]
        tiles = list(range(cfg.NT))
        if which == 2 and l == 1:
            tiles = list(range(cfg.NCT, cfg.NT))
        with ExitStack() as ph:
            gm = {}
            sh = {}
            for kind, row in (("lat", s), ("ctx", cfg.NSEQ)):
                if kind == "ctx" and which == 2 and l == 1:
                    continue
                g_t, g_r = tile1(ph, nc, "gm" + kind, [128, D], F32)
                s_t, s_r = tile1(ph, nc, "sh" + kind, [128, D], F32)
                n_t, n_r = tile1(ph, nc, "nw" + kind, [128, D], F32)
                self.S.dma("sp", lambda e, n_t=n_t: e.dma_start(out=n_t[:], in_=nw[l:l + 1, :].partition_broadcast(128)), writes=[n_r])
                self.S.dma("sp", lambda e, g_t=g_t, row=row: e.dma_start(out=g_t[:], in_=self.MOD[l, row:row + 1, (base + 1) * D:(base + 2) * D].partition_broadcast(128)), reads=[self.r_MOD], writes=[g_r])
                self.S.dma("sp", lambda e, s_t=s_t, row=row: e.dma_start(out=s_t[:], in_=self.MOD[l, row:row + 1, base * D:(base + 1) * D].partition_broadcast(128)), reads=[self.r_MOD], writes=[s_r])
                S.op("dve", lambda e, g_t=g_t, n_t=n_t: e.scalar_tensor_tensor(out=g_t[:], in0=g_t[:], scalar=1.0, in1=n_t[:], op0=ALU.add, op1=ALU.mult),
                     reads=[g_r, n_r], writes=[g_r])
                gm[kind] = (g_t, g_r)
                sh[kind] = (s_t, s_r)
            xpool = TPool(ph, nc, "xt", [128, D], F32, 2)
            junk, r_junk = tile1(ph, nc, "junk", [128, D], BF16)
            t1pool = TPool(ph, nc, "t1", [128, D], F32, 1)
            hpool = TPool(ph, nc, "hb", [128, D], BF16, 2)
            stpool = TPool(ph, nc, "st", [128, 4], F32, 2)
            pspool = TPool(ph, nc, "pT", [128, KC, 128], BF16, 2, psum=True)
            loaded = {}

            def load(i):
                tile = tiles[i]
                xt, r_xt = xpool.next()
                src = self.xsrc(first, s, tile)
                S.dma("sp", lambda e, xt=xt, src=src: e.dma_start(out=xt[:], in_=src), reads=[self.r_XR[s][tile]], writes=[r_xt])
                loaded[i] = (xt, r_xt)

            load(0)
            for i, tile in enumerate(tiles):
                if i + 1 < len(tiles):
                    load(i + 1)
                xt, r_xt = loaded.pop(i)
                kind = "ctx" if tile < cfg.NCT else "lat"
                g_t, g_r = gm[kind]
                s_t, s_r = sh[kind]
                st, r_st = stpool.next()
                S.op("act", lambda e, xt=xt, st=st: e.activation(out=junk[:], in_=xt[:], func=AF.Square, accum_out=st[:, 0:1]),
                     reads=[r_xt], writes=[r_junk, r_st])
                S.op("dve", lambda e, st=st: e.tensor_scalar(out=st[:, 1:2], in0=st[:, 0:1], scalar1=1.0 / D, scalar2=EPS, op0=ALU.mult, op1=ALU.add),
                     reads=[r_st], writes=[r_st])
                S.op("act", lambda e, st=st: e.activation(out=st[:, 2:3], in_=st[:, 1:2], func=AF.Sqrt), reads=[r_st], writes=[r_st])
                S.op("dve", lambda e, st=st: e.reciprocal(out=st[:, 3:4], in_=st[:, 2:3]), reads=[r_st], writes=[r_st])
                t1, r_t1 = t1pool.next()
                S.op("dve", lambda e, t1=t1, xt=xt, g_t=g_t: e.tensor_tensor(out=t1[:], in0=xt[:], in1=g_t[:], op=ALU.mult),
                     reads=[r_xt, g_r], writes=[r_t1])
                hb, r_hb = hpool.next()
                S.op("dve", lambda e, hb=hb, t1=t1, st=st, s_t=s_t: e.scalar_tensor_tensor(out=hb[:], in0=t1[:], scalar=st[:, 3:4], in1=s_t[:], op0=ALU.mult, op1=ALU.add),
                     reads=[r_t1, r_st, s_r], writes=[r_hb])
                if which == 2 and l == 1 and cfg.MOE == "routed":
                    g_ = s * (cfg.SEQ // 128) + (tile - cfg.NCT)
                    S.dma("sp", lambda e, hb=hb, g_=g_: e.dma_start(out=self.H2[g_ * 128:(g_ + 1) * 128, :], in_=hb[:]), reads=[r_hb], writes=[self.r_H2])
                ps, r_ps = pspool.next()
                for kc in range(KC):
                    S.op("pe", lambda e, ps=ps, hb=hb, kc=kc: e.transpose(out=ps[:, kc, :], in_=hb[:, kc * 128:(kc + 1) * 128], identity=ident[:]),
                         reads=[r_hb, r_ident], writes=[r_ps], signal=(kc == KC - 1))
                S.op("act", lambda e, ps=ps, tile=tile: e.copy(out=BIG[:, :, tile * 128:(tile + 1) * 128], in_=ps[:]),
                     reads=[r_ps], writes=[r_BIG])
            self.end_phase()

    def phase_inproj(self, l, s):
        cfg, nc, S = self.cfg, self.nc, self.S
        D, KC, T = cfg.D, cfg.KC, cfg.T
        BIG, r_BIG = self.BIG
        CTX, SEQ = cfg.CTX, cfg.SEQ
        pm, r_pm = self.C["pm"]
        w_in = self.w_in[l]
        with ExitStack() as ph:
            wpool = TPool(ph, nc, "win", [128, KC, 512], BF16, 3)
            pspool = TPool(ph, nc, "psin", [128, 512], F32, 4, psum=True)
            psrot = TPool(ph, nc, "psrot", [128, 512], F32, 2, psum=True)
            stb = TPool(ph, nc, "stb", [128, T], BF16, 4)
            stf = TPool(ph, nc, "stf", [128, T], F32, 2)
            tmpf = TPool(ph, nc, "tmpf", [128, 512], F32, 6)
            cosT, r_cos = tile1(ph, nc, "cosT", [128, SEQ], F32)
            sinT, r_sin = tile1(ph, nc, "sinT", [128, SEQ], F32)
            S.dma("sp", lambda e: e.dma_start(out=cosT[:], in_=self.cst_d["cosT"][:, :]), writes=[r_cos])
            S.dma("sp", lambda e: e.dma_start(out=sinT[:], in_=self.cst_d["sinT"][:, :]), writes=[r_sin])
            lbv, oml = [], []
            for di, lbsrc in enumerate((self.lb_f, self.lb_b)):
                lb_t, lb_r = tile1(ph, nc, "lb%d" % di, [128, 8], F32)
                om_t, om_r = tile1(ph, nc, "oml%d" % di, [128, 8], F32)
                if l == 0:
                    S.op("dve", lambda e, lb_t=lb_t: e.memset(lb_t[:], 0.0), writes=[lb_r])
                else:
                    r0_t, r0_r = tile1(ph, nc, "lr0%d" % di, [128, 8], F32)
                    r1_t, r1_r = tile1(ph, nc, "lr1%d" % di, [128, 8], F32)
                    S.dma("sp", lambda e, r0_t=r0_t, lbsrc=lbsrc: e.dma_start(out=r0_t[:], in_=lbsrc[0:1, :].rearrange("o (h p) -> p (o h)", p=128)), writes=[r0_r])
                    S.dma("sp", lambda e, r1_t=r1_t, lbsrc=lbsrc: e.dma_start(out=r1_t[:], in_=lbsrc[1:2, :].rearrange("o (h p) -> p (o h)", p=128)), writes=[r1_r])
                    S.op("dve", lambda e, r0_t=r0_t, r1_t=r1_t: e.tensor_tensor(out=r0_t[:], in0=r0_t[:], in1=r1_t[:], op=ALU.subtract), reads=[r0_r, r1_r], writes=[r0_r])
                    S.op("act", lambda e, r0_t=r0_t: e.activation(out=r0_t[:], in_=r0_t[:], func=AF.Exp), reads=[r0_r], writes=[r0_r])
                    S.op("dve", lambda e, r0_t=r0_t: e.tensor_scalar(out=r0_t[:], in0=r0_t[:], scalar1=1.0, scalar2=None, op0=ALU.add), reads=[r0_r], writes=[r0_r])
                    S.op("dve", lambda e, r0_t=r0_t, lb_t=lb_t: e.reciprocal(out=lb_t[:], in_=r0_t[:]), reads=[r0_r], writes=[lb_r])
                S.op("dve", lambda e, lb_t=lb_t, om_t=om_t: e.tensor_scalar(out=om_t[:], in0=lb_t[:], scalar1=-1.0, scalar2=1.0, op0=ALU.mult, op1=ALU.add), reads=[lb_r], writes=[om_r])
                lbv.append((lb_t, lb_r))
                oml.append((om_t, om_r))

            def proj_chunk(w, r_w, c, epi):
                for (t0, n) in cfg.tblocks:
                    ps, r_ps = pspool.next()
                    for kc in range(KC):
                        S.op("pe", lambda e, ps=ps, kc=kc, t0=t0, n=n: e.matmul(ps[:, 0:n], lhsT=w[:, kc, c * 128:(c + 1) * 128], rhs=BIG[:, kc, t0:t0 + n], start=(kc == 0), stop=(kc == KC - 1)),
                             reads=[r_w, r_BIG], writes=[r_ps], signal=(kc == KC - 1))
                    epi(ps, r_ps, t0, n)

            def store(dst, st, r_st, r_dst):
                S.dma("sp", lambda e: e.dma_start(out=dst, in_=st[:]), reads=[r_st], writes=[r_dst])

            def simple_job(col0, nchunks, func, scale, dst, r_dst):
                for b in range(0, nchunks, 4):
                    w, r_w = self.load_wblock(wpool, w_in, col0 + b * 128, 512)
                    for c in range(4):
                        st, r_st = stb.next()
                        def epi(ps, r_ps, t0, n, st=st, r_st=r_st):
                            S.op("act", lambda e: e.activation(out=st[:, t0:t0 + n], in_=ps[:, 0:n], func=func, scale=scale), reads=[r_ps], writes=[r_st])
                        proj_chunk(w, r_w, c, epi)
                        store(dst[b + c], st, r_st, r_dst[b + c])

            simple_job(COL["hq"], 8, AF.Copy, 128.0 ** -0.5, self.QT, self.r_QT)
            simple_job(COL["hg"], 8, AF.Silu, 1.0, self.SGT, self.r_SGT)
            simple_job(COL["ga"], 16, AF.Sigmoid, 1.0, self.GA, self.r_GA)
            simple_job(COL["gb"], 16, AF.Sigmoid, 1.0, self.GB, self.r_GB)
            for di, col0 in enumerate((COL["ff"], COL["fb"])):
                lb_t, lb_r = lbv[di]
                om_t, om_r = oml[di]
                for b in range(0, 8, 4):
                    w, r_w = self.load_wblock(wpool, w_in, col0 + b * 128, 512)
                    for c in range(4):
                        hd = b + c
                        sg_, r_sg = stf.next()
                        sk_, r_sk = stb.next()
                        def epi(ps, r_ps, t0, n, sg_=sg_, r_sg=r_sg, sk_=sk_, r_sk=r_sk, hd=hd, om_t=om_t, om_r=om_r):
                            e_t, r_e = tmpf.next()
                            t_t, r_t = tmpf.next()
                            k_t, r_k = tmpf.next()
                            S.op("act", lambda e: e.activation(out=e_t[:, 0:n], in_=ps[:, 0:n], func=AF.Exp, scale=-1.0), reads=[r_ps], writes=[r_e])
                            S.op("dve", lambda e: e.tensor_scalar(out=t_t[:, 0:n], in0=e_t[:, 0:n], scalar1=1.0, scalar2=None, op0=ALU.add), reads=[r_e], writes=[r_t])
                            S.op("dve", lambda e: e.reciprocal(out=t_t[:, 0:n], in_=t_t[:, 0:n]), reads=[r_t], writes=[r_t])
                            S.op("dve", lambda e: e.scalar_tensor_tensor(out=k_t[:, 0:n], in0=e_t[:, 0:n], scalar=om_t[:, hd:hd + 1], in1=t_t[:, 0:n], op0=ALU.mult, op1=ALU.mult),
                                 reads=[r_e, r_t, om_r], writes=[r_k])
                            S.op("act", lambda e: e.activation(out=sg_[:, t0:t0 + n], in_=k_t[:, 0:n], func=AF.Ln, scale=-1.0, bias=1.0), reads=[r_k], writes=[r_sg])
                            S.op("act", lambda e: e.copy(out=sk_[:, t0:t0 + n], in_=k_t[:, 0:n]), reads=[r_k], writes=[r_sk])
                        proj_chunk(w, r_w, c, epi)
                        store(self.GT[di, hd], sg_, r_sg, self.r_GT[di][hd])
                        store(self.KT[di, hd], sk_, r_sk, self.r_KT[di][hd])

            def rope_job(w, r_w, c, scale, dst, r_dst):
                raw, r_raw = stb.next()
                outt, r_out = stb.next()
                def epi(ps, r_ps, t0, n):
                    S.op("act", lambda e: e.activation(out=raw[:, t0:t0 + n], in_=ps[:, 0:n], func=AF.Copy, scale=scale), reads=[r_ps], writes=[r_raw])
                    if t0 < CTX:
                        S.op("act", lambda e: e.copy(out=outt[:, t0:t0 + n], in_=raw[:, t0:t0 + n]), reads=[r_raw], writes=[r_out])
                        return
                    p0 = t0 - CTX
                    pr, r_pr = psrot.next()
                    S.op("pe", lambda e: e.matmul(pr[:, 0:n], lhsT=pm[:], rhs=raw[:, t0:t0 + n], start=True, stop=True), reads=[r_pm, r_raw], writes=[r_pr])
                    a_t, r_a = tmpf.next()
                    b_t, r_b = tmpf.next()
                    S.op("dve", lambda e: e.tensor_tensor(out=a_t[:, 0:n], in0=raw[:, t0:t0 + n], in1=cosT[:, p0:p0 + n], op=ALU.mult), reads=[r_raw, r_cos], writes=[r_a])
                    S.op("dve", lambda e: e.tensor_tensor(out=b_t[:, 0:n], in0=pr[:, 0:n], in1=sinT[:, p0:p0 + n], op=ALU.mult), reads=[r_pr, r_sin], writes=[r_b])
                    S.op("dve", lambda e: e.tensor_tensor(out=outt[:, t0:t0 + n], in0=a_t[:, 0:n], in1=b_t[:, 0:n], op=ALU.add), reads=[r_a, r_b], writes=[r_out])
                proj_chunk(w, r_w, c, epi)
                store(dst, outt, r_out, r_dst)

            for b in range(0, 8, 4):
                w, r_w = self.load_wblock(wpool, w_in, COL["aq"] + b * 128, 512)
                for c in range(4):
                    rope_job(w, r_w, c, 64.0 ** -0.5, self.QA[b + c], self.r_QA[b + c])
            w, r_w = wpool.next()
            for (o, c0, nn) in ((0, COL["ak"], 128), (128, COL["ak"] + 64, 64), (192, COL["ak"], 64)):
                src = w_in[:, c0:c0 + nn].rearrange("(kc p) n -> p kc n", p=128)
                S.dma("pool", lambda e, o=o, nn=nn, src=src: e.dma_start(out=w[:, :, o:o + nn], in_=src), writes=[r_w])
            for c in range(2):
                rope_job(w, r_w, c, 1.0, self.KA[c], self.r_KA[c])

            vst = TPool(ph, nc, "vst", [128, 512], BF16, 3)
            for (col0, ncols, dst, r_dst) in ((COL["hv"], 512, self.V[:, 0:512], self.r_V), (COL["hv"] + 512, 512, self.V[:, 512:1024], self.r_V), (COL["av"], 128, self.VA, self.r_VA)):
                w, r_w = self.load_wblock(wpool, w_in, col0, ncols)
                for tile in range(cfg.NT):
                    ps, r_ps = pspool.next()
                    for kc in range(KC):
                        S.op("pe", lambda e, ps=ps, kc=kc, tile=tile, ncols=ncols, w=w: e.matmul(ps[:, 0:ncols], lhsT=BIG[:, kc, tile * 128:(tile + 1) * 128], rhs=w[:, kc, 0:ncols], start=(kc == 0), stop=(kc == KC - 1)),
                             reads=[r_w, r_BIG], writes=[r_ps], signal=(kc == KC - 1))
                    vt, r_vt = vst.next()
                    S.op("act", lambda e, vt=vt, ps=ps, ncols=ncols: e.copy(out=vt[:, 0:ncols], in_=ps[:, 0:ncols]), reads=[r_ps], writes=[r_vt])
                    S.dma("sp", lambda e, vt=vt, tile=tile, ncols=ncols, dst=dst: e.dma_start(out=dst[tile * 128:(tile + 1) * 128, :], in_=vt[:, 0:ncols]), reads=[r_vt], writes=[r_dst])
            self.end_phase()

    def phase_scan(self, l, s):
        cfg, nc, S = self.cfg, self.nc, self.S
        T, NCH = cfg.T, cfg.NCH
        NB16 = T // 16
        ident, r_ident = self.C["ident"]
        groups = cfg.groups
        ng = len(groups)
        order = [[list(g) for g in groups],
                 [list(reversed(groups[0]))] + [list(reversed(g)) for g in reversed(groups[1:])]]
        VN = ("qd", "kd", "qb", "qin", "kout", "k1", "k2", "k3")
        with ExitStack() as ph:
            qT, r_qT = tile1(ph, nc, "s_qT", [128, T], BF16)
            sgT, r_sgT = tile1(ph, nc, "s_sgT", [128, T], BF16)
            vt, r_vt = tile1(ph, nc, "s_v", [64, NCH, 128], BF16)
            gT = [tile1(ph, nc, "s_gT%d" % d, [128, T], F32) for d in range(2)]
            kT = [tile1(ph, nc, "s_kT%d" % d, [128, T], BF16) for d in range(2)]
            Zps = [tile1(ph, nc, "s_Zp%d" % d, [128, T + 1], F32) for d in range(2)]
            tmp = TPool(ph, nc, "s_tmp", [128, T], F32, 3)
            var = [{n: tile1(ph, nc, "s_%s%d" % (n, d), [128, T], BF16) for n in VN} for d in range(2)]
            tr = [tile1(ph, nc, "s_tr%d" % d, [128, NCH], F32) for d in range(2)]
            oall, r_oall = tile1(ph, nc, "s_oall", [128, T], F32)
            aT, r_aT = tile1(ph, nc, "s_aT", [128, T], BF16)
            ones, r_ones = tile1(ph, nc, "s_ones", [128, T], F32)
            onesf, r_onesf = tile1(ph, nc, "s_onesf", [128, 128], F32)
            gain, r_gain = tile1(ph, nc, "s_gain", [128, 8], F32)
            mk = {}
            for n in ("maskD_f", "maskO_f", "maskD_b", "maskO_b"):
                mk[n] = tile1(ph, nc, "s_" + n, [64, 64], BF16)
                S.dma("pool", lambda e, n=n: e.dma_start(out=mk[n][0][:], in_=self.cst_d[n][:, :]), writes=[mk[n][1]])
            Sf = [tile1(ph, nc, "s_Sf%d" % d, [128, 128], F32) for d in range(2)]
            Sb = [tile1(ph, nc, "s_Sb%d" % d, [128, 128], BF16) for d in range(2)]
            At = TPool(ph, nc, "s_At", [64, 8, 64], BF16, 4)
            At2 = TPool(ph, nc, "s_At2", [64, 8, 64], BF16, 2)
            ktok = TPool(ph, nc, "s_ktok", [64, 8, 128], BF16, 4)
            rs = TPool(ph, nc, "s_rs", [128, 512], F32, 2)
            psA = TPool(ph, nc, "s_psA", [64, 8, 64], F32, 1, psum=True)
            psA2 = [tile1(ph, nc, "s_psA2%d" % d, [64, 8, 64], F32, psum=True) for d in range(2)]
            psK = TPool(ph, nc, "s_psK", [64, 8, 128], BF16, 1, psum=True)
            psO = TPool(ph, nc, "s_psO", [128, 8, 64], F32, 2, psum=True)
            psD = TPool(ph, nc, "s_psD", [128, 128], F32, 1, psum=True)
            psS = TPool(ph, nc, "s_psS", [128, 512], F32, 1, psum=True)
            S.op("dve", lambda e: e.memset(ones[:], 1.0), writes=[r_ones])
            S.op("dve", lambda e: e.memset(onesf[:], 1.0), writes=[r_onesf])
            for d in range(2):
                S.op("dve", lambda e, d=d: e.memset(psA2[d][0][:], 0.0), writes=[psA2[d][1]])
                S.op("dve", lambda e, d=d: e.memset(Zps[d][0][:], 0.0), writes=[Zps[d][1]])
            S.dma("sp", lambda e: e.dma_start(out=gain[:], in_=self.hg_norm[l:l + 1, :].rearrange("o (h p) -> p (o h)", p=128)), writes=[r_gain])

            def v3(ap, j):
                return ap.rearrange("p (c j) -> p c j", j=j)

            for hd in range(8):
                self.pump(cfg.PUMP)
                S.dma("sp", lambda e, hd=hd: e.dma_start(out=qT[:], in_=self.QT[hd]), reads=[self.r_QT[hd]], writes=[r_qT])
                S.dma("sp", lambda e, hd=hd: e.dma_start(out=sgT[:], in_=self.SGT[hd]), reads=[self.r_SGT[hd]], writes=[r_sgT])
                S.dma("sp", lambda e, hd=hd: e.dma_start(out=vt[:], in_=self.V[:, hd * 128:(hd + 1) * 128].rearrange("(c p) v -> p c v", p=64)), reads=[self.r_V], writes=[r_vt])
                for d in range(2):
                    S.dma("sp", lambda e, hd=hd, d=d: e.dma_start(out=gT[d][0][:], in_=self.GT[d, hd]), reads=[self.r_GT[d][hd]], writes=[gT[d][1]])
                    S.dma("sp", lambda e, hd=hd, d=d: e.dma_start(out=kT[d][0][:], in_=self.KT[d, hd]), reads=[self.r_KT[d][hd]], writes=[kT[d][1]])
                for d in range(2):
                    g_t, g_r = gT[d]
                    k_t, k_r = kT[d]
                    Zp, r_Zp = Zps[d]
                    sg = 1.0 if d == 0 else -1.0
                    S.op("dve", lambda e, Zp=Zp, g_t=g_t: e.tensor_tensor_scan(out=Zp[:, 1:T + 1], data0=ones[:], data1=g_t[:], initial=0.0, op0=ALU.mult, op1=ALU.add),
                         reads=[r_ones, g_r], writes=[r_Zp])
                    X = Zp[:, 1:T + 1] if d == 0 else Zp[:, 0:T]
                    lo16 = v3(Zp[:, 0:T], 16)
                    hi16 = v3(Zp[:, 1:T + 1], 16)
                    lo64 = v3(Zp[:, 0:T], 64)
                    hi64 = v3(Zp[:, 1:T + 1], 64)
                    ref_mid = lo16[:, :, 8:9]
                    ref_qb = lo16[:, :, 0:1] if d == 0 else hi16[:, :, 15:16]
                    ref_qin = lo64[:, :, 0:1] if d == 0 else hi64[:, :, 63:64]
                    ref_kout = hi64[:, :, 63:64] if d == 0 else lo64[:, :, 0:1]
                    ref_k = [lo64[:, :, 16 * i:16 * i + 1] for i in (1, 2, 3)]

                    def make(name, src_t, src_r, ref, blk, scale, clamp=None, X=X, r_Zp=r_Zp, d=d):
                        nb = T // blk
                        D, r_D = tmp.next()
                        S.op("dve", lambda e: e.tensor_tensor(out=v3(D[:], blk), in0=v3(X, blk), in1=ref.broadcast_to([128, nb, blk]), op=ALU.subtract),
                             reads=[r_Zp], writes=[r_D])
                        if clamp is not None:
                            S.op("dve", lambda e: e.tensor_scalar(out=D[:], in0=D[:], scalar1=0.0, scalar2=None, op0=clamp), reads=[r_D], writes=[r_D])
                        S.op("act", lambda e: e.activation(out=D[:], in_=D[:], func=AF.Exp, scale=scale), reads=[r_D], writes=[r_D])
                        o_t, o_r = var[d][name]
                        S.op("pool", lambda e: e.tensor_tensor(out=o_t[:], in0=src_t[:], in1=D[:], op=ALU.mult), reads=[src_r, r_D], writes=[o_r])

                    make("qd", qT, r_qT, ref_mid, 16, sg)
                    make("kd", k_t, k_r, ref_mid, 16, -sg)
                    make("qb", qT, r_qT, ref_qb, 16, sg)
                    make("qin", qT, r_qT, ref_qin, 64, sg)
                    make("kout", k_t, k_r, ref_kout, 64, -sg)
                    for i in range(3):
                        make("k%d" % (i + 1), k_t, k_r, ref_k[i], 64, -sg, clamp=(ALU.max if d == 0 else ALU.min))
                    tr_t, tr_r = tr[d]
                    S.op("dve", lambda e, tr_t=tr_t, hi64=hi64, lo64=lo64: e.tensor_tensor(out=tr_t[:].unsqueeze(2), in0=hi64[:, :, 63:64], in1=lo64[:, :, 0:1], op=ALU.subtract), reads=[r_Zp], writes=[tr_r])
                    S.op("act", lambda e, tr_t=tr_t: e.activation(out=tr_t[:], in_=tr_t[:], func=AF.Exp), reads=[tr_r], writes=[tr_r])
                    S.op("dve", lambda e, d=d: e.memset(Sf[d][0][:], 0.0), writes=[Sf[d][1]])
                    S.op("dve", lambda e, d=d: e.memset(Sb[d][0][:], 0.0), writes=[Sb[d][1]])
                touched = set()
                for gi in range(ng):
                    ctxs = []
                    for d in range(2):
                        cl = order[d][gi]
                        g0 = min(cl)
                        n = len(cl)
                        V = var[d]
                        pa, r_pa = psA.next()
                        for c in cl:
                            S.op("pe", lambda e, pa=pa, c=c, g0=g0, V=V: e.matmul(pa[:, c - g0, :], lhsT=V["kd"][0][:, c * 64:(c + 1) * 64], rhs=V["qd"][0][:, c * 64:(c + 1) * 64], start=True, stop=True),
                                 reads=[V["kd"][1], V["qd"][1]], writes=[r_pa], signal=(c == cl[-1]))
                        pa2, r_pa2 = psA2[d]
                        subs = (1, 2, 3) if d == 0 else (0, 1, 2)
                        for c in cl:
                            for i in subs:
                                kv = V["k%d" % (i if d == 0 else i + 1)]
                                S.op("pe", lambda e, pa2=pa2, c=c, g0=g0, i=i, kv=kv, V=V: e.matmul(pa2[:, c - g0, 16 * i:16 * i + 16], lhsT=kv[0][:, c * 64:(c + 1) * 64], rhs=V["qb"][0][:, c * 64 + 16 * i:c * 64 + 16 * i + 16], start=True, stop=True),
                                     reads=[kv[1], V["qb"][1]], writes=[r_pa2], signal=(c == cl[-1] and i == subs[-1]))
                        at, r_at = At.next()
                        at2, r_at2 = At2.next()
                        mD, r_mD = mk["maskD_f" if d == 0 else "maskD_b"]
                        mO, r_mO = mk["maskO_f" if d == 0 else "maskO_b"]
                        S.op("dve", lambda e, at=at, pa=pa, n=n, mD=mD: e.tensor_tensor(out=at[:, 0:n, :], in0=pa[:, 0:n, :], in1=mD[:].unsqueeze(1).broadcast_to([64, n, 64]), op=ALU.mult),
                             reads=[r_pa, r_mD], writes=[r_at])
                        S.op("dve", lambda e, at2=at2, pa2=pa2, n=n, mO=mO: e.tensor_tensor(out=at2[:, 0:n, :], in0=pa2[:, 0:n, :], in1=mO[:].unsqueeze(1).broadcast_to([64, n, 64]), op=ALU.mult),
                             reads=[r_pa2, r_mO], writes=[r_at2])
                        S.op("pool", lambda e, at=at, at2=at2, n=n: e.tensor_tensor(out=at[:, 0:n, :], in0=at[:, 0:n, :], in1=at2[:, 0:n, :], op=ALU.add), reads=[r_at, r_at2], writes=[r_at])
                        pk, r_pk = psK.next()
                        for c in cl:
                            S.op("pe", lambda e, pk=pk, c=c, g0=g0, V=V: e.transpose(out=pk[:, c - g0, :], in_=V["kout"][0][:, c * 64:(c + 1) * 64], identity=ident[:]),
                                 reads=[V["kout"][1], r_ident], writes=[r_pk], signal=(c == cl[-1]))
                        kk, r_kk = ktok.next()
                        S.op("act", lambda e, kk=kk, pk=pk, n=n: e.copy(out=kk[:, 0:n, :], in_=pk[:, 0:n, :]), reads=[r_pk], writes=[r_kk])
                        po, r_po = psO.next()
                        ctxs.append((d, cl, g0, at, r_at, kk, r_kk, po, r_po))
                    nsteps = max(len(c[1]) for c in ctxs)
                    for j in range(nsteps):
                        for (d, cl, g0, at, r_at, kk, r_kk, po, r_po) in ctxs:
                            if j >= len(cl):
                                continue
                            c = cl[j]
                            pos = c - g0
                            V = var[d]
                            S.op("pe", lambda e, po=po, pos=pos, c=c, at=at: e.matmul(po[:, pos, :], lhsT=vt[:, c, :], rhs=at[:, pos, :], start=True, stop=False),
                                 reads=[r_vt, r_at], writes=[r_po], signal=False)
                            S.op("pe", lambda e, po=po, pos=pos, c=c, d=d, V=V: e.matmul(po[:, pos, :], lhsT=Sb[d][0][:], rhs=V["qin"][0][:, c * 64:(c + 1) * 64], start=False, stop=True),
                                 reads=[Sb[d][1], V["qin"][1]], writes=[r_po], signal=(j == len(cl) - 1))
                            last = (gi == ng - 1 and j == len(cl) - 1)
                            if not last:
                                pd, r_pd = psD.next()
                                S.op("pe", lambda e, pd=pd, kk=kk, pos=pos, c=c: e.matmul(pd[:], lhsT=kk[:, pos, :], rhs=vt[:, c, :], start=True, stop=True),
                                     reads=[r_kk, r_vt], writes=[r_pd])
                                S.op("dve", lambda e, pd=pd, d=d, c=c: e.scalar_tensor_tensor(out=Sf[d][0][:], in0=Sf[d][0][:], scalar=tr[d][0][:, c:c + 1], in1=pd[:], op0=ALU.mult, op1=ALU.add),
                                     reads=[r_pd, Sf[d][1], tr[d][1]], writes=[Sf[d][1]])
                                S.op("act", lambda e, d=d: e.copy(out=Sb[d][0][:], in_=Sf[d][0][:]), reads=[Sf[d][1]], writes=[Sb[d][1]])
                    for (d, cl, g0, at, r_at, kk, r_kk, po, r_po) in ctxs:
                        n = len(cl)
                        dst = v3(oall[:, g0 * 64:(g0 + n) * 64], 64)
                        if g0 not in touched:
                            touched.add(g0)
                            S.op("act", lambda e, dst=dst, po=po, n=n: e.copy(out=dst, in_=po[:, 0:n, :]), reads=[r_po], writes=[r_oall])
                        else:
                            S.op("dve", lambda e, dst=dst, po=po, n=n: e.tensor_tensor(out=dst, in0=po[:, 0:n, :], in1=dst, op=ALU.add), reads=[r_po, r_oall], writes=[r_oall])
                sq, r_sq = tmp.next()
                S.op("pool", lambda e, sq=sq: e.tensor_tensor(out=sq[:], in0=oall[:], in1=oall[:], op=ALU.mult), reads=[r_oall], writes=[r_sq])
                for (t0, n) in cfg.tblocks:
                    pss, r_pss = psS.next()
                    S.op("pe", lambda e, pss=pss, sq=sq, t0=t0, n=n: e.matmul(pss[:, 0:n], lhsT=onesf[:], rhs=sq[:, t0:t0 + n], start=True, stop=True), reads=[r_onesf, r_sq], writes=[r_pss])
                    r1, r_r1 = rs.next()
                    S.op("dve", lambda e, r1=r1, pss=pss, n=n: e.tensor_scalar(out=r1[:, 0:n], in0=pss[:, 0:n], scalar1=1.0 / 128, scalar2=EPS, op0=ALU.mult, op1=ALU.add), reads=[r_pss], writes=[r_r1])
                    S.op("act", lambda e, r1=r1, n=n: e.activation(out=r1[:, 0:n], in_=r1[:, 0:n], func=AF.Sqrt), reads=[r_r1], writes=[r_r1])
                    S.op("dve", lambda e, r1=r1, n=n: e.reciprocal(out=r1[:, 0:n], in_=r1[:, 0:n]), reads=[r_r1], writes=[r_r1])
                    S.op("dve", lambda e, r1=r1, t0=t0, n=n: e.tensor_tensor(out=r1[:, 0:n], in0=r1[:, 0:n], in1=oall[:, t0:t0 + n], op=ALU.mult), reads=[r_r1, r_oall], writes=[r_r1])
                    S.op("dve", lambda e, r1=r1, t0=t0, n=n, hd=hd: e.scalar_tensor_tensor(out=aT[:, t0:t0 + n], in0=r1[:, 0:n], scalar=gain[:, hd:hd + 1], in1=sgT[:, t0:t0 + n], op0=ALU.mult, op1=ALU.mult),
                         reads=[r_r1, r_gain, r_sgT], writes=[r_aT])
                S.dma("sp", lambda e, hd=hd: e.dma_start(out=self.AT[hd], in_=aT[:]), reads=[r_aT], writes=[self.r_AT[hd]])
            self.end_phase()

    def phase_attn(self, l, s):
        cfg, nc, S = self.cfg, self.nc, self.S
        T, CTX, NCT, NT = cfg.T, cfg.CTX, cfg.NCT, cfg.NT
        NQ = cfg.SEQ // 128
        ident, r_ident = self.C["ident"]
        mprev, r_mprev = self.C["mneg_prev"]
        mnext, r_mnext = self.C["mneg_next"]
        onesb, r_onesb = self.C["ones"]
        with ExitStack() as ph:
            kc_ = [tile1(ph, nc, "a_k%d" % i, [128, T], BF16) for i in range(2)]
            vtok, r_vtok = tile1(ph, nc, "a_v", [128, NT, 128], BF16)
            qpool = TPool(ph, nc, "a_q", [128, T], BF16, 2)
            opool = TPool(ph, nc, "a_o", [128, T], BF16, 2)
            esb, r_esb = tile1(ph, nc, "a_esb", [128, 16], F32)
            es2, r_es2 = tile1(ph, nc, "a_es2", [128, 8], F32)
            ppool = TPool(ph, nc, "a_p", [128, 5, 128], BF16, 4)
            rpool = TPool(ph, nc, "a_r", [128, 128], F32, 3)
            psS = TPool(ph, nc, "a_psS", [128, 8, 128], F32, 3, psum=True)
            psOD = TPool(ph, nc, "a_psOD", [128, 2, 128], F32, 2, psum=True)
            for i in range(2):
                S.dma("sp", lambda e, i=i: e.dma_start(out=kc_[i][0][:], in_=self.KA[i]), reads=[self.r_KA[i]], writes=[kc_[i][1]])
            S.dma("sp", lambda e: e.dma_start(out=vtok[:], in_=self.VA.rearrange("(c p) v -> p c v", p=128)), reads=[self.r_VA], writes=[r_vtok])
            S.dma("sp", lambda e: e.dma_start(out=esb[:], in_=self.sink[l:l + 1, :].partition_broadcast(128)), writes=[r_esb])
            S.op("act", lambda e: e.activation(out=esb[:], in_=esb[:], func=AF.Exp), reads=[r_esb], writes=[r_esb])
            esb3 = esb[:].rearrange("p (c two) -> p c two", two=2)
            S.op("dve", lambda e: e.tensor_copy(out=es2[0:64, :], in_=esb3[0:64, :, 0]), reads=[r_esb], writes=[r_es2])
            S.op("dve", lambda e: e.tensor_copy(out=es2[64:128, :], in_=esb3[64:128, :, 1]), reads=[r_esb], writes=[r_es2])
            qblocks = []
            if l == 0:
                for m in range(NCT):
                    qblocks.append((m * 128, [(t, None) for t in range(NCT)]))
            for n in range(NQ):
                tiles = [(t, None) for t in range(NCT)]
                if n > 0:
                    tiles.append((NCT + n - 1, "prev"))
                tiles.append((NCT + n, None))
                if n < NQ - 1:
                    tiles.append((NCT + n + 1, "next"))
                qblocks.append((CTX + n * 128, tiles))
            for c in range(8):
                g = c // 4
                self.pump(cfg.PUMP)
                qc, r_qc = qpool.next()
                S.dma("sp", lambda e, c=c, qc=qc: e.dma_start(out=qc[:], in_=self.QA[c]), reads=[self.r_QA[c]], writes=[r_qc])
                oc, r_oc = opool.next()
                kops = [(kc_[0] if g == 0 else kc_[1], 0), (kc_[1] if g == 0 else kc_[0], 64)]
                for (q0, tiles) in qblocks:
                    nt = len(tiles)
                    pts = []
                    for hh in range(2):
                        (k_t, k_r), r0 = kops[hh]
                        ps, r_ps = psS.next()
                        for ti, (tile, mkind) in enumerate(tiles):
                            S.op("pe", lambda e, ps=ps, ti=ti, tile=tile, k_t=k_t, r0=r0, q0=q0, qc=qc, mkind=mkind: e.matmul(ps[:, ti, :], lhsT=k_t[r0:r0 + 64, tile * 128:(tile + 1) * 128], rhs=qc[r0:r0 + 64, q0:q0 + 128], start=True, stop=(mkind is None)),
                                 reads=[k_r, r_qc], writes=[r_ps], signal=(mkind is None and ti == nt - 1))
                            if mkind is not None:
                                m_t, m_r = (mprev, r_mprev) if mkind == "prev" else (mnext, r_mnext)
                                S.op("pe", lambda e, ps=ps, ti=ti, m_t=m_t: e.matmul(ps[:, ti, :], lhsT=ident[:], rhs=m_t[:], start=False, stop=True),
                                     reads=[r_ident, m_r], writes=[r_ps], signal=(ti == nt - 1))
                        pt, r_pt = ppool.next()
                        S.op("act", lambda e, pt=pt, ps=ps, nt=nt: e.activation(out=pt[:, 0:nt, :], in_=ps[:, 0:nt, :], func=AF.Exp), reads=[r_ps], writes=[r_pt])
                        pts.append((pt, r_pt))
                    od, r_od = psOD.next()
                    for hh in range(2):
                        pt, r_pt = pts[hh]
                        r0 = 64 * hh
                        tp = None if hh == 0 else (0, 64)
                        for ti, (tile, mkind) in enumerate(tiles):
                            S.op("pe", lambda e, od=od, r0=r0, ti=ti, tile=tile, pt=pt, tp=tp, nt=nt, g=g: e.matmul(od[r0:r0 + 64, 0, :], lhsT=vtok[:, tile, g * 64:(g + 1) * 64], rhs=pt[:, ti, :], start=(ti == 0), stop=(ti == nt - 1), tile_position=tp),
                                 reads=[r_vtok, r_pt], writes=[r_od], signal=False)
                        for ti, (tile, mkind) in enumerate(tiles):
                            S.op("pe", lambda e, od=od, r0=r0, ti=ti, pt=pt, tp=tp, nt=nt: e.matmul(od[r0:r0 + 64, 1, :], lhsT=onesb[:, 0:64], rhs=pt[:, ti, :], start=(ti == 0), stop=(ti == nt - 1), tile_position=tp),
                                 reads=[r_onesb, r_pt], writes=[r_od], signal=(ti == nt - 1))
                    rc, r_rc = rpool.next()
                    S.op("dve", lambda e, rc=rc, od=od, c=c: e.tensor_scalar(out=rc[:], in0=od[:, 1, :], scalar1=es2[:, c:c + 1], scalar2=None, op0=ALU.add), reads=[r_od, r_es2], writes=[r_rc])
                    S.op("dve", lambda e, rc=rc: e.reciprocal(out=rc[:], in_=rc[:]), reads=[r_rc], writes=[r_rc])
                    S.op("dve", lambda e, rc=rc, od=od, oc=oc, q0=q0: e.tensor_tensor(out=oc[:, q0:q0 + 128], in0=od[:, 0, :], in1=rc[:], op=ALU.mult), reads=[r_od, r_rc], writes=[r_oc])
                if l == 0:
                    S.dma("sp", lambda e, c=c, oc=oc: e.dma_start(out=self.OAT[c], in_=oc[:]), reads=[r_oc], writes=[self.r_OAT[c]])
                else:
                    S.dma("sp", lambda e, c=c, oc=oc: e.dma_start(out=self.OAT[c][:, CTX:T], in_=oc[:, CTX:T]), reads=[r_oc], writes=[self.r_OAT[c]])
            self.end_phase()

    def phase_merge(self, l, s):
        cfg, nc, S = self.cfg, self.nc, self.S
        D, KC, T, CTX = cfg.D, cfg.KC, cfg.T, cfg.CTX
        BIG, r_BIG = self.BIG
        blocks = cfg.tblocks if l == 0 else cfg.lat_blocks
        tiles = list(range(cfg.NT)) if l == 0 else list(range(cfg.NCT, cfg.NT))
        with ExitStack() as ph:
            aT, r_aT = tile1(ph, nc, "m_aT", [128, 8, T], BF16)
            oT, r_oT = tile1(ph, nc, "m_oT", [128, 8, T], BF16)
            S.dma("sp", lambda e: e.dma_start(out=aT[:], in_=self.AT.rearrange("c p t -> p c t")), reads=self.r_AT, writes=[r_aT])
            S.dma("sp", lambda e: e.dma_start(out=oT[:], in_=self.OAT.rearrange("c p t -> p c t")), reads=self.r_OAT, writes=[r_oT])
            wap = TPool(ph, nc, "m_wa", [128, 8, 512], BF16, 2)
            wbp = TPool(ph, nc, "m_wb", [128, 8, 512], BF16, 2)
            gap = TPool(ph, nc, "m_ga", [128, T], BF16, 2)
            gbp = TPool(ph, nc, "m_gb", [128, T], BF16, 2)
            tp = TPool(ph, nc, "m_t", [128, 512], F32, 4)
            ps1 = TPool(ph, nc, "m_ps1", [128, 512], F32, 2, psum=True)
            ps2 = TPool(ph, nc, "m_ps2", [128, 512], F32, 2, psum=True)
            for jb in range(4):
                wa, r_wa = self.load_wblock(wap, self.w_a[l], jb * 512, 512)
                wb, r_wb = self.load_wblock(wbp, self.w_b[l], jb * 512, 512)
                for c in range(4):
                    j = jb * 4 + c
                    ga, r_ga = gap.next()
                    gb, r_gb = gbp.next()
                    S.dma("sp", lambda e, ga=ga, j=j: e.dma_start(out=ga[:], in_=self.GA[j]), reads=[self.r_GA[j]], writes=[r_ga])
                    S.dma("sp", lambda e, gb=gb, j=j: e.dma_start(out=gb[:], in_=self.GB[j]), reads=[self.r_GB[j]], writes=[r_gb])
                    for (t0, n) in blocks:
                        p1, r_p1 = ps1.next()
                        p2, r_p2 = ps2.next()
                        for kc in range(8):
                            S.op("pe", lambda e, p1=p1, wa=wa, kc=kc, c=c, t0=t0, n=n: e.matmul(p1[:, 0:n], lhsT=wa[:, kc, c * 128:(c + 1) * 128], rhs=aT[:, kc, t0:t0 + n], start=(kc == 0), stop=(kc == 7)),
                                 reads=[r_wa, r_aT], writes=[r_p1], signal=(kc == 7))
                        for kc in range(8):
                            S.op("pe", lambda e, p2=p2, wb=wb, kc=kc, c=c, t0=t0, n=n: e.matmul(p2[:, 0:n], lhsT=wb[:, kc, c * 128:(c + 1) * 128], rhs=oT[:, kc, t0:t0 + n], start=(kc == 0), stop=(kc == 7)),
                                 reads=[r_wb, r_oT], writes=[r_p2], signal=(kc == 7))
                        t1, r_t1 = tp.next()
                        t2, r_t2 = tp.next()
                        S.op("dve", lambda e, t1=t1, p1=p1, ga=ga, t0=t0, n=n: e.tensor_tensor(out=t1[:, 0:n], in0=p1[:, 0:n], in1=ga[:, t0:t0 + n], op=ALU.mult), reads=[r_p1, r_ga], writes=[r_t1])
                        S.op("dve", lambda e, t2=t2, p2=p2, gb=gb, t0=t0, n=n: e.tensor_tensor(out=t2[:, 0:n], in0=p2[:, 0:n], in1=gb[:, t0:t0 + n], op=ALU.mult), reads=[r_p2, r_gb], writes=[r_t2])
                        S.op("pool", lambda e, t1=t1, t2=t2, j=j, t0=t0, n=n: e.tensor_tensor(out=BIG[:, j, t0:t0 + n], in0=t1[:, 0:n], in1=t2[:, 0:n], op=ALU.add), reads=[r_t1, r_t2], writes=[r_BIG])
            self.end_phase()
        with ExitStack() as ph:
            wop = TPool(ph, nc, "m_wo", [128, KC, 512], BF16, 2)
            g1 = {}
            for kind, row in (("lat", s), ("ctx", cfg.NSEQ)):
                g1[kind] = tile1(ph, nc, "m_g1" + kind, [128, D], F32)
                self.bcast_row(g1[kind][0], g1[kind][1], self.MOD[l, row:row + 1, 2 * D:3 * D], D)
            xp = TPool(ph, nc, "m_x", [128, 512], F32, 3)
            tp = TPool(ph, nc, "m_t2", [128, 512], F32, 2)
            xo = TPool(ph, nc, "m_xo", [128, 512], F32, 3)
            pso = TPool(ph, nc, "m_pso", [128, 512], F32, 3, psum=True)
            first = (l == 0)
            for nb in range(4):
                wo, r_wo = self.load_wblock(wop, self.w_o[l], nb * 512, 512)
                loaded = {}

                def load(i, nb=nb, loaded=loaded):
                    tile = tiles[i]
                    xt, r_xt = xp.next()
                    src = self.xsrc(first, s, tile)[:, nb * 512:(nb + 1) * 512]
                    S.dma("sp", lambda e, xt=xt, src=src: e.dma_start(out=xt[:], in_=src), reads=([] if first else [self.r_XR[s][tile]]), writes=[r_xt])
                    loaded[i] = (xt, r_xt)
                load(0)
                for i, tile in enumerate(tiles):
                    if i + 1 < len(tiles):
                        load(i + 1)
                    xt, r_xt = loaded.pop(i)
                    g_t, g_r = g1["ctx" if tile < cfg.NCT else "lat"]
                    ps, r_ps = pso.next()
                    for kc in range(KC):
                        S.op("pe", lambda e, ps=ps, kc=kc, tile=tile, wo=wo: e.matmul(ps[:], lhsT=BIG[:, kc, tile * 128:(tile + 1) * 128], rhs=wo[:, kc, :], start=(kc == 0), stop=(kc == KC - 1)),
                             reads=[r_BIG, r_wo], writes=[r_ps], signal=(kc == KC - 1))
                    t1, r_t1 = tp.next()
                    S.op("dve", lambda e, t1=t1, ps=ps, g_t=g_t, nb=nb: e.tensor_tensor(out=t1[:], in0=ps[:], in1=g_t[:, nb * 512:(nb + 1) * 512], op=ALU.mult), reads=[r_ps, g_r], writes=[r_t1])
                    xn, r_xn = xo.next()
                    S.op("dve", lambda e, xn=xn, t1=t1, xt=xt: e.tensor_tensor(out=xn[:], in0=t1[:], in1=xt[:], op=ALU.add), reads=[r_t1, r_xt], writes=[r_xn])
                    S.dma("sp", lambda e, xn=xn, tile=tile, nb=nb: e.dma_start(out=self.XR[s, tile * 128:(tile + 1) * 128, nb * 512:(nb + 1) * 512], in_=xn[:]), reads=[r_xn], writes=[self.r_XR[s][tile]])
            self.end_phase()

    def phase_swiglu(self, l, s):
        cfg, nc, S = self.cfg, self.nc, self.S
        D, KC, T, CTX, NCT = cfg.D, cfg.KC, cfg.T, cfg.CTX, cfg.NCT
        BIG, r_BIG = self.BIG
        moe = (l == 1)
        blocks = cfg.lat_blocks if moe else cfg.tblocks
        if moe:
            experts = [(self.moe_wg[0, e], self.moe_wu[0, e], self.moe_wd[0, e], cfg.DEXP, e) for e in range(8)]
        else:
            experts = [(self.ffn_wg[0], self.ffn_wu[0], self.ffn_wd[0], cfg.DFF, None)]
        GF = 256
        with ExitStack() as ph:
            g2 = {}
            for kind, row in (("lat", s), ("ctx", cfg.NSEQ)):
                if moe and kind == "ctx":
                    continue
                g2[kind] = tile1(ph, nc, "f_g2" + kind, [128, D], F32)
                self.bcast_row(g2[kind][0], g2[kind][1], self.MOD[l, row:row + 1, 5 * D:6 * D], D)
            comb = None
            if moe:
                comb = self.routing(ph, s)
            wgp = TPool(ph, nc, "f_wg", [128, KC, GF], BF16, 2)
            wup = TPool(ph, nc, "f_wu", [128, KC, GF], BF16, 2)
            wdp = TPool(ph, nc, "f_wd", [128, GF // 128, D], BF16, 2)
            acc, r_acc = tile1(ph, nc, "f_acc", [128, 4, D], F32)
            actp = TPool(ph, nc, "f_act", [128, GF // 128, 512], BF16, 2)
            sgp = TPool(ph, nc, "f_sg", [128, 512], F32, 2)
            xp = TPool(ph, nc, "f_x", [128, D], F32, 2)
            tp = TPool(ph, nc, "f_t", [128, D], F32, 1)
            psg = TPool(ph, nc, "f_psg", [128, 512], F32, 2, psum=True)
            psu = TPool(ph, nc, "f_psu", [128, 512], F32, 2, psum=True)
            pso = TPool(ph, nc, "f_pso", [128, 512], F32, 2 if moe else 3, psum=True)
            for (wg2d, wu2d, wd2d, dff, eidx) in experts:
                ngr = dff // GF
                for (t0, n) in blocks:
                    ntl = n // 128
                    for gr in range(ngr):
                        wg, r_wg = self.load_wblock(wgp, wg2d, gr * GF, GF)
                        wu, r_wu = self.load_wblock(wup, wu2d, gr * GF, GF)
                        wd, r_wd = wdp.next()
                        for hlf in range(2):
                            srcd = wd2d[gr * GF:(gr + 1) * GF, hlf * 1024:(hlf + 1) * 1024].rearrange("(c p) n -> p c n", p=128)
                            S.dma("pool", lambda e, wd=wd, srcd=srcd, hlf=hlf: e.dma_start(out=wd[:, :, hlf * 1024:(hlf + 1) * 1024], in_=srcd), writes=[r_wd])
                        act, r_act = actp.next()
                        for c in range(GF // 128):
                            pg, r_pg = psg.next()
                            pu, r_pu = psu.next()
                            for kc in range(KC):
                                S.op("pe", lambda e, pg=pg, wg=wg, kc=kc, c=c, t0=t0, n=n: e.matmul(pg[:, 0:n], lhsT=wg[:, kc, c * 128:(c + 1) * 128], rhs=BIG[:, kc, t0:t0 + n], start=(kc == 0), stop=(kc == KC - 1)),
                                     reads=[r_wg, r_BIG], writes=[r_pg], signal=(kc == KC - 1))
                            for kc in range(KC):
                                S.op("pe", lambda e, pu=pu, wu=wu, kc=kc, c=c, t0=t0, n=n: e.matmul(pu[:, 0:n], lhsT=wu[:, kc, c * 128:(c + 1) * 128], rhs=BIG[:, kc, t0:t0 + n], start=(kc == 0), stop=(kc == KC - 1)),
                                     reads=[r_wu, r_BIG], writes=[r_pu], signal=(kc == KC - 1))
                            sg, r_sg = sgp.next()
                            S.op("act", lambda e, sg=sg, pg=pg, n=n: e.activation(out=sg[:, 0:n], in_=pg[:, 0:n], func=AF.Silu), reads=[r_pg], writes=[r_sg])
                            S.op("dve", lambda e, act=act, sg=sg, pu=pu, c=c, n=n: e.tensor_tensor(out=act[:, c, 0:n], in0=sg[:, 0:n], in1=pu[:, 0:n], op=ALU.mult), reads=[r_sg, r_pu], writes=[r_act])
                        for tl in range(ntl):
                            for nb in range(4):
                                po, r_po = pso.next()
                                nch = GF // 128
                                for c in range(nch):
                                    S.op("pe", lambda e, po=po, act=act, wd=wd, c=c, tl=tl, nb=nb, nch=nch: e.matmul(po[:], lhsT=act[:, c, tl * 128:(tl + 1) * 128], rhs=wd[:, c, nb * 512:(nb + 1) * 512], start=(c == 0), stop=(c == nch - 1)),
                                         reads=[r_act, r_wd], writes=[r_po], signal=(c == nch - 1))
                                if gr == 0:
                                    S.op("act", lambda e, po=po, tl=tl, nb=nb: e.copy(out=acc[:, tl, nb * 512:(nb + 1) * 512], in_=po[:]), reads=[r_po], writes=[r_acc])
                                else:
                                    S.op("dve", lambda e, po=po, tl=tl, nb=nb: e.tensor_tensor(out=acc[:, tl, nb * 512:(nb + 1) * 512], in0=po[:], in1=acc[:, tl, nb * 512:(nb + 1) * 512], op=ALU.add), reads=[r_po, r_acc], writes=[r_acc])
                    for tl in range(ntl):
                        tile = t0 // 128 + tl
                        g_t, g_r = g2["ctx" if tile < NCT else "lat"]
                        xt, r_xt = xp.next()
                        S.dma("sp", lambda e, xt=xt, tile=tile: e.dma_start(out=xt[:], in_=self.XR[s, tile * 128:(tile + 1) * 128, :]), reads=[self.r_XR[s][tile]], writes=[r_xt])
                        t1, r_t1 = tp.next()
                        S.op("dve", lambda e, t1=t1, tl=tl, g_t=g_t: e.tensor_tensor(out=t1[:], in0=acc[:, tl, :], in1=g_t[:], op=ALU.mult), reads=[r_acc, g_r], writes=[r_t1])
                        if eidx is None:
                            S.op("dve", lambda e, t1=t1, xt=xt: e.tensor_tensor(out=xt[:], in0=t1[:], in1=xt[:], op=ALU.add), reads=[r_t1, r_xt], writes=[r_xt])
                        else:
                            cb, r_cb = comb
                            lt = tile - NCT
                            S.op("dve", lambda e, t1=t1, xt=xt, cb=cb, lt=lt, eidx=eidx: e.scalar_tensor_tensor(out=xt[:], in0=t1[:], scalar=cb[:, lt, eidx:eidx + 1], in1=xt[:], op0=ALU.mult, op1=ALU.add),
                                 reads=[r_t1, r_xt, r_cb], writes=[r_xt])
                        S.dma("sp", lambda e, xt=xt, tile=tile: e.dma_start(out=self.XR[s, tile * 128:(tile + 1) * 128, :], in_=xt[:]), reads=[r_xt], writes=[self.r_XR[s][tile]])
            self.end_phase()

    def routing(self, ph, s, want_sel=False):
        cfg, nc, S = self.cfg, self.nc, self.S
        D, KC, NCT = cfg.D, cfg.KC, cfg.NCT
        BIG, r_BIG = self.BIG
        NTl = cfg.SEQ // 128
        rf, r_rf = tile1(ph, nc, "r_rf", [128, KC, 8], F32)
        rh, r_rh = tile1(ph, nc, "r_rh", [128, KC, 8], BF16)
        rl, r_rl = tile1(ph, nc, "r_rl", [128, KC, 8], BF16)
        S.dma("sp", lambda e: e.dma_start(out=rf[:], in_=self.router[0].rearrange("(kc p) e -> p kc e", p=128)), writes=[r_rf])
        S.op("dve", lambda e: e.tensor_copy(out=rh[:], in_=rf[:]), reads=[r_rf], writes=[r_rh])
        S.op("dve", lambda e: e.tensor_tensor(out=rl[:], in0=rf[:], in1=rh[:], op=ALU.subtract), reads=[r_rf, r_rh], writes=[r_rl])
        lg, r_lg = tile1(ph, nc, "r_lg", [128, NTl, 8], F32)
        psl = TPool(ph, nc, "r_ps", [128, 8], F32, 2, psum=True)
        for lt in range(NTl):
            tile = NCT + lt
            ps, r_ps = psl.next()
            for i, (rt, rr) in enumerate(((rh, r_rh), (rl, r_rl))):
                for kc in range(KC):
                    S.op("pe", lambda e, ps=ps, rt=rt, kc=kc, tile=tile, i=i: e.matmul(ps[:], lhsT=BIG[:, kc, tile * 128:(tile + 1) * 128], rhs=rt[:, kc, :], start=(i == 0 and kc == 0), stop=(i == 1 and kc == KC - 1)),
                         reads=[r_BIG, rr], writes=[r_ps], signal=(i == 1 and kc == KC - 1))
            S.op("act", lambda e, ps=ps, lt=lt: e.copy(out=lg[:, lt, :], in_=ps[:]), reads=[r_ps], writes=[r_lg])
        shp = [128, NTl, 8]
        m1, r_m1 = tile1(ph, nc, "r_m1", [128, NTl], F32)
        m2, r_m2 = tile1(ph, nc, "r_m2", [128, NTl], F32)
        t8, r_t8 = tile1(ph, nc, "r_t8", shp, F32)
        l2, r_l2 = tile1(ph, nc, "r_l2", shp, F32)
        sel, r_sel = tile1(ph, nc, "r_sel", shp, F32)
        cb, r_cb = tile1(ph, nc, "r_cb", shp, F32)
        bc = lambda t: t[:].unsqueeze(2).broadcast_to(shp)
        S.op("dve", lambda e: e.tensor_reduce(out=m1[:], in_=lg[:], axis=AX.X, op=ALU.max), reads=[r_lg], writes=[r_m1])
        S.op("dve", lambda e: e.tensor_tensor(out=t8[:], in0=lg[:], in1=bc(m1), op=ALU.is_equal), reads=[r_lg, r_m1], writes=[r_t8])
        S.op("dve", lambda e: e.scalar_tensor_tensor(out=l2[:], in0=t8[:], scalar=-1e30, in1=lg[:], op0=ALU.mult, op1=ALU.add), reads=[r_t8, r_lg], writes=[r_l2])
        S.op("dve", lambda e: e.tensor_reduce(out=m2[:], in_=l2[:], axis=AX.X, op=ALU.max), reads=[r_l2], writes=[r_m2])
        S.op("dve", lambda e: e.tensor_tensor(out=sel[:], in0=lg[:], in1=bc(m2), op=ALU.is_ge), reads=[r_lg, r_m2], writes=[r_sel])
        S.op("dve", lambda e: e.tensor_tensor(out=t8[:], in0=lg[:], in1=bc(m1), op=ALU.subtract), reads=[r_lg, r_m1], writes=[r_t8])
        S.op("act", lambda e: e.activation(out=t8[:], in_=t8[:], func=AF.Exp), reads=[r_t8], writes=[r_t8])
        S.op("dve", lambda e: e.tensor_tensor(out=t8[:], in0=t8[:], in1=sel[:], op=ALU.mult), reads=[r_t8, r_sel], writes=[r_t8])
        S.op("dve", lambda e: e.tensor_tensor(out=m2[:], in0=m2[:], in1=m1[:], op=ALU.subtract), reads=[r_m2, r_m1], writes=[r_m2])
        S.op("act", lambda e: e.activation(out=m2[:], in_=m2[:], func=AF.Exp), reads=[r_m2], writes=[r_m2])
        S.op("dve", lambda e: e.tensor_scalar(out=m2[:], in0=m2[:], scalar1=1.0, scalar2=None, op0=ALU.add), reads=[r_m2], writes=[r_m2])
        S.op("dve", lambda e: e.reciprocal(out=m2[:], in_=m2[:]), reads=[r_m2], writes=[r_m2])
        S.op("dve", lambda e: e.tensor_tensor(out=cb[:], in0=t8[:], in1=bc(m2), op=ALU.mult), reads=[r_t8, r_m2], writes=[r_cb])
        if want_sel:
            return cb, r_cb, sel, r_sel
        return cb, r_cb

    def phase_final(self):
        cfg, nc, S = self.cfg, self.nc, self.S
        D, NCT = cfg.D, cfg.NCT
        with ExitStack() as ph:
            fn, r_fn = tile1(ph, nc, "z_fn", [128, D], F32)
            S.dma("sp", lambda e: e.dma_start(out=fn[:], in_=self.final_norm.unsqueeze(0).partition_broadcast(128)), writes=[r_fn])
            xp = TPool(ph, nc, "z_x", [128, D], F32, 3)
            op_ = TPool(ph, nc, "z_o", [128, D], F32, 2)
            junk, r_junk = tile1(ph, nc, "z_junk", [128, D], BF16)
            stp = TPool(ph, nc, "z_st", [128, 4], F32, 2)
            jobs = [(s, tile) for s in range(cfg.NSEQ) for tile in range(NCT, cfg.NT)]
            loaded = {}

            def load(i):
                s, tile = jobs[i]
                xt, r_xt = xp.next()
                S.dma("sp", lambda e, xt=xt, s=s, tile=tile: e.dma_start(out=xt[:], in_=self.XR[s, tile * 128:(tile + 1) * 128, :]), reads=[self.r_XR[s][tile]], writes=[r_xt])
                loaded[i] = (xt, r_xt)
            load(0)
            for i, (s, tile) in enumerate(jobs):
                if i + 1 < len(jobs):
                    load(i + 1)
                xt, r_xt = loaded.pop(i)
                st, r_st = stp.next()
                S.op("act", lambda e, xt=xt, st=st: e.activation(out=junk[:], in_=xt[:], func=AF.Square, accum_out=st[:, 0:1]), reads=[r_xt], writes=[r_junk, r_st])
                S.op("dve", lambda e, st=st: e.tensor_scalar(out=st[:, 1:2], in0=st[:, 0:1], scalar1=1.0 / D, scalar2=EPS, op0=ALU.mult, op1=ALU.add), reads=[r_st], writes=[r_st])
                S.op("act", lambda e, st=st: e.activation(out=st[:, 2:3], in_=st[:, 1:2], func=AF.Sqrt), reads=[r_st], writes=[r_st])
                S.op("dve", lambda e, st=st: e.reciprocal(out=st[:, 3:4], in_=st[:, 2:3]), reads=[r_st], writes=[r_st])
                ot, r_ot = op_.next()
                S.op("dve", lambda e, ot=ot, xt=xt, st=st: e.scalar_tensor_tensor(out=ot[:], in0=xt[:], scalar=st[:, 3:4], in1=fn[:], op0=ALU.mult, op1=ALU.mult), reads=[r_xt, r_st, r_fn], writes=[r_ot])
                lt = tile - NCT
                S.dma("sp", lambda e, ot=ot, s=s, lt=lt: e.dma_start(out=self.out[s, lt * 128:(lt + 1) * 128, :], in_=ot[:]), reads=[r_ot], writes=[self.r_out])
            self.end_phase()

    def prepass_jobs(self):
        cfg = self.cfg
        NG = cfg.DEXP // 512
        jobs = []
        for e_ in range(8):
            for g in range(NG):
                row0 = (e_ * NG + g) * 128
                for (dst_t, src_t) in ((self.WGB, self.moe_wg), (self.WUB, self.moe_wu)):
                    dst = dst_t[row0:row0 + 128, :].rearrange("p (kc n) -> p kc n", n=512)
                    src = src_t[0, e_, :, g * 512:(g + 1) * 512].rearrange("(kc p) n -> p kc n", p=128)
                    jobs.append((dst, src))
                for hlf in range(2):
                    dst = self.WDB[row0:row0 + 128, :].rearrange("p (c n) -> p c n", n=2048)[:, :, hlf * 1024:(hlf + 1) * 1024]
                    src = self.moe_wd[0, e_, g * 512:(g + 1) * 512, hlf * 1024:(hlf + 1) * 1024].rearrange("(c p) n -> p c n", p=128)
                    jobs.append((dst, src))
        return jobs

    def pump(self, k):
        if self.cfg.MOE != "routed":
            return
        for _ in range(k):
            if not self.pre_jobs:
                return
            dst, src = self.pre_jobs.pop(0)
            self.S.dma("pool", lambda e, dst=dst, src=src: e.dma_start(out=dst, in_=src))

    def route_local(self, s):
        cfg, nc, S = self.cfg, self.nc, self.S
        NTl = cfg.SEQ // 128
        with ExitStack() as ph:
            cb, r_cb, sel, r_sel = self.routing(ph, s, want_sel=True)
            S.dma("sp", lambda e: e.dma_start(out=self.SELD[:, s * NTl:(s + 1) * NTl, :], in_=sel[:]), reads=[r_sel], writes=[self.r_SELD])
            S.dma("sp", lambda e: e.dma_start(out=self.CBD[:, s * NTl:(s + 1) * NTl, :], in_=cb[:]), reads=[r_cb], writes=[self.r_SELD])
            self.end_phase()

    def phase_moe_routed(self):
        cfg, nc, S = self.cfg, self.nc, self.S
        D, KC, NCT = cfg.D, cfg.KC, cfg.NCT
        NTl = cfg.SEQ // 128
        NTg = cfg.NSEQ * NTl
        NSLOT = cfg.NSLOT
        NG = cfg.DEXP // 512
        ident, r_ident = self.C["ident"]
        ustrict, r_us = self.C["ustrict"]
        onesb, r_onesb = self.C["ones"]
        shp = [128, NTg, 8]
        self.pump(100000)
        with ExitStack() as ms:
            posA, r_posA = tile1(ms, nc, "q_posA", [128, NTg], I32)
            posB, r_posB = tile1(ms, nc, "q_posB", [128, NTg], I32)
            wA, r_wA = tile1(ms, nc, "q_wA", [128, NTg], F32)
            wB, r_wB = tile1(ms, nc, "q_wB", [128, NTg], F32)
            idxW, r_idxW = tile1(ms, nc, "q_idxW", [128, NSLOT, NG], I32)
            with ExitStack() as ph:
                sel, r_sel = tile1(ph, nc, "q_sel", shp, F32)
                cb, r_cb = tile1(ph, nc, "q_cb", shp, F32)
                selb, r_selb = tile1(ph, nc, "q_selb", shp, BF16)
                S.dma("sp", lambda e: e.dma_start(out=sel[:], in_=self.SELD), reads=[self.r_SELD], writes=[r_sel])
                S.dma("sp", lambda e: e.dma_start(out=cb[:], in_=self.CBD), reads=[self.r_SELD], writes=[r_cb])
                S.op("dve", lambda e: e.tensor_copy(out=selb[:], in_=sel[:]), reads=[r_sel], writes=[r_selb])
                pR, r_pR = tile1(ph, nc, "q_pR", [128, NTg * 8], F32, psum=True)
                pC, r_pC = tile1(ph, nc, "q_pC", [128, NTg * 8], F32, psum=True)
                flat = lambda t: t[:].rearrange("p g e -> p (g e)")
                S.op("pe", lambda e: e.matmul(pR[:], lhsT=ustrict[:], rhs=flat(selb), start=True, stop=True), reads=[r_us, r_selb], writes=[r_pR])
                S.op("pe", lambda e: e.matmul(pC[:], lhsT=onesb[:], rhs=flat(selb), start=True, stop=True), reads=[r_onesb, r_selb], writes=[r_pC])
                Cs, r_Cs = tile1(ph, nc, "q_Cs", shp, F32)
                incl, r_incl = tile1(ph, nc, "q_incl", shp, F32)
                pos, r_pos = tile1(ph, nc, "q_pos", shp, F32)
                v, r_v = tile1(ph, nc, "q_v", shp, F32)
                t8, r_t8 = tile1(ph, nc, "q_t8", shp, F32)
                o32, r_o32 = tile1(ph, nc, "q_o32", [128, NTg], F32)
                S.op("dve", lambda e: e.memset(o32[:], 1.0), writes=[r_o32])
                S.op("act", lambda e: e.copy(out=flat(Cs), in_=pC[:]), reads=[r_pC], writes=[r_Cs])
                for e_ in range(8):
                    S.op("dve", lambda e, e_=e_: e.tensor_tensor_scan(out=incl[:, :, e_], data0=o32[:], data1=Cs[:, :, e_], initial=0.0, op0=ALU.mult, op1=ALU.add),
                         reads=[r_o32, r_Cs], writes=[r_incl])
                sm = lambda name, w: tile1(ph, nc, name, [128, w], F32)
                n_e, r_n = sm("q_n", 8)
                np_e, r_np = sm("q_np", 8)
                base, r_base = sm("q_base", 8)
                cs, r_cs = sm("q_cs", 8)
                S.op("dve", lambda e: e.tensor_copy(out=n_e[:], in_=incl[:, NTg - 1, :]), reads=[r_incl], writes=[r_n])
                KMAX = max(1, (cfg.NSEQ * cfg.SEQ) // 512)
                kg, r_kg = tile1(ph, nc, "q_kg", [128, 8, KMAX], F32)
                S.dma("sp", lambda e: e.dma_start(out=kg[:], in_=self.cst_d["kgrid"].rearrange("p (e k) -> p e k", k=KMAX)), writes=[r_kg])
                S.op("dve", lambda e: e.tensor_tensor(out=kg[:], in0=n_e[:].unsqueeze(2).broadcast_to([128, 8, KMAX]), in1=kg[:], op=ALU.is_gt), reads=[r_n, r_kg], writes=[r_kg])
                S.op("dve", lambda e: e.tensor_reduce(out=np_e[:], in_=kg[:], axis=AX.X, op=ALU.add), reads=[r_kg], writes=[r_np])
                S.op("dve", lambda e: e.tensor_scalar(out=np_e[:], in0=np_e[:], scalar1=512.0, scalar2=None, op0=ALU.mult), reads=[r_np], writes=[r_np])
                S.op("dve", lambda e: e.memset(base[:], 0.0), writes=[r_base])
                for e_ in range(1, 8):
                    S.op("dve", lambda e, e_=e_: e.tensor_tensor(out=base[:, e_:e_ + 1], in0=base[:, e_ - 1:e_], in1=np_e[:, e_ - 1:e_], op=ALU.add), reads=[r_base, r_np], writes=[r_base])
                S.op("dve", lambda e: e.tensor_tensor(out=pos[:], in0=incl[:], in1=Cs[:], op=ALU.subtract), reads=[r_incl, r_Cs], writes=[r_pos])
                S.op("dve", lambda e: e.tensor_tensor(out=flat(pos), in0=pR[:], in1=flat(pos), op=ALU.add), reads=[r_pR, r_pos], writes=[r_pos])
                S.op("dve", lambda e: e.tensor_tensor(out=pos[:], in0=pos[:], in1=base[:].unsqueeze(1).broadcast_to(shp), op=ALU.add), reads=[r_pos, r_base], writes=[r_pos])
                S.op("dve", lambda e: e.scalar_tensor_tensor(out=v[:], in0=pos[:], scalar=1.0, in1=sel[:], op0=ALU.add, op1=ALU.mult), reads=[r_pos, r_sel], writes=[r_v])
                pA1, r_pA1 = sm("q_pA1", NTg)
                pB1, r_pB1 = sm("q_pB1", NTg)
                bc = lambda t: t[:].unsqueeze(2).broadcast_to(shp)
                S.op("dve", lambda e: e.tensor_reduce(out=pA1[:], in_=v[:], axis=AX.X, op=ALU.max), reads=[r_v], writes=[r_pA1])
                S.op("dve", lambda e: e.tensor_tensor(out=t8[:], in0=v[:], in1=bc(pA1), op=ALU.is_equal), reads=[r_v, r_pA1], writes=[r_t8])
                S.op("dve", lambda e: e.tensor_tensor(out=pos[:], in0=t8[:], in1=cb[:], op=ALU.mult), reads=[r_t8, r_cb], writes=[r_pos])
                S.op("dve", lambda e: e.tensor_reduce(out=wA[:], in_=pos[:], axis=AX.X, op=ALU.add), reads=[r_pos], writes=[r_wA])
                S.op("dve", lambda e: e.tensor_scalar(out=wB[:], in0=wA[:], scalar1=-1.0, scalar2=1.0, op0=ALU.mult, op1=ALU.add), reads=[r_wA], writes=[r_wB])
                S.op("dve", lambda e: e.tensor_tensor(out=t8[:], in0=t8[:], in1=v[:], op=ALU.mult), reads=[r_t8, r_v], writes=[r_t8])
                S.op("dve", lambda e: e.tensor_tensor(out=t8[:], in0=v[:], in1=t8[:], op=ALU.subtract), reads=[r_t8, r_v], writes=[r_t8])
                S.op("dve", lambda e: e.tensor_reduce(out=pB1[:], in_=t8[:], axis=AX.X, op=ALU.max), reads=[r_t8], writes=[r_pB1])
                S.op("dve", lambda e: e.tensor_scalar(out=posA[:], in0=pA1[:], scalar1=-1.0, scalar2=None, op0=ALU.add), reads=[r_pA1], writes=[r_posA])
                S.op("dve", lambda e: e.tensor_scalar(out=posB[:], in0=pB1[:], scalar1=-1.0, scalar2=None, op0=ALU.add), reads=[r_pB1], writes=[r_posB])
                S.op("dve", lambda e: e.tensor_tensor(out=cs[:], in0=base[:], in1=np_e[:], op=ALU.add), reads=[r_base, r_np], writes=[r_cs])
                jg, r_jg = tile1(ph, nc, "q_jg", [128, NSLOT, 8], F32)
                gp, r_gp = tile1(ph, nc, "q_gp", [128, NG], F32)
                S.dma("sp", lambda e: e.dma_start(out=jg[:], in_=self.cst_d["jgrid"].rearrange("p (j e) -> p j e", e=8)), writes=[r_jg])
                S.dma("sp", lambda e: e.dma_start(out=gp[:], in_=self.cst_d["gp"][:, :]), writes=[r_gp])
                S.op("dve", lambda e: e.tensor_tensor(out=jg[:], in0=cs[:].unsqueeze(1).broadcast_to([128, NSLOT, 8]), in1=jg[:], op=ALU.is_le), reads=[r_cs, r_jg], writes=[r_jg])
                eid, r_eid = sm("q_eid", NSLOT)
                S.op("dve", lambda e: e.tensor_reduce(out=eid[:], in_=jg[:], axis=AX.X, op=ALU.add), reads=[r_jg], writes=[r_eid])
                S.op("dve", lambda e: e.tensor_scalar(out=eid[:], in0=eid[:], scalar1=7.0, scalar2=None, op0=ALU.min), reads=[r_eid], writes=[r_eid])
                S.op("dve", lambda e: e.scalar_tensor_tensor(out=idxW[:], in0=eid[:].unsqueeze(2).broadcast_to([128, NSLOT, NG]), scalar=float(NG * 128), in1=gp[:].unsqueeze(1).broadcast_to([128, NSLOT, NG]), op0=ALU.mult, op1=ALU.add),
                     reads=[r_eid, r_gp], writes=[r_idxW])
                if cfg.DEBUG:
                    self.dump("posA", posA, r_posA, [128, NTg], I32)
                    self.dump("posB", posB, r_posB, [128, NTg], I32)
                    self.dump("wA", wA, r_wA, [128, NTg], F32)
                    self.dump("idxW", idxW, r_idxW, [128, NSLOT, NG], I32)
                    self.dump("eid", eid, r_eid, [128, NSLOT], F32)
                    self.dump("cs", cs, r_cs, [128, 8], F32)
                    self.dump("jg", jg, r_jg, [128, NSLOT, 8], F32)
                    self.dump("incl", incl, r_incl, shp, F32)
                    self.dump("Cs", Cs, r_Cs, shp, F32)
                    self.dump("np", np_e, r_np, [128, 8], F32)
                    self.dump("base", base, r_base, [128, 8], F32)
                self.end_phase()
                if cfg.STOP == "route":
                    return
            with ExitStack() as ph:
                hp = TPool(ph, nc, "q_h", [128, D], BF16, 3)
                zt, r_zt = tile1(ph, nc, "q_z", [128, 4, D], BF16)
                S.op("dve", lambda e: e.memset(zt[:], 0.0), writes=[r_zt])
                for j in range(NSLOT):
                    S.dma("sp", lambda e, j=j: e.dma_start(out=self.HS[j * 512:(j + 1) * 512, :].rearrange("(a p) d -> p a d", p=128), in_=zt[:]), reads=[r_zt], writes=[self.r_HS])
                for g in range(NTg):
                    ht, r_ht = hp.next()
                    S.dma("sp", lambda e, ht=ht, g=g: e.dma_start(out=ht[:], in_=self.H2[g * 128:(g + 1) * 128, :]), reads=[self.r_H2], writes=[r_ht])
                    for (pt, pr) in ((posA, r_posA), (posB, r_posB)):
                        S.dma("pool", lambda e, ht=ht, g=g, pt=pt: e.indirect_dma_start(out=self.HS, out_offset=bass.IndirectOffsetOnAxis(ap=pt[:, g:g + 1], axis=0), in_=ht[:], in_offset=None),
                              reads=[r_ht, pr], writes=[self.r_HS])
                self.end_phase()
                if cfg.STOP == "scatter":
                    return
            with ExitStack() as ph:
                hrp = TPool(ph, nc, "q_hr", [128, D], BF16, 2)
                hTp = TPool(ph, nc, "q_hT", [128, KC, 512], BF16, 2)
                wgp = TPool(ph, nc, "q_wg", [128, KC * 512], BF16, 2)
                wup = TPool(ph, nc, "q_wu", [128, KC * 512], BF16, 2)
                wdp = TPool(ph, nc, "q_wd", [128, 4 * D], BF16, 2)
                acc, r_acc = tile1(ph, nc, "q_acc", [128, 4, D], F32)
                actp = TPool(ph, nc, "q_act", [128, 4, 512], BF16, 2)
                sgp = TPool(ph, nc, "q_sg", [128, 512], F32, 2)
                psT = TPool(ph, nc, "q_psT", [128, KC, 128], BF16, 1, psum=True)
                psg = TPool(ph, nc, "q_psg", [128, 512], F32, 2, psum=True)
                psu = TPool(ph, nc, "q_psu", [128, 512], F32, 2, psum=True)
                pso = TPool(ph, nc, "q_pso", [128, 512], F32, 2, psum=True)
                for j in range(NSLOT):
                    hT, r_hT = hTp.next()
                    for tl in range(4):
                        hr, r_hr = hrp.next()
                        S.dma("sp", lambda e, hr=hr, j=j, tl=tl: e.dma_start(out=hr[:], in_=self.HS[j * 512 + tl * 128:j * 512 + (tl + 1) * 128, :]), reads=[self.r_HS], writes=[r_hr])
                        ps, r_ps = psT.next()
                        for kc in range(KC):
                            S.op("pe", lambda e, ps=ps, hr=hr, kc=kc: e.transpose(out=ps[:, kc, :], in_=hr[:, kc * 128:(kc + 1) * 128], identity=ident[:]),
                                 reads=[r_hr, r_ident], writes=[r_ps], signal=(kc == KC - 1))
                        S.op("act", lambda e, ps=ps, hT=hT, tl=tl: e.copy(out=hT[:, :, tl * 128:(tl + 1) * 128], in_=ps[:]), reads=[r_ps], writes=[r_hT])
                    for gr in range(NG):
                        wts = []
                        for (pool_, src_) in ((wgp, self.WGB), (wup, self.WUB), (wdp, self.WDB)):
                            wt, r_wt = pool_.next()
                            S.dma("pool", lambda e, wt=wt, src_=src_, j=j, gr=gr: e.indirect_dma_start(out=wt[:], out_offset=None, in_=src_, in_offset=bass.IndirectOffsetOnAxis(ap=idxW[:, j, gr:gr + 1], axis=0)),
                                  reads=[r_idxW, self.r_WB], writes=[r_wt])
                            wts.append((wt, r_wt))
                        (wg_, r_wg), (wu_, r_wu), (wd_, r_wd) = wts
                        wg = wg_[:].rearrange("p (kc n) -> p kc n", n=512)
                        wu = wu_[:].rearrange("p (kc n) -> p kc n", n=512)
                        wd = wd_[:].rearrange("p (c n) -> p c n", n=D)
                        act, r_act = actp.next()
                        for c in range(4):
                            pg, r_pg = psg.next()
                            pu, r_pu = psu.next()
                            for kc in range(KC):
                                S.op("pe", lambda e, pg=pg, wg=wg, kc=kc, c=c, hT=hT: e.matmul(pg[:], lhsT=wg[:, kc, c * 128:(c + 1) * 128], rhs=hT[:, kc, :], start=(kc == 0), stop=(kc == KC - 1)),
                                     reads=[r_wg, r_hT], writes=[r_pg], signal=(kc == KC - 1))
                            for kc in range(KC):
                                S.op("pe", lambda e, pu=pu, wu=wu, kc=kc, c=c, hT=hT: e.matmul(pu[:], lhsT=wu[:, kc, c * 128:(c + 1) * 128], rhs=hT[:, kc, :], start=(kc == 0), stop=(kc == KC - 1)),
                                     reads=[r_wu, r_hT], writes=[r_pu], signal=(kc == KC - 1))
                            sg, r_sg = sgp.next()
                            S.op("act", lambda e, sg=sg, pg=pg: e.activation(out=sg[:], in_=pg[:], func=AF.Silu), reads=[r_pg], writes=[r_sg])
                            S.op("dve", lambda e, act=act, sg=sg, pu=pu, c=c: e.tensor_tensor(out=act[:, c, :], in0=sg[:], in1=pu[:], op=ALU.mult), reads=[r_sg, r_pu], writes=[r_act])
                        for tl in range(4):
                            for nb in range(4):
                                po, r_po = pso.next()
                                for c in range(4):
                                    S.op("pe", lambda e, po=po, act=act, wd=wd, c=c, tl=tl, nb=nb: e.matmul(po[:], lhsT=act[:, c, tl * 128:(tl + 1) * 128], rhs=wd[:, c, nb * 512:(nb + 1) * 512], start=(c == 0), stop=(c == 3)),
                                         reads=[r_act, r_wd], writes=[r_po], signal=(c == 3))
                                if gr == 0:
                                    S.op("act", lambda e, po=po, tl=tl, nb=nb: e.copy(out=acc[:, tl, nb * 512:(nb + 1) * 512], in_=po[:]), reads=[r_po], writes=[r_acc])
                                else:
                                    S.op("dve", lambda e, po=po, tl=tl, nb=nb: e.tensor_tensor(out=acc[:, tl, nb * 512:(nb + 1) * 512], in0=po[:], in1=acc[:, tl, nb * 512:(nb + 1) * 512], op=ALU.add), reads=[r_po, r_acc], writes=[r_acc])
                    for tl in range(4):
                        S.dma("sp", lambda e, j=j, tl=tl: e.dma_start(out=self.YP[j * 512 + tl * 128:j * 512 + (tl + 1) * 128, :], in_=acc[:, tl, :]), reads=[r_acc], writes=[self.r_YP])
                self.end_phase()
            if cfg.STOP == "slots":
                return
            with ExitStack() as ph:
                fn, r_fn = tile1(ph, nc, "z_fn", [128, D], F32)
                S.dma("sp", lambda e: e.dma_start(out=fn[:], in_=self.final_norm.unsqueeze(0).partition_broadcast(128)), writes=[r_fn])
                g2 = []
                for s in range(cfg.NSEQ):
                    g2.append(tile1(ph, nc, "z_g2%d" % s, [128, D], F32))
                    self.bcast_row(g2[s][0], g2[s][1], self.MOD[1, s:s + 1, 5 * D:6 * D], D)
                xp = TPool(ph, nc, "z_x", [128, D], F32, 2)
                yap = TPool(ph, nc, "z_ya", [128, D], F32, 2)
                ybp = TPool(ph, nc, "z_yb", [128, D], F32, 2)
                op_ = TPool(ph, nc, "z_o", [128, D], F32, 2)
                junk, r_junk = tile1(ph, nc, "z_junk", [128, D], BF16)
                stp = TPool(ph, nc, "z_st", [128, 4], F32, 2)
                for g in range(NTg):
                    s, lt = g // NTl, g % NTl
                    tile = NCT + lt
                    xt, r_xt = xp.next()
                    S.dma("sp", lambda e, xt=xt, s=s, tile=tile: e.dma_start(out=xt[:], in_=self.XR[s, tile * 128:(tile + 1) * 128, :]), reads=[self.r_XR[s][tile]], writes=[r_xt])
                    ya, r_ya = yap.next()
                    yb, r_yb = ybp.next()
                    for (yt, r_yt, pt, pr) in ((ya, r_ya, posA, r_posA), (yb, r_yb, posB, r_posB)):
                        S.dma("pool", lambda e, yt=yt, pt=pt, g=g: e.indirect_dma_start(out=yt[:], out_offset=None, in_=self.YP, in_offset=bass.IndirectOffsetOnAxis(ap=pt[:, g:g + 1], axis=0)),
                              reads=[self.r_YP, pr], writes=[r_yt])
                    S.op("dve", lambda e, ya=ya, g=g: e.tensor_scalar(out=ya[:], in0=ya[:], scalar1=wA[:, g:g + 1], scalar2=None, op0=ALU.mult), reads=[r_ya, r_wA], writes=[r_ya])
                    S.op("dve", lambda e, ya=ya, yb=yb, g=g: e.scalar_tensor_tensor(out=ya[:], in0=yb[:], scalar=wB[:, g:g + 1], in1=ya[:], op0=ALU.mult, op1=ALU.add), reads=[r_ya, r_yb, r_wB], writes=[r_ya])
                    S.op("dve", lambda e, ya=ya, s=s: e.tensor_tensor(out=ya[:], in0=ya[:], in1=g2[s][0][:], op=ALU.mult), reads=[r_ya, g2[s][1]], writes=[r_ya])
                    S.op("dve", lambda e, ya=ya, xt=xt: e.tensor_tensor(out=xt[:], in0=ya[:], in1=xt[:], op=ALU.add), reads=[r_ya, r_xt], writes=[r_xt])
                    st, r_st = stp.next()
                    S.op("act", lambda e, xt=xt, st=st: e.activation(out=junk[:], in_=xt[:], func=AF.Square, accum_out=st[:, 0:1]), reads=[r_xt], writes=[r_junk, r_st])
                    S.op("dve", lambda e, st=st: e.tensor_scalar(out=st[:, 1:2], in0=st[:, 0:1], scalar1=1.0 / D, scalar2=EPS, op0=ALU.mult, op1=ALU.add), reads=[r_st], writes=[r_st])
                    S.op("act", lambda e, st=st: e.activation(out=st[:, 2:3], in_=st[:, 1:2], func=AF.Sqrt), reads=[r_st], writes=[r_st])
                    S.op("dve", lambda e, st=st: e.reciprocal(out=st[:, 3:4], in_=st[:, 2:3]), reads=[r_st], writes=[r_st])
                    ot, r_ot = op_.next()
                    S.op("dve", lambda e, ot=ot, xt=xt, st=st: e.scalar_tensor_tensor(out=ot[:], in0=xt[:], scalar=st[:, 3:4], in1=fn[:], op0=ALU.mult, op1=ALU.mult), reads=[r_xt, r_st, r_fn], writes=[r_ot])
                    S.dma("sp", lambda e, ot=ot, s=s, lt=lt: e.dma_start(out=self.out[s, lt * 128:(lt + 1) * 128, :], in_=ot[:]), reads=[r_ot], writes=[self.r_out])
                self.end_phase()


def make_in_maps(cfg, inputs, ncores):
    consts = host_consts(cfg)
    maps = []
    for core in range(ncores):
        b0 = core * cfg.NSEQ
        m = {}
        m["x_in"] = np.ascontiguousarray(inputs["x"][b0:b0 + cfg.NSEQ])
        m["ctx_in"] = np.ascontiguousarray(inputs["ctx"][b0:b0 + cfg.NSEQ])
        m["cvec"] = np.ascontiguousarray(np.concatenate([inputs["c"][b0:b0 + cfg.NSEQ], inputs["c_ctx"][None, :]], 0))
        for k in ("w_mod", "b_mod", "norm_mix", "norm_ffn", "w_in", "hg_lb_fwd", "hg_lb_bwd", "hg_norm", "attn_sink",
                  "w_branch_a", "w_branch_b", "w_out", "ffn_w_gate", "ffn_w_up", "ffn_w_down", "moe_router",
                  "moe_w_gate", "moe_w_up", "moe_w_down", "final_norm"):
            m[k] = inputs[k]
        for k, v in consts.items():
            m["c_" + k] = v
        maps.append(m)
    return maps


_CACHE = {}


def kernel(**inputs):
    cfg = Cfg()
    inputs = {k: np.asarray(v) for k, v in inputs.items()}
    if "nc" not in _CACHE:
        _CACHE["nc"] = K(cfg).build()
    nc = _CACHE["nc"]
    ncores = 16 // cfg.NSEQ
    maps = make_in_maps(cfg, inputs, ncores)
    res = run_bass_kernel_spmd(nc, maps, core_ids=list(range(ncores)))
    out = np.concatenate([np.asarray(r["out"]) for r in res.results], axis=0)
    return out.astype(np.float32, copy=False)
```

```python
import numpy as np
from contextlib import ExitStack
import concourse.bass as bass
import concourse.mybir as mybir
from concourse.bass_utils import run_bass_kernel_spmd

F32 = mybir.dt.float32
BF16 = mybir.dt.bfloat16
I32 = mybir.dt.int32
AF = mybir.ActivationFunctionType
ALU = mybir.AluOpType
AX = mybir.AxisListType

EPS = 1e-6
NEG = -30000.0


class Res:
    __slots__ = ("name", "last_w", "readers")

    def __init__(self, name=""):
        self.name = name
        self.last_w = None
        self.readers = {}


class Sched:
    COMPUTE = ("pe", "act", "dve", "pool")
    NDMA = 8

    def __init__(self, nc, es):
        self.nc = nc
        self.sem = {}
        for e in self.COMPUTE:
            self.sem[e] = es.enter_context(nc.semaphore("sem_" + e))
        self.count = {e: 0 for e in self.COMPUTE}
        self.engs = ("pe", "act", "dve", "pool", "sp")
        self.seen = {e: {} for e in self.engs}
        self.q = {e: [] for e in self.engs}
        self.dma_uses = {}
        self.dma_i = {}
        for e in ("sp", "act", "pool"):
            for i in range(self.NDMA):
                self.sem[("d", e, i)] = es.enter_context(nc.semaphore("dsem_%s%d" % (e, i)))
            self.dma_uses[e] = [0] * self.NDMA
            self.dma_i[e] = 0
        self.ninstr = 0

    def _deps(self, reads, writes):
        deps = {}
        for r in reads:
            ev = r.last_w
            if ev is not None and deps.get(ev[0], 0) < ev[1]:
                deps[ev[0]] = ev[1]
        for w in writes:
            ev = w.last_w
            if ev is not None and deps.get(ev[0], 0) < ev[1]:
                deps[ev[0]] = ev[1]
            for k, v in w.readers.items():
                if deps.get(k, 0) < v:
                    deps[k] = v
        return deps

    def _waits(self, eng, deps, skip=None):
        waits = []
        seen = self.seen[eng]
        for k, v in deps.items():
            if k == skip or seen.get(k, 0) >= v:
                continue
            seen[k] = v
            waits.append((k, v))
        return waits

    def _commit(self, ev, reads, writes):
        k, v = ev
        for r in reads:
            if r.readers.get(k, 0) < v:
                r.readers[k] = v
        for w in writes:
            w.last_w = ev
            w.readers = {}

    def op(self, eng, fn, reads=(), writes=(), signal=True):
        deps = self._deps(reads, writes)
        waits = self._waits(eng, deps, skip=("pe" if eng == "pe" else None))
        sem = self.sem
        if signal:
            self.count[eng] += 1
            ev = (eng, self.count[eng])
            own = sem[eng]
        else:
            ev = (eng, self.count[eng] + 1)
            own = None

        def run(e, waits=waits, fn=fn, own=own):
            for k, v in waits:
                e.wait_ge(sem[k], v)
            ins = fn(e)
            if own is not None:
                ins.then_inc(own, 1)
        self.q[eng].append(run)
        self._commit(ev, reads, writes)
        self.ninstr += 1
        return ev

    def dma(self, eng, fn, reads=(), writes=()):
        i = self.dma_i[eng] % self.NDMA
        self.dma_i[eng] += 1
        key = ("d", eng, i)
        prev = 16 * self.dma_uses[eng][i]
        self.dma_uses[eng][i] += 1
        ev = (key, prev + 16)
        deps = self._deps(reads, writes)
        if prev > 0 and deps.get(key, 0) < prev:
            deps[key] = prev
        waits = self._waits(eng, deps)
        sem = self.sem
        own = sem[key]

        def run(e, waits=waits, fn=fn, own=own):
            for k, v in waits:
                e.wait_ge(sem[k], v)
            fn(e).then_inc(own, 16)
        self.q[eng].append(run)
        self._commit(ev, reads, writes)
        self.ninstr += 1
        return ev

    def barrier(self):
        targets = {}
        for e in self.COMPUTE:
            if self.count[e] > 0:
                targets[e] = self.count[e]
        for e in ("sp", "act", "pool"):
            for i in range(self.NDMA):
                if self.dma_uses[e][i] > 0:
                    targets[("d", e, i)] = 16 * self.dma_uses[e][i]
        sem = self.sem
        for eng in self.engs:
            waits = self._waits(eng, targets, skip=(eng if eng in self.COMPUTE else None))
            if waits:
                def run(e, waits=waits):
                    for k, v in waits:
                        e.wait_ge(sem[k], v)
                self.q[eng].append(run)

    def flush(self):
        nc = self.nc
        q = self.q
        with nc.Block() as block:
            if q["pe"]:
                @block.tensor
                def _(e):
                    for f in q["pe"]:
                        f(e)
            if q["act"]:
                @block.scalar
                def _(e):
                    for f in q["act"]:
                        f(e)
            if q["dve"]:
                @block.vector
                def _(e):
                    for f in q["dve"]:
                        f(e)
            if q["pool"]:
                @block.gpsimd
                def _(e):
                    for f in q["pool"]:
                        f(e)
            if q["sp"]:
                @block.sync
                def _(e):
                    for f in q["sp"]:
                        f(e)
        self.q = {e: [] for e in self.engs}


_uid = [0]


class TPool:
    def __init__(self, es, nc, name, shape, dtype, n, psum=False):
        self.tiles = []
        for i in range(n):
            _uid[0] += 1
            nm = "%s_%d" % (name, _uid[0])
            mk = nc.psum_tensor if psum else nc.sbuf_tensor
            t = es.enter_context(mk(nm, list(shape), dtype))
            self.tiles.append((t, Res(nm)))
        self.i = 0

    def next(self):
        t = self.tiles[self.i % len(self.tiles)]
        self.i += 1
        return t


def tile1(es, nc, name, shape, dtype, psum=False):
    return TPool(es, nc, name, shape, dtype, 1, psum).tiles[0]


class Cfg:
    def __init__(self, NSEQ=2, SEQ=2048, CTX=256, DFF=5632, DEXP=7168, MOE="routed", DEBUG=False, STOP=None):
        self.DEBUG = DEBUG
        self.STOP = STOP
        self.D = 2048
        self.KC = 16
        self.NSEQ = NSEQ
        self.NR = NSEQ + 1
        self.SEQ = SEQ
        self.CTX = CTX
        self.T = CTX + SEQ
        self.NT = self.T // 128
        self.NCT = CTX // 128
        self.DFF = DFF
        self.DEXP = DEXP
        self.NE = 8
        self.NIN = 10496
        self.NCH = self.T // 64
        self.NCC = CTX // 64
        self.MOE = MOE
        self.PUMP = 7
        self.NSLOT = (2 * NSEQ * SEQ) // 512 + 8
        self.tblocks = [(0, CTX)] + [(CTX + i * 512, 512) for i in range(SEQ // 512)]
        self.lat_blocks = self.tblocks[1:]
        self.groups = [list(range(0, self.NCC))] + [list(range(self.NCC + 8 * i, self.NCC + 8 * i + 8))
                                                    for i in range((SEQ // 64) // 8)]


COL = dict(hq=0, ff=1024, fb=2048, hv=3072, hg=4096, aq=5120, ak=6144, av=6272, ga=6400, gb=8448)


def host_consts(cfg):
    c = {}
    c["ident"] = np.eye(128, dtype=np.float32)
    i = np.arange(64)
    sb_, tb_ = i[:, None] // 16, i[None, :] // 16
    c["maskD_f"] = ((i[:, None] <= i[None, :]) & (sb_ == tb_)).astype(np.float32)
    c["maskO_f"] = (sb_ < tb_).astype(np.float32)
    c["maskD_b"] = ((i[:, None] >= i[None, :]) & (sb_ == tb_)).astype(np.float32)
    c["maskO_b"] = (sb_ > tb_).astype(np.float32)
    j = np.arange(128)
    c["mneg_prev"] = np.where(j[None, :] <= j[:, None], 0.0, NEG).astype(np.float32)
    c["mneg_next"] = np.where(j[:, None] <= j[None, :], 0.0, NEG).astype(np.float32)
    c["ustrict"] = (j[:, None] < j[None, :]).astype(np.float32)
    t = np.arange(cfg.SEQ)
    row = (t // 64).astype(np.float32)
    col = (t % 64).astype(np.float32)
    inv = (10000.0 ** (-np.arange(16, dtype=np.float32) / 16)).astype(np.float32)
    d = np.arange(64)
    axis = d // 32
    freq = d % 16
    second = (d % 32) >= 16
    pos = np.where(axis[:, None] == 0, row[None, :], col[None, :]).astype(np.float32)
    ang = (pos * inv[freq][:, None]).astype(np.float32)
    cos = np.cos(ang).astype(np.float32)
    sin = np.sin(ang).astype(np.float32)
    sin_s = np.where(second[:, None], sin, -sin).astype(np.float32)
    c["cosT"] = np.concatenate([cos, cos], 0)
    c["sinT"] = np.concatenate([sin_s, sin_s], 0)
    pm = np.zeros((128, 128), np.float32)
    for m in range(128):
        dd = m % 64
        partner = dd - 16 if (dd % 32) >= 16 else dd + 16
        pm[(m // 64) * 64 + partner, m] = 1.0
    c["pm"] = pm
    c["ones"] = np.ones((128, 128), np.float32)
    NG = cfg.DEXP // 512
    c["jgrid"] = np.broadcast_to(np.repeat(512.0 * np.arange(cfg.NSLOT, dtype=np.float32), 8)[None, :], (128, cfg.NSLOT * 8)).copy()
    KMAX = max(1, (cfg.NSEQ * cfg.SEQ) // 512)
    c["kgrid"] = np.broadcast_to(np.tile(512.0 * np.arange(KMAX, dtype=np.float32), 8)[None, :], (128, 8 * KMAX)).copy()
    c["gp"] = (np.arange(NG, dtype=np.float32)[None, :] * 128 + np.arange(128, dtype=np.float32)[:, None]).astype(np.float32)
    return c


CONST_SHAPES = lambda cfg: dict(ident=[128, 128], maskD_f=[64, 64], maskO_f=[64, 64], maskD_b=[64, 64], maskO_b=[64, 64], mneg_prev=[128, 128],
                                mneg_next=[128, 128], ustrict=[128, 128], cosT=[128, cfg.SEQ],
                                sinT=[128, cfg.SEQ], pm=[128, 128], ones=[128, 128], jgrid=[128, cfg.NSLOT * 8], gp=[128, cfg.DEXP // 512], kgrid=[128, 8 * max(1, (cfg.NSEQ * cfg.SEQ) // 512)])


class K:
    def __init__(self, cfg):
        self.cfg = cfg
        nc = self.nc = bass.Bass("TRN2", target_bir_lowering=False)
        D = cfg.D
        T = cfg.T
        def din(name, shape, dt=F32):
            return nc.dram_tensor(name, list(shape), dt, kind="ExternalInput").ap()
        def scr(name, shape, dt):
            return nc.dram_tensor(name, list(shape), dt, kind=("ExternalOutput" if cfg.DEBUG else "Internal")).ap()
        self.x_in = din("x_in", [cfg.NSEQ, cfg.SEQ, D])
        self.ctx_in = din("ctx_in", [cfg.NSEQ, cfg.CTX, D])
        self.cvec = din("cvec", [cfg.NR, D])
        self.w_mod = din("w_mod", [2, D, 6 * D])
        self.b_mod = din("b_mod", [2, 6 * D])
        self.norm_mix = din("norm_mix", [2, D])
        self.norm_ffn = din("norm_ffn", [2, D])
        self.w_in = din("w_in", [2, D, cfg.NIN])
        self.lb_f = din("hg_lb_fwd", [2, 1024])
        self.lb_b = din("hg_lb_bwd", [2, 1024])
        self.hg_norm = din("hg_norm", [2, 1024])
        self.sink = din("attn_sink", [2, 16])
        self.w_a = din("w_branch_a", [2, 1024, D])
        self.w_b = din("w_branch_b", [2, 1024, D])
        self.w_o = din("w_out", [2, D, D])
        self.ffn_wg = din("ffn_w_gate", [1, D, cfg.DFF])
        self.ffn_wu = din("ffn_w_up", [1, D, cfg.DFF])
        self.ffn_wd = din("ffn_w_down", [1, cfg.DFF, D])
        self.router = din("moe_router", [1, D, 8])
        self.moe_wg = din("moe_w_gate", [1, 8, D, cfg.DEXP])
        self.moe_wu = din("moe_w_up", [1, 8, D, cfg.DEXP])
        self.moe_wd = din("moe_w_down", [1, 8, cfg.DEXP, D])
        self.final_norm = din("final_norm", [D])
        self.cst_d = {k: din("c_" + k, s) for k, s in CONST_SHAPES(cfg).items()}
        self.out = nc.dram_tensor("out", [cfg.NSEQ, cfg.SEQ, D], F32, kind="ExternalOutput").ap()
        self.MOD = scr("MOD", [2, cfg.NR, 6 * D], F32)
        self.XR = scr("XR", [cfg.NSEQ, T, D], F32)
        self.QT = scr("QT", [8, 128, T], BF16)
        self.GT = scr("GT", [2, 8, 128, T], F32)
        self.KT = scr("KT", [2, 8, 128, T], BF16)
        self.SGT = scr("SGT", [8, 128, T], BF16)
        self.V = scr("V", [T, 1024], BF16)
        self.QA = scr("QA", [8, 128, T], BF16)
        self.KA = scr("KA", [2, 128, T], BF16)
        self.VA = scr("VA", [T, 128], BF16)
        self.GA = scr("GA", [16, 128, T], BF16)
        self.GB = scr("GB", [16, 128, T], BF16)
        self.AT = scr("AT", [8, 128, T], BF16)
        self.OAT = scr("OAT", [8, 128, T], BF16)
        NG = cfg.DEXP // 512
        NTg = cfg.NSEQ * cfg.SEQ // 128
        if cfg.MOE == "routed":
            self.H2 = scr("H2", [cfg.NSEQ * cfg.SEQ, D], BF16)
            self.HS = scr("HS", [cfg.NSLOT * 512, D], BF16)
            self.YP = scr("YP", [cfg.NSLOT * 512, D], F32)
            self.SELD = scr("SELD", [128, NTg, 8], F32)
            self.CBD = scr("CBD", [128, NTg, 8], F32)
            self.WGB = scr("WGB", [8 * NG * 128, 16 * 512], BF16)
            self.WUB = scr("WUB", [8 * NG * 128, 16 * 512], BF16)
            self.WDB = scr("WDB", [8 * NG * 128, 4 * D], BF16)
        self.r_H2 = Res(); self.r_HS = Res(); self.r_YP = Res(); self.r_SELD = Res(); self.r_WB = Res()
        self.pre_jobs = self.prepass_jobs() if cfg.MOE == "routed" else []
        self.dbg = {}
        self.r_MOD = Res('MOD')
        self.r_out = Res('out')
        self.r_QT = [Res() for _ in range(8)]
        self.r_SGT = [Res() for _ in range(8)]
        self.r_GA = [Res() for _ in range(16)]
        self.r_GB = [Res() for _ in range(16)]
        self.r_GT = [[Res() for _ in range(8)] for _ in range(2)]
        self.r_KT = [[Res() for _ in range(8)] for _ in range(2)]
        self.r_QA = [Res() for _ in range(8)]
        self.r_KA = [Res() for _ in range(2)]
        self.r_V = Res()
        self.r_VA = Res()
        self.r_AT = [Res() for _ in range(8)]
        self.r_OAT = [Res() for _ in range(8)]
        self.r_XR = [[Res('XR') for _ in range(cfg.NT)] for _ in range(cfg.NSEQ)]

    def xsrc(self, first, s, tile):
        cfg = self.cfg
        if first:
            if tile < cfg.NCT:
                return self.ctx_in[s, tile * 128:(tile + 1) * 128, :]
            tt = tile - cfg.NCT
            return self.x_in[s, tt * 128:(tt + 1) * 128, :]
        return self.XR[s, tile * 128:(tile + 1) * 128, :]

    def load_wblock(self, pool, w2d, n0, nn, nk=None, k0=0, eng="pool"):
        S = self.S
        nk = nk if nk is not None else w2d.shape[0] // 128
        t, r = pool.next()
        src = w2d[k0 * 128:(k0 + nk) * 128, n0:n0 + nn].rearrange("(kc p) n -> p kc n", p=128)
        S.dma(eng, lambda e: e.dma_start(out=t[:, 0:nk, 0:nn], in_=src), writes=[r])
        return t, r

    def bcast_row(self, t, r, row_ap, n, eng="sp"):
        self.S.dma(eng, lambda e: e.dma_start(out=t[:, 0:n], in_=row_ap.partition_broadcast(128)), writes=[r])

    def build(self):
        cfg = self.cfg
        nc = self.nc
        with ExitStack() as es:
            self.S = S = Sched(nc, es)
            es.enter_context(nc.allow_non_contiguous_dma(reason='small strided layout loads'))
            self.C = {}
            for k in ("ident", "mneg_prev", "mneg_next", "ustrict", "pm", "ones"):
                shp = CONST_SHAPES(cfg)[k]
                t, r = tile1(es, nc, "c_" + k, shp, BF16)
                S.dma("pool", lambda e, t=t, k=k: e.dma_start(out=t[:], in_=self.cst_d[k][:, :]), writes=[r])
                self.C[k] = (t, r)
            stop = cfg.STOP
            self.phase_mod()
            if stop == "mod":
                return nc
            for l in range(2):
                for s in range(cfg.NSEQ):
                    with ExitStack() as bs:
                        self.BIG = tile1(bs, nc, "BIG", [128, cfg.KC, cfg.T], BF16)
                        self.phase_norm(l, s, 1)
                        if stop == "norm":
                            self.dump("BIG", self.BIG[0], self.BIG[1], [128, cfg.KC, cfg.T], BF16)
                            self.end_phase()
                            return nc
                        self.phase_inproj(l, s)
                    if stop == "inproj":
                        return nc
                    self.phase_scan(l, s)
                    if stop in ("scan", "scanprep"):
                        return nc
                    self.phase_attn(l, s)
                    if stop == "attn":
                        return nc
                    with ExitStack() as bs:
                        self.BIG = tile1(bs, nc, "BIG", [128, cfg.KC, cfg.T], BF16)
                        self.phase_merge(l, s)
                        if stop == "merge":
                            return nc
                        self.phase_norm(l, s, 2)
                        if l == 1 and cfg.MOE == "routed":
                            self.route_local(s)
                        else:
                            self.phase_swiglu(l, s)
                    if stop == "ffn":
                        return nc
                if stop == "layer0":
                    return nc
            if cfg.MOE == "routed":
                self.phase_moe_routed()
            else:
                self.phase_final()
        return nc

    def dump(self, name, tile, r, shape, dt):
        if not self.cfg.DEBUG:
            return
        d = self.nc.dram_tensor("dbg_" + name, list(shape), dt, kind="ExternalOutput").ap()
        self.S.dma("sp", lambda e: e.dma_start(out=d, in_=tile[:]), reads=[r])

    def end_phase(self):
        self.S.barrier()
        self.S.flush()

    def phase_mod(self):
        cfg, nc, S = self.cfg, self.nc, self.S
        D, KC, NR = cfg.D, cfg.KC, cfg.NR
        with ExitStack() as ph:
            if cfg.MOE == "routed":
                zt, r_zt = tile1(ph, nc, "q_z", [128, 4, D], BF16)
                S.op("dve", lambda e: e.memset(zt[:], 0.0), writes=[r_zt])
                for j in range(cfg.NSLOT):
                    S.dma("sp", lambda e, j=j: e.dma_start(out=self.HS[j * 512:(j + 1) * 512, :].rearrange("(a p) d -> p a d", p=128), in_=zt[:]), reads=[r_zt])
            cT, r_cT = tile1(ph, nc, "cT", [128, KC, NR], F32)
            cTb, r_cTb = tile1(ph, nc, "cTb", [128, KC, NR], BF16)
            for r in range(NR):
                src = self.cvec[r:r + 1, :].rearrange("o (kc p) -> p kc o", p=128)
                S.dma("sp", lambda e, src=src, r=r: e.dma_start(out=cT[:, :, r:r + 1], in_=src), writes=[r_cT])
            S.op("act", lambda e: e.activation(out=cTb[:], in_=cT[:], func=AF.Silu), reads=[r_cT], writes=[r_cTb])
            wpool = TPool(ph, nc, "wmod", [128, KC, 512], BF16, 3)
            pspool = TPool(ph, nc, "psmod", [NR, 512], F32, 2, psum=True)
            bias, r_bias = tile1(ph, nc, "bmod", [NR, 6 * D], F32)
            res, r_res = tile1(ph, nc, "resmod", [NR, 6 * D], F32)
            for l in range(2):
                S.dma("sp", lambda e, l=l: e.dma_start(out=bias[:], in_=self.b_mod[l:l + 1, :].partition_broadcast(NR)), writes=[r_bias])
                nb = 6 * D // 512
                for b in range(nb):
                    w, r_w = self.load_wblock(wpool, self.w_mod[l], b * 512, 512)
                    ps, r_ps = pspool.next()
                    for kc in range(KC):
                        S.op("pe", lambda e, ps=ps, w=w, kc=kc: e.matmul(ps[:], lhsT=cTb[:, kc, :], rhs=w[:, kc, :], start=(kc == 0), stop=(kc == KC - 1)),
                             reads=[r_cTb, r_w], writes=[r_ps], signal=(kc == KC - 1))
                    S.op("dve", lambda e, ps=ps, b=b: e.tensor_tensor(out=res[:, b * 512:(b + 1) * 512], in0=ps[:], in1=bias[:, b * 512:(b + 1) * 512], op=ALU.add),
                         reads=[r_ps, r_bias], writes=[r_res])
                S.dma("sp", lambda e, l=l: e.dma_start(out=self.MOD[l], in_=res[:]), reads=[r_res], writes=[self.r_MOD])
            self.end_phase()

    def phase_norm(self, l, s, which):
        cfg, nc, S = self.cfg, self.nc, self.S
        D, KC = cfg.D, cfg.KC
        first = (l == 0 and which == 1)
        nw = self.norm_mix if which == 1 else self.norm_ffn
        base = 0 if which == 1 else 3
        BIG, r_BIG = self.BIG
        ident, r_ident = self.C["ident"]
        tiles = list(range(cfg.NT))
        if which == 2 and l == 1:
            tiles = list(range(cfg.NCT, cfg.NT))
        with ExitStack() as ph:
            gm = {}
            sh = {}
            for kind, row in (("lat", s), ("ctx", cfg.NSEQ)):
                if kind == "ctx" and which == 2 and l == 1:
                    continue
                g_t, g_r = tile1(ph, nc, "gm" + kind, [128, D], F32)
                s_t, s_r = tile1(ph, nc, "sh" + kind, [128, D], F32)
                n_t, n_r = tile1(ph, nc, "nw" + kind, [128, D], F32)
                self.S.dma("sp", lambda e, n_t=n_t: e.dma_start(out=n_t[:], in_=nw[l:l + 1, :].partition_broadcast(128)), writes=[n_r])
                self.S.dma("sp", lambda e, g_t=g_t, row=row: e.dma_start(out=g_t[:], in_=self.MOD[l, row:row + 1, (base + 1) * D:(base + 2) * D].partition_broadcast(128)), reads=[self.r_MOD], writes=[g_r])
                self.S.dma("sp", lambda e, s_t=s_t, row=row: e.dma_start(out=s_t[:], in_=self.MOD[l, row:row + 1, base * D:(base + 1) * D].partition_broadcast(128)), reads=[self.r_MOD], writes=[s_r])
                S.op("dve", lambda e, g_t=g_t, n_t=n_t: e.scalar_tensor_tensor(out=g_t[:], in0=g_t[:], scalar=1.0, in1=n_t[:], op0=ALU.add, op1=ALU.mult),
                     reads=[g_r, n_r], writes=[g_r])
                gm[kind] = (g_t, g_r)
                sh[kind] = (s_t, s_r)
            xpool = TPool(ph, nc, "xt", [128, D], F32, 2)
            junk, r_junk = tile1(ph, nc, "junk", [128, D], BF16)
            t1pool = TPool(ph, nc, "t1", [128, D], F32, 1)
            hpool = TPool(ph, nc, "hb", [128, D], BF16, 2)
            stpool = TPool(ph, nc, "st", [128, 4], F32, 2)
            pspool = TPool(ph, nc, "pT", [128, KC, 128], BF16, 2, psum=True)
            loaded = {}

            def load(i):
                tile = tiles[i]
                xt, r_xt = xpool.next()
                src = self.xsrc(first, s, tile)
                S.dma("sp", lambda e, xt=xt, src=src: e.dma_start(out=xt[:], in_=src), reads=[self.r_XR[s][tile]], writes=[r_xt])
                loaded[i] = (xt, r_xt)

            load(0)
            for i, tile in enumerate(tiles):
                if i + 1 < len(tiles):
                    load(i + 1)
                xt, r_xt = loaded.pop(i)
                kind = "ctx" if tile < cfg.NCT else "lat"
                g_t, g_r = gm[kind]
                s_t, s_r = sh[kind]
                st, r_st = stpool.next()
                S.op("act", lambda e, xt=xt, st=st: e.activation(out=junk[:], in_=xt[:], func=AF.Square, accum_out=st[:, 0:1]),
                     reads=[r_xt], writes=[r_junk, r_st])
                S.op("dve", lambda e, st=st: e.tensor_scalar(out=st[:, 1:2], in0=st[:, 0:1], scalar1=1.0 / D, scalar2=EPS, op0=ALU.mult, op1=ALU.add),
                     reads=[r_st], writes=[r_st])
                S.op("act", lambda e, st=st: e.activation(out=st[:, 2:3], in_=st[:, 1:2], func=AF.Sqrt), reads=[r_st], writes=[r_st])
                S.op("dve", lambda e, st=st: e.reciprocal(out=st[:, 3:4], in_=st[:, 2:3]), reads=[r_st], writes=[r_st])
                t1, r_t1 = t1pool.next()
                S.op("dve", lambda e, t1=t1, xt=xt, g_t=g_t: e.tensor_tensor(out=t1[:], in0=xt[:], in1=g_t[:], op=ALU.mult),
                     reads=[r_xt, g_r], writes=[r_t1])
                hb, r_hb = hpool.next()
                S.op("dve", lambda e, hb=hb, t1=t1, st=st, s_t=s_t: e.scalar_tensor_tensor(out=hb[:], in0=t1[:], scalar=st[:, 3:4], in1=s_t[:], op0=ALU.mult, op1=ALU.add),
                     reads=[r_t1, r_st, s_r], writes=[r_hb])
                if which == 2 and l == 1 and cfg.MOE == "routed":
                    g_ = s * (cfg.SEQ // 128) + (tile - cfg.NCT)
                    S.dma("sp", lambda e, hb=hb, g_=g_: e.dma_start(out=self.H2[g_ * 128:(g_ + 1) * 128, :], in_=hb[:]), reads=[r_hb], writes=[self.r_H2])
                ps, r_ps = pspool.next()
                for kc in range(KC):
                    S.op("pe", lambda e, ps=ps, hb=hb, kc=kc: e.transpose(out=ps[:, kc, :], in_=hb[:, kc * 128:(kc + 1) * 128], identity=ident[:]),
                         reads=[r_hb, r_ident], writes=[r_ps], signal=(kc == KC - 1))
                S.op("act", lambda e, ps=ps, tile=tile: e.copy(out=BIG[:, :, tile * 128:(tile + 1) * 128], in_=ps[:]),
                     reads=[r_ps], writes=[r_BIG])
            self.end_phase()

    def phase_inproj(self, l, s):
        cfg, nc, S = self.cfg, self.nc, self.S
        D, KC, T = cfg.D, cfg.KC, cfg.T
        BIG, r_BIG = self.BIG
        CTX, SEQ = cfg.CTX, cfg.SEQ
        pm, r_pm = self.C["pm"]
        w_in = self.w_in[l]
        with ExitStack() as ph:
            wpool = TPool(ph, nc, "win", [128, KC, 512], BF16, 3)
            pspool = TPool(ph, nc, "psin", [128, 512], F32, 4, psum=True)
            psrot = TPool(ph, nc, "psrot", [128, 512], F32, 2, psum=True)
            stb = TPool(ph, nc, "stb", [128, T], BF16, 4)
            stf = TPool(ph, nc, "stf", [128, T], F32, 2)
            tmpf = TPool(ph, nc, "tmpf", [128, 512], F32, 6)
            cosT, r_cos = tile1(ph, nc, "cosT", [128, SEQ], F32)
            sinT, r_sin = tile1(ph, nc, "sinT", [128, SEQ], F32)
            S.dma("sp", lambda e: e.dma_start(out=cosT[:], in_=self.cst_d["cosT"][:, :]), writes=[r_cos])
            S.dma("sp", lambda e: e.dma_start(out=sinT[:], in_=self.cst_d["sinT"][:, :]), writes=[r_sin])
            lbv, oml = [], []
            for di, lbsrc in enumerate((self.lb_f, self.lb_b)):
                lb_t, lb_r = tile1(ph, nc, "lb%d" % di, [128, 8], F32)
                om_t, om_r = tile1(ph, nc, "oml%d" % di, [128, 8], F32)
                if l == 0:
                    S.op("dve", lambda e, lb_t=lb_t: e.memset(lb_t[:], 0.0), writes=[lb_r])
                else:
                    r0_t, r0_r = tile1(ph, nc, "lr0%d" % di, [128, 8], F32)
                    r1_t, r1_r = tile1(ph, nc, "lr1%d" % di, [128, 8], F32)
                    S.dma("sp", lambda e, r0_t=r0_t, lbsrc=lbsrc: e.dma_start(out=r0_t[:], in_=lbsrc[0:1, :].rearrange("o (h p) -> p (o h)", p=128)), writes=[r0_r])
                    S.dma("sp", lambda e, r1_t=r1_t, lbsrc=lbsrc: e.dma_start(out=r1_t[:], in_=lbsrc[1:2, :].rearrange("o (h p) -> p (o h)", p=128)), writes=[r1_r])
                    S.op("dve", lambda e, r0_t=r0_t, r1_t=r1_t: e.tensor_tensor(out=r0_t[:], in0=r0_t[:], in1=r1_t[:], op=ALU.subtract), reads=[r0_r, r1_r], writes=[r0_r])
                    S.op("act", lambda e, r0_t=r0_t: e.activation(out=r0_t[:], in_=r0_t[:], func=AF.Exp), reads=[r0_r], writes=[r0_r])
                    S.op("dve", lambda e, r0_t=r0_t: e.tensor_scalar(out=r0_t[:], in0=r0_t[:], scalar1=1.0, scalar2=None, op0=ALU.add), reads=[r0_r], writes=[r0_r])
                    S.op("dve", lambda e, r0_t=r0_t, lb_t=lb_t: e.reciprocal(out=lb_t[:], in_=r0_t[:]), reads=[r0_r], writes=[lb_r])
                S.op("dve", lambda e, lb_t=lb_t, om_t=om_t: e.tensor_scalar(out=om_t[:], in0=lb_t[:], scalar1=-1.0, scalar2=1.0, op0=ALU.mult, op1=ALU.add), reads=[lb_r], writes=[om_r])
                lbv.append((lb_t, lb_r))
                oml.append((om_t, om_r))

            def proj_chunk(w, r_w, c, epi):
                for (t0, n) in cfg.tblocks:
                    ps, r_ps = pspool.next()
                    for kc in range(KC):
                        S.op("pe", lambda e, ps=ps, kc=kc, t0=t0, n=n: e.matmul(ps[:, 0:n], lhsT=w[:, kc, c * 128:(c + 1) * 128], rhs=BIG[:, kc, t0:t0 + n], start=(kc == 0), stop=(kc == KC - 1)),
                             reads=[r_w, r_BIG], writes=[r_ps], signal=(kc == KC - 1))
                    epi(ps, r_ps, t0, n)

            def store(dst, st, r_st, r_dst):
                S.dma("sp", lambda e: e.dma_start(out=dst, in_=st[:]), reads=[r_st], writes=[r_dst])

            def simple_job(col0, nchunks, func, scale, dst, r_dst):
                for b in range(0, nchunks, 4):
                    w, r_w = self.load_wblock(wpool, w_in, col0 + b * 128, 512)
                    for c in range(4):
                        st, r_st = stb.next()
                        def epi(ps, r_ps, t0, n, st=st, r_st=r_st):
                            S.op("act", lambda e: e.activation(out=st[:, t0:t0 + n], in_=ps[:, 0:n], func=func, scale=scale), reads=[r_ps], writes=[r_st])
                        proj_chunk(w, r_w, c, epi)
                        store(dst[b + c], st, r_st, r_dst[b + c])

            simple_job(COL["hq"], 8, AF.Copy, 128.0 ** -0.5, self.QT, self.r_QT)
            simple_job(COL["hg"], 8, AF.Silu, 1.0, self.SGT, self.r_SGT)
            simple_job(COL["ga"], 16, AF.Sigmoid, 1.0, self.GA, self.r_GA)
            simple_job(COL["gb"], 16, AF.Sigmoid, 1.0, self.GB, self.r_GB)
            for di, col0 in enumerate((COL["ff"], COL["fb"])):
                lb_t, lb_r = lbv[di]
                om_t, om_r = oml[di]
                for b in range(0, 8, 4):
                    w, r_w = self.load_wblock(wpool, w_in, col0 + b * 128, 512)
                    for c in range(4):
                        hd = b + c
                        sg_, r_sg = stf.next()
                        sk_, r_sk = stb.next()
                        def epi(ps, r_ps, t0, n, sg_=sg_, r_sg=r_sg, sk_=sk_, r_sk=r_sk, hd=hd, om_t=om_t, om_r=om_r):
                            e_t, r_e = tmpf.next()
                            t_t, r_t = tmpf.next()
                            k_t, r_k = tmpf.next()
                            S.op("act", lambda e: e.activation(out=e_t[:, 0:n], in_=ps[:, 0:n], func=AF.Exp, scale=-1.0), reads=[r_ps], writes=[r_e])
                            S.op("dve", lambda e: e.tensor_scalar(out=t_t[:, 0:n], in0=e_t[:, 0:n], scalar1=1.0, scalar2=None, op0=ALU.add), reads=[r_e], writes=[r_t])
                            S.op("dve", lambda e: e.reciprocal(out=t_t[:, 0:n], in_=t_t[:, 0:n]), reads=[r_t], writes=[r_t])
                            S.op("dve", lambda e: e.scalar_tensor_tensor(out=k_t[:, 0:n], in0=e_t[:, 0:n], scalar=om_t[:, hd:hd + 1], in1=t_t[:, 0:n], op0=ALU.mult, op1=ALU.mult),
                                 reads=[r_e, r_t, om_r], writes=[r_k])
                            S.op("act", lambda e: e.activation(out=sg_[:, t0:t0 + n], in_=k_t[:, 0:n], func=AF.Ln, scale=-1.0, bias=1.0), reads=[r_k], writes=[r_sg])
                            S.op("act", lambda e: e.copy(out=sk_[:, t0:t0 + n], in_=k_t[:, 0:n]), reads=[r_k], writes=[r_sk])
                        proj_chunk(w, r_w, c, epi)
                        store(self.GT[di, hd], sg_, r_sg, self.r_GT[di][hd])
                        store(self.KT[di, hd], sk_, r_sk, self.r_KT[di][hd])

            def rope_job(w, r_w, c, scale, dst, r_dst):
                raw, r_raw = stb.next()
                outt, r_out = stb.next()
                def epi(ps, r_ps, t0, n):
                    S.op("act", lambda e: e.activation(out=raw[:, t0:t0 + n], in_=ps[:, 0:n], func=AF.Copy, scale=scale), reads=[r_ps], writes=[r_raw])
                    if t0 < CTX:
                        S.op("act", lambda e: e.copy(out=outt[:, t0:t0 + n], in_=raw[:, t0:t0 + n]), reads=[r_raw], writes=[r_out])
                        return
                    p0 = t0 - CTX
                    pr, r_pr = psrot.next()
                    S.op("pe", lambda e: e.matmul(pr[:, 0:n], lhsT=pm[:], rhs=raw[:, t0:t0 + n], start=True, stop=True), reads=[r_pm, r_raw], writes=[r_pr])
                    a_t, r_a = tmpf.next()
                    b_t, r_b = tmpf.next()
                    S.op("dve", lambda e: e.tensor_tensor(out=a_t[:, 0:n], in0=raw[:, t0:t0 + n], in1=cosT[:, p0:p0 + n], op=ALU.mult), reads=[r_raw, r_cos], writes=[r_a])
                    S.op("dve", lambda e: e.tensor_tensor(out=b_t[:, 0:n], in0=pr[:, 0:n], in1=sinT[:, p0:p0 + n], op=ALU.mult), reads=[r_pr, r_sin], writes=[r_b])
                    S.op("dve", lambda e: e.tensor_tensor(out=outt[:, t0:t0 + n], in0=a_t[:, 0:n], in1=b_t[:, 0:n], op=ALU.add), reads=[r_a, r_b], writes=[r_out])
                proj_chunk(w, r_w, c, epi)
                store(dst, outt, r_out, r_dst)

            for b in range(0, 8, 4):
                w, r_w = self.load_wblock(wpool, w_in, COL["aq"] + b * 128, 512)
                for c in range(4):
                    rope_job(w, r_w, c, 64.0 ** -0.5, self.QA[b + c], self.r_QA[b + c])
            w, r_w = wpool.next()
            for (o, c0, nn) in ((0, COL["ak"], 128), (128, COL["ak"] + 64, 64), (192, COL["ak"], 64)):
                src = w_in[:, c0:c0 + nn].rearrange("(kc p) n -> p kc n", p=128)
                S.dma("pool", lambda e, o=o, nn=nn, src=src: e.dma_start(out=w[:, :, o:o + nn], in_=src), writes=[r_w])
            for c in range(2):
                rope_job(w, r_w, c, 1.0, self.KA[c], self.r_KA[c])

            vst = TPool(ph, nc, "vst", [128, 512], BF16, 3)
            for (col0, ncols, dst, r_dst) in ((COL["hv"], 512, self.V[:, 0:512], self.r_V), (COL["hv"] + 512, 512, self.V[:, 512:1024], self.r_V), (COL["av"], 128, self.VA, self.r_VA)):
                w, r_w = self.load_wblock(wpool, w_in, col0, ncols)
                for tile in range(cfg.NT):
                    ps, r_ps = pspool.next()
                    for kc in range(KC):
                        S.op("pe", lambda e, ps=ps, kc=kc, tile=tile, ncols=ncols, w=w: e.matmul(ps[:, 0:ncols], lhsT=BIG[:, kc, tile * 128:(tile + 1) * 128], rhs=w[:, kc, 0:ncols], start=(kc == 0), stop=(kc == KC - 1)),
                             reads=[r_w, r_BIG], writes=[r_ps], signal=(kc == KC - 1))
                    vt, r_vt = vst.next()
                    S.op("act", lambda e, vt=vt, ps=ps, ncols=ncols: e.copy(out=vt[:, 0:ncols], in_=ps[:, 0:ncols]), reads=[r_ps], writes=[r_vt])
                    S.dma("sp", lambda e, vt=vt, tile=tile, ncols=ncols, dst=dst: e.dma_start(out=dst[tile * 128:(tile + 1) * 128, :], in_=vt[:, 0:ncols]), reads=[r_vt], writes=[r_dst])
            self.end_phase()

    def phase_scan(self, l, s):
        cfg, nc, S = self.cfg, self.nc, self.S
        T, NCH = cfg.T, cfg.NCH
        NB16 = T // 16
        ident, r_ident = self.C["ident"]
        groups = cfg.groups
        ng = len(groups)
        order = [[list(g) for g in groups],
                 [list(reversed(groups[0]))] + [list(reversed(g)) for g in reversed(groups[1:])]]
        VN = ("qd", "kd", "qb", "qin", "kout", "k1", "k2", "k3")
        with ExitStack() as ph:
            qT, r_qT = tile1(ph, nc, "s_qT", [128, T], BF16)
            sgT, r_sgT = tile1(ph, nc, "s_sgT", [128, T], BF16)
            vt, r_vt = tile1(ph, nc, "s_v", [64, NCH, 128], BF16)
            gT = [tile1(ph, nc, "s_gT%d" % d, [128, T], F32) for d in range(2)]
            kT = [tile1(ph, nc, "s_kT%d" % d, [128, T], BF16) for d in range(2)]
            Zps = [tile1(ph, nc, "s_Zp%d" % d, [128, T + 1], F32) for d in range(2)]
            tmp = TPool(ph, nc, "s_tmp", [128, T], F32, 3)
            var = [{n: tile1(ph, nc, "s_%s%d" % (n, d), [128, T], BF16) for n in VN} for d in range(2)]
            tr = [tile1(ph, nc, "s_tr%d" % d, [128, NCH], F32) for d in range(2)]
            oall, r_oall = tile1(ph, nc, "s_oall", [128, T], F32)
            aT, r_aT = tile1(ph, nc, "s_aT", [128, T], BF16)
            ones, r_ones = tile1(ph, nc, "s_ones", [128, T], F32)
            onesf, r_onesf = tile1(ph, nc, "s_onesf", [128, 128], F32)
            gain, r_gain = tile1(ph, nc, "s_gain", [128, 8], F32)
            mk = {}
            for n in ("maskD_f", "maskO_f", "maskD_b", "maskO_b"):
                mk[n] = tile1(ph, nc, "s_" + n, [64, 64], BF16)
                S.dma("pool", lambda e, n=n: e.dma_start(out=mk[n][0][:], in_=self.cst_d[n][:, :]), writes=[mk[n][1]])
            Sf = [tile1(ph, nc, "s_Sf%d" % d, [128, 128], F32) for d in range(2)]
            Sb = [tile1(ph, nc, "s_Sb%d" % d, [128, 128], BF16) for d in range(2)]
            At = TPool(ph, nc, "s_At", [64, 8, 64], BF16, 4)
            At2 = TPool(ph, nc, "s_At2", [64, 8, 64], BF16, 2)
            ktok = TPool(ph, nc, "s_ktok", [64, 8, 128], BF16, 4)
            rs = TPool(ph, nc, "s_rs", [128, 512], F32, 2)
            psA = TPool(ph, nc, "s_psA", [64, 8, 64], F32, 1, psum=True)
            psA2 = [tile1(ph, nc, "s_psA2%d" % d, [64, 8, 64], F32, psum=True) for d in range(2)]
            psK = TPool(ph, nc, "s_psK", [64, 8, 128], BF16, 1, psum=True)
            psO = TPool(ph, nc, "s_psO", [128, 8, 64], F32, 2, psum=True)
            psD = TPool(ph, nc, "s_psD", [128, 128], F32, 1, psum=True)
            psS = TPool(ph, nc, "s_psS", [128, 512], F32, 1, psum=True)
            S.op("dve", lambda e: e.memset(ones[:], 1.0), writes=[r_ones])
            S.op("dve", lambda e: e.memset(onesf[:], 1.0), writes=[r_onesf])
            for d in range(2):
                S.op("dve", lambda e, d=d: e.memset(psA2[d][0][:], 0.0), writes=[psA2[d][1]])
                S.op("dve", lambda e, d=d: e.memset(Zps[d][0][:], 0.0), writes=[Zps[d][1]])
            S.dma("sp", lambda e: e.dma_start(out=gain[:], in_=self.hg_norm[l:l + 1, :].rearrange("o (h p) -> p (o h)", p=128)), writes=[r_gain])

            def v3(ap, j):
                return ap.rearrange("p (c j) -> p c j", j=j)

            def load_pre(hd):
                for d in range(2):
                    S.dma("sp", lambda e, hd=hd, d=d: e.dma_start(out=gT[d][0][:], in_=self.GT[d, hd]), reads=[self.r_GT[d][hd]], writes=[gT[d][1]])
                S.dma("sp", lambda e, hd=hd: e.dma_start(out=qT[:], in_=self.QT[hd]), reads=[self.r_QT[hd]], writes=[r_qT])
                for d in range(2):
                    S.dma("sp", lambda e, hd=hd, d=d: e.dma_start(out=kT[d][0][:], in_=self.KT[d, hd]), reads=[self.r_KT[d][hd]], writes=[kT[d][1]])

            load_pre(0)
            for hd in range(8):
                self.pump(cfg.PUMP)
                S.dma("sp", lambda e, hd=hd: e.dma_start(out=vt[:], in_=self.V[:, hd * 128:(hd + 1) * 128].rearrange("(c p) v -> p c v", p=64)), reads=[self.r_V], writes=[r_vt])
                S.dma("sp", lambda e, hd=hd: e.dma_start(out=sgT[:], in_=self.SGT[hd]), reads=[self.r_SGT[hd]], writes=[r_sgT])
                for d in range(2):
                    g_t, g_r = gT[d]
                    k_t, k_r = kT[d]
                    Zp, r_Zp = Zps[d]
                    sg = 1.0 if d == 0 else -1.0
                    S.op("dve", lambda e, Zp=Zp, g_t=g_t: e.tensor_tensor_scan(out=Zp[:, 1:T + 1], data0=ones[:], data1=g_t[:], initial=0.0, op0=ALU.mult, op1=ALU.add),
                         reads=[r_ones, g_r], writes=[r_Zp])
                    X = Zp[:, 1:T + 1] if d == 0 else Zp[:, 0:T]
                    lo16 = v3(Zp[:, 0:T], 16)
                    hi16 = v3(Zp[:, 1:T + 1], 16)
                    lo64 = v3(Zp[:, 0:T], 64)
                    hi64 = v3(Zp[:, 1:T + 1], 64)
                    ref_mid = lo16[:, :, 8:9]
                    ref_qb = lo16[:, :, 0:1] if d == 0 else hi16[:, :, 15:16]
                    ref_qin = lo64[:, :, 0:1] if d == 0 else hi64[:, :, 63:64]
                    ref_kout = hi64[:, :, 63:64] if d == 0 else lo64[:, :, 0:1]
                    ref_k = [lo64[:, :, 16 * i:16 * i + 1] for i in (1, 2, 3)]

                    def make(name, src_t, src_r, ref, blk, scale, clamp=None, X=X, r_Zp=r_Zp, d=d):
                        nb = T // blk
                        D, r_D = tmp.next()
                        S.op("dve", lambda e: e.tensor_tensor(out=v3(D[:], blk), in0=v3(X, blk), in1=ref.broadcast_to([128, nb, blk]), op=ALU.subtract),
                             reads=[r_Zp], writes=[r_D])
                        S.op("act", lambda e: e.activation(out=D[:], in_=D[:], func=AF.Exp, scale=scale), reads=[r_D], writes=[r_D])
                        o_t, o_r = var[d][name]
                        if clamp is None:
                            S.op("dve", lambda e: e.tensor_tensor(out=o_t[:], in0=src_t[:], in1=D[:], op=ALU.mult), reads=[src_r, r_D], writes=[o_r])
                        else:
                            S.op("dve", lambda e: e.scalar_tensor_tensor(out=o_t[:], in0=D[:], scalar=1.0, in1=src_t[:], op0=ALU.min, op1=ALU.mult), reads=[src_r, r_D], writes=[o_r])

                    make("qd", qT, r_qT, ref_mid, 16, sg)
                    make("kd", k_t, k_r, ref_mid, 16, -sg)
                    make("qb", qT, r_qT, ref_qb, 16, sg)
                    make("qin", qT, r_qT, ref_qin, 64, sg)
                    make("kout", k_t, k_r, ref_kout, 64, -sg)
                    for i in range(3):
                        make("k%d" % (i + 1), k_t, k_r, ref_k[i], 64, -sg, clamp=(ALU.max if d == 0 else ALU.min))
                    tr_t, tr_r = tr[d]
                    S.op("dve", lambda e, tr_t=tr_t, hi64=hi64, lo64=lo64: e.tensor_tensor(out=tr_t[:].unsqueeze(2), in0=hi64[:, :, 63:64], in1=lo64[:, :, 0:1], op=ALU.subtract), reads=[r_Zp], writes=[tr_r])
                    S.op("act", lambda e, tr_t=tr_t: e.activation(out=tr_t[:], in_=tr_t[:], func=AF.Exp), reads=[tr_r], writes=[tr_r])
                    S.op("dve", lambda e, d=d: e.memset(Sf[d][0][:], 0.0), writes=[Sf[d][1]])
                    S.op("dve", lambda e, d=d: e.memset(Sb[d][0][:], 0.0), writes=[Sb[d][1]])
                if hd + 1 < 8:
                    load_pre(hd + 1)
                touched = set()
                for gi in range(ng):
                    ctxs = []
                    for d in range(2):
                        cl = order[d][gi]
                        g0 = min(cl)
                        n = len(cl)
                        V = var[d]
                        pa, r_pa = psA.next()
                        for c in cl:
                            S.op("pe", lambda e, pa=pa, c=c, g0=g0, V=V: e.matmul(pa[:, c - g0, :], lhsT=V["kd"][0][:, c * 64:(c + 1) * 64], rhs=V["qd"][0][:, c * 64:(c + 1) * 64], start=True, stop=True),
                                 reads=[V["kd"][1], V["qd"][1]], writes=[r_pa], signal=(c == cl[-1]))
                        pa2, r_pa2 = psA2[d]
                        subs = (1, 2, 3) if d == 0 else (0, 1, 2)
                        for c in cl:
                            for i in subs:
                                kv = V["k%d" % (i if d == 0 else i + 1)]
                                S.op("pe", lambda e, pa2=pa2, c=c, g0=g0, i=i, kv=kv, V=V: e.matmul(pa2[:, c - g0, 16 * i:16 * i + 16], lhsT=kv[0][:, c * 64:(c + 1) * 64], rhs=V["qb"][0][:, c * 64 + 16 * i:c * 64 + 16 * i + 16], start=True, stop=True),
                                     reads=[kv[1], V["qb"][1]], writes=[r_pa2], signal=(c == cl[-1] and i == subs[-1]))
                        at, r_at = At.next()
                        at2, r_at2 = At2.next()
                        mD, r_mD = mk["maskD_f" if d == 0 else "maskD_b"]
                        mO, r_mO = mk["maskO_f" if d == 0 else "maskO_b"]
                        S.op("dve", lambda e, at=at, pa=pa, n=n, mD=mD: e.tensor_tensor(out=at[:, 0:n, :], in0=pa[:, 0:n, :], in1=mD[:].unsqueeze(1).broadcast_to([64, n, 64]), op=ALU.mult),
                             reads=[r_pa, r_mD], writes=[r_at])
                        S.op("dve", lambda e, at2=at2, pa2=pa2, n=n, mO=mO: e.tensor_tensor(out=at2[:, 0:n, :], in0=pa2[:, 0:n, :], in1=mO[:].unsqueeze(1).broadcast_to([64, n, 64]), op=ALU.mult),
                             reads=[r_pa2, r_mO], writes=[r_at2])
                        S.op("pool", lambda e, at=at, at2=at2, n=n: e.tensor_tensor(out=at[:, 0:n, :], in0=at[:, 0:n, :], in1=at2[:, 0:n, :], op=ALU.add), reads=[r_at, r_at2], writes=[r_at])
                        pk, r_pk = psK.next()
                        for c in cl:
                            S.op("pe", lambda e, pk=pk, c=c, g0=g0, V=V: e.transpose(out=pk[:, c - g0, :], in_=V["kout"][0][:, c * 64:(c + 1) * 64], identity=ident[:]),
                                 reads=[V["kout"][1], r_ident], writes=[r_pk], signal=(c == cl[-1]))
                        kk, r_kk = ktok.next()
                        S.op("act", lambda e, kk=kk, pk=pk, n=n: e.copy(out=kk[:, 0:n, :], in_=pk[:, 0:n, :]), reads=[r_pk], writes=[r_kk])
                        po, r_po = psO.next()
                        ctxs.append((d, cl, g0, at, r_at, kk, r_kk, po, r_po))
                    nsteps = max(len(c[1]) for c in ctxs)
                    for j in range(nsteps):
                        for (d, cl, g0, at, r_at, kk, r_kk, po, r_po) in ctxs:
                            if j >= len(cl):
                                continue
                            c = cl[j]
                            pos = c - g0
                            V = var[d]
                            S.op("pe", lambda e, po=po, pos=pos, c=c, at=at: e.matmul(po[:, pos, :], lhsT=vt[:, c, :], rhs=at[:, pos, :], start=True, stop=False),
                                 reads=[r_vt, r_at], writes=[r_po], signal=False)
                            S.op("pe", lambda e, po=po, pos=pos, c=c, d=d, V=V: e.matmul(po[:, pos, :], lhsT=Sb[d][0][:], rhs=V["qin"][0][:, c * 64:(c + 1) * 64], start=False, stop=True),
                                 reads=[Sb[d][1], V["qin"][1]], writes=[r_po], signal=(j == len(cl) - 1))
                            last = (gi == ng - 1 and j == len(cl) - 1)
                            if not last:
                                pd, r_pd = psD.next()
                                S.op("pe", lambda e, pd=pd, kk=kk, pos=pos, c=c: e.matmul(pd[:], lhsT=kk[:, pos, :], rhs=vt[:, c, :], start=True, stop=True),
                                     reads=[r_kk, r_vt], writes=[r_pd])
                                S.op("dve", lambda e, pd=pd, d=d, c=c: e.scalar_tensor_tensor(out=Sf[d][0][:], in0=Sf[d][0][:], scalar=tr[d][0][:, c:c + 1], in1=pd[:], op0=ALU.mult, op1=ALU.add),
                                     reads=[r_pd, Sf[d][1], tr[d][1]], writes=[Sf[d][1]])
                                S.op("act", lambda e, d=d: e.copy(out=Sb[d][0][:], in_=Sf[d][0][:]), reads=[Sf[d][1]], writes=[Sb[d][1]])
                    for (d, cl, g0, at, r_at, kk, r_kk, po, r_po) in ctxs:
                        n = len(cl)
                        dst = v3(oall[:, g0 * 64:(g0 + n) * 64], 64)
                        if g0 not in touched:
                            touched.add(g0)
                            S.op("act", lambda e, dst=dst, po=po, n=n: e.copy(out=dst, in_=po[:, 0:n, :]), reads=[r_po], writes=[r_oall])
                        else:
                            S.op("dve", lambda e, dst=dst, po=po, n=n: e.tensor_tensor(out=dst, in0=po[:, 0:n, :], in1=dst, op=ALU.add), reads=[r_po, r_oall], writes=[r_oall])
                sq, r_sq = tmp.next()
                S.op("act", lambda e, sq=sq: e.activation(out=sq[:], in_=oall[:], func=AF.Square), reads=[r_oall], writes=[r_sq])
                for (t0, n) in cfg.tblocks:
                    pss, r_pss = psS.next()
                    S.op("pe", lambda e, pss=pss, sq=sq, t0=t0, n=n: e.matmul(pss[:, 0:n], lhsT=onesf[:], rhs=sq[:, t0:t0 + n], start=True, stop=True), reads=[r_onesf, r_sq], writes=[r_pss])
                    r1, r_r1 = rs.next()
                    S.op("dve", lambda e, r1=r1, pss=pss, n=n: e.tensor_scalar(out=r1[:, 0:n], in0=pss[:, 0:n], scalar1=1.0 / 128, scalar2=EPS, op0=ALU.mult, op1=ALU.add), reads=[r_pss], writes=[r_r1])
                    S.op("act", lambda e, r1=r1, n=n: e.activation(out=r1[:, 0:n], in_=r1[:, 0:n], func=AF.Sqrt), reads=[r_r1], writes=[r_r1])
                    S.op("dve", lambda e, r1=r1, n=n: e.reciprocal(out=r1[:, 0:n], in_=r1[:, 0:n]), reads=[r_r1], writes=[r_r1])
                    S.op("dve", lambda e, r1=r1, t0=t0, n=n: e.tensor_tensor(out=r1[:, 0:n], in0=r1[:, 0:n], in1=oall[:, t0:t0 + n], op=ALU.mult), reads=[r_r1, r_oall], writes=[r_r1])
                    S.op("dve", lambda e, r1=r1, t0=t0, n=n, hd=hd: e.scalar_tensor_tensor(out=aT[:, t0:t0 + n], in0=r1[:, 0:n], scalar=gain[:, hd:hd + 1], in1=sgT[:, t0:t0 + n], op0=ALU.mult, op1=ALU.mult),
                         reads=[r_r1, r_gain, r_sgT], writes=[r_aT])
                S.dma("sp", lambda e, hd=hd: e.dma_start(out=self.AT[hd], in_=aT[:]), reads=[r_aT], writes=[self.r_AT[hd]])
            self.end_phase()

    def phase_attn(self, l, s):
        cfg, nc, S = self.cfg, self.nc, self.S
        T, CTX, NCT, NT = cfg.T, cfg.CTX, cfg.NCT, cfg.NT
        NQ = cfg.SEQ // 128
        ident, r_ident = self.C["ident"]
        mprev, r_mprev = self.C["mneg_prev"]
        mnext, r_mnext = self.C["mneg_next"]
        onesb, r_onesb = self.C["ones"]
        with ExitStack() as ph:
            kc_ = [tile1(ph, nc, "a_k%d" % i, [128, T], BF16) for i in range(2)]
            vtok, r_vtok = tile1(ph, nc, "a_v", [128, NT, 128], BF16)
            qpool = TPool(ph, nc, "a_q", [128, T], BF16, 2)
            opool = TPool(ph, nc, "a_o", [128, T], BF16, 2)
            esb, r_esb = tile1(ph, nc, "a_esb", [128, 16], F32)
            es2, r_es2 = tile1(ph, nc, "a_es2", [128, 8], F32)
            ppool = TPool(ph, nc, "a_p", [128, 5, 128], BF16, 4)
            rpool = TPool(ph, nc, "a_r", [128, 128], F32, 3)
            psS = TPool(ph, nc, "a_psS", [128, 8, 128], F32, 3, psum=True)
            psOD = TPool(ph, nc, "a_psOD", [128, 2, 128], F32, 2, psum=True)
            for i in range(2):
                S.dma("sp", lambda e, i=i: e.dma_start(out=kc_[i][0][:], in_=self.KA[i]), reads=[self.r_KA[i]], writes=[kc_[i][1]])
            S.dma("sp", lambda e: e.dma_start(out=vtok[:], in_=self.VA.rearrange("(c p) v -> p c v", p=128)), reads=[self.r_VA], writes=[r_vtok])
            S.dma("sp", lambda e: e.dma_start(out=esb[:], in_=self.sink[l:l + 1, :].partition_broadcast(128)), writes=[r_esb])
            S.op("act", lambda e: e.activation(out=esb[:], in_=esb[:], func=AF.Exp), reads=[r_esb], writes=[r_esb])
            esb3 = esb[:].rearrange("p (c two) -> p c two", two=2)
            S.op("dve", lambda e: e.tensor_copy(out=es2[0:64, :], in_=esb3[0:64, :, 0]), reads=[r_esb], writes=[r_es2])
            S.op("dve", lambda e: e.tensor_copy(out=es2[64:128, :], in_=esb3[64:128, :, 1]), reads=[r_esb], writes=[r_es2])
            qblocks = []
            if l == 0:
                for m in range(NCT):
                    qblocks.append((m * 128, [(t, None) for t in range(NCT)]))
            for n in range(NQ):
                tiles = [(t, None) for t in range(NCT)]
                if n > 0:
                    tiles.append((NCT + n - 1, "prev"))
                tiles.append((NCT + n, None))
                if n < NQ - 1:
                    tiles.append((NCT + n + 1, "next"))
                qblocks.append((CTX + n * 128, tiles))
            for c in range(8):
                g = c // 4
                self.pump(cfg.PUMP)
                qc, r_qc = qpool.next()
                S.dma("sp", lambda e, c=c, qc=qc: e.dma_start(out=qc[:], in_=self.QA[c]), reads=[self.r_QA[c]], writes=[r_qc])
                oc, r_oc = opool.next()
                kops = [(kc_[0] if g == 0 else kc_[1], 0), (kc_[1] if g == 0 else kc_[0], 64)]
                for (q0, tiles) in qblocks:
                    nt = len(tiles)
                    pts = []
                    for hh in range(2):
                        (k_t, k_r), r0 = kops[hh]
                        ps, r_ps = psS.next()
                        for ti, (tile, mkind) in enumerate(tiles):
                            S.op("pe", lambda e, ps=ps, ti=ti, tile=tile, k_t=k_t, r0=r0, q0=q0, qc=qc, mkind=mkind: e.matmul(ps[:, ti, :], lhsT=k_t[r0:r0 + 64, tile * 128:(tile + 1) * 128], rhs=qc[r0:r0 + 64, q0:q0 + 128], start=True, stop=(mkind is None)),
                                 reads=[k_r, r_qc], writes=[r_ps], signal=(mkind is None and ti == nt - 1))
                            if mkind is not None:
                                m_t, m_r = (mprev, r_mprev) if mkind == "prev" else (mnext, r_mnext)
                                S.op("pe", lambda e, ps=ps, ti=ti, m_t=m_t: e.matmul(ps[:, ti, :], lhsT=ident[:], rhs=m_t[:], start=False, stop=True),
                                     reads=[r_ident, m_r], writes=[r_ps], signal=(ti == nt - 1))
                        pt, r_pt = ppool.next()
                        S.op("act", lambda e, pt=pt, ps=ps, nt=nt: e.activation(out=pt[:, 0:nt, :], in_=ps[:, 0:nt, :], func=AF.Exp), reads=[r_ps], writes=[r_pt])
                        pts.append((pt, r_pt))
                    od, r_od = psOD.next()
                    for hh in range(2):
                        pt, r_pt = pts[hh]
                        r0 = 64 * hh
                        tp = None if hh == 0 else (0, 64)
                        for ti, (tile, mkind) in enumerate(tiles):
                            S.op("pe", lambda e, od=od, r0=r0, ti=ti, tile=tile, pt=pt, tp=tp, nt=nt, g=g: e.matmul(od[r0:r0 + 64, 0, :], lhsT=vtok[:, tile, g * 64:(g + 1) * 64], rhs=pt[:, ti, :], start=(ti == 0), stop=(ti == nt - 1), tile_position=tp),
                                 reads=[r_vtok, r_pt], writes=[r_od], signal=False)
                        for ti, (tile, mkind) in enumerate(tiles):
                            S.op("pe", lambda e, od=od, r0=r0, ti=ti, pt=pt, tp=tp, nt=nt: e.matmul(od[r0:r0 + 64, 1, :], lhsT=onesb[:, 0:64], rhs=pt[:, ti, :], start=(ti == 0), stop=(ti == nt - 1), tile_position=tp),
                                 reads=[r_onesb, r_pt], writes=[r_od], signal=(ti == nt - 1))
                    rc, r_rc = rpool.next()
                    S.op("dve", lambda e, rc=rc, od=od, c=c: e.tensor_scalar(out=rc[:], in0=od[:, 1, :], scalar1=es2[:, c:c + 1], scalar2=None, op0=ALU.add), reads=[r_od, r_es2], writes=[r_rc])
                    S.op("dve", lambda e, rc=rc: e.reciprocal(out=rc[:], in_=rc[:]), reads=[r_rc], writes=[r_rc])
                    S.op("dve", lambda e, rc=rc, od=od, oc=oc, q0=q0: e.tensor_tensor(out=oc[:, q0:q0 + 128], in0=od[:, 0, :], in1=rc[:], op=ALU.mult), reads=[r_od, r_rc], writes=[r_oc])
                if l == 0:
                    S.dma("sp", lambda e, c=c, oc=oc: e.dma_start(out=self.OAT[c], in_=oc[:]), reads=[r_oc], writes=[self.r_OAT[c]])
                else:
                    S.dma("sp", lambda e, c=c, oc=oc: e.dma_start(out=self.OAT[c][:, CTX:T], in_=oc[:, CTX:T]), reads=[r_oc], writes=[self.r_OAT[c]])
            self.end_phase()

    def phase_merge(self, l, s):
        cfg, nc, S = self.cfg, self.nc, self.S
        D, KC, T, CTX = cfg.D, cfg.KC, cfg.T, cfg.CTX
        BIG, r_BIG = self.BIG
        blocks = cfg.tblocks if l == 0 else cfg.lat_blocks
        tiles = list(range(cfg.NT)) if l == 0 else list(range(cfg.NCT, cfg.NT))
        with ExitStack() as ph:
            aT, r_aT = tile1(ph, nc, "m_aT", [128, 8, T], BF16)
            oT, r_oT = tile1(ph, nc, "m_oT", [128, 8, T], BF16)
            S.dma("sp", lambda e: e.dma_start(out=aT[:], in_=self.AT.rearrange("c p t -> p c t")), reads=self.r_AT, writes=[r_aT])
            S.dma("sp", lambda e: e.dma_start(out=oT[:], in_=self.OAT.rearrange("c p t -> p c t")), reads=self.r_OAT, writes=[r_oT])
            wap = TPool(ph, nc, "m_wa", [128, 8, 512], BF16, 2)
            wbp = TPool(ph, nc, "m_wb", [128, 8, 512], BF16, 2)
            gap = TPool(ph, nc, "m_ga", [128, T], BF16, 2)
            gbp = TPool(ph, nc, "m_gb", [128, T], BF16, 2)
            tp = TPool(ph, nc, "m_t", [128, 512], F32, 4)
            ps1 = TPool(ph, nc, "m_ps1", [128, 512], F32, 2, psum=True)
            ps2 = TPool(ph, nc, "m_ps2", [128, 512], F32, 2, psum=True)
            for jb in range(4):
                wa, r_wa = self.load_wblock(wap, self.w_a[l], jb * 512, 512)
                wb, r_wb = self.load_wblock(wbp, self.w_b[l], jb * 512, 512)
                for c in range(4):
                    j = jb * 4 + c
                    ga, r_ga = gap.next()
                    gb, r_gb = gbp.next()
                    S.dma("sp", lambda e, ga=ga, j=j: e.dma_start(out=ga[:], in_=self.GA[j]), reads=[self.r_GA[j]], writes=[r_ga])
                    S.dma("sp", lambda e, gb=gb, j=j: e.dma_start(out=gb[:], in_=self.GB[j]), reads=[self.r_GB[j]], writes=[r_gb])
                    for (t0, n) in blocks:
                        p1, r_p1 = ps1.next()
                        p2, r_p2 = ps2.next()
                        for kc in range(8):
                            S.op("pe", lambda e, p1=p1, wa=wa, kc=kc, c=c, t0=t0, n=n: e.matmul(p1[:, 0:n], lhsT=wa[:, kc, c * 128:(c + 1) * 128], rhs=aT[:, kc, t0:t0 + n], start=(kc == 0), stop=(kc == 7)),
                                 reads=[r_wa, r_aT], writes=[r_p1], signal=(kc == 7))
                        for kc in range(8):
                            S.op("pe", lambda e, p2=p2, wb=wb, kc=kc, c=c, t0=t0, n=n: e.matmul(p2[:, 0:n], lhsT=wb[:, kc, c * 128:(c + 1) * 128], rhs=oT[:, kc, t0:t0 + n], start=(kc == 0), stop=(kc == 7)),
                                 reads=[r_wb, r_oT], writes=[r_p2], signal=(kc == 7))
                        t1, r_t1 = tp.next()
                        t2, r_t2 = tp.next()
                        S.op("dve", lambda e, t1=t1, p1=p1, ga=ga, t0=t0, n=n: e.tensor_tensor(out=t1[:, 0:n], in0=p1[:, 0:n], in1=ga[:, t0:t0 + n], op=ALU.mult), reads=[r_p1, r_ga], writes=[r_t1])
                        S.op("dve", lambda e, t2=t2, p2=p2, gb=gb, t0=t0, n=n: e.tensor_tensor(out=t2[:, 0:n], in0=p2[:, 0:n], in1=gb[:, t0:t0 + n], op=ALU.mult), reads=[r_p2, r_gb], writes=[r_t2])
                        S.op("pool", lambda e, t1=t1, t2=t2, j=j, t0=t0, n=n: e.tensor_tensor(out=BIG[:, j, t0:t0 + n], in0=t1[:, 0:n], in1=t2[:, 0:n], op=ALU.add), reads=[r_t1, r_t2], writes=[r_BIG])
            self.end_phase()
        with ExitStack() as ph:
            wop = TPool(ph, nc, "m_wo", [128, KC, 512], BF16, 2)
            g1 = {}
            for kind, row in (("lat", s), ("ctx", cfg.NSEQ)):
                g1[kind] = tile1(ph, nc, "m_g1" + kind, [128, D], F32)
                self.bcast_row(g1[kind][0], g1[kind][1], self.MOD[l, row:row + 1, 2 * D:3 * D], D)
            xp = TPool(ph, nc, "m_x", [128, 512], F32, 3)
            tp = TPool(ph, nc, "m_t2", [128, 512], F32, 2)
            xo = TPool(ph, nc, "m_xo", [128, 512], F32, 3)
            pso = TPool(ph, nc, "m_pso", [128, 512], F32, 3, psum=True)
            first = (l == 0)
            for nb in range(4):
                wo, r_wo = self.load_wblock(wop, self.w_o[l], nb * 512, 512)
                loaded = {}

                def load(i, nb=nb, loaded=loaded):
                    tile = tiles[i]
                    xt, r_xt = xp.next()
                    src = self.xsrc(first, s, tile)[:, nb * 512:(nb + 1) * 512]
                    S.dma("sp", lambda e, xt=xt, src=src: e.dma_start(out=xt[:], in_=src), reads=([] if first else [self.r_XR[s][tile]]), writes=[r_xt])
                    loaded[i] = (xt, r_xt)
                load(0)
                for i, tile in enumerate(tiles):
                    if i + 1 < len(tiles):
                        load(i + 1)
                    xt, r_xt = loaded.pop(i)
                    g_t, g_r = g1["ctx" if tile < cfg.NCT else "lat"]
                    ps, r_ps = pso.next()
                    for kc in range(KC):
                        S.op("pe", lambda e, ps=ps, kc=kc, tile=tile, wo=wo: e.matmul(ps[:], lhsT=BIG[:, kc, tile * 128:(tile + 1) * 128], rhs=wo[:, kc, :], start=(kc == 0), stop=(kc == KC - 1)),
                             reads=[r_BIG, r_wo], writes=[r_ps], signal=(kc == KC - 1))
                    t1, r_t1 = tp.next()
                    S.op("dve", lambda e, t1=t1, ps=ps, g_t=g_t, nb=nb: e.tensor_tensor(out=t1[:], in0=ps[:], in1=g_t[:, nb * 512:(nb + 1) * 512], op=ALU.mult), reads=[r_ps, g_r], writes=[r_t1])
                    xn, r_xn = xo.next()
                    S.op("dve", lambda e, xn=xn, t1=t1, xt=xt: e.tensor_tensor(out=xn[:], in0=t1[:], in1=xt[:], op=ALU.add), reads=[r_t1, r_xt], writes=[r_xn])
                    S.dma("sp", lambda e, xn=xn, tile=tile, nb=nb: e.dma_start(out=self.XR[s, tile * 128:(tile + 1) * 128, nb * 512:(nb + 1) * 512], in_=xn[:]), reads=[r_xn], writes=[self.r_XR[s][tile]])
            self.end_phase()

    def phase_swiglu(self, l, s):
        cfg, nc, S = self.cfg, self.nc, self.S
        D, KC, T, CTX, NCT = cfg.D, cfg.KC, cfg.T, cfg.CTX, cfg.NCT
        BIG, r_BIG = self.BIG
        moe = (l == 1)
        blocks = cfg.lat_blocks if moe else cfg.tblocks
        if moe:
            experts = [(self.moe_wg[0, e], self.moe_wu[0, e], self.moe_wd[0, e], cfg.DEXP, e) for e in range(8)]
        else:
            experts = [(self.ffn_wg[0], self.ffn_wu[0], self.ffn_wd[0], cfg.DFF, None)]
        GF = 256
        with ExitStack() as ph:
            g2 = {}
            for kind, row in (("lat", s), ("ctx", cfg.NSEQ)):
                if moe and kind == "ctx":
                    continue
                g2[kind] = tile1(ph, nc, "f_g2" + kind, [128, D], F32)
                self.bcast_row(g2[kind][0], g2[kind][1], self.MOD[l, row:row + 1, 5 * D:6 * D], D)
            comb = None
            if moe:
                comb = self.routing(ph, s)
            wgp = TPool(ph, nc, "f_wg", [128, KC, GF], BF16, 2)
            wup = TPool(ph, nc, "f_wu", [128, KC, GF], BF16, 2)
            wdp = TPool(ph, nc, "f_wd", [128, GF // 128, D], BF16, 2)
            acc, r_acc = tile1(ph, nc, "f_acc", [128, 4, D], F32)
            actp = TPool(ph, nc, "f_act", [128, GF // 128, 512], BF16, 2)
            sgp = TPool(ph, nc, "f_sg", [128, 512], F32, 2)
            xp = TPool(ph, nc, "f_x", [128, D], F32, 2)
            tp = TPool(ph, nc, "f_t", [128, D], F32, 1)
            psg = TPool(ph, nc, "f_psg", [128, 512], F32, 2, psum=True)
            psu = TPool(ph, nc, "f_psu", [128, 512], F32, 2, psum=True)
            pso = TPool(ph, nc, "f_pso", [128, 512], F32, 2 if moe else 3, psum=True)
            for (wg2d, wu2d, wd2d, dff, eidx) in experts:
                ngr = dff // GF
                for (t0, n) in blocks:
                    ntl = n // 128
                    for gr in range(ngr):
                        wg, r_wg = self.load_wblock(wgp, wg2d, gr * GF, GF)
                        wu, r_wu = self.load_wblock(wup, wu2d, gr * GF, GF)
                        wd, r_wd = wdp.next()
                        for hlf in range(2):
                            srcd = wd2d[gr * GF:(gr + 1) * GF, hlf * 1024:(hlf + 1) * 1024].rearrange("(c p) n -> p c n", p=128)
                            S.dma("pool", lambda e, wd=wd, srcd=srcd, hlf=hlf: e.dma_start(out=wd[:, :, hlf * 1024:(hlf + 1) * 1024], in_=srcd), writes=[r_wd])
                        act, r_act = actp.next()
                        for c in range(GF // 128):
                            pg, r_pg = psg.next()
                            pu, r_pu = psu.next()
                            for kc in range(KC):
                                S.op("pe", lambda e, pg=pg, wg=wg, kc=kc, c=c, t0=t0, n=n: e.matmul(pg[:, 0:n], lhsT=wg[:, kc, c * 128:(c + 1) * 128], rhs=BIG[:, kc, t0:t0 + n], start=(kc == 0), stop=(kc == KC - 1)),
                                     reads=[r_wg, r_BIG], writes=[r_pg], signal=(kc == KC - 1))
                            for kc in range(KC):
                                S.op("pe", lambda e, pu=pu, wu=wu, kc=kc, c=c, t0=t0, n=n: e.matmul(pu[:, 0:n], lhsT=wu[:, kc, c * 128:(c + 1) * 128], rhs=BIG[:, kc, t0:t0 + n], start=(kc == 0), stop=(kc == KC - 1)),
                                     reads=[r_wu, r_BIG], writes=[r_pu], signal=(kc == KC - 1))
                            sg, r_sg = sgp.next()
                            S.op("act", lambda e, sg=sg, pg=pg, n=n: e.activation(out=sg[:, 0:n], in_=pg[:, 0:n], func=AF.Silu), reads=[r_pg], writes=[r_sg])
                            S.op("dve", lambda e, act=act, sg=sg, pu=pu, c=c, n=n: e.tensor_tensor(out=act[:, c, 0:n], in0=sg[:, 0:n], in1=pu[:, 0:n], op=ALU.mult), reads=[r_sg, r_pu], writes=[r_act])
                        for tl in range(ntl):
                            for nb in range(4):
                                po, r_po = pso.next()
                                nch = GF // 128
                                for c in range(nch):
                                    S.op("pe", lambda e, po=po, act=act, wd=wd, c=c, tl=tl, nb=nb, nch=nch: e.matmul(po[:], lhsT=act[:, c, tl * 128:(tl + 1) * 128], rhs=wd[:, c, nb * 512:(nb + 1) * 512], start=(c == 0), stop=(c == nch - 1)),
                                         reads=[r_act, r_wd], writes=[r_po], signal=(c == nch - 1))
                                if gr == 0:
                                    S.op("act", lambda e, po=po, tl=tl, nb=nb: e.copy(out=acc[:, tl, nb * 512:(nb + 1) * 512], in_=po[:]), reads=[r_po], writes=[r_acc])
                                else:
                                    S.op("dve", lambda e, po=po, tl=tl, nb=nb: e.tensor_tensor(out=acc[:, tl, nb * 512:(nb + 1) * 512], in0=po[:], in1=acc[:, tl, nb * 512:(nb + 1) * 512], op=ALU.add), reads=[r_po, r_acc], writes=[r_acc])
                    for tl in range(ntl):
                        tile = t0 // 128 + tl
                        g_t, g_r = g2["ctx" if tile < NCT else "lat"]
                        xt, r_xt = xp.next()
                        S.dma("sp", lambda e, xt=xt, tile=tile: e.dma_start(out=xt[:], in_=self.XR[s, tile * 128:(tile + 1) * 128, :]), reads=[self.r_XR[s][tile]], writes=[r_xt])
                        t1, r_t1 = tp.next()
                        S.op("dve", lambda e, t1=t1, tl=tl, g_t=g_t: e.tensor_tensor(out=t1[:], in0=acc[:, tl, :], in1=g_t[:], op=ALU.mult), reads=[r_acc, g_r], writes=[r_t1])
                        if eidx is None:
                            S.op("dve", lambda e, t1=t1, xt=xt: e.tensor_tensor(out=xt[:], in0=t1[:], in1=xt[:], op=ALU.add), reads=[r_t1, r_xt], writes=[r_xt])
                        else:
                            cb, r_cb = comb
                            lt = tile - NCT
                            S.op("dve", lambda e, t1=t1, xt=xt, cb=cb, lt=lt, eidx=eidx: e.scalar_tensor_tensor(out=xt[:], in0=t1[:], scalar=cb[:, lt, eidx:eidx + 1], in1=xt[:], op0=ALU.mult, op1=ALU.add),
                                 reads=[r_t1, r_xt, r_cb], writes=[r_xt])
                        S.dma("sp", lambda e, xt=xt, tile=tile: e.dma_start(out=self.XR[s, tile * 128:(tile + 1) * 128, :], in_=xt[:]), reads=[r_xt], writes=[self.r_XR[s][tile]])
            self.end_phase()

    def routing(self, ph, s, want_sel=False):
        cfg, nc, S = self.cfg, self.nc, self.S
        D, KC, NCT = cfg.D, cfg.KC, cfg.NCT
        BIG, r_BIG = self.BIG
        NTl = cfg.SEQ // 128
        rf, r_rf = tile1(ph, nc, "r_rf", [128, KC, 8], F32)
        rh, r_rh = tile1(ph, nc, "r_rh", [128, KC, 8], BF16)
        rl, r_rl = tile1(ph, nc, "r_rl", [128, KC, 8], BF16)
        S.dma("sp", lambda e: e.dma_start(out=rf[:], in_=self.router[0].rearrange("(kc p) e -> p kc e", p=128)), writes=[r_rf])
        S.op("dve", lambda e: e.tensor_copy(out=rh[:], in_=rf[:]), reads=[r_rf], writes=[r_rh])
        S.op("dve", lambda e: e.tensor_tensor(out=rl[:], in0=rf[:], in1=rh[:], op=ALU.subtract), reads=[r_rf, r_rh], writes=[r_rl])
        lg, r_lg = tile1(ph, nc, "r_lg", [128, NTl, 8], F32)
        psl = TPool(ph, nc, "r_ps", [128, 8], F32, 2, psum=True)
        for lt in range(NTl):
            tile = NCT + lt
            ps, r_ps = psl.next()
            for i, (rt, rr) in enumerate(((rh, r_rh), (rl, r_rl))):
                for kc in range(KC):
                    S.op("pe", lambda e, ps=ps, rt=rt, kc=kc, tile=tile, i=i: e.matmul(ps[:], lhsT=BIG[:, kc, tile * 128:(tile + 1) * 128], rhs=rt[:, kc, :], start=(i == 0 and kc == 0), stop=(i == 1 and kc == KC - 1)),
                         reads=[r_BIG, rr], writes=[r_ps], signal=(i == 1 and kc == KC - 1))
            S.op("act", lambda e, ps=ps, lt=lt: e.copy(out=lg[:, lt, :], in_=ps[:]), reads=[r_ps], writes=[r_lg])
        shp = [128, NTl, 8]
        m1, r_m1 = tile1(ph, nc, "r_m1", [128, NTl], F32)
        m2, r_m2 = tile1(ph, nc, "r_m2", [128, NTl], F32)
        t8, r_t8 = tile1(ph, nc, "r_t8", shp, F32)
        l2, r_l2 = tile1(ph, nc, "r_l2", shp, F32)
        sel, r_sel = tile1(ph, nc, "r_sel", shp, F32)
        cb, r_cb = tile1(ph, nc, "r_cb", shp, F32)
        bc = lambda t: t[:].unsqueeze(2).broadcast_to(shp)
        S.op("dve", lambda e: e.tensor_reduce(out=m1[:], in_=lg[:], axis=AX.X, op=ALU.max), reads=[r_lg], writes=[r_m1])
        S.op("dve", lambda e: e.tensor_tensor(out=t8[:], in0=lg[:], in1=bc(m1), op=ALU.is_equal), reads=[r_lg, r_m1], writes=[r_t8])
        S.op("dve", lambda e: e.scalar_tensor_tensor(out=l2[:], in0=t8[:], scalar=-1e30, in1=lg[:], op0=ALU.mult, op1=ALU.add), reads=[r_t8, r_lg], writes=[r_l2])
        S.op("dve", lambda e: e.tensor_reduce(out=m2[:], in_=l2[:], axis=AX.X, op=ALU.max), reads=[r_l2], writes=[r_m2])
        S.op("dve", lambda e: e.tensor_tensor(out=sel[:], in0=lg[:], in1=bc(m2), op=ALU.is_ge), reads=[r_lg, r_m2], writes=[r_sel])
        S.op("dve", lambda e: e.tensor_tensor(out=t8[:], in0=lg[:], in1=bc(m1), op=ALU.subtract), reads=[r_lg, r_m1], writes=[r_t8])
        S.op("act", lambda e: e.activation(out=t8[:], in_=t8[:], func=AF.Exp), reads=[r_t8], writes=[r_t8])
        S.op("dve", lambda e: e.tensor_tensor(out=t8[:], in0=t8[:], in1=sel[:], op=ALU.mult), reads=[r_t8, r_sel], writes=[r_t8])
        S.op("dve", lambda e: e.tensor_tensor(out=m2[:], in0=m2[:], in1=m1[:], op=ALU.subtract), reads=[r_m2, r_m1], writes=[r_m2])
        S.op("act", lambda e: e.activation(out=m2[:], in_=m2[:], func=AF.Exp), reads=[r_m2], writes=[r_m2])
        S.op("dve", lambda e: e.tensor_scalar(out=m2[:], in0=m2[:], scalar1=1.0, scalar2=None, op0=ALU.add), reads=[r_m2], writes=[r_m2])
        S.op("dve", lambda e: e.reciprocal(out=m2[:], in_=m2[:]), reads=[r_m2], writes=[r_m2])
        S.op("dve", lambda e: e.tensor_tensor(out=cb[:], in0=t8[:], in1=bc(m2), op=ALU.mult), reads=[r_t8, r_m2], writes=[r_cb])
        if want_sel:
            return cb, r_cb, sel, r_sel
        return cb, r_cb

    def phase_final(self):
        cfg, nc, S = self.cfg, self.nc, self.S
        D, NCT = cfg.D, cfg.NCT
        with ExitStack() as ph:
            fn, r_fn = tile1(ph, nc, "z_fn", [128, D], F32)
            S.dma("sp", lambda e: e.dma_start(out=fn[:], in_=self.final_norm.unsqueeze(0).partition_broadcast(128)), writes=[r_fn])
            xp = TPool(ph, nc, "z_x", [128, D], F32, 3)
            op_ = TPool(ph, nc, "z_o", [128, D], F32, 2)
            junk, r_junk = tile1(ph, nc, "z_junk", [128, D], BF16)
            stp = TPool(ph, nc, "z_st", [128, 4], F32, 2)
            jobs = [(s, tile) for s in range(cfg.NSEQ) for tile in range(NCT, cfg.NT)]
            loaded = {}

            def load(i):
                s, tile = jobs[i]
                xt, r_xt = xp.next()
                S.dma("sp", lambda e, xt=xt, s=s, tile=tile: e.dma_start(out=xt[:], in_=self.XR[s, tile * 128:(tile + 1) * 128, :]), reads=[self.r_XR[s][tile]], writes=[r_xt])
                loaded[i] = (xt, r_xt)
            load(0)
            for i, (s, tile) in enumerate(jobs):
                if i + 1 < len(jobs):
                    load(i + 1)
                xt, r_xt = loaded.pop(i)
                st, r_st = stp.next()
                S.op("act", lambda e, xt=xt, st=st: e.activation(out=junk[:], in_=xt[:], func=AF.Square, accum_out=st[:, 0:1]), reads=[r_xt], writes=[r_junk, r_st])
                S.op("dve", lambda e, st=st: e.tensor_scalar(out=st[:, 1:2], in0=st[:, 0:1], scalar1=1.0 / D, scalar2=EPS, op0=ALU.mult, op1=ALU.add), reads=[r_st], writes=[r_st])
                S.op("act", lambda e, st=st: e.activation(out=st[:, 2:3], in_=st[:, 1:2], func=AF.Sqrt), reads=[r_st], writes=[r_st])
                S.op("dve", lambda e, st=st: e.reciprocal(out=st[:, 3:4], in_=st[:, 2:3]), reads=[r_st], writes=[r_st])
                ot, r_ot = op_.next()
                S.op("dve", lambda e, ot=ot, xt=xt, st=st: e.scalar_tensor_tensor(out=ot[:], in0=xt[:], scalar=st[:, 3:4], in1=fn[:], op0=ALU.mult, op1=ALU.mult), reads=[r_xt, r_st, r_fn], writes=[r_ot])
                lt = tile - NCT
                S.dma("sp", lambda e, ot=ot, s=s, lt=lt: e.dma_start(out=self.out[s, lt * 128:(lt + 1) * 128, :], in_=ot[:]), reads=[r_ot], writes=[self.r_out])
            self.end_phase()

    def prepass_jobs(self):
        cfg = self.cfg
        NG = cfg.DEXP // 512
        jobs = []
        for e_ in range(8):
            for g in range(NG):
                row0 = (e_ * NG + g) * 128
                for (dst_t, src_t) in ((self.WGB, self.moe_wg), (self.WUB, self.moe_wu)):
                    dst = dst_t[row0:row0 + 128, :].rearrange("p (kc n) -> p kc n", n=512)
                    src = src_t[0, e_, :, g * 512:(g + 1) * 512].rearrange("(kc p) n -> p kc n", p=128)
                    jobs.append((dst, src))
                for hlf in range(2):
                    dst = self.WDB[row0:row0 + 128, :].rearrange("p (c n) -> p c n", n=2048)[:, :, hlf * 1024:(hlf + 1) * 1024]
                    src = self.moe_wd[0, e_, g * 512:(g + 1) * 512, hlf * 1024:(hlf + 1) * 1024].rearrange("(c p) n -> p c n", p=128)
                    jobs.append((dst, src))
        return jobs

    def pump(self, k):
        if self.cfg.MOE != "routed":
            return
        for _ in range(k):
            if not self.pre_jobs:
                return
            dst, src = self.pre_jobs.pop(0)
            self.S.dma("pool", lambda e, dst=dst, src=src: e.dma_start(out=dst, in_=src))

    def route_local(self, s):
        cfg, nc, S = self.cfg, self.nc, self.S
        NTl = cfg.SEQ // 128
        with ExitStack() as ph:
            cb, r_cb, sel, r_sel = self.routing(ph, s, want_sel=True)
            S.dma("sp", lambda e: e.dma_start(out=self.SELD[:, s * NTl:(s + 1) * NTl, :], in_=sel[:]), reads=[r_sel], writes=[self.r_SELD])
            S.dma("sp", lambda e: e.dma_start(out=self.CBD[:, s * NTl:(s + 1) * NTl, :], in_=cb[:]), reads=[r_cb], writes=[self.r_SELD])
            self.end_phase()

    def phase_moe_routed(self):
        cfg, nc, S = self.cfg, self.nc, self.S
        D, KC, NCT = cfg.D, cfg.KC, cfg.NCT
        NTl = cfg.SEQ // 128
        NTg = cfg.NSEQ * NTl
        NSLOT = cfg.NSLOT
        NG = cfg.DEXP // 512
        ident, r_ident = self.C["ident"]
        ustrict, r_us = self.C["ustrict"]
        onesb, r_onesb = self.C["ones"]
        shp = [128, NTg, 8]
        self.pump(100000)
        with ExitStack() as ms:
            posA, r_posA = tile1(ms, nc, "q_posA", [128, NTg], I32)
            posB, r_posB = tile1(ms, nc, "q_posB", [128, NTg], I32)
            wA, r_wA = tile1(ms, nc, "q_wA", [128, NTg], F32)
            wB, r_wB = tile1(ms, nc, "q_wB", [128, NTg], F32)
            idxW, r_idxW = tile1(ms, nc, "q_idxW", [128, NSLOT, NG], I32)
            with ExitStack() as ph:
                sel, r_sel = tile1(ph, nc, "q_sel", shp, F32)
                cb, r_cb = tile1(ph, nc, "q_cb", shp, F32)
                selb, r_selb = tile1(ph, nc, "q_selb", shp, BF16)
                S.dma("sp", lambda e: e.dma_start(out=sel[:], in_=self.SELD), reads=[self.r_SELD], writes=[r_sel])
                S.dma("sp", lambda e: e.dma_start(out=cb[:], in_=self.CBD), reads=[self.r_SELD], writes=[r_cb])
                S.op("dve", lambda e: e.tensor_copy(out=selb[:], in_=sel[:]), reads=[r_sel], writes=[r_selb])
                pR, r_pR = tile1(ph, nc, "q_pR", [128, NTg * 8], F32, psum=True)
                pC, r_pC = tile1(ph, nc, "q_pC", [128, NTg * 8], F32, psum=True)
                flat = lambda t: t[:].rearrange("p g e -> p (g e)")
                S.op("pe", lambda e: e.matmul(pR[:], lhsT=ustrict[:], rhs=flat(selb), start=True, stop=True), reads=[r_us, r_selb], writes=[r_pR])
                S.op("pe", lambda e: e.matmul(pC[:], lhsT=onesb[:], rhs=flat(selb), start=True, stop=True), reads=[r_onesb, r_selb], writes=[r_pC])
                Cs, r_Cs = tile1(ph, nc, "q_Cs", shp, F32)
                incl, r_incl = tile1(ph, nc, "q_incl", shp, F32)
                pos, r_pos = tile1(ph, nc, "q_pos", shp, F32)
                v, r_v = tile1(ph, nc, "q_v", shp, F32)
                t8, r_t8 = tile1(ph, nc, "q_t8", shp, F32)
                o32, r_o32 = tile1(ph, nc, "q_o32", [128, NTg], F32)
                S.op("dve", lambda e: e.memset(o32[:], 1.0), writes=[r_o32])
                S.op("act", lambda e: e.copy(out=flat(Cs), in_=pC[:]), reads=[r_pC], writes=[r_Cs])
                for e_ in range(8):
                    S.op("dve", lambda e, e_=e_: e.tensor_tensor_scan(out=incl[:, :, e_], data0=o32[:], data1=Cs[:, :, e_], initial=0.0, op0=ALU.mult, op1=ALU.add),
                         reads=[r_o32, r_Cs], writes=[r_incl])
                sm = lambda name, w: tile1(ph, nc, name, [128, w], F32)
                n_e, r_n = sm("q_n", 8)
                np_e, r_np = sm("q_np", 8)
                base, r_base = sm("q_base", 8)
                cs, r_cs = sm("q_cs", 8)
                S.op("dve", lambda e: e.tensor_copy(out=n_e[:], in_=incl[:, NTg - 1, :]), reads=[r_incl], writes=[r_n])
                KMAX = max(1, (cfg.NSEQ * cfg.SEQ) // 512)
                kg, r_kg = tile1(ph, nc, "q_kg", [128, 8, KMAX], F32)
                S.dma("sp", lambda e: e.dma_start(out=kg[:], in_=self.cst_d["kgrid"].rearrange("p (e k) -> p e k", k=KMAX)), writes=[r_kg])
                S.op("dve", lambda e: e.tensor_tensor(out=kg[:], in0=n_e[:].unsqueeze(2).broadcast_to([128, 8, KMAX]), in1=kg[:], op=ALU.is_gt), reads=[r_n, r_kg], writes=[r_kg])
                S.op("dve", lambda e: e.tensor_reduce(out=np_e[:], in_=kg[:], axis=AX.X, op=ALU.add), reads=[r_kg], writes=[r_np])
                S.op("dve", lambda e: e.tensor_scalar(out=np_e[:], in0=np_e[:], scalar1=512.0, scalar2=None, op0=ALU.mult), reads=[r_np], writes=[r_np])
                S.op("dve", lambda e: e.memset(base[:], 0.0), writes=[r_base])
                for e_ in range(1, 8):
                    S.op("dve", lambda e, e_=e_: e.tensor_tensor(out=base[:, e_:e_ + 1], in0=base[:, e_ - 1:e_], in1=np_e[:, e_ - 1:e_], op=ALU.add), reads=[r_base, r_np], writes=[r_base])
                S.op("dve", lambda e: e.tensor_tensor(out=pos[:], in0=incl[:], in1=Cs[:], op=ALU.subtract), reads=[r_incl, r_Cs], writes=[r_pos])
                S.op("dve", lambda e: e.tensor_tensor(out=flat(pos), in0=pR[:], in1=flat(pos), op=ALU.add), reads=[r_pR, r_pos], writes=[r_pos])
                S.op("dve", lambda e: e.tensor_tensor(out=pos[:], in0=pos[:], in1=base[:].unsqueeze(1).broadcast_to(shp), op=ALU.add), reads=[r_pos, r_base], writes=[r_pos])
                S.op("dve", lambda e: e.scalar_tensor_tensor(out=v[:], in0=pos[:], scalar=1.0, in1=sel[:], op0=ALU.add, op1=ALU.mult), reads=[r_pos, r_sel], writes=[r_v])
                pA1, r_pA1 = sm("q_pA1", NTg)
                pB1, r_pB1 = sm("q_pB1", NTg)
                bc = lambda t: t[:].unsqueeze(2).broadcast_to(shp)
                S.op("dve", lambda e: e.tensor_reduce(out=pA1[:], in_=v[:], axis=AX.X, op=ALU.max), reads=[r_v], writes=[r_pA1])
                S.op("dve", lambda e: e.tensor_tensor(out=t8[:], in0=v[:], in1=bc(pA1), op=ALU.is_equal), reads=[r_v, r_pA1], writes=[r_t8])
                S.op("dve", lambda e: e.tensor_tensor(out=pos[:], in0=t8[:], in1=cb[:], op=ALU.mult), reads=[r_t8, r_cb], writes=[r_pos])
                S.op("dve", lambda e: e.tensor_reduce(out=wA[:], in_=pos[:], axis=AX.X, op=ALU.add), reads=[r_pos], writes=[r_wA])
                S.op("dve", lambda e: e.tensor_scalar(out=wB[:], in0=wA[:], scalar1=-1.0, scalar2=1.0, op0=ALU.mult, op1=ALU.add), reads=[r_wA], writes=[r_wB])
                S.op("dve", lambda e: e.tensor_tensor(out=t8[:], in0=t8[:], in1=v[:], op=ALU.mult), reads=[r_t8, r_v], writes=[r_t8])
                S.op("dve", lambda e: e.tensor_tensor(out=t8[:], in0=v[:], in1=t8[:], op=ALU.subtract), reads=[r_t8, r_v], writes=[r_t8])
                S.op("dve", lambda e: e.tensor_reduce(out=pB1[:], in_=t8[:], axis=AX.X, op=ALU.max), reads=[r_t8], writes=[r_pB1])
                S.op("dve", lambda e: e.tensor_scalar(out=posA[:], in0=pA1[:], scalar1=-1.0, scalar2=None, op0=ALU.add), reads=[r_pA1], writes=[r_posA])
                S.op("dve", lambda e: e.tensor_scalar(out=posB[:], in0=pB1[:], scalar1=-1.0, scalar2=None, op0=ALU.add), reads=[r_pB1], writes=[r_posB])
                S.op("dve", lambda e: e.tensor_tensor(out=cs[:], in0=base[:], in1=np_e[:], op=ALU.add), reads=[r_base, r_np], writes=[r_cs])
                jg, r_jg = tile1(ph, nc, "q_jg", [128, NSLOT, 8], F32)
                gp, r_gp = tile1(ph, nc, "q_gp", [128, NG], F32)
                S.dma("sp", lambda e: e.dma_start(out=jg[:], in_=self.cst_d["jgrid"].rearrange("p (j e) -> p j e", e=8)), writes=[r_jg])
                S.dma("sp", lambda e: e.dma_start(out=gp[:], in_=self.cst_d["gp"][:, :]), writes=[r_gp])
                S.op("dve", lambda e: e.tensor_tensor(out=jg[:], in0=cs[:].unsqueeze(1).broadcast_to([128, NSLOT, 8]), in1=jg[:], op=ALU.is_le), reads=[r_cs, r_jg], writes=[r_jg])
                eid, r_eid = sm("q_eid", NSLOT)
                S.op("dve", lambda e: e.tensor_reduce(out=eid[:], in_=jg[:], axis=AX.X, op=ALU.add), reads=[r_jg], writes=[r_eid])
                S.op("dve", lambda e: e.tensor_scalar(out=eid[:], in0=eid[:], scalar1=7.0, scalar2=None, op0=ALU.min), reads=[r_eid], writes=[r_eid])
                S.op("dve", lambda e: e.scalar_tensor_tensor(out=idxW[:], in0=eid[:].unsqueeze(2).broadcast_to([128, NSLOT, NG]), scalar=float(NG * 128), in1=gp[:].unsqueeze(1).broadcast_to([128, NSLOT, NG]), op0=ALU.mult, op1=ALU.add),
                     reads=[r_eid, r_gp], writes=[r_idxW])
                if cfg.DEBUG:
                    self.dump("posA", posA, r_posA, [128, NTg], I32)
                    self.dump("posB", posB, r_posB, [128, NTg], I32)
                    self.dump("wA", wA, r_wA, [128, NTg], F32)
                    self.dump("idxW", idxW, r_idxW, [128, NSLOT, NG], I32)
                    self.dump("eid", eid, r_eid, [128, NSLOT], F32)
                    self.dump("cs", cs, r_cs, [128, 8], F32)
                    self.dump("jg", jg, r_jg, [128, NSLOT, 8], F32)
                    self.dump("incl", incl, r_incl, shp, F32)
                    self.dump("Cs", Cs, r_Cs, shp, F32)
                    self.dump("np", np_e, r_np, [128, 8], F32)
                    self.dump("base", base, r_base, [128, 8], F32)
                self.end_phase()
                if cfg.STOP == "route":
                    return
            with ExitStack() as ph:
                hp = TPool(ph, nc, "q_h", [128, D], BF16, 3)
                for g in range(NTg):
                    ht, r_ht = hp.next()
                    S.dma("sp", lambda e, ht=ht, g=g: e.dma_start(out=ht[:], in_=self.H2[g * 128:(g + 1) * 128, :]), reads=[self.r_H2], writes=[r_ht])
                    for (pt, pr) in ((posA, r_posA), (posB, r_posB)):
                        S.dma("pool", lambda e, ht=ht, g=g, pt=pt: e.indirect_dma_start(out=self.HS, out_offset=bass.IndirectOffsetOnAxis(ap=pt[:, g:g + 1], axis=0), in_=ht[:], in_offset=None),
                              reads=[r_ht, pr], writes=[self.r_HS])
                self.end_phase()
                if cfg.STOP == "scatter":
                    return
            with ExitStack() as ph:
                hrp = TPool(ph, nc, "q_hr", [128, D], BF16, 2)
                hTp = TPool(ph, nc, "q_hT", [128, KC, 512], BF16, 2)
                wgp = TPool(ph, nc, "q_wg", [128, KC * 512], BF16, 2)
                wup = TPool(ph, nc, "q_wu", [128, KC * 512], BF16, 2)
                wdp = TPool(ph, nc, "q_wd", [128, 4 * D], BF16, 2)
                acc, r_acc = tile1(ph, nc, "q_acc", [128, 4, D], F32)
                actp = TPool(ph, nc, "q_act", [128, 4, 512], BF16, 2)
                sgp = TPool(ph, nc, "q_sg", [128, 512], F32, 2)
                psT = TPool(ph, nc, "q_psT", [128, KC, 128], BF16, 1, psum=True)
                psg = TPool(ph, nc, "q_psg", [128, 512], F32, 2, psum=True)
                psu = TPool(ph, nc, "q_psu", [128, 512], F32, 2, psum=True)
                pso = TPool(ph, nc, "q_pso", [128, 512], F32, 2, psum=True)
                for j in range(NSLOT):
                    hT, r_hT = hTp.next()
                    for tl in range(4):
                        hr, r_hr = hrp.next()
                        S.dma("sp", lambda e, hr=hr, j=j, tl=tl: e.dma_start(out=hr[:], in_=self.HS[j * 512 + tl * 128:j * 512 + (tl + 1) * 128, :]), reads=[self.r_HS], writes=[r_hr])
                        ps, r_ps = psT.next()
                        for kc in range(KC):
                            S.op("pe", lambda e, ps=ps, hr=hr, kc=kc: e.transpose(out=ps[:, kc, :], in_=hr[:, kc * 128:(kc + 1) * 128], identity=ident[:]),
                                 reads=[r_hr, r_ident], writes=[r_ps], signal=(kc == KC - 1))
                        S.op("act", lambda e, ps=ps, hT=hT, tl=tl: e.copy(out=hT[:, :, tl * 128:(tl + 1) * 128], in_=ps[:]), reads=[r_ps], writes=[r_hT])
                    for gr in range(NG):
                        wts = []
                        for (pool_, src_) in ((wgp, self.WGB), (wup, self.WUB), (wdp, self.WDB)):
                            wt, r_wt = pool_.next()
                            S.dma("pool", lambda e, wt=wt, src_=src_, j=j, gr=gr: e.indirect_dma_start(out=wt[:], out_offset=None, in_=src_, in_offset=bass.IndirectOffsetOnAxis(ap=idxW[:, j, gr:gr + 1], axis=0)),
                                  reads=[r_idxW, self.r_WB], writes=[r_wt])
                            wts.append((wt, r_wt))
                        (wg_, r_wg), (wu_, r_wu), (wd_, r_wd) = wts
                        wg = wg_[:].rearrange("p (kc n) -> p kc n", n=512)
                        wu = wu_[:].rearrange("p (kc n) -> p kc n", n=512)
                        wd = wd_[:].rearrange("p (c n) -> p c n", n=D)
                        act, r_act = actp.next()
                        for c in range(4):
                            pg, r_pg = psg.next()
                            pu, r_pu = psu.next()
                            for kc in range(KC):
                                S.op("pe", lambda e, pg=pg, wg=wg, kc=kc, c=c, hT=hT: e.matmul(pg[:], lhsT=wg[:, kc, c * 128:(c + 1) * 128], rhs=hT[:, kc, :], start=(kc == 0), stop=(kc == KC - 1)),
                                     reads=[r_wg, r_hT], writes=[r_pg], signal=(kc == KC - 1))
                            for kc in range(KC):
                                S.op("pe", lambda e, pu=pu, wu=wu, kc=kc, c=c, hT=hT: e.matmul(pu[:], lhsT=wu[:, kc, c * 128:(c + 1) * 128], rhs=hT[:, kc, :], start=(kc == 0), stop=(kc == KC - 1)),
                                     reads=[r_wu, r_hT], writes=[r_pu], signal=(kc == KC - 1))
                            sg, r_sg = sgp.next()
                            S.op("act", lambda e, sg=sg, pg=pg: e.activation(out=sg[:], in_=pg[:], func=AF.Silu), reads=[r_pg], writes=[r_sg])
                            S.op("dve", lambda e, act=act, sg=sg, pu=pu, c=c: e.tensor_tensor(out=act[:, c, :], in0=sg[:], in1=pu[:], op=ALU.mult), reads=[r_sg, r_pu], writes=[r_act])
                        for tl in range(4):
                            for nb in range(4):
                                po, r_po = pso.next()
                                for c in range(4):
                                    S.op("pe", lambda e, po=po, act=act, wd=wd, c=c, tl=tl, nb=nb: e.matmul(po[:], lhsT=act[:, c, tl * 128:(tl + 1) * 128], rhs=wd[:, c, nb * 512:(nb + 1) * 512], start=(c == 0), stop=(c == 3)),
                                         reads=[r_act, r_wd], writes=[r_po], signal=(c == 3))
                                if gr == 0:
                                    S.op("act", lambda e, po=po, tl=tl, nb=nb: e.copy(out=acc[:, tl, nb * 512:(nb + 1) * 512], in_=po[:]), reads=[r_po], writes=[r_acc])
                                else:
                                    S.op("dve", lambda e, po=po, tl=tl, nb=nb: e.tensor_tensor(out=acc[:, tl, nb * 512:(nb + 1) * 512], in0=po[:], in1=acc[:, tl, nb * 512:(nb + 1) * 512], op=ALU.add), reads=[r_po, r_acc], writes=[r_acc])
                    for tl in range(4):
                        S.dma("sp", lambda e, j=j, tl=tl: e.dma_start(out=self.YP[j * 512 + tl * 128:j * 512 + (tl + 1) * 128, :], in_=acc[:, tl, :]), reads=[r_acc], writes=[self.r_YP])
                self.end_phase()
            if cfg.STOP == "slots":
                return
            with ExitStack() as ph:
                fn, r_fn = tile1(ph, nc, "z_fn", [128, D], F32)
                S.dma("sp", lambda e: e.dma_start(out=fn[:], in_=self.final_norm.unsqueeze(0).partition_broadcast(128)), writes=[r_fn])
                g2 = []
                for s in range(cfg.NSEQ):
                    g2.append(tile1(ph, nc, "z_g2%d" % s, [128, D], F32))
                    self.bcast_row(g2[s][0], g2[s][1], self.MOD[1, s:s + 1, 5 * D:6 * D], D)
                xp = TPool(ph, nc, "z_x", [128, D], F32, 2)
                yap = TPool(ph, nc, "z_ya", [128, D], F32, 2)
                ybp = TPool(ph, nc, "z_yb", [128, D], F32, 2)
                op_ = TPool(ph, nc, "z_o", [128, D], F32, 2)
                junk, r_junk = tile1(ph, nc, "z_junk", [128, D], BF16)
                stp = TPool(ph, nc, "z_st", [128, 4], F32, 2)
                for g in range(NTg):
                    s, lt = g // NTl, g % NTl
                    tile = NCT + lt
                    xt, r_xt = xp.next()
                    S.dma("sp", lambda e, xt=xt, s=s, tile=tile: e.dma_start(out=xt[:], in_=self.XR[s, tile * 128:(tile + 1) * 128, :]), reads=[self.r_XR[s][tile]], writes=[r_xt])
                    ya, r_ya = yap.next()
                    yb, r_yb = ybp.next()
                    for (yt, r_yt, pt, pr) in ((ya, r_ya, posA, r_posA), (yb, r_yb, posB, r_posB)):
                        S.dma("pool", lambda e, yt=yt, pt=pt, g=g: e.indirect_dma_start(out=yt[:], out_offset=None, in_=self.YP, in_offset=bass.IndirectOffsetOnAxis(ap=pt[:, g:g + 1], axis=0)),
                              reads=[self.r_YP, pr], writes=[r_yt])
                    S.op("dve", lambda e, ya=ya, g=g: e.tensor_scalar(out=ya[:], in0=ya[:], scalar1=wA[:, g:g + 1], scalar2=None, op0=ALU.mult), reads=[r_ya, r_wA], writes=[r_ya])
                    S.op("dve", lambda e, ya=ya, yb=yb, g=g: e.scalar_tensor_tensor(out=ya[:], in0=yb[:], scalar=wB[:, g:g + 1], in1=ya[:], op0=ALU.mult, op1=ALU.add), reads=[r_ya, r_yb, r_wB], writes=[r_ya])
                    S.op("dve", lambda e, ya=ya, s=s: e.tensor_tensor(out=ya[:], in0=ya[:], in1=g2[s][0][:], op=ALU.mult), reads=[r_ya, g2[s][1]], writes=[r_ya])
                    S.op("dve", lambda e, ya=ya, xt=xt: e.tensor_tensor(out=xt[:], in0=ya[:], in1=xt[:], op=ALU.add), reads=[r_ya, r_xt], writes=[r_xt])
                    st, r_st = stp.next()
                    S.op("act", lambda e, xt=xt, st=st: e.activation(out=junk[:], in_=xt[:], func=AF.Square, accum_out=st[:, 0:1]), reads=[r_xt], writes=[r_junk, r_st])
                    S.op("dve", lambda e, st=st: e.tensor_scalar(out=st[:, 1:2], in0=st[:, 0:1], scalar1=1.0 / D, scalar2=EPS, op0=ALU.mult, op1=ALU.add), reads=[r_st], writes=[r_st])
                    S.op("act", lambda e, st=st: e.activation(out=st[:, 2:3], in_=st[:, 1:2], func=AF.Sqrt), reads=[r_st], writes=[r_st])
                    S.op("dve", lambda e, st=st: e.reciprocal(out=st[:, 3:4], in_=st[:, 2:3]), reads=[r_st], writes=[r_st])
                    ot, r_ot = op_.next()
                    S.op("dve", lambda e, ot=ot, xt=xt, st=st: e.scalar_tensor_tensor(out=ot[:], in0=xt[:], scalar=st[:, 3:4], in1=fn[:], op0=ALU.mult, op1=ALU.mult), reads=[r_xt, r_st, r_fn], writes=[r_ot])
                    S.dma("sp", lambda e, ot=ot, s=s, lt=lt: e.dma_start(out=self.out[s, lt * 128:(lt + 1) * 128, :], in_=ot[:]), reads=[r_ot], writes=[self.r_out])
                self.end_phase()


def make_in_maps(cfg, inputs, ncores):
    consts = host_consts(cfg)
    maps = []
    for core in range(ncores):
        b0 = core * cfg.NSEQ
        m = {}
        m["x_in"] = np.ascontiguousarray(inputs["x"][b0:b0 + cfg.NSEQ])
        m["ctx_in"] = np.ascontiguousarray(inputs["ctx"][b0:b0 + cfg.NSEQ])
        m["cvec"] = np.ascontiguousarray(np.concatenate([inputs["c"][b0:b0 + cfg.NSEQ], inputs["c_ctx"][None, :]], 0))
        for k in ("w_mod", "b_mod", "norm_mix", "norm_ffn", "w_in", "hg_lb_fwd", "hg_lb_bwd", "hg_norm", "attn_sink",
                  "w_branch_a", "w_branch_b", "w_out", "ffn_w_gate", "ffn_w_up", "ffn_w_down", "moe_router",
                  "moe_w_gate", "moe_w_up", "moe_w_down", "final_norm"):
            m[k] = inputs[k]
        for k, v in consts.items():
            m["c_" + k] = v
        maps.append(m)
    return maps


_CACHE = {}


def kernel(**inputs):
    cfg = Cfg()
    inputs = {k: np.asarray(v) for k, v in inputs.items()}
    if "nc" not in _CACHE:
        _CACHE["nc"] = K(cfg).build()
    nc = _CACHE["nc"]
    ncores = 16 // cfg.NSEQ
    maps = make_in_maps(cfg, inputs, ncores)
    res = run_bass_kernel_spmd(nc, maps, core_ids=list(range(ncores)))
    out = np.concatenate([np.asarray(r["out"]) for r in res.results], axis=0)
    return out.astype(np.float32, copy=False)
```

```python
import numpy as np
from contextlib import ExitStack
import concourse.bass as bass
import concourse.mybir as mybir
from concourse.bass_utils import run_bass_kernel_spmd

F32 = mybir.dt.float32
BF16 = mybir.dt.bfloat16
I32 = mybir.dt.int32
AF = mybir.ActivationFunctionType
ALU = mybir.AluOpType
AX = mybir.AxisListType

EPS = 1e-6
NEG = -30000.0


class Res:
    __slots__ = ("name", "last_w", "readers")

    def __init__(self, name=""):
        self.name = name
        self.last_w = None
        self.readers = {}


class Sched:
    COMPUTE = ("pe", "act", "dve", "pool")
    NDMA = 8

    def __init__(self, nc, es):
        self.nc = nc
        self.sem = {}
        for e in self.COMPUTE:
            self.sem[e] = es.enter_context(nc.semaphore("sem_" + e))
        self.count = {e: 0 for e in self.COMPUTE}
        self.engs = ("pe", "act", "dve", "pool", "sp")
        self.seen = {e: {} for e in self.engs}
        self.q = {e: [] for e in self.engs}
        self.dma_uses = {}
        self.dma_i = {}
        for e in ("sp", "act", "pool"):
            for i in range(self.NDMA):
                self.sem[("d", e, i)] = es.enter_context(nc.semaphore("dsem_%s%d" % (e, i)))
            self.dma_uses[e] = [0] * self.NDMA
            self.dma_i[e] = 0
        self.ninstr = 0

    def _deps(self, reads, writes):
        deps = {}
        for r in reads:
            ev = r.last_w
            if ev is not None and deps.get(ev[0], 0) < ev[1]:
                deps[ev[0]] = ev[1]
        for w in writes:
            ev = w.last_w
            if ev is not None and deps.get(ev[0], 0) < ev[1]:
                deps[ev[0]] = ev[1]
            for k, v in w.readers.items():
                if deps.get(k, 0) < v:
                    deps[k] = v
        return deps

    def _waits(self, eng, deps, skip=None):
        waits = []
        seen = self.seen[eng]
        for k, v in deps.items():
            if k == skip or seen.get(k, 0) >= v:
                continue
            seen[k] = v
            waits.append((k, v))
        return waits

    def _commit(self, ev, reads, writes):
        k, v = ev
        for r in reads:
            if r.readers.get(k, 0) < v:
                r.readers[k] = v
        for w in writes:
            w.last_w = ev
            w.readers = {}

    def op(self, eng, fn, reads=(), writes=(), signal=True):
        deps = self._deps(reads, writes)
        waits = self._waits(eng, deps, skip=("pe" if eng == "pe" else None))
        sem = self.sem
        if signal:
            self.count[eng] += 1
            ev = (eng, self.count[eng])
            own = sem[eng]
        else:
            ev = (eng, self.count[eng] + 1)
            own = None

        def run(e, waits=waits, fn=fn, own=own):
            for k, v in waits:
                e.wait_ge(sem[k], v)
            ins = fn(e)
            if own is not None:
                ins.then_inc(own, 1)
        self.q[eng].append(run)
        self._commit(ev, reads, writes)
        self.ninstr += 1
        return ev

    def dma(self, eng, fn, reads=(), writes=()):
        i = self.dma_i[eng] % self.NDMA
        self.dma_i[eng] += 1
        key = ("d", eng, i)
        prev = 16 * self.dma_uses[eng][i]
        self.dma_uses[eng][i] += 1
        ev = (key, prev + 16)
        deps = self._deps(reads, writes)
        if prev > 0 and deps.get(key, 0) < prev:
            deps[key] = prev
        waits = self._waits(eng, deps)
        sem = self.sem
        own = sem[key]

        def run(e, waits=waits, fn=fn, own=own):
            for k, v in waits:
                e.wait_ge(sem[k], v)
            fn(e).then_inc(own, 16)
        self.q[eng].append(run)
        self._commit(ev, reads, writes)
        self.ninstr += 1
        return ev

    def barrier(self):
        targets = {}
        for e in self.COMPUTE:
            if self.count[e] > 0:
                targets[e] = self.count[e]
        for e in ("sp", "act", "pool"):
            for i in range(self.NDMA):
                if self.dma_uses[e][i] > 0:
                    targets[("d", e, i)] = 16 * self.dma_uses[e][i]
        sem = self.sem
        for eng in self.engs:
            waits = self._waits(eng, targets, skip=(eng if eng in self.COMPUTE else None))
            if waits:
                def run(e, waits=waits):
                    for k, v in waits:
                        e.wait_ge(sem[k], v)
                self.q[eng].append(run)

    def flush(self):
        nc = self.nc
        q = self.q
        with nc.Block() as block:
            if q["pe"]:
                @block.tensor
                def _(e):
                    for f in q["pe"]:
                        f(e)
            if q["act"]:
                @block.scalar
                def _(e):
                    for f in q["act"]:
                        f(e)
            if q["dve"]:
                @block.vector
                def _(e):
                    for f in q["dve"]:
                        f(e)
            if q["pool"]:
                @block.gpsimd
                def _(e):
                    for f in q["pool"]:
                        f(e)
            if q["sp"]:
                @block.sync
                def _(e):
                    for f in q["sp"]:
                        f(e)
        self.q = {e: [] for e in self.engs}


_uid = [0]


class TPool:
    def __init__(self, es, nc, name, shape, dtype, n, psum=False):
        self.tiles = []
        for i in range(n):
            _uid[0] += 1
            nm = "%s_%d" % (name, _uid[0])
            mk = nc.psum_tensor if psum else nc.sbuf_tensor
            t = es.enter_context(mk(nm, list(shape), dtype))
            self.tiles.append((t, Res(nm)))
        self.i = 0

    def next(self):
        t = self.tiles[self.i % len(self.tiles)]
        self.i += 1
        return t


def tile1(es, nc, name, shape, dtype, psum=False):
    return TPool(es, nc, name, shape, dtype, 1, psum).tiles[0]


class Cfg:
    def __init__(self, NSEQ=2, SEQ=2048, CTX=256, DFF=5632, DEXP=7168, MOE="routed", DEBUG=False, STOP=None):
        self.DEBUG = DEBUG
        self.STOP = STOP
        self.D = 2048
        self.KC = 16
        self.NSEQ = NSEQ
        self.NR = NSEQ + 1
        self.SEQ = SEQ
        self.CTX = CTX
        self.T = CTX + SEQ
        self.NT = self.T // 128
        self.NCT = CTX // 128
        self.DFF = DFF
        self.DEXP = DEXP
        self.NE = 8
        self.NIN = 10496
        self.NCH = self.T // 64
        self.NCC = CTX // 64
        self.MOE = MOE
        self.PUMP = 7
        self.NSLOT = (2 * NSEQ * SEQ) // 512 + 7
        self.tblocks = [(0, CTX)] + [(CTX + i * 512, 512) for i in range(SEQ // 512)]
        self.lat_blocks = self.tblocks[1:]
        self.groups = [list(range(0, self.NCC))] + [list(range(self.NCC + 8 * i, self.NCC + 8 * i + 8))
                                                    for i in range((SEQ // 64) // 8)]


COL = dict(hq=0, ff=1024, fb=2048, hv=3072, hg=4096, aq=5120, ak=6144, av=6272, ga=6400, gb=8448)


def host_consts(cfg):
    c = {}
    c["ident"] = np.eye(128, dtype=np.float32)
    i = np.arange(64)
    sb_, tb_ = i[:, None] // 16, i[None, :] // 16
    c["maskD_f"] = ((i[:, None] <= i[None, :]) & (sb_ == tb_)).astype(np.float32)
    c["maskO_f"] = (sb_ < tb_).astype(np.float32)
    c["maskD_b"] = ((i[:, None] >= i[None, :]) & (sb_ == tb_)).astype(np.float32)
    c["maskO_b"] = (sb_ > tb_).astype(np.float32)
    j = np.arange(128)
    c["mneg_prev"] = np.where(j[None, :] <= j[:, None], 0.0, NEG).astype(np.float32)
    c["mneg_next"] = np.where(j[:, None] <= j[None, :], 0.0, NEG).astype(np.float32)
    c["ustrict"] = (j[:, None] < j[None, :]).astype(np.float32)
    t = np.arange(cfg.SEQ)
    row = (t // 64).astype(np.float32)
    col = (t % 64).astype(np.float32)
    inv = (10000.0 ** (-np.arange(16, dtype=np.float32) / 16)).astype(np.float32)
    d = np.arange(64)
    axis = d // 32
    freq = d % 16
    second = (d % 32) >= 16
    pos = np.where(axis[:, None] == 0, row[None, :], col[None, :]).astype(np.float32)
    ang = (pos * inv[freq][:, None]).astype(np.float32)
    cos = np.cos(ang).astype(np.float32)
    sin = np.sin(ang).astype(np.float32)
    sin_s = np.where(second[:, None], sin, -sin).astype(np.float32)
    c["cosT"] = np.concatenate([cos, cos], 0)
    c["sinT"] = np.concatenate([sin_s, sin_s], 0)
    pm = np.zeros((128, 128), np.float32)
    for m in range(128):
        dd = m % 64
        partner = dd - 16 if (dd % 32) >= 16 else dd + 16
        pm[(m // 64) * 64 + partner, m] = 1.0
    c["pm"] = pm
    c["ones"] = np.ones((128, 128), np.float32)
    NG = cfg.DEXP // 512
    c["jgrid"] = np.broadcast_to(np.repeat(512.0 * np.arange(cfg.NSLOT, dtype=np.float32), 8)[None, :], (128, cfg.NSLOT * 8)).copy()
    KMAX = max(1, (cfg.NSEQ * cfg.SEQ) // 512)
    c["kgrid"] = np.broadcast_to(np.tile(512.0 * np.arange(KMAX, dtype=np.float32), 8)[None, :], (128, 8 * KMAX)).copy()
    c["gp"] = (np.arange(NG, dtype=np.float32)[None, :] * 128 + np.arange(128, dtype=np.float32)[:, None]).astype(np.float32)
    return c


CONST_SHAPES = lambda cfg: dict(ident=[128, 128], maskD_f=[64, 64], maskO_f=[64, 64], maskD_b=[64, 64], maskO_b=[64, 64], mneg_prev=[128, 128],
                                mneg_next=[128, 128], ustrict=[128, 128], cosT=[128, cfg.SEQ],
                                sinT=[128, cfg.SEQ], pm=[128, 128], ones=[128, 128], jgrid=[128, cfg.NSLOT * 8], gp=[128, cfg.DEXP // 512], kgrid=[128, 8 * max(1, (cfg.NSEQ * cfg.SEQ) // 512)])


class K:
    def __init__(self, cfg):
        self.cfg = cfg
        nc = self.nc = bass.Bass("TRN2", target_bir_lowering=False)
        D = cfg.D
        T = cfg.T
        def din(name, shape, dt=F32):
            return nc.dram_tensor(name, list(shape), dt, kind="ExternalInput").ap()
        def scr(name, shape, dt):
            return nc.dram_tensor(name, list(shape), dt, kind=("ExternalOutput" if cfg.DEBUG else "Internal")).ap()
        self.x_in = din("x_in", [cfg.NSEQ, cfg.SEQ, D])
        self.ctx_in = din("ctx_in", [cfg.NSEQ, cfg.CTX, D])
        self.cvec = din("cvec", [cfg.NR, D])
        self.w_mod = din("w_mod", [2, D, 6 * D])
        self.b_mod = din("b_mod", [2, 6 * D])
        self.norm_mix = din("norm_mix", [2, D])
        self.norm_ffn = din("norm_ffn", [2, D])
        self.w_in = din("w_in", [2, D, cfg.NIN])
        self.lb_f = din("hg_lb_fwd", [2, 1024])
        self.lb_b = din("hg_lb_bwd", [2, 1024])
        self.hg_norm = din("hg_norm", [2, 1024])
        self.sink = din("attn_sink", [2, 16])
        self.w_a = din("w_branch_a", [2, 1024, D])
        self.w_b = din("w_branch_b", [2, 1024, D])
        self.w_o = din("w_out", [2, D, D])
        self.ffn_wg = din("ffn_w_gate", [1, D, cfg.DFF])
        self.ffn_wu = din("ffn_w_up", [1, D, cfg.DFF])
        self.ffn_wd = din("ffn_w_down", [1, cfg.DFF, D])
        self.router = din("moe_router", [1, D, 8])
        self.moe_wg = din("moe_w_gate", [1, 8, D, cfg.DEXP])
        self.moe_wu = din("moe_w_up", [1, 8, D, cfg.DEXP])
        self.moe_wd = din("moe_w_down", [1, 8, cfg.DEXP, D])
        self.final_norm = din("final_norm", [D])
        self.cst_d = {k: din("c_" + k, s) for k, s in CONST_SHAPES(cfg).items()}
        self.out = nc.dram_tensor("out", [cfg.NSEQ, cfg.SEQ, D], F32, kind="ExternalOutput").ap()
        self.MOD = scr("MOD", [2, cfg.NR, 6 * D], F32)
        self.XR = scr("XR", [cfg.NSEQ, T, D], F32)
        self.QT = scr("QT", [8, 128, T], BF16)
        self.GT = scr("GT", [2, 8, 128, T], F32)
        self.KT = scr("KT", [2, 8, 128, T], BF16)
        self.SGT = scr("SGT", [8, 128, T], BF16)
        self.V = scr("V", [T, 1024], BF16)
        self.QA = scr("QA", [8, 128, T], BF16)
        self.KA = scr("KA", [2, 128, T], BF16)
        self.VA = scr("VA", [T, 128], BF16)
        self.GA = scr("GA", [16, 128, T], BF16)
        self.GB = scr("GB", [16, 128, T], BF16)
        self.AT = scr("AT", [8, 128, T], BF16)
        self.OAT = scr("OAT", [8, 128, T], BF16)
        NG = cfg.DEXP // 512
        NTg = cfg.NSEQ * cfg.SEQ // 128
        if cfg.MOE == "routed":
            self.H2 = scr("H2", [cfg.NSEQ * cfg.SEQ, D], BF16)
            self.HS = scr("HS", [cfg.NSLOT * 512, D], BF16)
            self.YP = scr("YP", [cfg.NSLOT * 512, D], F32)
            self.SELD = scr("SELD", [128, NTg, 8], F32)
            self.CBD = scr("CBD", [128, NTg, 8], F32)
            self.WGB = scr("WGB", [8 * NG * 128, 16 * 512], BF16)
            self.WUB = scr("WUB", [8 * NG * 128, 16 * 512], BF16)
            self.WDB = scr("WDB", [8 * NG * 128, 4 * D], BF16)
        self.r_H2 = Res(); self.r_HS = Res(); self.r_YP = Res(); self.r_SELD = Res(); self.r_WB = Res()
        self.pre_jobs = self.prepass_jobs() if cfg.MOE == "routed" else []
        self.dbg = {}
        self.r_MOD = Res('MOD')
        self.r_out = Res('out')
        self.r_QT = [Res() for _ in range(8)]
        self.r_SGT = [Res() for _ in range(8)]
        self.r_GA = [Res() for _ in range(16)]
        self.r_GB = [Res() for _ in range(16)]
        self.r_GT = [[Res() for _ in range(8)] for _ in range(2)]
        self.r_KT = [[Res() for _ in range(8)] for _ in range(2)]
        self.r_QA = [Res() for _ in range(8)]
        self.r_KA = [Res() for _ in range(2)]
        self.r_V = Res()
        self.r_VA = Res()
        self.r_AT = [Res() for _ in range(8)]
        self.r_OAT = [Res() for _ in range(8)]
        self.r_XR = [[Res('XR') for _ in range(cfg.NT)] for _ in range(cfg.NSEQ)]

    def xsrc(self, first, s, tile):
        cfg = self.cfg
        if first:
            if tile < cfg.NCT:
                return self.ctx_in[s, tile * 128:(tile + 1) * 128, :]
            tt = tile - cfg.NCT
            return self.x_in[s, tt * 128:(tt + 1) * 128, :]
        return self.XR[s, tile * 128:(tile + 1) * 128, :]

    def load_wblock(self, pool, w2d, n0, nn, nk=None, k0=0, eng="pool"):
        S = self.S
        nk = nk if nk is not None else w2d.shape[0] // 128
        t, r = pool.next()
        src = w2d[k0 * 128:(k0 + nk) * 128, n0:n0 + nn].rearrange("(kc p) n -> p kc n", p=128)
        S.dma(eng, lambda e: e.dma_start(out=t[:, 0:nk, 0:nn], in_=src), writes=[r])
        return t, r

    def bcast_row(self, t, r, row_ap, n, eng="sp"):
        self.S.dma(eng, lambda e: e.dma_start(out=t[:, 0:n], in_=row_ap.partition_broadcast(128)), writes=[r])

    def build(self):
        cfg = self.cfg
        nc = self.nc
        with ExitStack() as es:
            self.S = S = Sched(nc, es)
            es.enter_context(nc.allow_non_contiguous_dma(reason='small strided layout loads'))
            self.C = {}
            for k in ("ident", "mneg_prev", "mneg_next", "ustrict", "pm", "ones"):
                shp = CONST_SHAPES(cfg)[k]
                t, r = tile1(es, nc, "c_" + k, shp, BF16)
                S.dma("pool", lambda e, t=t, k=k: e.dma_start(out=t[:], in_=self.cst_d[k][:, :]), writes=[r])
                self.C[k] = (t, r)
            stop = cfg.STOP
            self.phase_mod()
            if stop == "mod":
                return nc
            for l in range(2):
                for s in range(cfg.NSEQ):
                    with ExitStack() as bs:
                        self.BIG = tile1(bs, nc, "BIG", [128, cfg.KC, cfg.T], BF16)
                        self.phase_norm(l, s, 1)
                        if stop == "norm":
                            self.dump("BIG", self.BIG[0], self.BIG[1], [128, cfg.KC, cfg.T], BF16)
                            self.end_phase()
                            return nc
                        self.phase_inproj(l, s)
                    if stop == "inproj":
                        return nc
                    self.phase_scan(l, s)
                    if stop in ("scan", "scanprep"):
                        return nc
                    self.phase_attn(l, s)
                    if stop == "attn":
                        return nc
                    with ExitStack() as bs:
                        self.BIG = tile1(bs, nc, "BIG", [128, cfg.KC, cfg.T], BF16)
                        self.phase_merge(l, s)
                        if stop == "merge":
                            return nc
                        self.phase_norm(l, s, 2)
                        if l == 1 and cfg.MOE == "routed":
                            self.route_local(s)
                        else:
                            self.phase_swiglu(l, s)
                    if stop == "ffn":
                        return nc
                if stop == "layer0":
                    return nc
            if cfg.MOE == "routed":
                self.phase_moe_routed()
            else:
                self.phase_final()
        return nc

    def dump(self, name, tile, r, shape, dt):
        if not self.cfg.DEBUG:
            return
        d = self.nc.dram_tensor("dbg_" + name, list(shape), dt, kind="ExternalOutput").ap()
        self.S.dma("sp", lambda e: e.dma_start(out=d, in_=tile[:]), reads=[r])

    def end_phase(self):
        self.S.barrier()
        self.S.flush()

    def phase_mod(self):
        cfg, nc, S = self.cfg, self.nc, self.S
        D, KC, NR = cfg.D, cfg.KC, cfg.NR
        with ExitStack() as ph:
            if cfg.MOE == "routed":
                zt, r_zt = tile1(ph, nc, "q_z", [128, 4, D], BF16)
                S.op("dve", lambda e: e.memset(zt[:], 0.0), writes=[r_zt])
                for j in range(cfg.NSLOT):
                    S.dma("sp", lambda e, j=j: e.dma_start(out=self.HS[j * 512:(j + 1) * 512, :].rearrange("(a p) d -> p a d", p=128), in_=zt[:]), reads=[r_zt])
            cT, r_cT = tile1(ph, nc, "cT", [128, KC, NR], F32)
            cTb, r_cTb = tile1(ph, nc, "cTb", [128, KC, NR], BF16)
            for r in range(NR):
                src = self.cvec[r:r + 1, :].rearrange("o (kc p) -> p kc o", p=128)
                S.dma("sp", lambda e, src=src, r=r: e.dma_start(out=cT[:, :, r:r + 1], in_=src), writes=[r_cT])
            S.op("act", lambda e: e.activation(out=cTb[:], in_=cT[:], func=AF.Silu), reads=[r_cT], writes=[r_cTb])
            wpool = TPool(ph, nc, "wmod", [128, KC, 512], BF16, 3)
            pspool = TPool(ph, nc, "psmod", [NR, 512], F32, 2, psum=True)
            bias, r_bias = tile1(ph, nc, "bmod", [NR, 6 * D], F32)
            res, r_res = tile1(ph, nc, "resmod", [NR, 6 * D], F32)
            for l in range(2):
                S.dma("sp", lambda e, l=l: e.dma_start(out=bias[:], in_=self.b_mod[l:l + 1, :].partition_broadcast(NR)), writes=[r_bias])
                nb = 6 * D // 512
                for b in range(nb):
                    w, r_w = self.load_wblock(wpool, self.w_mod[l], b * 512, 512)
                    ps, r_ps = pspool.next()
                    for kc in range(KC):
                        S.op("pe", lambda e, ps=ps, w=w, kc=kc: e.matmul(ps[:], lhsT=cTb[:, kc, :], rhs=w[:, kc, :], start=(kc == 0), stop=(kc == KC - 1)),
                             reads=[r_cTb, r_w], writes=[r_ps], signal=(kc == KC - 1))
                    S.op("dve", lambda e, ps=ps, b=b: e.tensor_tensor(out=res[:, b * 512:(b + 1) * 512], in0=ps[:], in1=bias[:, b * 512:(b + 1) * 512], op=ALU.add),
                         reads=[r_ps, r_bias], writes=[r_res])
                S.dma("sp", lambda e, l=l: e.dma_start(out=self.MOD[l], in_=res[:]), reads=[r_res], writes=[self.r_MOD])
            self.end_phase()

    def phase_norm(self, l, s, which):
        cfg, nc, S = self.cfg, self.nc, self.S
        D, KC = cfg.D, cfg.KC
        first = (l == 0 and which == 1)
        nw = self.norm_mix if which == 1 else self.norm_ffn
        base = 0 if which == 1 else 3
        BIG, r_BIG = self.BIG
        ident, r_ident = self.C["ident"]
        tiles = list(range(cfg.NT))
        if which == 2 and l == 1:
            tiles = list(range(cfg.NCT, cfg.NT))
        with ExitStack() as ph:
            gm = {}
            sh = {}
            for kind, row in (("lat", s), ("ctx", cfg.NSEQ)):
                if kind == "ctx" and which == 2 and l == 1:
                    continue
                g_t, g_r = tile1(ph, nc, "gm" + kind, [128, D], F32)
                s_t, s_r = tile1(ph, nc, "sh" + kind, [128, D], F32)
                n_t, n_r = tile1(ph, nc, "nw" + kind, [128, D], F32)
                self.S.dma("sp", lambda e, n_t=n_t: e.dma_start(out=n_t[:], in_=nw[l:l + 1, :].partition_broadcast(128)), writes=[n_r])
                self.S.dma("sp", lambda e, g_t=g_t, row=row: e.dma_start(out=g_t[:], in_=self.MOD[l, row:row + 1, (base + 1) * D:(base + 2) * D].partition_broadcast(128)), reads=[self.r_MOD], writes=[g_r])
                self.S.dma("sp", lambda e, s_t=s_t, row=row: e.dma_start(out=s_t[:], in_=self.MOD[l, row:row + 1, base * D:(base + 1) * D].partition_broadcast(128)), reads=[self.r_MOD], writes=[s_r])
                S.op("dve", lambda e, g_t=g_t, n_t=n_t: e.scalar_tensor_tensor(out=g_t[:], in0=g_t[:], scalar=1.0, in1=n_t[:], op0=ALU.add, op1=ALU.mult),
                     reads=[g_r, n_r], writes=[g_r])
                gm[kind] = (g_t, g_r)
                sh[kind] = (s_t, s_r)
            xpool = TPool(ph, nc, "xt", [128, D], F32, 2)
            junk, r_junk = tile1(ph, nc, "junk", [128, D], BF16)
            t1pool = TPool(ph, nc, "t1", [128, D], F32, 1)
            hpool = TPool(ph, nc, "hb", [128, D], BF16, 2)
            stpool = TPool(ph, nc, "st", [128, 4], F32, 2)
            pspool = TPool(ph, nc, "pT", [128, KC, 128], BF16, 2, psum=True)
            loaded = {}

            def load(i):
                tile = tiles[i]
                xt, r_xt = xpool.next()
                src = self.xsrc(first, s, tile)
                S.dma("sp", lambda e, xt=xt, src=src: e.dma_start(out=xt[:], in_=src), reads=[self.r_XR[s][tile]], writes=[r_xt])
                loaded[i] = (xt, r_xt)

            load(0)
            for i, tile in enumerate(tiles):
                if i + 1 < len(tiles):
                    load(i + 1)
                xt, r_xt = loaded.pop(i)
                kind = "ctx" if tile < cfg.NCT else "lat"
                g_t, g_r = gm[kind]
                s_t, s_r = sh[kind]
                st, r_st = stpool.next()
                S.op("act", lambda e, xt=xt, st=st: e.activation(out=junk[:], in_=xt[:], func=AF.Square, accum_out=st[:, 0:1]),
                     reads=[r_xt], writes=[r_junk, r_st])
                S.op("dve", lambda e, st=st: e.tensor_scalar(out=st[:, 1:2], in0=st[:, 0:1], scalar1=1.0 / D, scalar2=EPS, op0=ALU.mult, op1=ALU.add),
                     reads=[r_st], writes=[r_st])
                S.op("act", lambda e, st=st: e.activation(out=st[:, 2:3], in_=st[:, 1:2], func=AF.Sqrt), reads=[r_st], writes=[r_st])
                S.op("dve", lambda e, st=st: e.reciprocal(out=st[:, 3:4], in_=st[:, 2:3]), reads=[r_st], writes=[r_st])
                t1, r_t1 = t1pool.next()
                S.op("dve", lambda e, t1=t1, xt=xt, g_t=g_t: e.tensor_tensor(out=t1[:], in0=xt[:], in1=g_t[:], op=ALU.mult),
                     reads=[r_xt, g_r], writes=[r_t1])
                hb, r_hb = hpool.next()
                S.op("dve", lambda e, hb=hb, t1=t1, st=st, s_t=s_t: e.scalar_tensor_tensor(out=hb[:], in0=t1[:], scalar=st[:, 3:4], in1=s_t[:], op0=ALU.mult, op1=ALU.add),
                     reads=[r_t1, r_st, s_r], writes=[r_hb])
                if which == 2 and l == 1 and cfg.MOE == "routed":
                    g_ = s * (cfg.SEQ // 128) + (tile - cfg.NCT)
                    S.dma("sp", lambda e, hb=hb, g_=g_: e.dma_start(out=self.H2[g_ * 128:(g_ + 1) * 128, :], in_=hb[:]), reads=[r_hb], writes=[self.r_H2])
                ps, r_ps = pspool.next()
                for kc in range(KC):
                    S.op("pe", lambda e, ps=ps, hb=hb, kc=kc: e.transpose(out=ps[:, kc, :], in_=hb[:, kc * 128:(kc + 1) * 128], identity=ident[:]),
                         reads=[r_hb, r_ident], writes=[r_ps], signal=(kc == KC - 1))
                S.op("act", lambda e, ps=ps, tile=tile: e.copy(out=BIG[:, :, tile * 128:(tile + 1) * 128], in_=ps[:]),
                     reads=[r_ps], writes=[r_BIG])
            self.end_phase()

    def phase_inproj(self, l, s):
        cfg, nc, S = self.cfg, self.nc, self.S
        D, KC, T = cfg.D, cfg.KC, cfg.T
        BIG, r_BIG = self.BIG
        CTX, SEQ = cfg.CTX, cfg.SEQ
        pm, r_pm = self.C["pm"]
        w_in = self.w_in[l]
        with ExitStack() as ph:
            wpool = TPool(ph, nc, "win", [128, KC, 512], BF16, 3)
            pspool = TPool(ph, nc, "psin", [128, 512], F32, 4, psum=True)
            psrot = TPool(ph, nc, "psrot", [128, 512], F32, 2, psum=True)
            stb = TPool(ph, nc, "stb", [128, T], BF16, 4)
            stf = TPool(ph, nc, "stf", [128, T], F32, 2)
            tmpf = TPool(ph, nc, "tmpf", [128, 512], F32, 6)
            cosT, r_cos = tile1(ph, nc, "cosT", [128, SEQ], F32)
            sinT, r_sin = tile1(ph, nc, "sinT", [128, SEQ], F32)
            S.dma("sp", lambda e: e.dma_start(out=cosT[:], in_=self.cst_d["cosT"][:, :]), writes=[r_cos])
            S.dma("sp", lambda e: e.dma_start(out=sinT[:], in_=self.cst_d["sinT"][:, :]), writes=[r_sin])
            lbv, oml = [], []
            for di, lbsrc in enumerate((self.lb_f, self.lb_b)):
                lb_t, lb_r = tile1(ph, nc, "lb%d" % di, [128, 8], F32)
                om_t, om_r = tile1(ph, nc, "oml%d" % di, [128, 8], F32)
                if l == 0:
                    S.op("dve", lambda e, lb_t=lb_t: e.memset(lb_t[:], 0.0), writes=[lb_r])
                else:
                    r0_t, r0_r = tile1(ph, nc, "lr0%d" % di, [128, 8], F32)
                    r1_t, r1_r = tile1(ph, nc, "lr1%d" % di, [128, 8], F32)
                    S.dma("sp", lambda e, r0_t=r0_t, lbsrc=lbsrc: e.dma_start(out=r0_t[:], in_=lbsrc[0:1, :].rearrange("o (h p) -> p (o h)", p=128)), writes=[r0_r])
                    S.dma("sp", lambda e, r1_t=r1_t, lbsrc=lbsrc: e.dma_start(out=r1_t[:], in_=lbsrc[1:2, :].rearrange("o (h p) -> p (o h)", p=128)), writes=[r1_r])
                    S.op("dve", lambda e, r0_t=r0_t, r1_t=r1_t: e.tensor_tensor(out=r0_t[:], in0=r0_t[:], in1=r1_t[:], op=ALU.subtract), reads=[r0_r, r1_r], writes=[r0_r])
                    S.op("act", lambda e, r0_t=r0_t: e.activation(out=r0_t[:], in_=r0_t[:], func=AF.Exp), reads=[r0_r], writes=[r0_r])
                    S.op("dve", lambda e, r0_t=r0_t: e.tensor_scalar(out=r0_t[:], in0=r0_t[:], scalar1=1.0, scalar2=None, op0=ALU.add), reads=[r0_r], writes=[r0_r])
                    S.op("dve", lambda e, r0_t=r0_t, lb_t=lb_t: e.reciprocal(out=lb_t[:], in_=r0_t[:]), reads=[r0_r], writes=[lb_r])
                S.op("dve", lambda e, lb_t=lb_t, om_t=om_t: e.tensor_scalar(out=om_t[:], in0=lb_t[:], scalar1=-1.0, scalar2=1.0, op0=ALU.mult, op1=ALU.add), reads=[lb_r], writes=[om_r])
                lbv.append((lb_t, lb_r))
                oml.append((om_t, om_r))

            def proj_chunk(w, r_w, c, epi):
                for (t0, n) in cfg.tblocks:
                    ps, r_ps = pspool.next()
                    for kc in range(KC):
                        S.op("pe", lambda e, ps=ps, kc=kc, t0=t0, n=n: e.matmul(ps[:, 0:n], lhsT=w[:, kc, c * 128:(c + 1) * 128], rhs=BIG[:, kc, t0:t0 + n], start=(kc == 0), stop=(kc == KC - 1)),
                             reads=[r_w, r_BIG], writes=[r_ps], signal=(kc == KC - 1))
                    epi(ps, r_ps, t0, n)

            def store(dst, st, r_st, r_dst):
                S.dma("sp", lambda e: e.dma_start(out=dst, in_=st[:]), reads=[r_st], writes=[r_dst])

            def simple_job(col0, nchunks, func, scale, dst, r_dst):
                for b in range(0, nchunks, 4):
                    w, r_w = self.load_wblock(wpool, w_in, col0 + b * 128, 512)
                    for c in range(4):
                        st, r_st = stb.next()
                        def epi(ps, r_ps, t0, n, st=st, r_st=r_st):
                            S.op("act", lambda e: e.activation(out=st[:, t0:t0 + n], in_=ps[:, 0:n], func=func, scale=scale), reads=[r_ps], writes=[r_st])
                        proj_chunk(w, r_w, c, epi)
                        store(dst[b + c], st, r_st, r_dst[b + c])

            simple_job(COL["hq"], 8, AF.Copy, 128.0 ** -0.5, self.QT, self.r_QT)
            simple_job(COL["hg"], 8, AF.Silu, 1.0, self.SGT, self.r_SGT)
            simple_job(COL["ga"], 16, AF.Sigmoid, 1.0, self.GA, self.r_GA)
            simple_job(COL["gb"], 16, AF.Sigmoid, 1.0, self.GB, self.r_GB)
            for di, col0 in enumerate((COL["ff"], COL["fb"])):
                lb_t, lb_r = lbv[di]
                om_t, om_r = oml[di]
                for b in range(0, 8, 4):
                    w, r_w = self.load_wblock(wpool, w_in, col0 + b * 128, 512)
                    for c in range(4):
                        hd = b + c
                        sg_, r_sg = stf.next()
                        sk_, r_sk = stb.next()
                        def epi(ps, r_ps, t0, n, sg_=sg_, r_sg=r_sg, sk_=sk_, r_sk=r_sk, hd=hd, om_t=om_t, om_r=om_r):
                            e_t, r_e = tmpf.next()
                            t_t, r_t = tmpf.next()
                            k_t, r_k = tmpf.next()
                            S.op("act", lambda e: e.activation(out=e_t[:, 0:n], in_=ps[:, 0:n], func=AF.Exp, scale=-1.0), reads=[r_ps], writes=[r_e])
                            S.op("dve", lambda e: e.tensor_scalar(out=t_t[:, 0:n], in0=e_t[:, 0:n], scalar1=1.0, scalar2=None, op0=ALU.add), reads=[r_e], writes=[r_t])
                            S.op("dve", lambda e: e.reciprocal(out=t_t[:, 0:n], in_=t_t[:, 0:n]), reads=[r_t], writes=[r_t])
                            S.op("dve", lambda e: e.scalar_tensor_tensor(out=k_t[:, 0:n], in0=e_t[:, 0:n], scalar=om_t[:, hd:hd + 1], in1=t_t[:, 0:n], op0=ALU.mult, op1=ALU.mult),
                                 reads=[r_e, r_t, om_r], writes=[r_k])
                            S.op("act", lambda e: e.activation(out=sg_[:, t0:t0 + n], in_=k_t[:, 0:n], func=AF.Ln, scale=-1.0, bias=1.0), reads=[r_k], writes=[r_sg])
                            S.op("act", lambda e: e.copy(out=sk_[:, t0:t0 + n], in_=k_t[:, 0:n]), reads=[r_k], writes=[r_sk])
                        proj_chunk(w, r_w, c, epi)
                        store(self.GT[di, hd], sg_, r_sg, self.r_GT[di][hd])
                        store(self.KT[di, hd], sk_, r_sk, self.r_KT[di][hd])

            def rope_job(w, r_w, c, scale, dst, r_dst):
                raw, r_raw = stb.next()
                outt, r_out = stb.next()
                def epi(ps, r_ps, t0, n):
                    S.op("act", lambda e: e.activation(out=raw[:, t0:t0 + n], in_=ps[:, 0:n], func=AF.Copy, scale=scale), reads=[r_ps], writes=[r_raw])
                    if t0 < CTX:
                        S.op("act", lambda e: e.copy(out=outt[:, t0:t0 + n], in_=raw[:, t0:t0 + n]), reads=[r_raw], writes=[r_out])
                        return
                    p0 = t0 - CTX
                    pr, r_pr = psrot.next()
                    S.op("pe", lambda e: e.matmul(pr[:, 0:n], lhsT=pm[:], rhs=raw[:, t0:t0 + n], start=True, stop=True), reads=[r_pm, r_raw], writes=[r_pr])
                    a_t, r_a = tmpf.next()
                    b_t, r_b = tmpf.next()
                    S.op("dve", lambda e: e.tensor_tensor(out=a_t[:, 0:n], in0=raw[:, t0:t0 + n], in1=cosT[:, p0:p0 + n], op=ALU.mult), reads=[r_raw, r_cos], writes=[r_a])
                    S.op("dve", lambda e: e.tensor_tensor(out=b_t[:, 0:n], in0=pr[:, 0:n], in1=sinT[:, p0:p0 + n], op=ALU.mult), reads=[r_pr, r_sin], writes=[r_b])
                    S.op("dve", lambda e: e.tensor_tensor(out=outt[:, t0:t0 + n], in0=a_t[:, 0:n], in1=b_t[:, 0:n], op=ALU.add), reads=[r_a, r_b], writes=[r_out])
                proj_chunk(w, r_w, c, epi)
                store(dst, outt, r_out, r_dst)

            for b in range(0, 8, 4):
                w, r_w = self.load_wblock(wpool, w_in, COL["aq"] + b * 128, 512)
                for c in range(4):
                    rope_job(w, r_w, c, 64.0 ** -0.5, self.QA[b + c], self.r_QA[b + c])
            w, r_w = wpool.next()
            for (o, c0, nn) in ((0, COL["ak"], 128), (128, COL["ak"] + 64, 64), (192, COL["ak"], 64)):
                src = w_in[:, c0:c0 + nn].rearrange("(kc p) n -> p kc n", p=128)
                S.dma("pool", lambda e, o=o, nn=nn, src=src: e.dma_start(out=w[:, :, o:o + nn], in_=src), writes=[r_w])
            for c in range(2):
                rope_job(w, r_w, c, 1.0, self.KA[c], self.r_KA[c])

            vst = TPool(ph, nc, "vst", [128, 512], BF16, 3)
            for (col0, ncols, dst, r_dst) in ((COL["hv"], 512, self.V[:, 0:512], self.r_V), (COL["hv"] + 512, 512, self.V[:, 512:1024], self.r_V), (COL["av"], 128, self.VA, self.r_VA)):
                w, r_w = self.load_wblock(wpool, w_in, col0, ncols)
                for tile in range(cfg.NT):
                    ps, r_ps = pspool.next()
                    for kc in range(KC):
                        S.op("pe", lambda e, ps=ps, kc=kc, tile=tile, ncols=ncols, w=w: e.matmul(ps[:, 0:ncols], lhsT=BIG[:, kc, tile * 128:(tile + 1) * 128], rhs=w[:, kc, 0:ncols], start=(kc == 0), stop=(kc == KC - 1)),
                             reads=[r_w, r_BIG], writes=[r_ps], signal=(kc == KC - 1))
                    vt, r_vt = vst.next()
                    S.op("act", lambda e, vt=vt, ps=ps, ncols=ncols: e.copy(out=vt[:, 0:ncols], in_=ps[:, 0:ncols]), reads=[r_ps], writes=[r_vt])
                    S.dma("sp", lambda e, vt=vt, tile=tile, ncols=ncols, dst=dst: e.dma_start(out=dst[tile * 128:(tile + 1) * 128, :], in_=vt[:, 0:ncols]), reads=[r_vt], writes=[r_dst])
            self.end_phase()

    def phase_scan(self, l, s):
        cfg, nc, S = self.cfg, self.nc, self.S
        T, NCH = cfg.T, cfg.NCH
        NB16 = T // 16
        ident, r_ident = self.C["ident"]
        groups = cfg.groups
        ng = len(groups)
        order = [[list(g) for g in groups],
                 [list(reversed(groups[0]))] + [list(reversed(g)) for g in reversed(groups[1:])]]
        VN = ("qd", "kd", "qb", "qin", "kout", "k1", "k2", "k3")
        with ExitStack() as ph:
            qT, r_qT = tile1(ph, nc, "s_qT", [128, T], BF16)
            sgT, r_sgT = tile1(ph, nc, "s_sgT", [128, T], BF16)
            vt, r_vt = tile1(ph, nc, "s_v", [64, NCH, 128], BF16)
            gT = [tile1(ph, nc, "s_gT%d" % d, [128, T], F32) for d in range(2)]
            kT = [tile1(ph, nc, "s_kT%d" % d, [128, T], BF16) for d in range(2)]
            Zps = [tile1(ph, nc, "s_Zp%d" % d, [128, T + 1], F32) for d in range(2)]
            tmp = TPool(ph, nc, "s_tmp", [128, T], F32, 3)
            var = [{n: tile1(ph, nc, "s_%s%d" % (n, d), [128, T], BF16) for n in VN} for d in range(2)]
            tr = [tile1(ph, nc, "s_tr%d" % d, [128, NCH], F32) for d in range(2)]
            oall, r_oall = tile1(ph, nc, "s_oall", [128, T], F32)
            aT, r_aT = tile1(ph, nc, "s_aT", [128, T], BF16)
            ones, r_ones = tile1(ph, nc, "s_ones", [128, T], F32)
            onesf, r_onesf = tile1(ph, nc, "s_onesf", [128, 128], F32)
            gain, r_gain = tile1(ph, nc, "s_gain", [128, 8], F32)
            mk = {}
            for n in ("maskD_f", "maskO_f", "maskD_b", "maskO_b"):
                mk[n] = tile1(ph, nc, "s_" + n, [64, 64], BF16)
                S.dma("pool", lambda e, n=n: e.dma_start(out=mk[n][0][:], in_=self.cst_d[n][:, :]), writes=[mk[n][1]])
            Sf = [tile1(ph, nc, "s_Sf%d" % d, [128, 128], F32) for d in range(2)]
            Sb = [tile1(ph, nc, "s_Sb%d" % d, [128, 128], BF16) for d in range(2)]
            At = TPool(ph, nc, "s_At", [64, 8, 64], BF16, 4)
            At2 = TPool(ph, nc, "s_At2", [64, 8, 64], BF16, 2)
            ktok = TPool(ph, nc, "s_ktok", [64, 8, 128], BF16, 4)
            rs = TPool(ph, nc, "s_rs", [128, 512], F32, 2)
            psA = TPool(ph, nc, "s_psA", [64, 8, 64], F32, 1, psum=True)
            psA2 = [tile1(ph, nc, "s_psA2%d" % d, [64, 8, 64], F32, psum=True) for d in range(2)]
            psK = TPool(ph, nc, "s_psK", [64, 8, 128], BF16, 1, psum=True)
            psO = TPool(ph, nc, "s_psO", [128, 8, 64], F32, 2, psum=True)
            psD = TPool(ph, nc, "s_psD", [128, 128], F32, 1, psum=True)
            psS = TPool(ph, nc, "s_psS", [128, 512], F32, 1, psum=True)
            S.op("dve", lambda e: e.memset(ones[:], 1.0), writes=[r_ones])
            S.op("dve", lambda e: e.memset(onesf[:], 1.0), writes=[r_onesf])
            for d in range(2):
                S.op("dve", lambda e, d=d: e.memset(psA2[d][0][:], 0.0), writes=[psA2[d][1]])
                S.op("dve", lambda e, d=d: e.memset(Zps[d][0][:], 0.0), writes=[Zps[d][1]])
            S.dma("sp", lambda e: e.dma_start(out=gain[:], in_=self.hg_norm[l:l + 1, :].rearrange("o (h p) -> p (o h)", p=128)), writes=[r_gain])

            def v3(ap, j):
                return ap.rearrange("p (c j) -> p c j", j=j)

            def load_pre(hd):
                for d in range(2):
                    S.dma("sp", lambda e, hd=hd, d=d: e.dma_start(out=gT[d][0][:], in_=self.GT[d, hd]), reads=[self.r_GT[d][hd]], writes=[gT[d][1]])
                S.dma("sp", lambda e, hd=hd: e.dma_start(out=qT[:], in_=self.QT[hd]), reads=[self.r_QT[hd]], writes=[r_qT])
                for d in range(2):
                    S.dma("sp", lambda e, hd=hd, d=d: e.dma_start(out=kT[d][0][:], in_=self.KT[d, hd]), reads=[self.r_KT[d][hd]], writes=[kT[d][1]])

            load_pre(0)
            for hd in range(8):
                self.pump(cfg.PUMP)
                S.dma("sp", lambda e, hd=hd: e.dma_start(out=vt[:], in_=self.V[:, hd * 128:(hd + 1) * 128].rearrange("(c p) v -> p c v", p=64)), reads=[self.r_V], writes=[r_vt])
                S.dma("sp", lambda e, hd=hd: e.dma_start(out=sgT[:], in_=self.SGT[hd]), reads=[self.r_SGT[hd]], writes=[r_sgT])
                mjobs = []
                for d in range(2):
                    g_t, g_r = gT[d]
                    k_t, k_r = kT[d]
                    Zp, r_Zp = Zps[d]
                    sg = 1.0 if d == 0 else -1.0
                    S.op("dve", lambda e, Zp=Zp, g_t=g_t: e.tensor_tensor_scan(out=Zp[:, 1:T + 1], data0=ones[:], data1=g_t[:], initial=0.0, op0=ALU.mult, op1=ALU.add),
                         reads=[r_ones, g_r], writes=[r_Zp])
                    X = Zp[:, 1:T + 1] if d == 0 else Zp[:, 0:T]
                    lo16 = v3(Zp[:, 0:T], 16)
                    hi16 = v3(Zp[:, 1:T + 1], 16)
                    lo64 = v3(Zp[:, 0:T], 64)
                    hi64 = v3(Zp[:, 1:T + 1], 64)
                    ref_mid = lo16[:, :, 8:9]
                    ref_qb = lo16[:, :, 0:1] if d == 0 else hi16[:, :, 15:16]
                    ref_qin = lo64[:, :, 0:1] if d == 0 else hi64[:, :, 63:64]
                    ref_kout = hi64[:, :, 63:64] if d == 0 else lo64[:, :, 0:1]
                    ref_k = [lo64[:, :, 16 * i:16 * i + 1] for i in (1, 2, 3)]

                    def make(name, src_t, src_r, ref, blk, scale, clamp=None, X=X, r_Zp=r_Zp, d=d):
                        nb = T // blk
                        st_ = {}

                        def s1():
                            D, r_D = tmp.next()
                            st_["D"] = (D, r_D)
                            S.op("dve", lambda e: e.tensor_tensor(out=v3(D[:], blk), in0=v3(X, blk), in1=ref.broadcast_to([128, nb, blk]), op=ALU.subtract),
                                 reads=[r_Zp], writes=[r_D])
                            S.op("act", lambda e: e.activation(out=D[:], in_=D[:], func=AF.Exp, scale=scale), reads=[r_D], writes=[r_D])

                        def s2():
                            D, r_D = st_["D"]
                            o_t, o_r = var[d][name]
                            if clamp is None:
                                S.op("dve", lambda e: e.tensor_tensor(out=o_t[:], in0=src_t[:], in1=D[:], op=ALU.mult), reads=[src_r, r_D], writes=[o_r])
                            else:
                                S.op("dve", lambda e: e.scalar_tensor_tensor(out=o_t[:], in0=D[:], scalar=1.0, in1=src_t[:], op0=ALU.min, op1=ALU.mult), reads=[src_r, r_D], writes=[o_r])
                        mjobs.append((s1, s2))

                    make("qd", qT, r_qT, ref_mid, 16, sg)
                    make("kd", k_t, k_r, ref_mid, 16, -sg)
                    make("qb", qT, r_qT, ref_qb, 16, sg)
                    make("qin", qT, r_qT, ref_qin, 64, sg)
                    make("kout", k_t, k_r, ref_kout, 64, -sg)
                    for i in range(3):
                        make("k%d" % (i + 1), k_t, k_r, ref_k[i], 64, -sg, clamp=(ALU.max if d == 0 else ALU.min))
                    tr_t, tr_r = tr[d]
                    S.op("dve", lambda e, tr_t=tr_t, hi64=hi64, lo64=lo64: e.tensor_tensor(out=tr_t[:].unsqueeze(2), in0=hi64[:, :, 63:64], in1=lo64[:, :, 0:1], op=ALU.subtract), reads=[r_Zp], writes=[tr_r])
                    S.op("act", lambda e, tr_t=tr_t: e.activation(out=tr_t[:], in_=tr_t[:], func=AF.Exp), reads=[tr_r], writes=[tr_r])
                    S.op("dve", lambda e, d=d: e.memset(Sf[d][0][:], 0.0), writes=[Sf[d][1]])
                    S.op("dve", lambda e, d=d: e.memset(Sb[d][0][:], 0.0), writes=[Sb[d][1]])
                for i in range(len(mjobs) + 1):
                    if i < len(mjobs):
                        mjobs[i][0]()
                    if i >= 1:
                        mjobs[i - 1][1]()
                if hd + 1 < 8:
                    load_pre(hd + 1)
                touched = set()
                for gi in range(ng):
                    ctxs = []
                    for d in range(2):
                        cl = order[d][gi]
                        g0 = min(cl)
                        n = len(cl)
                        V = var[d]
                        pa, r_pa = psA.next()
                        for c in cl:
                            S.op("pe", lambda e, pa=pa, c=c, g0=g0, V=V: e.matmul(pa[:, c - g0, :], lhsT=V["kd"][0][:, c * 64:(c + 1) * 64], rhs=V["qd"][0][:, c * 64:(c + 1) * 64], start=True, stop=True),
                                 reads=[V["kd"][1], V["qd"][1]], writes=[r_pa], signal=(c == cl[-1]))
                        pa2, r_pa2 = psA2[d]
                        subs = (1, 2, 3) if d == 0 else (0, 1, 2)
                        for c in cl:
                            for i in subs:
                                kv = V["k%d" % (i if d == 0 else i + 1)]
                                S.op("pe", lambda e, pa2=pa2, c=c, g0=g0, i=i, kv=kv, V=V: e.matmul(pa2[:, c - g0, 16 * i:16 * i + 16], lhsT=kv[0][:, c * 64:(c + 1) * 64], rhs=V["qb"][0][:, c * 64 + 16 * i:c * 64 + 16 * i + 16], start=True, stop=True),
                                     reads=[kv[1], V["qb"][1]], writes=[r_pa2], signal=(c == cl[-1] and i == subs[-1]))
                        at, r_at = At.next()
                        at2, r_at2 = At2.next()
                        mD, r_mD = mk["maskD_f" if d == 0 else "maskD_b"]
                        mO, r_mO = mk["maskO_f" if d == 0 else "maskO_b"]
                        S.op("dve", lambda e, at=at, pa=pa, n=n, mD=mD: e.tensor_tensor(out=at[:, 0:n, :], in0=pa[:, 0:n, :], in1=mD[:].unsqueeze(1).broadcast_to([64, n, 64]), op=ALU.mult),
                             reads=[r_pa, r_mD], writes=[r_at])
                        S.op("dve", lambda e, at2=at2, pa2=pa2, n=n, mO=mO: e.tensor_tensor(out=at2[:, 0:n, :], in0=pa2[:, 0:n, :], in1=mO[:].unsqueeze(1).broadcast_to([64, n, 64]), op=ALU.mult),
                             reads=[r_pa2, r_mO], writes=[r_at2])
                        S.op("pool", lambda e, at=at, at2=at2, n=n: e.tensor_tensor(out=at[:, 0:n, :], in0=at[:, 0:n, :], in1=at2[:, 0:n, :], op=ALU.add), reads=[r_at, r_at2], writes=[r_at])
                        pk, r_pk = psK.next()
                        for c in cl:
                            S.op("pe", lambda e, pk=pk, c=c, g0=g0, V=V: e.transpose(out=pk[:, c - g0, :], in_=V["kout"][0][:, c * 64:(c + 1) * 64], identity=ident[:]),
                                 reads=[V["kout"][1], r_ident], writes=[r_pk], signal=(c == cl[-1]))
                        kk, r_kk = ktok.next()
                        S.op("act", lambda e, kk=kk, pk=pk, n=n: e.copy(out=kk[:, 0:n, :], in_=pk[:, 0:n, :]), reads=[r_pk], writes=[r_kk])
                        po, r_po = psO.next()
                        ctxs.append((d, cl, g0, at, r_at, kk, r_kk, po, r_po))
                    nsteps = max(len(c[1]) for c in ctxs)
                    for j in range(nsteps):
                        for (d, cl, g0, at, r_at, kk, r_kk, po, r_po) in ctxs:
                            if j >= len(cl):
                                continue
                            c = cl[j]
                            pos = c - g0
                            V = var[d]
                            S.op("pe", lambda e, po=po, pos=pos, c=c, at=at: e.matmul(po[:, pos, :], lhsT=vt[:, c, :], rhs=at[:, pos, :], start=True, stop=False),
                                 reads=[r_vt, r_at], writes=[r_po], signal=False)
                            S.op("pe", lambda e, po=po, pos=pos, c=c, d=d, V=V: e.matmul(po[:, pos, :], lhsT=Sb[d][0][:], rhs=V["qin"][0][:, c * 64:(c + 1) * 64], start=False, stop=True),
                                 reads=[Sb[d][1], V["qin"][1]], writes=[r_po], signal=(j == len(cl) - 1))
                            last = (gi == ng - 1 and j == len(cl) - 1)
                            if not last:
                                pd, r_pd = psD.next()
                                S.op("pe", lambda e, pd=pd, kk=kk, pos=pos, c=c: e.matmul(pd[:], lhsT=kk[:, pos, :], rhs=vt[:, c, :], start=True, stop=True),
                                     reads=[r_kk, r_vt], writes=[r_pd])
                                S.op("dve", lambda e, pd=pd, d=d, c=c: e.scalar_tensor_tensor(out=Sf[d][0][:], in0=Sf[d][0][:], scalar=tr[d][0][:, c:c + 1], in1=pd[:], op0=ALU.mult, op1=ALU.add),
                                     reads=[r_pd, Sf[d][1], tr[d][1]], writes=[Sf[d][1]])
                                S.op("act", lambda e, d=d: e.copy(out=Sb[d][0][:], in_=Sf[d][0][:]), reads=[Sf[d][1]], writes=[Sb[d][1]])
                    for (d, cl, g0, at, r_at, kk, r_kk, po, r_po) in ctxs:
                        n = len(cl)
                        dst = v3(oall[:, g0 * 64:(g0 + n) * 64], 64)
                        if g0 not in touched:
                            touched.add(g0)
                            S.op("act", lambda e, dst=dst, po=po, n=n: e.copy(out=dst, in_=po[:, 0:n, :]), reads=[r_po], writes=[r_oall])
                        else:
                            S.op("dve", lambda e, dst=dst, po=po, n=n: e.tensor_tensor(out=dst, in0=po[:, 0:n, :], in1=dst, op=ALU.add), reads=[r_po, r_oall], writes=[r_oall])
                sq, r_sq = tmp.next()
                S.op("act", lambda e, sq=sq: e.activation(out=sq[:], in_=oall[:], func=AF.Square), reads=[r_oall], writes=[r_sq])
                for (t0, n) in cfg.tblocks:
                    pss, r_pss = psS.next()
                    S.op("pe", lambda e, pss=pss, sq=sq, t0=t0, n=n: e.matmul(pss[:, 0:n], lhsT=onesf[:], rhs=sq[:, t0:t0 + n], start=True, stop=True), reads=[r_onesf, r_sq], writes=[r_pss])
                    r1, r_r1 = rs.next()
                    S.op("dve", lambda e, r1=r1, pss=pss, n=n: e.tensor_scalar(out=r1[:, 0:n], in0=pss[:, 0:n], scalar1=1.0 / 128, scalar2=EPS, op0=ALU.mult, op1=ALU.add), reads=[r_pss], writes=[r_r1])
                    S.op("act", lambda e, r1=r1, n=n: e.activation(out=r1[:, 0:n], in_=r1[:, 0:n], func=AF.Sqrt), reads=[r_r1], writes=[r_r1])
                    S.op("dve", lambda e, r1=r1, n=n: e.reciprocal(out=r1[:, 0:n], in_=r1[:, 0:n]), reads=[r_r1], writes=[r_r1])
                    S.op("dve", lambda e, r1=r1, t0=t0, n=n: e.tensor_tensor(out=r1[:, 0:n], in0=r1[:, 0:n], in1=oall[:, t0:t0 + n], op=ALU.mult), reads=[r_r1, r_oall], writes=[r_r1])
                    S.op("dve", lambda e, r1=r1, t0=t0, n=n, hd=hd: e.scalar_tensor_tensor(out=aT[:, t0:t0 + n], in0=r1[:, 0:n], scalar=gain[:, hd:hd + 1], in1=sgT[:, t0:t0 + n], op0=ALU.mult, op1=ALU.mult),
                         reads=[r_r1, r_gain, r_sgT], writes=[r_aT])
                S.dma("sp", lambda e, hd=hd: e.dma_start(out=self.AT[hd], in_=aT[:]), reads=[r_aT], writes=[self.r_AT[hd]])
            self.end_phase()

    def phase_attn(self, l, s):
        cfg, nc, S = self.cfg, self.nc, self.S
        T, CTX, NCT, NT = cfg.T, cfg.CTX, cfg.NCT, cfg.NT
        NQ = cfg.SEQ // 128
        ident, r_ident = self.C["ident"]
        mprev, r_mprev = self.C["mneg_prev"]
        mnext, r_mnext = self.C["mneg_next"]
        onesb, r_onesb = self.C["ones"]
        with ExitStack() as ph:
            kc_ = [tile1(ph, nc, "a_k%d" % i, [128, T], BF16) for i in range(2)]
            vtok, r_vtok = tile1(ph, nc, "a_v", [128, NT, 128], BF16)
            qpool = TPool(ph, nc, "a_q", [128, T], BF16, 2)
            opool = TPool(ph, nc, "a_o", [128, T], BF16, 2)
            esb, r_esb = tile1(ph, nc, "a_esb", [128, 16], F32)
            es2, r_es2 = tile1(ph, nc, "a_es2", [128, 8], F32)
            ppool = TPool(ph, nc, "a_p", [128, 5, 128], BF16, 4)
            rpool = TPool(ph, nc, "a_r", [128, 128], F32, 3)
            psS = TPool(ph, nc, "a_psS", [128, 8, 128], F32, 3, psum=True)
            psOD = TPool(ph, nc, "a_psOD", [128, 2, 128], F32, 2, psum=True)
            for i in range(2):
                S.dma("sp", lambda e, i=i: e.dma_start(out=kc_[i][0][:], in_=self.KA[i]), reads=[self.r_KA[i]], writes=[kc_[i][1]])
            S.dma("sp", lambda e: e.dma_start(out=vtok[:], in_=self.VA.rearrange("(c p) v -> p c v", p=128)), reads=[self.r_VA], writes=[r_vtok])
            S.dma("sp", lambda e: e.dma_start(out=esb[:], in_=self.sink[l:l + 1, :].partition_broadcast(128)), writes=[r_esb])
            S.op("act", lambda e: e.activation(out=esb[:], in_=esb[:], func=AF.Exp), reads=[r_esb], writes=[r_esb])
            esb3 = esb[:].rearrange("p (c two) -> p c two", two=2)
            S.op("dve", lambda e: e.tensor_copy(out=es2[0:64, :], in_=esb3[0:64, :, 0]), reads=[r_esb], writes=[r_es2])
            S.op("dve", lambda e: e.tensor_copy(out=es2[64:128, :], in_=esb3[64:128, :, 1]), reads=[r_esb], writes=[r_es2])
            qblocks = []
            if l == 0:
                for m in range(NCT):
                    qblocks.append((m * 128, [(t, None) for t in range(NCT)]))
            for n in range(NQ):
                tiles = [(t, None) for t in range(NCT)]
                if n > 0:
                    tiles.append((NCT + n - 1, "prev"))
                tiles.append((NCT + n, None))
                if n < NQ - 1:
                    tiles.append((NCT + n + 1, "next"))
                qblocks.append((CTX + n * 128, tiles))
            for c in range(8):
                g = c // 4
                self.pump(cfg.PUMP)
                qc, r_qc = qpool.next()
                S.dma("sp", lambda e, c=c, qc=qc: e.dma_start(out=qc[:], in_=self.QA[c]), reads=[self.r_QA[c]], writes=[r_qc])
                oc, r_oc = opool.next()
                kops = [(kc_[0] if g == 0 else kc_[1], 0), (kc_[1] if g == 0 else kc_[0], 64)]
                for (q0, tiles) in qblocks:
                    nt = len(tiles)
                    pts = []
                    for hh in range(2):
                        (k_t, k_r), r0 = kops[hh]
                        ps, r_ps = psS.next()
                        for ti, (tile, mkind) in enumerate(tiles):
                            S.op("pe", lambda e, ps=ps, ti=ti, tile=tile, k_t=k_t, r0=r0, q0=q0, qc=qc, mkind=mkind: e.matmul(ps[:, ti, :], lhsT=k_t[r0:r0 + 64, tile * 128:(tile + 1) * 128], rhs=qc[r0:r0 + 64, q0:q0 + 128], start=True, stop=(mkind is None)),
                                 reads=[k_r, r_qc], writes=[r_ps], signal=(mkind is None and ti == nt - 1))
                            if mkind is not None:
                                m_t, m_r = (mprev, r_mprev) if mkind == "prev" else (mnext, r_mnext)
                                S.op("pe", lambda e, ps=ps, ti=ti, m_t=m_t: e.matmul(ps[:, ti, :], lhsT=ident[:], rhs=m_t[:], start=False, stop=True),
                                     reads=[r_ident, m_r], writes=[r_ps], signal=(ti == nt - 1))
                        pt, r_pt = ppool.next()
                        S.op("act", lambda e, pt=pt, ps=ps, nt=nt: e.activation(out=pt[:, 0:nt, :], in_=ps[:, 0:nt, :], func=AF.Exp), reads=[r_ps], writes=[r_pt])
                        pts.append((pt, r_pt))
                    od, r_od = psOD.next()
                    for hh in range(2):
                        pt, r_pt = pts[hh]
                        r0 = 64 * hh
                        tp = None if hh == 0 else (0, 64)
                        for ti, (tile, mkind) in enumerate(tiles):
                            S.op("pe", lambda e, od=od, r0=r0, ti=ti, tile=tile, pt=pt, tp=tp, nt=nt, g=g: e.matmul(od[r0:r0 + 64, 0, :], lhsT=vtok[:, tile, g * 64:(g + 1) * 64], rhs=pt[:, ti, :], start=(ti == 0), stop=(ti == nt - 1), tile_position=tp),
                                 reads=[r_vtok, r_pt], writes=[r_od], signal=False)
                        for ti, (tile, mkind) in enumerate(tiles):
                            S.op("pe", lambda e, od=od, r0=r0, ti=ti, pt=pt, tp=tp, nt=nt: e.matmul(od[r0:r0 + 64, 1, :], lhsT=onesb[:, 0:64], rhs=pt[:, ti, :], start=(ti == 0), stop=(ti == nt - 1), tile_position=tp),
                                 reads=[r_onesb, r_pt], writes=[r_od], signal=(ti == nt - 1))
                    rc, r_rc = rpool.next()
                    S.op("dve", lambda e, rc=rc, od=od, c=c: e.tensor_scalar(out=rc[:], in0=od[:, 1, :], scalar1=es2[:, c:c + 1], scalar2=None, op0=ALU.add), reads=[r_od, r_es2], writes=[r_rc])
                    S.op("dve", lambda e, rc=rc: e.reciprocal(out=rc[:], in_=rc[:]), reads=[r_rc], writes=[r_rc])
                    S.op("dve", lambda e, rc=rc, od=od, oc=oc, q0=q0: e.tensor_tensor(out=oc[:, q0:q0 + 128], in0=od[:, 0, :], in1=rc[:], op=ALU.mult), reads=[r_od, r_rc], writes=[r_oc])
                if l == 0:
                    S.dma("sp", lambda e, c=c, oc=oc: e.dma_start(out=self.OAT[c], in_=oc[:]), reads=[r_oc], writes=[self.r_OAT[c]])
                else:
                    S.dma("sp", lambda e, c=c, oc=oc: e.dma_start(out=self.OAT[c][:, CTX:T], in_=oc[:, CTX:T]), reads=[r_oc], writes=[self.r_OAT[c]])
            self.end_phase()

    def phase_merge(self, l, s):
        cfg, nc, S = self.cfg, self.nc, self.S
        D, KC, T, CTX = cfg.D, cfg.KC, cfg.T, cfg.CTX
        BIG, r_BIG = self.BIG
        blocks = cfg.tblocks if l == 0 else cfg.lat_blocks
        tiles = list(range(cfg.NT)) if l == 0 else list(range(cfg.NCT, cfg.NT))
        with ExitStack() as ph:
            aT, r_aT = tile1(ph, nc, "m_aT", [128, 8, T], BF16)
            oT, r_oT = tile1(ph, nc, "m_oT", [128, 8, T], BF16)
            S.dma("sp", lambda e: e.dma_start(out=aT[:], in_=self.AT.rearrange("c p t -> p c t")), reads=self.r_AT, writes=[r_aT])
            S.dma("sp", lambda e: e.dma_start(out=oT[:], in_=self.OAT.rearrange("c p t -> p c t")), reads=self.r_OAT, writes=[r_oT])
            wap = TPool(ph, nc, "m_wa", [128, 8, 512], BF16, 2)
            wbp = TPool(ph, nc, "m_wb", [128, 8, 512], BF16, 2)
            gap = TPool(ph, nc, "m_ga", [128, T], BF16, 2)
            gbp = TPool(ph, nc, "m_gb", [128, T], BF16, 2)
            tp = TPool(ph, nc, "m_t", [128, 512], F32, 4)
            ps1 = TPool(ph, nc, "m_ps1", [128, 512], F32, 2, psum=True)
            ps2 = TPool(ph, nc, "m_ps2", [128, 512], F32, 2, psum=True)
            for jb in range(4):
                wa, r_wa = self.load_wblock(wap, self.w_a[l], jb * 512, 512)
                wb, r_wb = self.load_wblock(wbp, self.w_b[l], jb * 512, 512)
                for c in range(4):
                    j = jb * 4 + c
                    ga, r_ga = gap.next()
                    gb, r_gb = gbp.next()
                    S.dma("sp", lambda e, ga=ga, j=j: e.dma_start(out=ga[:], in_=self.GA[j]), reads=[self.r_GA[j]], writes=[r_ga])
                    S.dma("sp", lambda e, gb=gb, j=j: e.dma_start(out=gb[:], in_=self.GB[j]), reads=[self.r_GB[j]], writes=[r_gb])
                    for (t0, n) in blocks:
                        p1, r_p1 = ps1.next()
                        p2, r_p2 = ps2.next()
                        for kc in range(8):
                            S.op("pe", lambda e, p1=p1, wa=wa, kc=kc, c=c, t0=t0, n=n: e.matmul(p1[:, 0:n], lhsT=wa[:, kc, c * 128:(c + 1) * 128], rhs=aT[:, kc, t0:t0 + n], start=(kc == 0), stop=(kc == 7)),
                                 reads=[r_wa, r_aT], writes=[r_p1], signal=(kc == 7))
                        for kc in range(8):
                            S.op("pe", lambda e, p2=p2, wb=wb, kc=kc, c=c, t0=t0, n=n: e.matmul(p2[:, 0:n], lhsT=wb[:, kc, c * 128:(c + 1) * 128], rhs=oT[:, kc, t0:t0 + n], start=(kc == 0), stop=(kc == 7)),
                                 reads=[r_wb, r_oT], writes=[r_p2], signal=(kc == 7))
                        t1, r_t1 = tp.next()
                        t2, r_t2 = tp.next()
                        S.op("dve", lambda e, t1=t1, p1=p1, ga=ga, t0=t0, n=n: e.tensor_tensor(out=t1[:, 0:n], in0=p1[:, 0:n], in1=ga[:, t0:t0 + n], op=ALU.mult), reads=[r_p1, r_ga], writes=[r_t1])
                        S.op("dve", lambda e, t2=t2, p2=p2, gb=gb, t0=t0, n=n: e.tensor_tensor(out=t2[:, 0:n], in0=p2[:, 0:n], in1=gb[:, t0:t0 + n], op=ALU.mult), reads=[r_p2, r_gb], writes=[r_t2])
                        S.op("pool", lambda e, t1=t1, t2=t2, j=j, t0=t0, n=n: e.tensor_tensor(out=BIG[:, j, t0:t0 + n], in0=t1[:, 0:n], in1=t2[:, 0:n], op=ALU.add), reads=[r_t1, r_t2], writes=[r_BIG])
            self.end_phase()
        with ExitStack() as ph:
            wop = TPool(ph, nc, "m_wo", [128, KC, 512], BF16, 2)
            g1 = {}
            for kind, row in (("lat", s), ("ctx", cfg.NSEQ)):
                g1[kind] = tile1(ph, nc, "m_g1" + kind, [128, D], F32)
                self.bcast_row(g1[kind][0], g1[kind][1], self.MOD[l, row:row + 1, 2 * D:3 * D], D)
            xp = TPool(ph, nc, "m_x", [128, 512], F32, 3)
            tp = TPool(ph, nc, "m_t2", [128, 512], F32, 2)
            xo = TPool(ph, nc, "m_xo", [128, 512], F32, 3)
            pso = TPool(ph, nc, "m_pso", [128, 512], F32, 3, psum=True)
            first = (l == 0)
            for nb in range(4):
                wo, r_wo = self.load_wblock(wop, self.w_o[l], nb * 512, 512)
                loaded = {}

                def load(i, nb=nb, loaded=loaded):
                    tile = tiles[i]
                    xt, r_xt = xp.next()
                    src = self.xsrc(first, s, tile)[:, nb * 512:(nb + 1) * 512]
                    S.dma("sp", lambda e, xt=xt, src=src: e.dma_start(out=xt[:], in_=src), reads=([] if first else [self.r_XR[s][tile]]), writes=[r_xt])
                    loaded[i] = (xt, r_xt)
                load(0)
                for i, tile in enumerate(tiles):
                    if i + 1 < len(tiles):
                        load(i + 1)
                    xt, r_xt = loaded.pop(i)
                    g_t, g_r = g1["ctx" if tile < cfg.NCT else "lat"]
                    ps, r_ps = pso.next()
                    for kc in range(KC):
                        S.op("pe", lambda e, ps=ps, kc=kc, tile=tile, wo=wo: e.matmul(ps[:], lhsT=BIG[:, kc, tile * 128:(tile + 1) * 128], rhs=wo[:, kc, :], start=(kc == 0), stop=(kc == KC - 1)),
                             reads=[r_BIG, r_wo], writes=[r_ps], signal=(kc == KC - 1))
                    t1, r_t1 = tp.next()
                    S.op("dve", lambda e, t1=t1, ps=ps, g_t=g_t, nb=nb: e.tensor_tensor(out=t1[:], in0=ps[:], in1=g_t[:, nb * 512:(nb + 1) * 512], op=ALU.mult), reads=[r_ps, g_r], writes=[r_t1])
                    xn, r_xn = xo.next()
                    S.op("dve", lambda e, xn=xn, t1=t1, xt=xt: e.tensor_tensor(out=xn[:], in0=t1[:], in1=xt[:], op=ALU.add), reads=[r_t1, r_xt], writes=[r_xn])
                    S.dma("sp", lambda e, xn=xn, tile=tile, nb=nb: e.dma_start(out=self.XR[s, tile * 128:(tile + 1) * 128, nb * 512:(nb + 1) * 512], in_=xn[:]), reads=[r_xn], writes=[self.r_XR[s][tile]])
            self.end_phase()

    def phase_swiglu(self, l, s):
        cfg, nc, S = self.cfg, self.nc, self.S
        D, KC, T, CTX, NCT = cfg.D, cfg.KC, cfg.T, cfg.CTX, cfg.NCT
        BIG, r_BIG = self.BIG
        moe = (l == 1)
        blocks = cfg.lat_blocks if moe else cfg.tblocks
        if moe:
            experts = [(self.moe_wg[0, e], self.moe_wu[0, e], self.moe_wd[0, e], cfg.DEXP, e) for e in range(8)]
        else:
            experts = [(self.ffn_wg[0], self.ffn_wu[0], self.ffn_wd[0], cfg.DFF, None)]
        GF = 256
        with ExitStack() as ph:
            g2 = {}
            for kind, row in (("lat", s), ("ctx", cfg.NSEQ)):
                if moe and kind == "ctx":
                    continue
                g2[kind] = tile1(ph, nc, "f_g2" + kind, [128, D], F32)
                self.bcast_row(g2[kind][0], g2[kind][1], self.MOD[l, row:row + 1, 5 * D:6 * D], D)
            comb = None
            if moe:
                comb = self.routing(ph, s)
            wgp = TPool(ph, nc, "f_wg", [128, KC, GF], BF16, 2)
            wup = TPool(ph, nc, "f_wu", [128, KC, GF], BF16, 2)
            wdp = TPool(ph, nc, "f_wd", [128, GF // 128, D], BF16, 2)
            acc, r_acc = tile1(ph, nc, "f_acc", [128, 4, D], F32)
            actp = TPool(ph, nc, "f_act", [128, GF // 128, 512], BF16, 2)
            sgp = TPool(ph, nc, "f_sg", [128, 512], F32, 2)
            xp = TPool(ph, nc, "f_x", [128, D], F32, 2)
            tp = TPool(ph, nc, "f_t", [128, D], F32, 1)
            psg = TPool(ph, nc, "f_psg", [128, 512], F32, 2, psum=True)
            psu = TPool(ph, nc, "f_psu", [128, 512], F32, 2, psum=True)
            pso = TPool(ph, nc, "f_pso", [128, 512], F32, 2 if moe else 3, psum=True)
            for (wg2d, wu2d, wd2d, dff, eidx) in experts:
                ngr = dff // GF
                for (t0, n) in blocks:
                    ntl = n // 128
                    for gr in range(ngr):
                        wg, r_wg = self.load_wblock(wgp, wg2d, gr * GF, GF)
                        wu, r_wu = self.load_wblock(wup, wu2d, gr * GF, GF)
                        wd, r_wd = wdp.next()
                        for hlf in range(2):
                            srcd = wd2d[gr * GF:(gr + 1) * GF, hlf * 1024:(hlf + 1) * 1024].rearrange("(c p) n -> p c n", p=128)
                            S.dma("pool", lambda e, wd=wd, srcd=srcd, hlf=hlf: e.dma_start(out=wd[:, :, hlf * 1024:(hlf + 1) * 1024], in_=srcd), writes=[r_wd])
                        act, r_act = actp.next()
                        for c in range(GF // 128):
                            pg, r_pg = psg.next()
                            pu, r_pu = psu.next()
                            for kc in range(KC):
                                S.op("pe", lambda e, pg=pg, wg=wg, kc=kc, c=c, t0=t0, n=n: e.matmul(pg[:, 0:n], lhsT=wg[:, kc, c * 128:(c + 1) * 128], rhs=BIG[:, kc, t0:t0 + n], start=(kc == 0), stop=(kc == KC - 1)),
                                     reads=[r_wg, r_BIG], writes=[r_pg], signal=(kc == KC - 1))
                            for kc in range(KC):
                                S.op("pe", lambda e, pu=pu, wu=wu, kc=kc, c=c, t0=t0, n=n: e.matmul(pu[:, 0:n], lhsT=wu[:, kc, c * 128:(c + 1) * 128], rhs=BIG[:, kc, t0:t0 + n], start=(kc == 0), stop=(kc == KC - 1)),
                                     reads=[r_wu, r_BIG], writes=[r_pu], signal=(kc == KC - 1))
                            sg, r_sg = sgp.next()
                            S.op("act", lambda e, sg=sg, pg=pg, n=n: e.activation(out=sg[:, 0:n], in_=pg[:, 0:n], func=AF.Silu), reads=[r_pg], writes=[r_sg])
                            S.op("dve", lambda e, act=act, sg=sg, pu=pu, c=c, n=n: e.tensor_tensor(out=act[:, c, 0:n], in0=sg[:, 0:n], in1=pu[:, 0:n], op=ALU.mult), reads=[r_sg, r_pu], writes=[r_act])
                        for tl in range(ntl):
                            for nb in range(4):
                                po, r_po = pso.next()
                                nch = GF // 128
                                for c in range(nch):
                                    S.op("pe", lambda e, po=po, act=act, wd=wd, c=c, tl=tl, nb=nb, nch=nch: e.matmul(po[:], lhsT=act[:, c, tl * 128:(tl + 1) * 128], rhs=wd[:, c, nb * 512:(nb + 1) * 512], start=(c == 0), stop=(c == nch - 1)),
                                         reads=[r_act, r_wd], writes=[r_po], signal=(c == nch - 1))
                                if gr == 0:
                                    S.op("act", lambda e, po=po, tl=tl, nb=nb: e.copy(out=acc[:, tl, nb * 512:(nb + 1) * 512], in_=po[:]), reads=[r_po], writes=[r_acc])
                                else:
                                    S.op("dve", lambda e, po=po, tl=tl, nb=nb: e.tensor_tensor(out=acc[:, tl, nb * 512:(nb + 1) * 512], in0=po[:], in1=acc[:, tl, nb * 512:(nb + 1) * 512], op=ALU.add), reads=[r_po, r_acc], writes=[r_acc])
                    for tl in range(ntl):
                        tile = t0 // 128 + tl
                        g_t, g_r = g2["ctx" if tile < NCT else "lat"]
                        xt, r_xt = xp.next()
                        S.dma("sp", lambda e, xt=xt, tile=tile: e.dma_start(out=xt[:], in_=self.XR[s, tile * 128:(tile + 1) * 128, :]), reads=[self.r_XR[s][tile]], writes=[r_xt])
                        t1, r_t1 = tp.next()
                        S.op("dve", lambda e, t1=t1, tl=tl, g_t=g_t: e.tensor_tensor(out=t1[:], in0=acc[:, tl, :], in1=g_t[:], op=ALU.mult), reads=[r_acc, g_r], writes=[r_t1])
                        if eidx is None:
                            S.op("dve", lambda e, t1=t1, xt=xt: e.tensor_tensor(out=xt[:], in0=t1[:], in1=xt[:], op=ALU.add), reads=[r_t1, r_xt], writes=[r_xt])
                        else:
                            cb, r_cb = comb
                            lt = tile - NCT
                            S.op("dve", lambda e, t1=t1, xt=xt, cb=cb, lt=lt, eidx=eidx: e.scalar_tensor_tensor(out=xt[:], in0=t1[:], scalar=cb[:, lt, eidx:eidx + 1], in1=xt[:], op0=ALU.mult, op1=ALU.add),
                                 reads=[r_t1, r_xt, r_cb], writes=[r_xt])
                        S.dma("sp", lambda e, xt=xt, tile=tile: e.dma_start(out=self.XR[s, tile * 128:(tile + 1) * 128, :], in_=xt[:]), reads=[r_xt], writes=[self.r_XR[s][tile]])
            self.end_phase()

    def routing(self, ph, s, want_sel=False):
        cfg, nc, S = self.cfg, self.nc, self.S
        D, KC, NCT = cfg.D, cfg.KC, cfg.NCT
        BIG, r_BIG = self.BIG
        NTl = cfg.SEQ // 128
        rf, r_rf = tile1(ph, nc, "r_rf", [128, KC, 8], F32)
        rh, r_rh = tile1(ph, nc, "r_rh", [128, KC, 8], BF16)
        rl, r_rl = tile1(ph, nc, "r_rl", [128, KC, 8], BF16)
        S.dma("sp", lambda e: e.dma_start(out=rf[:], in_=self.router[0].rearrange("(kc p) e -> p kc e", p=128)), writes=[r_rf])
        S.op("dve", lambda e: e.tensor_copy(out=rh[:], in_=rf[:]), reads=[r_rf], writes=[r_rh])
        S.op("dve", lambda e: e.tensor_tensor(out=rl[:], in0=rf[:], in1=rh[:], op=ALU.subtract), reads=[r_rf, r_rh], writes=[r_rl])
        lg, r_lg = tile1(ph, nc, "r_lg", [128, NTl, 8], F32)
        psl = TPool(ph, nc, "r_ps", [128, 8], F32, 2, psum=True)
        for lt in range(NTl):
            tile = NCT + lt
            ps, r_ps = psl.next()
            for i, (rt, rr) in enumerate(((rh, r_rh), (rl, r_rl))):
                for kc in range(KC):
                    S.op("pe", lambda e, ps=ps, rt=rt, kc=kc, tile=tile, i=i: e.matmul(ps[:], lhsT=BIG[:, kc, tile * 128:(tile + 1) * 128], rhs=rt[:, kc, :], start=(i == 0 and kc == 0), stop=(i == 1 and kc == KC - 1)),
                         reads=[r_BIG, rr], writes=[r_ps], signal=(i == 1 and kc == KC - 1))
            S.op("act", lambda e, ps=ps, lt=lt: e.copy(out=lg[:, lt, :], in_=ps[:]), reads=[r_ps], writes=[r_lg])
        shp = [128, NTl, 8]
        m1, r_m1 = tile1(ph, nc, "r_m1", [128, NTl], F32)
        m2, r_m2 = tile1(ph, nc, "r_m2", [128, NTl], F32)
        t8, r_t8 = tile1(ph, nc, "r_t8", shp, F32)
        l2, r_l2 = tile1(ph, nc, "r_l2", shp, F32)
        sel, r_sel = tile1(ph, nc, "r_sel", shp, F32)
        cb, r_cb = tile1(ph, nc, "r_cb", shp, F32)
        bc = lambda t: t[:].unsqueeze(2).broadcast_to(shp)
        S.op("dve", lambda e: e.tensor_reduce(out=m1[:], in_=lg[:], axis=AX.X, op=ALU.max), reads=[r_lg], writes=[r_m1])
        S.op("dve", lambda e: e.tensor_tensor(out=t8[:], in0=lg[:], in1=bc(m1), op=ALU.is_equal), reads=[r_lg, r_m1], writes=[r_t8])
        S.op("dve", lambda e: e.scalar_tensor_tensor(out=l2[:], in0=t8[:], scalar=-1e30, in1=lg[:], op0=ALU.mult, op1=ALU.add), reads=[r_t8, r_lg], writes=[r_l2])
        S.op("dve", lambda e: e.tensor_reduce(out=m2[:], in_=l2[:], axis=AX.X, op=ALU.max), reads=[r_l2], writes=[r_m2])
        S.op("dve", lambda e: e.tensor_tensor(out=sel[:], in0=lg[:], in1=bc(m2), op=ALU.is_ge), reads=[r_lg, r_m2], writes=[r_sel])
        S.op("dve", lambda e: e.tensor_tensor(out=t8[:], in0=lg[:], in1=bc(m1), op=ALU.subtract), reads=[r_lg, r_m1], writes=[r_t8])
        S.op("act", lambda e: e.activation(out=t8[:], in_=t8[:], func=AF.Exp), reads=[r_t8], writes=[r_t8])
        S.op("dve", lambda e: e.tensor_tensor(out=t8[:], in0=t8[:], in1=sel[:], op=ALU.mult), reads=[r_t8, r_sel], writes=[r_t8])
        S.op("dve", lambda e: e.tensor_tensor(out=m2[:], in0=m2[:], in1=m1[:], op=ALU.subtract), reads=[r_m2, r_m1], writes=[r_m2])
        S.op("act", lambda e: e.activation(out=m2[:], in_=m2[:], func=AF.Exp), reads=[r_m2], writes=[r_m2])
        S.op("dve", lambda e: e.tensor_scalar(out=m2[:], in0=m2[:], scalar1=1.0, scalar2=None, op0=ALU.add), reads=[r_m2], writes=[r_m2])
        S.op("dve", lambda e: e.reciprocal(out=m2[:], in_=m2[:]), reads=[r_m2], writes=[r_m2])
        S.op("dve", lambda e: e.tensor_tensor(out=cb[:], in0=t8[:], in1=bc(m2), op=ALU.mult), reads=[r_t8, r_m2], writes=[r_cb])
        if want_sel:
            return cb, r_cb, sel, r_sel
        return cb, r_cb

    def phase_final(self):
        cfg, nc, S = self.cfg, self.nc, self.S
        D, NCT = cfg.D, cfg.NCT
        with ExitStack() as ph:
            fn, r_fn = tile1(ph, nc, "z_fn", [128, D], F32)
            S.dma("sp", lambda e: e.dma_start(out=fn[:], in_=self.final_norm.unsqueeze(0).partition_broadcast(128)), writes=[r_fn])
            xp = TPool(ph, nc, "z_x", [128, D], F32, 3)
            op_ = TPool(ph, nc, "z_o", [128, D], F32, 2)
            junk, r_junk = tile1(ph, nc, "z_junk", [128, D], BF16)
            stp = TPool(ph, nc, "z_st", [128, 4], F32, 2)
            jobs = [(s, tile) for s in range(cfg.NSEQ) for tile in range(NCT, cfg.NT)]
            loaded = {}

            def load(i):
                s, tile = jobs[i]
                xt, r_xt = xp.next()
                S.dma("sp", lambda e, xt=xt, s=s, tile=tile: e.dma_start(out=xt[:], in_=self.XR[s, tile * 128:(tile + 1) * 128, :]), reads=[self.r_XR[s][tile]], writes=[r_xt])
                loaded[i] = (xt, r_xt)
            load(0)
            for i, (s, tile) in enumerate(jobs):
                if i + 1 < len(jobs):
                    load(i + 1)
                xt, r_xt = loaded.pop(i)
                st, r_st = stp.next()
                S.op("act", lambda e, xt=xt, st=st: e.activation(out=junk[:], in_=xt[:], func=AF.Square, accum_out=st[:, 0:1]), reads=[r_xt], writes=[r_junk, r_st])
                S.op("dve", lambda e, st=st: e.tensor_scalar(out=st[:, 1:2], in0=st[:, 0:1], scalar1=1.0 / D, scalar2=EPS, op0=ALU.mult, op1=ALU.add), reads=[r_st], writes=[r_st])
                S.op("act", lambda e, st=st: e.activation(out=st[:, 2:3], in_=st[:, 1:2], func=AF.Sqrt), reads=[r_st], writes=[r_st])
                S.op("dve", lambda e, st=st: e.reciprocal(out=st[:, 3:4], in_=st[:, 2:3]), reads=[r_st], writes=[r_st])
                ot, r_ot = op_.next()
                S.op("dve", lambda e, ot=ot, xt=xt, st=st: e.scalar_tensor_tensor(out=ot[:], in0=xt[:], scalar=st[:, 3:4], in1=fn[:], op0=ALU.mult, op1=ALU.mult), reads=[r_xt, r_st, r_fn], writes=[r_ot])
                lt = tile - NCT
                S.dma("sp", lambda e, ot=ot, s=s, lt=lt: e.dma_start(out=self.out[s, lt * 128:(lt + 1) * 128, :], in_=ot[:]), reads=[r_ot], writes=[self.r_out])
            self.end_phase()

    def prepass_jobs(self):
        cfg = self.cfg
        NG = cfg.DEXP // 512
        jobs = []
        for e_ in range(8):
            for g in range(NG):
                row0 = (e_ * NG + g) * 128
                for (dst_t, src_t) in ((self.WGB, self.moe_wg), (self.WUB, self.moe_wu)):
                    dst = dst_t[row0:row0 + 128, :].rearrange("p (kc n) -> p kc n", n=512)
                    src = src_t[0, e_, :, g * 512:(g + 1) * 512].rearrange("(kc p) n -> p kc n", p=128)
                    jobs.append((dst, src))
                for hlf in range(2):
                    dst = self.WDB[row0:row0 + 128, :].rearrange("p (c n) -> p c n", n=2048)[:, :, hlf * 1024:(hlf + 1) * 1024]
                    src = self.moe_wd[0, e_, g * 512:(g + 1) * 512, hlf * 1024:(hlf + 1) * 1024].rearrange("(c p) n -> p c n", p=128)
                    jobs.append((dst, src))
        return jobs

    def pump(self, k):
        if self.cfg.MOE != "routed":
            return
        for _ in range(k):
            if not self.pre_jobs:
                return
            dst, src = self.pre_jobs.pop(0)
            self.S.dma("pool", lambda e, dst=dst, src=src: e.dma_start(out=dst, in_=src))

    def route_local(self, s):
        cfg, nc, S = self.cfg, self.nc, self.S
        NTl = cfg.SEQ // 128
        with ExitStack() as ph:
            cb, r_cb, sel, r_sel = self.routing(ph, s, want_sel=True)
            S.dma("sp", lambda e: e.dma_start(out=self.SELD[:, s * NTl:(s + 1) * NTl, :], in_=sel[:]), reads=[r_sel], writes=[self.r_SELD])
            S.dma("sp", lambda e: e.dma_start(out=self.CBD[:, s * NTl:(s + 1) * NTl, :], in_=cb[:]), reads=[r_cb], writes=[self.r_SELD])
            self.end_phase()

    def phase_moe_routed(self):
        cfg, nc, S = self.cfg, self.nc, self.S
        D, KC, NCT = cfg.D, cfg.KC, cfg.NCT
        NTl = cfg.SEQ // 128
        NTg = cfg.NSEQ * NTl
        NSLOT = cfg.NSLOT
        NG = cfg.DEXP // 512
        ident, r_ident = self.C["ident"]
        ustrict, r_us = self.C["ustrict"]
        onesb, r_onesb = self.C["ones"]
        shp = [128, NTg, 8]
        self.pump(100000)
        with ExitStack() as ms:
            posA, r_posA = tile1(ms, nc, "q_posA", [128, NTg], I32)
            posB, r_posB = tile1(ms, nc, "q_posB", [128, NTg], I32)
            wA, r_wA = tile1(ms, nc, "q_wA", [128, NTg], F32)
            wB, r_wB = tile1(ms, nc, "q_wB", [128, NTg], F32)
            idxW, r_idxW = tile1(ms, nc, "q_idxW", [128, NSLOT, NG], I32)
            with ExitStack() as ph:
                sel, r_sel = tile1(ph, nc, "q_sel", shp, F32)
                cb, r_cb = tile1(ph, nc, "q_cb", shp, F32)
                selb, r_selb = tile1(ph, nc, "q_selb", shp, BF16)
                S.dma("sp", lambda e: e.dma_start(out=sel[:], in_=self.SELD), reads=[self.r_SELD], writes=[r_sel])
                S.dma("sp", lambda e: e.dma_start(out=cb[:], in_=self.CBD), reads=[self.r_SELD], writes=[r_cb])
                S.op("dve", lambda e: e.tensor_copy(out=selb[:], in_=sel[:]), reads=[r_sel], writes=[r_selb])
                pR, r_pR = tile1(ph, nc, "q_pR", [128, NTg * 8], F32, psum=True)
                pC, r_pC = tile1(ph, nc, "q_pC", [128, NTg * 8], F32, psum=True)
                flat = lambda t: t[:].rearrange("p g e -> p (g e)")
                S.op("pe", lambda e: e.matmul(pR[:], lhsT=ustrict[:], rhs=flat(selb), start=True, stop=True), reads=[r_us, r_selb], writes=[r_pR])
                S.op("pe", lambda e: e.matmul(pC[:], lhsT=onesb[:], rhs=flat(selb), start=True, stop=True), reads=[r_onesb, r_selb], writes=[r_pC])
                Cs, r_Cs = tile1(ph, nc, "q_Cs", shp, F32)
                incl, r_incl = tile1(ph, nc, "q_incl", shp, F32)
                pos, r_pos = tile1(ph, nc, "q_pos", shp, F32)
                v, r_v = tile1(ph, nc, "q_v", shp, F32)
                t8, r_t8 = tile1(ph, nc, "q_t8", shp, F32)
                o32, r_o32 = tile1(ph, nc, "q_o32", [128, NTg], F32)
                S.op("dve", lambda e: e.memset(o32[:], 1.0), writes=[r_o32])
                S.op("act", lambda e: e.copy(out=flat(Cs), in_=pC[:]), reads=[r_pC], writes=[r_Cs])
                for e_ in range(8):
                    S.op("dve", lambda e, e_=e_: e.tensor_tensor_scan(out=incl[:, :, e_], data0=o32[:], data1=Cs[:, :, e_], initial=0.0, op0=ALU.mult, op1=ALU.add),
                         reads=[r_o32, r_Cs], writes=[r_incl])
                sm = lambda name, w: tile1(ph, nc, name, [128, w], F32)
                n_e, r_n = sm("q_n", 8)
                np_e, r_np = sm("q_np", 8)
                base, r_base = sm("q_base", 8)
                cs, r_cs = sm("q_cs", 8)
                S.op("dve", lambda e: e.tensor_copy(out=n_e[:], in_=incl[:, NTg - 1, :]), reads=[r_incl], writes=[r_n])
                KMAX = max(1, (cfg.NSEQ * cfg.SEQ) // 512)
                kg, r_kg = tile1(ph, nc, "q_kg", [128, 8, KMAX], F32)
                S.dma("sp", lambda e: e.dma_start(out=kg[:], in_=self.cst_d["kgrid"].rearrange("p (e k) -> p e k", k=KMAX)), writes=[r_kg])
                S.op("dve", lambda e: e.tensor_tensor(out=kg[:], in0=n_e[:].unsqueeze(2).broadcast_to([128, 8, KMAX]), in1=kg[:], op=ALU.is_gt), reads=[r_n, r_kg], writes=[r_kg])
                S.op("dve", lambda e: e.tensor_reduce(out=np_e[:], in_=kg[:], axis=AX.X, op=ALU.add), reads=[r_kg], writes=[r_np])
                S.op("dve", lambda e: e.tensor_scalar(out=np_e[:], in0=np_e[:], scalar1=512.0, scalar2=None, op0=ALU.mult), reads=[r_np], writes=[r_np])
                S.op("dve", lambda e: e.memset(base[:], 0.0), writes=[r_base])
                for e_ in range(1, 8):
                    S.op("dve", lambda e, e_=e_: e.tensor_tensor(out=base[:, e_:e_ + 1], in0=base[:, e_ - 1:e_], in1=np_e[:, e_ - 1:e_], op=ALU.add), reads=[r_base, r_np], writes=[r_base])
                S.op("dve", lambda e: e.tensor_tensor(out=pos[:], in0=incl[:], in1=Cs[:], op=ALU.subtract), reads=[r_incl, r_Cs], writes=[r_pos])
                S.op("dve", lambda e: e.tensor_tensor(out=flat(pos), in0=pR[:], in1=flat(pos), op=ALU.add), reads=[r_pR, r_pos], writes=[r_pos])
                S.op("dve", lambda e: e.tensor_tensor(out=pos[:], in0=pos[:], in1=base[:].unsqueeze(1).broadcast_to(shp), op=ALU.add), reads=[r_pos, r_base], writes=[r_pos])
                S.op("dve", lambda e: e.scalar_tensor_tensor(out=v[:], in0=pos[:], scalar=1.0, in1=sel[:], op0=ALU.add, op1=ALU.mult), reads=[r_pos, r_sel], writes=[r_v])
                pA1, r_pA1 = sm("q_pA1", NTg)
                pB1, r_pB1 = sm("q_pB1", NTg)
                bc = lambda t: t[:].unsqueeze(2).broadcast_to(shp)
                S.op("dve", lambda e: e.tensor_reduce(out=pA1[:], in_=v[:], axis=AX.X, op=ALU.max), reads=[r_v], writes=[r_pA1])
                S.op("dve", lambda e: e.tensor_tensor(out=t8[:], in0=v[:], in1=bc(pA1), op=ALU.is_equal), reads=[r_v, r_pA1], writes=[r_t8])
                S.op("dve", lambda e: e.tensor_tensor(out=pos[:], in0=t8[:], in1=cb[:], op=ALU.mult), reads=[r_t8, r_cb], writes=[r_pos])
                S.op("dve", lambda e: e.tensor_reduce(out=wA[:], in_=pos[:], axis=AX.X, op=ALU.add), reads=[r_pos], writes=[r_wA])
                S.op("dve", lambda e: e.tensor_scalar(out=wB[:], in0=wA[:], scalar1=-1.0, scalar2=1.0, op0=ALU.mult, op1=ALU.add), reads=[r_wA], writes=[r_wB])
                S.op("dve", lambda e: e.tensor_tensor(out=t8[:], in0=t8[:], in1=v[:], op=ALU.mult), reads=[r_t8, r_v], writes=[r_t8])
                S.op("dve", lambda e: e.tensor_tensor(out=t8[:], in0=v[:], in1=t8[:], op=ALU.subtract), reads=[r_t8, r_v], writes=[r_t8])
                S.op("dve", lambda e: e.tensor_reduce(out=pB1[:], in_=t8[:], axis=AX.X, op=ALU.max), reads=[r_t8], writes=[r_pB1])
                S.op("dve", lambda e: e.tensor_scalar(out=posA[:], in0=pA1[:], scalar1=-1.0, scalar2=None, op0=ALU.add), reads=[r_pA1], writes=[r_posA])
                S.op("dve", lambda e: e.tensor_scalar(out=posB[:], in0=pB1[:], scalar1=-1.0, scalar2=None, op0=ALU.add), reads=[r_pB1], writes=[r_posB])
                S.op("dve", lambda e: e.tensor_tensor(out=cs[:], in0=base[:], in1=np_e[:], op=ALU.add), reads=[r_base, r_np], writes=[r_cs])
                jg, r_jg = tile1(ph, nc, "q_jg", [128, NSLOT, 8], F32)
                gp, r_gp = tile1(ph, nc, "q_gp", [128, NG], F32)
                S.dma("sp", lambda e: e.dma_start(out=jg[:], in_=self.cst_d["jgrid"].rearrange("p (j e) -> p j e", e=8)), writes=[r_jg])
                S.dma("sp", lambda e: e.dma_start(out=gp[:], in_=self.cst_d["gp"][:, :]), writes=[r_gp])
                S.op("dve", lambda e: e.tensor_tensor(out=jg[:], in0=cs[:].unsqueeze(1).broadcast_to([128, NSLOT, 8]), in1=jg[:], op=ALU.is_le), reads=[r_cs, r_jg], writes=[r_jg])
                eid, r_eid = sm("q_eid", NSLOT)
                S.op("dve", lambda e: e.tensor_reduce(out=eid[:], in_=jg[:], axis=AX.X, op=ALU.add), reads=[r_jg], writes=[r_eid])
                S.op("dve", lambda e: e.tensor_scalar(out=eid[:], in0=eid[:], scalar1=7.0, scalar2=None, op0=ALU.min), reads=[r_eid], writes=[r_eid])
                S.op("dve", lambda e: e.scalar_tensor_tensor(out=idxW[:], in0=eid[:].unsqueeze(2).broadcast_to([128, NSLOT, NG]), scalar=float(NG * 128), in1=gp[:].unsqueeze(1).broadcast_to([128, NSLOT, NG]), op0=ALU.mult, op1=ALU.add),
                     reads=[r_eid, r_gp], writes=[r_idxW])
                if cfg.DEBUG:
                    self.dump("posA", posA, r_posA, [128, NTg], I32)
                    self.dump("posB", posB, r_posB, [128, NTg], I32)
                    self.dump("wA", wA, r_wA, [128, NTg], F32)
                    self.dump("idxW", idxW, r_idxW, [128, NSLOT, NG], I32)
                    self.dump("eid", eid, r_eid, [128, NSLOT], F32)
                    self.dump("cs", cs, r_cs, [128, 8], F32)
                    self.dump("jg", jg, r_jg, [128, NSLOT, 8], F32)
                    self.dump("incl", incl, r_incl, shp, F32)
                    self.dump("Cs", Cs, r_Cs, shp, F32)
                    self.dump("np", np_e, r_np, [128, 8], F32)
                    self.dump("base", base, r_base, [128, 8], F32)
                self.end_phase()
                if cfg.STOP == "route":
                    return
            with ExitStack() as ph:
                hp = TPool(ph, nc, "q_h", [128, D], BF16, 3)
                for g in range(NTg):
                    ht, r_ht = hp.next()
                    S.dma("sp", lambda e, ht=ht, g=g: e.dma_start(out=ht[:], in_=self.H2[g * 128:(g + 1) * 128, :]), reads=[self.r_H2], writes=[r_ht])
                    for (pt, pr) in ((posA, r_posA), (posB, r_posB)):
                        S.dma("pool", lambda e, ht=ht, g=g, pt=pt: e.indirect_dma_start(out=self.HS, out_offset=bass.IndirectOffsetOnAxis(ap=pt[:, g:g + 1], axis=0), in_=ht[:], in_offset=None),
                              reads=[r_ht, pr], writes=[self.r_HS])
                self.end_phase()
                if cfg.STOP == "scatter":
                    return
            with ExitStack() as ph:
                hrp = TPool(ph, nc, "q_hr", [128, D], BF16, 2)
                hTp = TPool(ph, nc, "q_hT", [128, KC, 512], BF16, 2)
                wgp = TPool(ph, nc, "q_wg", [128, KC * 512], BF16, 2)
                wup = TPool(ph, nc, "q_wu", [128, KC * 512], BF16, 2)
                wdp = TPool(ph, nc, "q_wd", [128, 4 * D], BF16, 2)
                acc, r_acc = tile1(ph, nc, "q_acc", [128, 4, D], F32)
                actp = TPool(ph, nc, "q_act", [128, 4, 512], BF16, 2)
                sgp = TPool(ph, nc, "q_sg", [128, 512], F32, 2)
                psT = TPool(ph, nc, "q_psT", [128, KC, 128], BF16, 1, psum=True)
                psg = TPool(ph, nc, "q_psg", [128, 512], F32, 2, psum=True)
                psu = TPool(ph, nc, "q_psu", [128, 512], F32, 2, psum=True)
                pso = TPool(ph, nc, "q_pso", [128, 512], F32, 2, psum=True)
                def prep_slot(j):
                    hT, r_hT = hTp.next()
                    for tl in range(4):
                        hr, r_hr = hrp.next()
                        S.dma("sp", lambda e, hr=hr, j=j, tl=tl: e.dma_start(out=hr[:], in_=self.HS[j * 512 + tl * 128:j * 512 + (tl + 1) * 128, :]), reads=[self.r_HS], writes=[r_hr])
                        ps, r_ps = psT.next()
                        for kc in range(KC):
                            S.op("pe", lambda e, ps=ps, hr=hr, kc=kc: e.transpose(out=ps[:, kc, :], in_=hr[:, kc * 128:(kc + 1) * 128], identity=ident[:]),
                                 reads=[r_hr, r_ident], writes=[r_ps], signal=(kc == KC - 1))
                        S.op("act", lambda e, ps=ps, hT=hT, tl=tl: e.copy(out=hT[:, :, tl * 128:(tl + 1) * 128], in_=ps[:]), reads=[r_ps], writes=[r_hT])
                    return hT, r_hT

                nxt = prep_slot(0)
                for j in range(NSLOT):
                    hT, r_hT = nxt
                    for gr in range(NG):
                        if gr == max(NG - 2, 0) and j + 1 < NSLOT:
                            nxt = prep_slot(j + 1)
                        wts = []
                        for (pool_, src_) in ((wgp, self.WGB), (wup, self.WUB), (wdp, self.WDB)):
                            wt, r_wt = pool_.next()
                            S.dma("pool", lambda e, wt=wt, src_=src_, j=j, gr=gr: e.indirect_dma_start(out=wt[:], out_offset=None, in_=src_, in_offset=bass.IndirectOffsetOnAxis(ap=idxW[:, j, gr:gr + 1], axis=0)),
                                  reads=[r_idxW, self.r_WB], writes=[r_wt])
                            wts.append((wt, r_wt))
                        (wg_, r_wg), (wu_, r_wu), (wd_, r_wd) = wts
                        wg = wg_[:].rearrange("p (kc n) -> p kc n", n=512)
                        wu = wu_[:].rearrange("p (kc n) -> p kc n", n=512)
                        wd = wd_[:].rearrange("p (c n) -> p c n", n=D)
                        act, r_act = actp.next()
                        for c in range(4):
                            pg, r_pg = psg.next()
                            pu, r_pu = psu.next()
                            for kc in range(KC):
                                S.op("pe", lambda e, pg=pg, wg=wg, kc=kc, c=c, hT=hT: e.matmul(pg[:], lhsT=wg[:, kc, c * 128:(c + 1) * 128], rhs=hT[:, kc, :], start=(kc == 0), stop=(kc == KC - 1)),
                                     reads=[r_wg, r_hT], writes=[r_pg], signal=(kc == KC - 1))
                            for kc in range(KC):
                                S.op("pe", lambda e, pu=pu, wu=wu, kc=kc, c=c, hT=hT: e.matmul(pu[:], lhsT=wu[:, kc, c * 128:(c + 1) * 128], rhs=hT[:, kc, :], start=(kc == 0), stop=(kc == KC - 1)),
                                     reads=[r_wu, r_hT], writes=[r_pu], signal=(kc == KC - 1))
                            sg, r_sg = sgp.next()
                            S.op("act", lambda e, sg=sg, pg=pg: e.activation(out=sg[:], in_=pg[:], func=AF.Silu), reads=[r_pg], writes=[r_sg])
                            S.op("dve", lambda e, act=act, sg=sg, pu=pu, c=c: e.tensor_tensor(out=act[:, c, :], in0=sg[:], in1=pu[:], op=ALU.mult), reads=[r_sg, r_pu], writes=[r_act])
                        for tl in range(4):
                            for nb in range(4):
                                po, r_po = pso.next()
                                for c in range(4):
                                    S.op("pe", lambda e, po=po, act=act, wd=wd, c=c, tl=tl, nb=nb: e.matmul(po[:], lhsT=act[:, c, tl * 128:(tl + 1) * 128], rhs=wd[:, c, nb * 512:(nb + 1) * 512], start=(c == 0), stop=(c == 3)),
                                         reads=[r_act, r_wd], writes=[r_po], signal=(c == 3))
                                if gr == 0:
                                    S.op("act", lambda e, po=po, tl=tl, nb=nb: e.copy(out=acc[:, tl, nb * 512:(nb + 1) * 512], in_=po[:]), reads=[r_po], writes=[r_acc])
                                else:
                                    S.op("dve", lambda e, po=po, tl=tl, nb=nb: e.tensor_tensor(out=acc[:, tl, nb * 512:(nb + 1) * 512], in0=po[:], in1=acc[:, tl, nb * 512:(nb + 1) * 512], op=ALU.add), reads=[r_po, r_acc], writes=[r_acc])
                            if gr == NG - 1:
                                S.dma("sp", lambda e, j=j, tl=tl: e.dma_start(out=self.YP[j * 512 + tl * 128:j * 512 + (tl + 1) * 128, :], in_=acc[:, tl, :]), reads=[r_acc], writes=[self.r_YP])
                self.end_phase()
            if cfg.STOP == "slots":
                return
            with ExitStack() as ph:
                fn, r_fn = tile1(ph, nc, "z_fn", [128, D], F32)
                S.dma("sp", lambda e: e.dma_start(out=fn[:], in_=self.final_norm.unsqueeze(0).partition_broadcast(128)), writes=[r_fn])
                g2 = []
                for s in range(cfg.NSEQ):
                    g2.append(tile1(ph, nc, "z_g2%d" % s, [128, D], F32))
                    self.bcast_row(g2[s][0], g2[s][1], self.MOD[1, s:s + 1, 5 * D:6 * D], D)
                xp = TPool(ph, nc, "z_x", [128, D], F32, 2)
                yap = TPool(ph, nc, "z_ya", [128, D], F32, 2)
                ybp = TPool(ph, nc, "z_yb", [128, D], F32, 2)
                op_ = TPool(ph, nc, "z_o", [128, D], F32, 2)
                junk, r_junk = tile1(ph, nc, "z_junk", [128, D], BF16)
                stp = TPool(ph, nc, "z_st", [128, 4], F32, 2)
                for g in range(NTg):
                    s, lt = g // NTl, g % NTl
                    tile = NCT + lt
                    xt, r_xt = xp.next()
                    S.dma("sp", lambda e, xt=xt, s=s, tile=tile: e.dma_start(out=xt[:], in_=self.XR[s, tile * 128:(tile + 1) * 128, :]), reads=[self.r_XR[s][tile]], writes=[r_xt])
                    ya, r_ya = yap.next()
                    yb, r_yb = ybp.next()
                    for (yt, r_yt, pt, pr) in ((ya, r_ya, posA, r_posA), (yb, r_yb, posB, r_posB)):
                        S.dma("pool", lambda e, yt=yt, pt=pt, g=g: e.indirect_dma_start(out=yt[:], out_offset=None, in_=self.YP, in_offset=bass.IndirectOffsetOnAxis(ap=pt[:, g:g + 1], axis=0)),
                              reads=[self.r_YP, pr], writes=[r_yt])
                    S.op("dve", lambda e, ya=ya, g=g: e.tensor_scalar(out=ya[:], in0=ya[:], scalar1=wA[:, g:g + 1], scalar2=None, op0=ALU.mult), reads=[r_ya, r_wA], writes=[r_ya])
                    S.op("dve", lambda e, ya=ya, yb=yb, g=g: e.scalar_tensor_tensor(out=ya[:], in0=yb[:], scalar=wB[:, g:g + 1], in1=ya[:], op0=ALU.mult, op1=ALU.add), reads=[r_ya, r_yb, r_wB], writes=[r_ya])
                    S.op("dve", lambda e, ya=ya, s=s: e.tensor_tensor(out=ya[:], in0=ya[:], in1=g2[s][0][:], op=ALU.mult), reads=[r_ya, g2[s][1]], writes=[r_ya])
                    S.op("dve", lambda e, ya=ya, xt=xt: e.tensor_tensor(out=xt[:], in0=ya[:], in1=xt[:], op=ALU.add), reads=[r_ya, r_xt], writes=[r_xt])
                    st, r_st = stp.next()
                    S.op("act", lambda e, xt=xt, st=st: e.activation(out=junk[:], in_=xt[:], func=AF.Square, accum_out=st[:, 0:1]), reads=[r_xt], writes=[r_junk, r_st])
                    S.op("dve", lambda e, st=st: e.tensor_scalar(out=st[:, 1:2], in0=st[:, 0:1], scalar1=1.0 / D, scalar2=EPS, op0=ALU.mult, op1=ALU.add), reads=[r_st], writes=[r_st])
                    S.op("act", lambda e, st=st: e.activation(out=st[:, 2:3], in_=st[:, 1:2], func=AF.Sqrt), reads=[r_st], writes=[r_st])
                    S.op("dve", lambda e, st=st: e.reciprocal(out=st[:, 3:4], in_=st[:, 2:3]), reads=[r_st], writes=[r_st])
                    ot, r_ot = op_.next()
                    S.op("dve", lambda e, ot=ot, xt=xt, st=st: e.scalar_tensor_tensor(out=ot[:], in0=xt[:], scalar=st[:, 3:4], in1=fn[:], op0=ALU.mult, op1=ALU.mult), reads=[r_xt, r_st, r_fn], writes=[r_ot])
                    S.dma("sp", lambda e, ot=ot, s=s, lt=lt: e.dma_start(out=self.out[s, lt * 128:(lt + 1) * 128, :], in_=ot[:]), reads=[r_ot], writes=[self.r_out])
                self.end_phase()


def make_in_maps(cfg, inputs, ncores):
    consts = host_consts(cfg)
    maps = []
    for core in range(ncores):
        b0 = core * cfg.NSEQ
        m = {}
        m["x_in"] = np.ascontiguousarray(inputs["x"][b0:b0 + cfg.NSEQ])
        m["ctx_in"] = np.ascontiguousarray(inputs["ctx"][b0:b0 + cfg.NSEQ])
        m["cvec"] = np.ascontiguousarray(np.concatenate([inputs["c"][b0:b0 + cfg.NSEQ], inputs["c_ctx"][None, :]], 0))
        for k in ("w_mod", "b_mod", "norm_mix", "norm_ffn", "w_in", "hg_lb_fwd", "hg_lb_bwd", "hg_norm", "attn_sink",
                  "w_branch_a", "w_branch_b", "w_out", "ffn_w_gate", "ffn_w_up", "ffn_w_down", "moe_router",
                  "moe_w_gate", "moe_w_up", "moe_w_down", "final_norm"):
            m[k] = inputs[k]
        for k, v in consts.items():
            m["c_" + k] = v
        maps.append(m)
    return maps


_CACHE = {}


def kernel(**inputs):
    cfg = Cfg()
    inputs = {k: np.asarray(v) for k, v in inputs.items()}
    if "nc" not in _CACHE:
        _CACHE["nc"] = K(cfg).build()
    nc = _CACHE["nc"]
    ncores = 16 // cfg.NSEQ
    maps = make_in_maps(cfg, inputs, ncores)
    res = run_bass_kernel_spmd(nc, maps, core_ids=list(range(ncores)))
    out = np.concatenate([np.asarray(r["out"]) for r in res.results], axis=0)
    return out.astype(np.float32, copy=False)
```

```python
import numpy as np
from contextlib import ExitStack
import concourse.bass as bass
import concourse.mybir as mybir
from concourse.bass_utils import run_bass_kernel_spmd

F32 = mybir.dt.float32
BF16 = mybir.dt.bfloat16
I32 = mybir.dt.int32
AF = mybir.ActivationFunctionType
ALU = mybir.AluOpType
AX = mybir.AxisListType

EPS = 1e-6
NEG = -30000.0


class Res:
    __slots__ = ("name", "last_w", "readers")

    def __init__(self, name=""):
        self.name = name
        self.last_w = None
        self.readers = {}


class Sched:
    COMPUTE = ("pe", "act", "dve", "pool")
    NDMA = 8

    def __init__(self, nc, es):
        self.nc = nc
        self.sem = {}
        for e in self.COMPUTE:
            self.sem[e] = es.enter_context(nc.semaphore("sem_" + e))
        self.count = {e: 0 for e in self.COMPUTE}
        self.engs = ("pe", "act", "dve", "pool", "sp")
        self.seen = {e: {} for e in self.engs}
        self.q = {e: [] for e in self.engs}
        self.dma_uses = {}
        self.dma_i = {}
        for e in ("sp", "act", "pool"):
            for i in range(self.NDMA):
                self.sem[("d", e, i)] = es.enter_context(nc.semaphore("dsem_%s%d" % (e, i)))
            self.dma_uses[e] = [0] * self.NDMA
            self.dma_i[e] = 0
        self.ninstr = 0

    def _deps(self, reads, writes):
        deps = {}
        for r in reads:
            ev = r.last_w
            if ev is not None and deps.get(ev[0], 0) < ev[1]:
                deps[ev[0]] = ev[1]
        for w in writes:
            ev = w.last_w
            if ev is not None and deps.get(ev[0], 0) < ev[1]:
                deps[ev[0]] = ev[1]
            for k, v in w.readers.items():
                if deps.get(k, 0) < v:
                    deps[k] = v
        return deps

    def _waits(self, eng, deps, skip=None):
        waits = []
        seen = self.seen[eng]
        for k, v in deps.items():
            if k == skip or seen.get(k, 0) >= v:
                continue
            seen[k] = v
            waits.append((k, v))
        return waits

    def _commit(self, ev, reads, writes):
        k, v = ev
        for r in reads:
            if r.readers.get(k, 0) < v:
                r.readers[k] = v
        for w in writes:
            w.last_w = ev
            w.readers = {}

    def op(self, eng, fn, reads=(), writes=(), signal=True):
        deps = self._deps(reads, writes)
        waits = self._waits(eng, deps, skip=("pe" if eng == "pe" else None))
        sem = self.sem
        if signal:
            self.count[eng] += 1
            ev = (eng, self.count[eng])
            own = sem[eng]
        else:
            ev = (eng, self.count[eng] + 1)
            own = None

        def run(e, waits=waits, fn=fn, own=own):
            for k, v in waits:
                e.wait_ge(sem[k], v)
            ins = fn(e)
            if own is not None:
                ins.then_inc(own, 1)
        self.q[eng].append(run)
        self._commit(ev, reads, writes)
        self.ninstr += 1
        return ev

    def dma(self, eng, fn, reads=(), writes=()):
        i = self.dma_i[eng] % self.NDMA
        self.dma_i[eng] += 1
        key = ("d", eng, i)
        prev = 16 * self.dma_uses[eng][i]
        self.dma_uses[eng][i] += 1
        ev = (key, prev + 16)
        deps = self._deps(reads, writes)
        if prev > 0 and deps.get(key, 0) < prev:
            deps[key] = prev
        waits = self._waits(eng, deps)
        sem = self.sem
        own = sem[key]

        def run(e, waits=waits, fn=fn, own=own):
            for k, v in waits:
                e.wait_ge(sem[k], v)
            fn(e).then_inc(own, 16)
        self.q[eng].append(run)
        self._commit(ev, reads, writes)
        self.ninstr += 1
        return ev

    def barrier(self):
        targets = {}
        for e in self.COMPUTE:
            if self.count[e] > 0:
                targets[e] = self.count[e]
        for e in ("sp", "act", "pool"):
            for i in range(self.NDMA):
                if self.dma_uses[e][i] > 0:
                    targets[("d", e, i)] = 16 * self.dma_uses[e][i]
        sem = self.sem
        for eng in self.engs:
            waits = self._waits(eng, targets, skip=(eng if eng in self.COMPUTE else None))
            if waits:
                def run(e, waits=waits):
                    for k, v in waits:
                        e.wait_ge(sem[k], v)
                self.q[eng].append(run)

    def flush(self):
        nc = self.nc
        q = self.q
        with nc.Block() as block:
            if q["pe"]:
                @block.tensor
                def _(e):
                    for f in q["pe"]:
                        f(e)
            if q["act"]:
                @block.scalar
                def _(e):
                    for f in q["act"]:
                        f(e)
            if q["dve"]:
                @block.vector
                def _(e):
                    for f in q["dve"]:
                        f(e)
            if q["pool"]:
                @block.gpsimd
                def _(e):
                    for f in q["pool"]:
                        f(e)
            if q["sp"]:
                @block.sync
                def _(e):
                    for f in q["sp"]:
                        f(e)
        self.q = {e: [] for e in self.engs}


_uid = [0]


class TPool:
    def __init__(self, es, nc, name, shape, dtype, n, psum=False):
        self.tiles = []
        for i in range(n):
            _uid[0] += 1
            nm = "%s_%d" % (name, _uid[0])
            mk = nc.psum_tensor if psum else nc.sbuf_tensor
            t = es.enter_context(mk(nm, list(shape), dtype))
            self.tiles.append((t, Res(nm)))
        self.i = 0

    def next(self):
        t = self.tiles[self.i % len(self.tiles)]
        self.i += 1
        return t


def tile1(es, nc, name, shape, dtype, psum=False):
    return TPool(es, nc, name, shape, dtype, 1, psum).tiles[0]


class Cfg:
    def __init__(self, NSEQ=2, SEQ=2048, CTX=256, DFF=5632, DEXP=7168, MOE="routed", DEBUG=False, STOP=None):
        self.DEBUG = DEBUG
        self.STOP = STOP
        self.D = 2048
        self.KC = 16
        self.NSEQ = NSEQ
        self.NR = NSEQ + 1
        self.SEQ = SEQ
        self.CTX = CTX
        self.T = CTX + SEQ
        self.NT = self.T // 128
        self.NCT = CTX // 128
        self.DFF = DFF
        self.DEXP = DEXP
        self.NE = 8
        self.NIN = 10496
        self.NCH = self.T // 64
        self.NCC = CTX // 64
        self.MOE = MOE
        self.PUMP = 7
        self.NSLOT = (2 * NSEQ * SEQ) // 512 + 7
        self.tblocks = [(0, CTX)] + [(CTX + i * 512, 512) for i in range(SEQ // 512)]
        self.lat_blocks = self.tblocks[1:]
        self.groups = [list(range(0, self.NCC))] + [list(range(self.NCC + 8 * i, self.NCC + 8 * i + 8))
                                                    for i in range((SEQ // 64) // 8)]


COL = dict(hq=0, ff=1024, fb=2048, hv=3072, hg=4096, aq=5120, ak=6144, av=6272, ga=6400, gb=8448)


def host_consts(cfg):
    c = {}
    c["ident"] = np.eye(128, dtype=np.float32)
    i = np.arange(64)
    sb_, tb_ = i[:, None] // 16, i[None, :] // 16
    c["maskD_f"] = ((i[:, None] <= i[None, :]) & (sb_ == tb_)).astype(np.float32)
    c["maskO_f"] = (sb_ < tb_).astype(np.float32)
    c["maskD_b"] = ((i[:, None] >= i[None, :]) & (sb_ == tb_)).astype(np.float32)
    c["maskO_b"] = (sb_ > tb_).astype(np.float32)
    j = np.arange(128)
    c["mneg_prev"] = np.where(j[None, :] <= j[:, None], 0.0, NEG).astype(np.float32)
    c["mneg_next"] = np.where(j[:, None] <= j[None, :], 0.0, NEG).astype(np.float32)
    c["ustrict"] = (j[:, None] < j[None, :]).astype(np.float32)
    t = np.arange(cfg.SEQ)
    row = (t // 64).astype(np.float32)
    col = (t % 64).astype(np.float32)
    inv = (10000.0 ** (-np.arange(16, dtype=np.float32) / 16)).astype(np.float32)
    d = np.arange(64)
    axis = d // 32
    freq = d % 16
    second = (d % 32) >= 16
    pos = np.where(axis[:, None] == 0, row[None, :], col[None, :]).astype(np.float32)
    ang = (pos * inv[freq][:, None]).astype(np.float32)
    cos = np.cos(ang).astype(np.float32)
    sin = np.sin(ang).astype(np.float32)
    sin_s = np.where(second[:, None], sin, -sin).astype(np.float32)
    c["cosT"] = np.concatenate([cos, cos], 0)
    c["sinT"] = np.concatenate([sin_s, sin_s], 0)
    pm = np.zeros((128, 128), np.float32)
    for m in range(128):
        dd = m % 64
        partner = dd - 16 if (dd % 32) >= 16 else dd + 16
        pm[(m // 64) * 64 + partner, m] = 1.0
    c["pm"] = pm
    c["ones"] = np.ones((128, 128), np.float32)
    NG = cfg.DEXP // 512
    c["jgrid"] = np.broadcast_to(np.repeat(512.0 * np.arange(cfg.NSLOT, dtype=np.float32), 8)[None, :], (128, cfg.NSLOT * 8)).copy()
    KMAX = max(1, (cfg.NSEQ * cfg.SEQ) // 512)
    c["kgrid"] = np.broadcast_to(np.tile(512.0 * np.arange(KMAX, dtype=np.float32), 8)[None, :], (128, 8 * KMAX)).copy()
    c["gp"] = (np.arange(NG, dtype=np.float32)[None, :] * 128 + np.arange(128, dtype=np.float32)[:, None]).astype(np.float32)
    return c


CONST_SHAPES = lambda cfg: dict(ident=[128, 128], maskD_f=[64, 64], maskO_f=[64, 64], maskD_b=[64, 64], maskO_b=[64, 64], mneg_prev=[128, 128],
                                mneg_next=[128, 128], ustrict=[128, 128], cosT=[128, cfg.SEQ],
                                sinT=[128, cfg.SEQ], pm=[128, 128], ones=[128, 128], jgrid=[128, cfg.NSLOT * 8], gp=[128, cfg.DEXP // 512], kgrid=[128, 8 * max(1, (cfg.NSEQ * cfg.SEQ) // 512)])


class K:
    def __init__(self, cfg):
        self.cfg = cfg
        nc = self.nc = bass.Bass("TRN2", target_bir_lowering=False)
        D = cfg.D
        T = cfg.T
        def din(name, shape, dt=F32):
            return nc.dram_tensor(name, list(shape), dt, kind="ExternalInput").ap()
        def scr(name, shape, dt):
            return nc.dram_tensor(name, list(shape), dt, kind=("ExternalOutput" if cfg.DEBUG else "Internal")).ap()
        self.x_in = din("x_in", [cfg.NSEQ, cfg.SEQ, D])
        self.ctx_in = din("ctx_in", [cfg.NSEQ, cfg.CTX, D])
        self.cvec = din("cvec", [cfg.NR, D])
        self.w_mod = din("w_mod", [2, D, 6 * D])
        self.b_mod = din("b_mod", [2, 6 * D])
        self.norm_mix = din("norm_mix", [2, D])
        self.norm_ffn = din("norm_ffn", [2, D])
        self.w_in = din("w_in", [2, D, cfg.NIN])
        self.lb_f = din("hg_lb_fwd", [2, 1024])
        self.lb_b = din("hg_lb_bwd", [2, 1024])
        self.hg_norm = din("hg_norm", [2, 1024])
        self.sink = din("attn_sink", [2, 16])
        self.w_a = din("w_branch_a", [2, 1024, D])
        self.w_b = din("w_branch_b", [2, 1024, D])
        self.w_o = din("w_out", [2, D, D])
        self.ffn_wg = din("ffn_w_gate", [1, D, cfg.DFF])
        self.ffn_wu = din("ffn_w_up", [1, D, cfg.DFF])
        self.ffn_wd = din("ffn_w_down", [1, cfg.DFF, D])
        self.router = din("moe_router", [1, D, 8])
        self.moe_wg = din("moe_w_gate", [1, 8, D, cfg.DEXP])
        self.moe_wu = din("moe_w_up", [1, 8, D, cfg.DEXP])
        self.moe_wd = din("moe_w_down", [1, 8, cfg.DEXP, D])
        self.final_norm = din("final_norm", [D])
        self.cst_d = {k: din("c_" + k, s) for k, s in CONST_SHAPES(cfg).items()}
        self.out = nc.dram_tensor("out", [cfg.NSEQ, cfg.SEQ, D], F32, kind="ExternalOutput").ap()
        self.MOD = scr("MOD", [2, cfg.NR, 6 * D], F32)
        self.XR = scr("XR", [cfg.NSEQ, T, D], F32)
        self.QT = scr("QT", [8, 128, T], BF16)
        self.GT = scr("GT", [2, 8, 128, T], F32)
        self.KT = scr("KT", [2, 8, 128, T], BF16)
        self.SGT = scr("SGT", [8, 128, T], BF16)
        self.V = scr("V", [T, 1024], BF16)
        self.QA = scr("QA", [8, 128, T], BF16)
        self.KA = scr("KA", [2, 128, T], BF16)
        self.VA = scr("VA", [T, 128], BF16)
        self.GA = scr("GA", [16, 128, T], BF16)
        self.GB = scr("GB", [16, 128, T], BF16)
        self.AT = scr("AT", [8, 128, T], BF16)
        self.OAT = scr("OAT", [8, 128, T], BF16)
        NG = cfg.DEXP // 512
        NTg = cfg.NSEQ * cfg.SEQ // 128
        if cfg.MOE == "routed":
            self.H2 = scr("H2", [cfg.NSEQ * cfg.SEQ, D], BF16)
            self.HS = scr("HS", [cfg.NSLOT * 512, D], BF16)
            self.YP = scr("YP", [cfg.NSLOT * 512, D], F32)
            self.SELD = scr("SELD", [128, NTg, 8], F32)
            self.CBD = scr("CBD", [128, NTg, 8], F32)
            self.WGB = scr("WGB", [8 * NG * 128, 16 * 512], BF16)
            self.WUB = scr("WUB", [8 * NG * 128, 16 * 512], BF16)
            self.WDB = scr("WDB", [8 * NG * 128, 4 * D], BF16)
        self.r_H2 = Res(); self.r_HS = Res(); self.r_YP = Res(); self.r_SELD = Res(); self.r_WB = Res()
        self.pre_jobs = self.prepass_jobs() if cfg.MOE == "routed" else []
        self.pump_n = 0
        self.dbg = {}
        self.r_MOD = Res('MOD')
        self.r_out = Res('out')
        self.r_QT = [Res() for _ in range(8)]
        self.r_SGT = [Res() for _ in range(8)]
        self.r_GA = [Res() for _ in range(16)]
        self.r_GB = [Res() for _ in range(16)]
        self.r_GT = [[Res() for _ in range(8)] for _ in range(2)]
        self.r_KT = [[Res() for _ in range(8)] for _ in range(2)]
        self.r_QA = [Res() for _ in range(8)]
        self.r_KA = [Res() for _ in range(2)]
        self.r_V = Res()
        self.r_VA = Res()
        self.r_AT = [Res() for _ in range(8)]
        self.r_OAT = [Res() for _ in range(8)]
        self.r_XR = [[Res('XR') for _ in range(cfg.NT)] for _ in range(cfg.NSEQ)]

    def xsrc(self, first, s, tile):
        cfg = self.cfg
        if first:
            if tile < cfg.NCT:
                return self.ctx_in[s, tile * 128:(tile + 1) * 128, :]
            tt = tile - cfg.NCT
            return self.x_in[s, tt * 128:(tt + 1) * 128, :]
        return self.XR[s, tile * 128:(tile + 1) * 128, :]

    def load_wblock(self, pool, w2d, n0, nn, nk=None, k0=0, eng="pool"):
        S = self.S
        nk = nk if nk is not None else w2d.shape[0] // 128
        t, r = pool.next()
        src = w2d[k0 * 128:(k0 + nk) * 128, n0:n0 + nn].rearrange("(kc p) n -> p kc n", p=128)
        S.dma(eng, lambda e: e.dma_start(out=t[:, 0:nk, 0:nn], in_=src), writes=[r])
        if self.pump_n:
            self.pump(self.pump_n)
        return t, r

    def bcast_row(self, t, r, row_ap, n, eng="sp"):
        self.S.dma(eng, lambda e: e.dma_start(out=t[:, 0:n], in_=row_ap.partition_broadcast(128)), writes=[r])

    def build(self):
        cfg = self.cfg
        nc = self.nc
        with ExitStack() as es:
            self.S = S = Sched(nc, es)
            es.enter_context(nc.allow_non_contiguous_dma(reason='small strided layout loads'))
            self.C = {}
            for k in ("ident", "mneg_prev", "mneg_next", "ustrict", "pm", "ones"):
                shp = CONST_SHAPES(cfg)[k]
                t, r = tile1(es, nc, "c_" + k, shp, BF16)
                S.dma("pool", lambda e, t=t, k=k: e.dma_start(out=t[:], in_=self.cst_d[k][:, :]), writes=[r])
                self.C[k] = (t, r)
            stop = cfg.STOP
            self.phase_mod()
            if stop == "mod":
                return nc
            for l in range(2):
                for s in range(cfg.NSEQ):
                    with ExitStack() as bs:
                        self.BIG = tile1(bs, nc, "BIG", [128, cfg.KC, cfg.T], BF16)
                        self.phase_norm(l, s, 1)
                        if stop == "norm":
                            self.dump("BIG", self.BIG[0], self.BIG[1], [128, cfg.KC, cfg.T], BF16)
                            self.end_phase()
                            return nc
                        self.pump_n = 3
                        self.phase_inproj(l, s)
                        self.pump_n = 0
                    if stop == "inproj":
                        return nc
                    self.phase_scan(l, s)
                    if stop in ("scan", "scanprep"):
                        return nc
                    self.phase_attn(l, s)
                    if stop == "attn":
                        return nc
                    with ExitStack() as bs:
                        self.BIG = tile1(bs, nc, "BIG", [128, cfg.KC, cfg.T], BF16)
                        self.phase_merge(l, s)
                        if stop == "merge":
                            return nc
                        self.phase_norm(l, s, 2)
                        if l == 1 and cfg.MOE == "routed":
                            self.route_local(s)
                        else:
                            self.phase_swiglu(l, s)
                    if stop == "ffn":
                        return nc
                if stop == "layer0":
                    return nc
            if cfg.MOE == "routed":
                self.phase_moe_routed()
            else:
                self.phase_final()
        return nc

    def dump(self, name, tile, r, shape, dt):
        if not self.cfg.DEBUG:
            return
        d = self.nc.dram_tensor("dbg_" + name, list(shape), dt, kind="ExternalOutput").ap()
        self.S.dma("sp", lambda e: e.dma_start(out=d, in_=tile[:]), reads=[r])

    def end_phase(self):
        self.S.barrier()
        self.S.flush()

    def phase_mod(self):
        cfg, nc, S = self.cfg, self.nc, self.S
        D, KC, NR = cfg.D, cfg.KC, cfg.NR
        with ExitStack() as ph:
            if cfg.MOE == "routed":
                zt, r_zt = tile1(ph, nc, "q_z", [128, 4, D], BF16)
                S.op("dve", lambda e: e.memset(zt[:], 0.0), writes=[r_zt])
                for j in range(cfg.NSLOT):
                    S.dma("sp", lambda e, j=j: e.dma_start(out=self.HS[j * 512:(j + 1) * 512, :].rearrange("(a p) d -> p a d", p=128), in_=zt[:]), reads=[r_zt])
            cT, r_cT = tile1(ph, nc, "cT", [128, KC, NR], F32)
            cTb, r_cTb = tile1(ph, nc, "cTb", [128, KC, NR], BF16)
            for r in range(NR):
                src = self.cvec[r:r + 1, :].rearrange("o (kc p) -> p kc o", p=128)
                S.dma("sp", lambda e, src=src, r=r: e.dma_start(out=cT[:, :, r:r + 1], in_=src), writes=[r_cT])
            S.op("act", lambda e: e.activation(out=cTb[:], in_=cT[:], func=AF.Silu), reads=[r_cT], writes=[r_cTb])
            wpool = TPool(ph, nc, "wmod", [128, KC, 512], BF16, 3)
            pspool = TPool(ph, nc, "psmod", [NR, 512], F32, 2, psum=True)
            bias, r_bias = tile1(ph, nc, "bmod", [NR, 6 * D], F32)
            res, r_res = tile1(ph, nc, "resmod", [NR, 6 * D], F32)
            for l in range(2):
                S.dma("sp", lambda e, l=l: e.dma_start(out=bias[:], in_=self.b_mod[l:l + 1, :].partition_broadcast(NR)), writes=[r_bias])
                nb = 6 * D // 512
                for b in range(nb):
                    w, r_w = self.load_wblock(wpool, self.w_mod[l], b * 512, 512)
                    ps, r_ps = pspool.next()
                    for kc in range(KC):
                        S.op("pe", lambda e, ps=ps, w=w, kc=kc: e.matmul(ps[:], lhsT=cTb[:, kc, :], rhs=w[:, kc, :], start=(kc == 0), stop=(kc == KC - 1)),
                             reads=[r_cTb, r_w], writes=[r_ps], signal=(kc == KC - 1))
                    S.op("dve", lambda e, ps=ps, b=b: e.tensor_tensor(out=res[:, b * 512:(b + 1) * 512], in0=ps[:], in1=bias[:, b * 512:(b + 1) * 512], op=ALU.add),
                         reads=[r_ps, r_bias], writes=[r_res])
                S.dma("sp", lambda e, l=l: e.dma_start(out=self.MOD[l], in_=res[:]), reads=[r_res], writes=[self.r_MOD])
            self.end_phase()

    def phase_norm(self, l, s, which):
        cfg, nc, S = self.cfg, self.nc, self.S
        D, KC = cfg.D, cfg.KC
        first = (l == 0 and which == 1)
        nw = self.norm_mix if which == 1 else self.norm_ffn
        base = 0 if which == 1 else 3
        BIG, r_BIG = self.BIG
        ident, r_ident = self.C["ident"]
        tiles = list(range(cfg.NT))
        if which == 2 and l == 1:
            tiles = list(range(cfg.NCT, cfg.NT))
        with ExitStack() as ph:
            gm = {}
            sh = {}
            for kind, row in (("lat", s), ("ctx", cfg.NSEQ)):
                if kind == "ctx" and which == 2 and l == 1:
                    continue
                g_t, g_r = tile1(ph, nc, "gm" + kind, [128, D], F32)
                s_t, s_r = tile1(ph, nc, "sh" + kind, [128, D], F32)
                n_t, n_r = tile1(ph, nc, "nw" + kind, [128, D], F32)
                self.S.dma("sp", lambda e, n_t=n_t: e.dma_start(out=n_t[:], in_=nw[l:l + 1, :].partition_broadcast(128)), writes=[n_r])
                self.S.dma("sp", lambda e, g_t=g_t, row=row: e.dma_start(out=g_t[:], in_=self.MOD[l, row:row + 1, (base + 1) * D:(base + 2) * D].partition_broadcast(128)), reads=[self.r_MOD], writes=[g_r])
                self.S.dma("sp", lambda e, s_t=s_t, row=row: e.dma_start(out=s_t[:], in_=self.MOD[l, row:row + 1, base * D:(base + 1) * D].partition_broadcast(128)), reads=[self.r_MOD], writes=[s_r])
                S.op("dve", lambda e, g_t=g_t, n_t=n_t: e.scalar_tensor_tensor(out=g_t[:], in0=g_t[:], scalar=1.0, in1=n_t[:], op0=ALU.add, op1=ALU.mult),
                     reads=[g_r, n_r], writes=[g_r])
                gm[kind] = (g_t, g_r)
                sh[kind] = (s_t, s_r)
            xpool = TPool(ph, nc, "xt", [128, D], F32, 2)
            junk, r_junk = tile1(ph, nc, "junk", [128, D], BF16)
            t1pool = TPool(ph, nc, "t1", [128, D], F32, 1)
            hpool = TPool(ph, nc, "hb", [128, D], BF16, 2)
            stpool = TPool(ph, nc, "st", [128, 4], F32, 2)
            pspool = TPool(ph, nc, "pT", [128, KC, 128], BF16, 2, psum=True)
            loaded = {}

            def load(i):
                tile = tiles[i]
                xt, r_xt = xpool.next()
                src = self.xsrc(first, s, tile)
                S.dma("sp", lambda e, xt=xt, src=src: e.dma_start(out=xt[:], in_=src), reads=[self.r_XR[s][tile]], writes=[r_xt])
                loaded[i] = (xt, r_xt)

            load(0)
            pend = None
            for i, tile in enumerate(tiles):
                if i + 1 < len(tiles):
                    load(i + 1)
                xt, r_xt = loaded.pop(i)
                kind = "ctx" if tile < cfg.NCT else "lat"
                g_t, g_r = gm[kind]
                s_t, s_r = sh[kind]
                st, r_st = stpool.next()
                S.op("act", lambda e, xt=xt, st=st: e.activation(out=junk[:], in_=xt[:], func=AF.Square, accum_out=st[:, 0:1]),
                     reads=[r_xt], writes=[r_junk, r_st])
                t1, r_t1 = t1pool.next()
                S.op("dve", lambda e, t1=t1, xt=xt, g_t=g_t: e.tensor_tensor(out=t1[:], in0=xt[:], in1=g_t[:], op=ALU.mult),
                     reads=[r_xt, g_r], writes=[r_t1])
                S.op("dve", lambda e, st=st: e.tensor_scalar(out=st[:, 1:2], in0=st[:, 0:1], scalar1=1.0 / D, scalar2=EPS, op0=ALU.mult, op1=ALU.add),
                     reads=[r_st], writes=[r_st])
                S.op("act", lambda e, st=st: e.activation(out=st[:, 2:3], in_=st[:, 1:2], func=AF.Sqrt), reads=[r_st], writes=[r_st])
                S.op("dve", lambda e, st=st: e.reciprocal(out=st[:, 3:4], in_=st[:, 2:3]), reads=[r_st], writes=[r_st])
                hb, r_hb = hpool.next()
                S.op("dve", lambda e, hb=hb, t1=t1, st=st, s_t=s_t: e.scalar_tensor_tensor(out=hb[:], in0=t1[:], scalar=st[:, 3:4], in1=s_t[:], op0=ALU.mult, op1=ALU.add),
                     reads=[r_t1, r_st, s_r], writes=[r_hb])
                if which == 2 and l == 1 and cfg.MOE == "routed":
                    g_ = s * (cfg.SEQ // 128) + (tile - cfg.NCT)
                    S.dma("sp", lambda e, hb=hb, g_=g_: e.dma_start(out=self.H2[g_ * 128:(g_ + 1) * 128, :], in_=hb[:]), reads=[r_hb], writes=[self.r_H2])
                def back(hb=hb, r_hb=r_hb, tile=tile):
                    ps, r_ps = pspool.next()
                    for kc in range(KC):
                        S.op("pe", lambda e, ps=ps, hb=hb, kc=kc: e.transpose(out=ps[:, kc, :], in_=hb[:, kc * 128:(kc + 1) * 128], identity=ident[:]),
                             reads=[r_hb, r_ident], writes=[r_ps], signal=(kc == KC - 1))
                    S.op("act", lambda e, ps=ps, tile=tile: e.copy(out=BIG[:, :, tile * 128:(tile + 1) * 128], in_=ps[:]),
                         reads=[r_ps], writes=[r_BIG])
                if pend is not None:
                    pend()
                pend = back
            if pend is not None:
                pend()
            self.end_phase()

    def phase_inproj(self, l, s):
        cfg, nc, S = self.cfg, self.nc, self.S
        D, KC, T = cfg.D, cfg.KC, cfg.T
        BIG, r_BIG = self.BIG
        CTX, SEQ = cfg.CTX, cfg.SEQ
        pm, r_pm = self.C["pm"]
        w_in = self.w_in[l]
        with ExitStack() as ph:
            wpool = TPool(ph, nc, "win", [128, KC, 512], BF16, 3)
            pspool = TPool(ph, nc, "psin", [128, 512], F32, 4, psum=True)
            psrot = TPool(ph, nc, "psrot", [128, 512], F32, 2, psum=True)
            stb = TPool(ph, nc, "stb", [128, T], BF16, 4)
            stf = TPool(ph, nc, "stf", [128, T], F32, 2)
            tmpf = TPool(ph, nc, "tmpf", [128, 512], F32, 6)
            cosT, r_cos = tile1(ph, nc, "cosT", [128, SEQ], F32)
            sinT, r_sin = tile1(ph, nc, "sinT", [128, SEQ], F32)
            S.dma("sp", lambda e: e.dma_start(out=cosT[:], in_=self.cst_d["cosT"][:, :]), writes=[r_cos])
            S.dma("sp", lambda e: e.dma_start(out=sinT[:], in_=self.cst_d["sinT"][:, :]), writes=[r_sin])
            lbv, oml = [], []
            for di, lbsrc in enumerate((self.lb_f, self.lb_b)):
                lb_t, lb_r = tile1(ph, nc, "lb%d" % di, [128, 8], F32)
                om_t, om_r = tile1(ph, nc, "oml%d" % di, [128, 8], F32)
                if l == 0:
                    S.op("dve", lambda e, lb_t=lb_t: e.memset(lb_t[:], 0.0), writes=[lb_r])
                else:
                    r0_t, r0_r = tile1(ph, nc, "lr0%d" % di, [128, 8], F32)
                    r1_t, r1_r = tile1(ph, nc, "lr1%d" % di, [128, 8], F32)
                    S.dma("sp", lambda e, r0_t=r0_t, lbsrc=lbsrc: e.dma_start(out=r0_t[:], in_=lbsrc[0:1, :].rearrange("o (h p) -> p (o h)", p=128)), writes=[r0_r])
                    S.dma("sp", lambda e, r1_t=r1_t, lbsrc=lbsrc: e.dma_start(out=r1_t[:], in_=lbsrc[1:2, :].rearrange("o (h p) -> p (o h)", p=128)), writes=[r1_r])
                    S.op("dve", lambda e, r0_t=r0_t, r1_t=r1_t: e.tensor_tensor(out=r0_t[:], in0=r0_t[:], in1=r1_t[:], op=ALU.subtract), reads=[r0_r, r1_r], writes=[r0_r])
                    S.op("act", lambda e, r0_t=r0_t: e.activation(out=r0_t[:], in_=r0_t[:], func=AF.Exp), reads=[r0_r], writes=[r0_r])
                    S.op("dve", lambda e, r0_t=r0_t: e.tensor_scalar(out=r0_t[:], in0=r0_t[:], scalar1=1.0, scalar2=None, op0=ALU.add), reads=[r0_r], writes=[r0_r])
                    S.op("dve", lambda e, r0_t=r0_t, lb_t=lb_t: e.reciprocal(out=lb_t[:], in_=r0_t[:]), reads=[r0_r], writes=[lb_r])
                S.op("dve", lambda e, lb_t=lb_t, om_t=om_t: e.tensor_scalar(out=om_t[:], in0=lb_t[:], scalar1=-1.0, scalar2=1.0, op0=ALU.mult, op1=ALU.add), reads=[lb_r], writes=[om_r])
                lbv.append((lb_t, lb_r))
                oml.append((om_t, om_r))

            def proj_chunk(w, r_w, c, epi):
                for (t0, n) in cfg.tblocks:
                    ps, r_ps = pspool.next()
                    for kc in range(KC):
                        S.op("pe", lambda e, ps=ps, kc=kc, t0=t0, n=n: e.matmul(ps[:, 0:n], lhsT=w[:, kc, c * 128:(c + 1) * 128], rhs=BIG[:, kc, t0:t0 + n], start=(kc == 0), stop=(kc == KC - 1)),
                             reads=[r_w, r_BIG], writes=[r_ps], signal=(kc == KC - 1))
                    epi(ps, r_ps, t0, n)

            def store(dst, st, r_st, r_dst):
                S.dma("sp", lambda e: e.dma_start(out=dst, in_=st[:]), reads=[r_st], writes=[r_dst])

            def simple_job(col0, nchunks, func, scale, dst, r_dst):
                for b in range(0, nchunks, 4):
                    w, r_w = self.load_wblock(wpool, w_in, col0 + b * 128, 512)
                    for c in range(4):
                        st, r_st = stb.next()
                        def epi(ps, r_ps, t0, n, st=st, r_st=r_st):
                            S.op("act", lambda e: e.activation(out=st[:, t0:t0 + n], in_=ps[:, 0:n], func=func, scale=scale), reads=[r_ps], writes=[r_st])
                        proj_chunk(w, r_w, c, epi)
                        store(dst[b + c], st, r_st, r_dst[b + c])

            simple_job(COL["hq"], 8, AF.Copy, 128.0 ** -0.5, self.QT, self.r_QT)
            simple_job(COL["hg"], 8, AF.Silu, 1.0, self.SGT, self.r_SGT)
            simple_job(COL["ga"], 16, AF.Sigmoid, 1.0, self.GA, self.r_GA)
            simple_job(COL["gb"], 16, AF.Sigmoid, 1.0, self.GB, self.r_GB)
            for di, col0 in enumerate((COL["ff"], COL["fb"])):
                lb_t, lb_r = lbv[di]
                om_t, om_r = oml[di]
                for b in range(0, 8, 4):
                    w, r_w = self.load_wblock(wpool, w_in, col0 + b * 128, 512)
                    for c in range(4):
                        hd = b + c
                        sg_, r_sg = stf.next()
                        sk_, r_sk = stb.next()
                        def epi(ps, r_ps, t0, n, sg_=sg_, r_sg=r_sg, sk_=sk_, r_sk=r_sk, hd=hd, om_t=om_t, om_r=om_r):
                            e_t, r_e = tmpf.next()
                            t_t, r_t = tmpf.next()
                            k_t, r_k = tmpf.next()
                            S.op("act", lambda e: e.activation(out=e_t[:, 0:n], in_=ps[:, 0:n], func=AF.Exp, scale=-1.0), reads=[r_ps], writes=[r_e])
                            S.op("dve", lambda e: e.tensor_scalar(out=t_t[:, 0:n], in0=e_t[:, 0:n], scalar1=1.0, scalar2=None, op0=ALU.add), reads=[r_e], writes=[r_t])
                            S.op("dve", lambda e: e.reciprocal(out=t_t[:, 0:n], in_=t_t[:, 0:n]), reads=[r_t], writes=[r_t])
                            S.op("dve", lambda e: e.scalar_tensor_tensor(out=k_t[:, 0:n], in0=e_t[:, 0:n], scalar=om_t[:, hd:hd + 1], in1=t_t[:, 0:n], op0=ALU.mult, op1=ALU.mult),
                                 reads=[r_e, r_t, om_r], writes=[r_k])
                            S.op("act", lambda e: e.activation(out=sg_[:, t0:t0 + n], in_=k_t[:, 0:n], func=AF.Ln, scale=-1.0, bias=1.0), reads=[r_k], writes=[r_sg])
                            S.op("act", lambda e: e.copy(out=sk_[:, t0:t0 + n], in_=k_t[:, 0:n]), reads=[r_k], writes=[r_sk])
                        proj_chunk(w, r_w, c, epi)
                        store(self.GT[di, hd], sg_, r_sg, self.r_GT[di][hd])
                        store(self.KT[di, hd], sk_, r_sk, self.r_KT[di][hd])

            def rope_job(w, r_w, c, scale, dst, r_dst):
                raw, r_raw = stb.next()
                outt, r_out = stb.next()
                def epi(ps, r_ps, t0, n):
                    S.op("act", lambda e: e.activation(out=raw[:, t0:t0 + n], in_=ps[:, 0:n], func=AF.Copy, scale=scale), reads=[r_ps], writes=[r_raw])
                    if t0 < CTX:
                        S.op("act", lambda e: e.copy(out=outt[:, t0:t0 + n], in_=raw[:, t0:t0 + n]), reads=[r_raw], writes=[r_out])
                        return
                    p0 = t0 - CTX
                    pr, r_pr = psrot.next()
                    S.op("pe", lambda e: e.matmul(pr[:, 0:n], lhsT=pm[:], rhs=raw[:, t0:t0 + n], start=True, stop=True), reads=[r_pm, r_raw], writes=[r_pr])
                    a_t, r_a = tmpf.next()
                    b_t, r_b = tmpf.next()
                    S.op("dve", lambda e: e.tensor_tensor(out=a_t[:, 0:n], in0=raw[:, t0:t0 + n], in1=cosT[:, p0:p0 + n], op=ALU.mult), reads=[r_raw, r_cos], writes=[r_a])
                    S.op("dve", lambda e: e.tensor_tensor(out=b_t[:, 0:n], in0=pr[:, 0:n], in1=sinT[:, p0:p0 + n], op=ALU.mult), reads=[r_pr, r_sin], writes=[r_b])
                    S.op("dve", lambda e: e.tensor_tensor(out=outt[:, t0:t0 + n], in0=a_t[:, 0:n], in1=b_t[:, 0:n], op=ALU.add), reads=[r_a, r_b], writes=[r_out])
                proj_chunk(w, r_w, c, epi)
                store(dst, outt, r_out, r_dst)

            for b in range(0, 8, 4):
                w, r_w = self.load_wblock(wpool, w_in, COL["aq"] + b * 128, 512)
                for c in range(4):
                    rope_job(w, r_w, c, 64.0 ** -0.5, self.QA[b + c], self.r_QA[b + c])
            w, r_w = wpool.next()
            for (o, c0, nn) in ((0, COL["ak"], 128), (128, COL["ak"] + 64, 64), (192, COL["ak"], 64)):
                src = w_in[:, c0:c0 + nn].rearrange("(kc p) n -> p kc n", p=128)
                S.dma("pool", lambda e, o=o, nn=nn, src=src: e.dma_start(out=w[:, :, o:o + nn], in_=src), writes=[r_w])
            for c in range(2):
                rope_job(w, r_w, c, 1.0, self.KA[c], self.r_KA[c])

            vst = TPool(ph, nc, "vst", [128, 512], BF16, 3)
            for (col0, ncols, dst, r_dst) in ((COL["hv"], 512, self.V[:, 0:512], self.r_V), (COL["hv"] + 512, 512, self.V[:, 512:1024], self.r_V), (COL["av"], 128, self.VA, self.r_VA)):
                w, r_w = self.load_wblock(wpool, w_in, col0, ncols)
                for tile in range(cfg.NT):
                    ps, r_ps = pspool.next()
                    for kc in range(KC):
                        S.op("pe", lambda e, ps=ps, kc=kc, tile=tile, ncols=ncols, w=w: e.matmul(ps[:, 0:ncols], lhsT=BIG[:, kc, tile * 128:(tile + 1) * 128], rhs=w[:, kc, 0:ncols], start=(kc == 0), stop=(kc == KC - 1)),
                             reads=[r_w, r_BIG], writes=[r_ps], signal=(kc == KC - 1))
                    vt, r_vt = vst.next()
                    S.op("act", lambda e, vt=vt, ps=ps, ncols=ncols: e.copy(out=vt[:, 0:ncols], in_=ps[:, 0:ncols]), reads=[r_ps], writes=[r_vt])
                    S.dma("sp", lambda e, vt=vt, tile=tile, ncols=ncols, dst=dst: e.dma_start(out=dst[tile * 128:(tile + 1) * 128, :], in_=vt[:, 0:ncols]), reads=[r_vt], writes=[r_dst])
            self.end_phase()

    def phase_scan(self, l, s):
        cfg, nc, S = self.cfg, self.nc, self.S
        T, NCH = cfg.T, cfg.NCH
        NB16 = T // 16
        ident, r_ident = self.C["ident"]
        groups = cfg.groups
        ng = len(groups)
        order = [[list(g) for g in groups],
                 [list(reversed(groups[0]))] + [list(reversed(g)) for g in reversed(groups[1:])]]
        VN = ("qd", "kd", "qb", "qin", "kout", "k1", "k2", "k3")
        with ExitStack() as ph:
            qT, r_qT = tile1(ph, nc, "s_qT", [128, T], BF16)
            sgT, r_sgT = tile1(ph, nc, "s_sgT", [128, T], BF16)
            vt, r_vt = tile1(ph, nc, "s_v", [64, NCH, 128], BF16)
            gT = [tile1(ph, nc, "s_gT%d" % d, [128, T], F32) for d in range(2)]
            kT = [tile1(ph, nc, "s_kT%d" % d, [128, T], BF16) for d in range(2)]
            Zps = [tile1(ph, nc, "s_Zp%d" % d, [128, T + 1], F32) for d in range(2)]
            tmp = TPool(ph, nc, "s_tmp", [128, T], F32, 3)
            var = [{n: tile1(ph, nc, "s_%s%d" % (n, d), [128, T], BF16) for n in VN} for d in range(2)]
            tr = [tile1(ph, nc, "s_tr%d" % d, [128, NCH], F32) for d in range(2)]
            oall, r_oall = tile1(ph, nc, "s_oall", [128, T], F32)
            aT, r_aT = tile1(ph, nc, "s_aT", [128, T], BF16)
            ones, r_ones = tile1(ph, nc, "s_ones", [128, T], F32)
            onesf, r_onesf = tile1(ph, nc, "s_onesf", [128, 128], F32)
            gain, r_gain = tile1(ph, nc, "s_gain", [128, 8], F32)
            mk = {}
            for n in ("maskD_f", "maskO_f", "maskD_b", "maskO_b"):
                mk[n] = tile1(ph, nc, "s_" + n, [64, 64], BF16)
                S.dma("pool", lambda e, n=n: e.dma_start(out=mk[n][0][:], in_=self.cst_d[n][:, :]), writes=[mk[n][1]])
            Sf = [tile1(ph, nc, "s_Sf%d" % d, [128, 128], F32) for d in range(2)]
            Sb = [tile1(ph, nc, "s_Sb%d" % d, [128, 128], BF16) for d in range(2)]
            At = TPool(ph, nc, "s_At", [64, 8, 64], BF16, 4)
            At2 = TPool(ph, nc, "s_At2", [64, 8, 64], BF16, 2)
            ktok = TPool(ph, nc, "s_ktok", [64, 8, 128], BF16, 4)
            rs = TPool(ph, nc, "s_rs", [128, 512], F32, 2)
            psA = TPool(ph, nc, "s_psA", [64, 8, 64], F32, 1, psum=True)
            psA2 = [tile1(ph, nc, "s_psA2%d" % d, [64, 8, 64], F32, psum=True) for d in range(2)]
            psK = TPool(ph, nc, "s_psK", [64, 8, 128], BF16, 1, psum=True)
            psO = TPool(ph, nc, "s_psO", [128, 8, 64], F32, 2, psum=True)
            psD = TPool(ph, nc, "s_psD", [128, 128], F32, 1, psum=True)
            psS = TPool(ph, nc, "s_psS", [128, 512], F32, 1, psum=True)
            S.op("dve", lambda e: e.memset(ones[:], 1.0), writes=[r_ones])
            S.op("dve", lambda e: e.memset(onesf[:], 1.0), writes=[r_onesf])
            for d in range(2):
                S.op("dve", lambda e, d=d: e.memset(psA2[d][0][:], 0.0), writes=[psA2[d][1]])
                S.op("dve", lambda e, d=d: e.memset(Zps[d][0][:], 0.0), writes=[Zps[d][1]])
            S.dma("sp", lambda e: e.dma_start(out=gain[:], in_=self.hg_norm[l:l + 1, :].rearrange("o (h p) -> p (o h)", p=128)), writes=[r_gain])

            def v3(ap, j):
                return ap.rearrange("p (c j) -> p c j", j=j)

            def load_pre(hd):
                for d in range(2):
                    S.dma("sp", lambda e, hd=hd, d=d: e.dma_start(out=gT[d][0][:], in_=self.GT[d, hd]), reads=[self.r_GT[d][hd]], writes=[gT[d][1]])
                S.dma("sp", lambda e, hd=hd: e.dma_start(out=qT[:], in_=self.QT[hd]), reads=[self.r_QT[hd]], writes=[r_qT])
                for d in range(2):
                    S.dma("sp", lambda e, hd=hd, d=d: e.dma_start(out=kT[d][0][:], in_=self.KT[d, hd]), reads=[self.r_KT[d][hd]], writes=[kT[d][1]])

            load_pre(0)
            for hd in range(8):
                self.pump(4)
                S.dma("sp", lambda e, hd=hd: e.dma_start(out=vt[:], in_=self.V[:, hd * 128:(hd + 1) * 128].rearrange("(c p) v -> p c v", p=64)), reads=[self.r_V], writes=[r_vt])
                S.dma("sp", lambda e, hd=hd: e.dma_start(out=sgT[:], in_=self.SGT[hd]), reads=[self.r_SGT[hd]], writes=[r_sgT])
                mjobs = []
                for d in range(2):
                    g_t, g_r = gT[d]
                    k_t, k_r = kT[d]
                    Zp, r_Zp = Zps[d]
                    sg = 1.0 if d == 0 else -1.0
                    S.op("dve", lambda e, Zp=Zp, g_t=g_t: e.tensor_tensor_scan(out=Zp[:, 1:T + 1], data0=ones[:], data1=g_t[:], initial=0.0, op0=ALU.mult, op1=ALU.add),
                         reads=[r_ones, g_r], writes=[r_Zp])
                    X = Zp[:, 1:T + 1] if d == 0 else Zp[:, 0:T]
                    lo16 = v3(Zp[:, 0:T], 16)
                    hi16 = v3(Zp[:, 1:T + 1], 16)
                    lo64 = v3(Zp[:, 0:T], 64)
                    hi64 = v3(Zp[:, 1:T + 1], 64)
                    ref_mid = lo16[:, :, 8:9]
                    ref_qb = lo16[:, :, 0:1] if d == 0 else hi16[:, :, 15:16]
                    ref_qin = lo64[:, :, 0:1] if d == 0 else hi64[:, :, 63:64]
                    ref_kout = hi64[:, :, 63:64] if d == 0 else lo64[:, :, 0:1]
                    ref_k = [lo64[:, :, 16 * i:16 * i + 1] for i in (1, 2, 3)]

                    def make(name, src_t, src_r, ref, blk, scale, clamp=None, X=X, r_Zp=r_Zp, d=d):
                        nb = T // blk
                        st_ = {}

                        def s1():
                            D, r_D = tmp.next()
                            st_["D"] = (D, r_D)
                            S.op("dve", lambda e: e.tensor_tensor(out=v3(D[:], blk), in0=v3(X, blk), in1=ref.broadcast_to([128, nb, blk]), op=ALU.subtract),
                                 reads=[r_Zp], writes=[r_D])
                            S.op("act", lambda e: e.activation(out=D[:], in_=D[:], func=AF.Exp, scale=scale), reads=[r_D], writes=[r_D])

                        def s2():
                            D, r_D = st_["D"]
                            o_t, o_r = var[d][name]
                            if clamp is None:
                                S.op("dve", lambda e: e.tensor_tensor(out=o_t[:], in0=src_t[:], in1=D[:], op=ALU.mult), reads=[src_r, r_D], writes=[o_r])
                            else:
                                S.op("dve", lambda e: e.scalar_tensor_tensor(out=o_t[:], in0=D[:], scalar=1.0, in1=src_t[:], op0=ALU.min, op1=ALU.mult), reads=[src_r, r_D], writes=[o_r])
                        mjobs.append((s1, s2))

                    make("qd", qT, r_qT, ref_mid, 16, sg)
                    make("kd", k_t, k_r, ref_mid, 16, -sg)
                    make("qb", qT, r_qT, ref_qb, 16, sg)
                    make("qin", qT, r_qT, ref_qin, 64, sg)
                    make("kout", k_t, k_r, ref_kout, 64, -sg)
                    for i in range(3):
                        make("k%d" % (i + 1), k_t, k_r, ref_k[i], 64, -sg, clamp=(ALU.max if d == 0 else ALU.min))
                    tr_t, tr_r = tr[d]
                    S.op("dve", lambda e, tr_t=tr_t, hi64=hi64, lo64=lo64: e.tensor_tensor(out=tr_t[:].unsqueeze(2), in0=hi64[:, :, 63:64], in1=lo64[:, :, 0:1], op=ALU.subtract), reads=[r_Zp], writes=[tr_r])
                    S.op("act", lambda e, tr_t=tr_t: e.activation(out=tr_t[:], in_=tr_t[:], func=AF.Exp), reads=[tr_r], writes=[tr_r])
                    S.op("dve", lambda e, d=d: e.memset(Sf[d][0][:], 0.0), writes=[Sf[d][1]])
                    S.op("dve", lambda e, d=d: e.memset(Sb[d][0][:], 0.0), writes=[Sb[d][1]])
                for i in range(len(mjobs) + 1):
                    if i < len(mjobs):
                        mjobs[i][0]()
                    if i >= 1:
                        mjobs[i - 1][1]()
                if hd + 1 < 8:
                    load_pre(hd + 1)
                touched = set()
                for gi in range(ng):
                    ctxs = []
                    for d in range(2):
                        cl = order[d][gi]
                        g0 = min(cl)
                        n = len(cl)
                        V = var[d]
                        pa, r_pa = psA.next()
                        for c in cl:
                            S.op("pe", lambda e, pa=pa, c=c, g0=g0, V=V: e.matmul(pa[:, c - g0, :], lhsT=V["kd"][0][:, c * 64:(c + 1) * 64], rhs=V["qd"][0][:, c * 64:(c + 1) * 64], start=True, stop=True),
                                 reads=[V["kd"][1], V["qd"][1]], writes=[r_pa], signal=(c == cl[-1]))
                        pa2, r_pa2 = psA2[d]
                        subs = (1, 2, 3) if d == 0 else (0, 1, 2)
                        for c in cl:
                            for i in subs:
                                kv = V["k%d" % (i if d == 0 else i + 1)]
                                S.op("pe", lambda e, pa2=pa2, c=c, g0=g0, i=i, kv=kv, V=V: e.matmul(pa2[:, c - g0, 16 * i:16 * i + 16], lhsT=kv[0][:, c * 64:(c + 1) * 64], rhs=V["qb"][0][:, c * 64 + 16 * i:c * 64 + 16 * i + 16], start=True, stop=True),
                                     reads=[kv[1], V["qb"][1]], writes=[r_pa2], signal=(c == cl[-1] and i == subs[-1]))
                        at, r_at = At.next()
                        at2, r_at2 = At2.next()
                        mD, r_mD = mk["maskD_f" if d == 0 else "maskD_b"]
                        mO, r_mO = mk["maskO_f" if d == 0 else "maskO_b"]
                        S.op("dve", lambda e, at=at, pa=pa, n=n, mD=mD: e.tensor_tensor(out=at[:, 0:n, :], in0=pa[:, 0:n, :], in1=mD[:].unsqueeze(1).broadcast_to([64, n, 64]), op=ALU.mult),
                             reads=[r_pa, r_mD], writes=[r_at])
                        S.op("dve", lambda e, at2=at2, pa2=pa2, n=n, mO=mO: e.tensor_tensor(out=at2[:, 0:n, :], in0=pa2[:, 0:n, :], in1=mO[:].unsqueeze(1).broadcast_to([64, n, 64]), op=ALU.mult),
                             reads=[r_pa2, r_mO], writes=[r_at2])
                        S.op("pool", lambda e, at=at, at2=at2, n=n: e.tensor_tensor(out=at[:, 0:n, :], in0=at[:, 0:n, :], in1=at2[:, 0:n, :], op=ALU.add), reads=[r_at, r_at2], writes=[r_at])
                        pk, r_pk = psK.next()
                        for c in cl:
                            S.op("pe", lambda e, pk=pk, c=c, g0=g0, V=V: e.transpose(out=pk[:, c - g0, :], in_=V["kout"][0][:, c * 64:(c + 1) * 64], identity=ident[:]),
                                 reads=[V["kout"][1], r_ident], writes=[r_pk], signal=(c == cl[-1]))
                        kk, r_kk = ktok.next()
                        S.op("act", lambda e, kk=kk, pk=pk, n=n: e.copy(out=kk[:, 0:n, :], in_=pk[:, 0:n, :]), reads=[r_pk], writes=[r_kk])
                        po, r_po = psO.next()
                        ctxs.append((d, cl, g0, at, r_at, kk, r_kk, po, r_po))
                    nsteps = max(len(c[1]) for c in ctxs)
                    for j in range(nsteps):
                        for (d, cl, g0, at, r_at, kk, r_kk, po, r_po) in ctxs:
                            if j >= len(cl):
                                continue
                            c = cl[j]
                            pos = c - g0
                            V = var[d]
                            S.op("pe", lambda e, po=po, pos=pos, c=c, at=at: e.matmul(po[:, pos, :], lhsT=vt[:, c, :], rhs=at[:, pos, :], start=True, stop=False),
                                 reads=[r_vt, r_at], writes=[r_po], signal=False)
                            S.op("pe", lambda e, po=po, pos=pos, c=c, d=d, V=V: e.matmul(po[:, pos, :], lhsT=Sb[d][0][:], rhs=V["qin"][0][:, c * 64:(c + 1) * 64], start=False, stop=True),
                                 reads=[Sb[d][1], V["qin"][1]], writes=[r_po], signal=(j == len(cl) - 1))
                            last = (gi == ng - 1 and j == len(cl) - 1)
                            if not last:
                                pd, r_pd = psD.next()
                                S.op("pe", lambda e, pd=pd, kk=kk, pos=pos, c=c: e.matmul(pd[:], lhsT=kk[:, pos, :], rhs=vt[:, c, :], start=True, stop=True),
                                     reads=[r_kk, r_vt], writes=[r_pd])
                                S.op("dve", lambda e, pd=pd, d=d, c=c: e.scalar_tensor_tensor(out=Sf[d][0][:], in0=Sf[d][0][:], scalar=tr[d][0][:, c:c + 1], in1=pd[:], op0=ALU.mult, op1=ALU.add),
                                     reads=[r_pd, Sf[d][1], tr[d][1]], writes=[Sf[d][1]])
                                S.op("act", lambda e, d=d: e.copy(out=Sb[d][0][:], in_=Sf[d][0][:]), reads=[Sf[d][1]], writes=[Sb[d][1]])
                    for (d, cl, g0, at, r_at, kk, r_kk, po, r_po) in ctxs:
                        n = len(cl)
                        dst = v3(oall[:, g0 * 64:(g0 + n) * 64], 64)
                        if g0 not in touched:
                            touched.add(g0)
                            S.op("act", lambda e, dst=dst, po=po, n=n: e.copy(out=dst, in_=po[:, 0:n, :]), reads=[r_po], writes=[r_oall])
                        else:
                            S.op("dve", lambda e, dst=dst, po=po, n=n: e.tensor_tensor(out=dst, in0=po[:, 0:n, :], in1=dst, op=ALU.add), reads=[r_po, r_oall], writes=[r_oall])
                sq, r_sq = tmp.next()
                S.op("act", lambda e, sq=sq: e.activation(out=sq[:], in_=oall[:], func=AF.Square), reads=[r_oall], writes=[r_sq])
                for (t0, n) in cfg.tblocks:
                    pss, r_pss = psS.next()
                    S.op("pe", lambda e, pss=pss, sq=sq, t0=t0, n=n: e.matmul(pss[:, 0:n], lhsT=onesf[:], rhs=sq[:, t0:t0 + n], start=True, stop=True), reads=[r_onesf, r_sq], writes=[r_pss])
                    r1, r_r1 = rs.next()
                    S.op("dve", lambda e, r1=r1, pss=pss, n=n: e.tensor_scalar(out=r1[:, 0:n], in0=pss[:, 0:n], scalar1=1.0 / 128, scalar2=EPS, op0=ALU.mult, op1=ALU.add), reads=[r_pss], writes=[r_r1])
                    S.op("act", lambda e, r1=r1, n=n: e.activation(out=r1[:, 0:n], in_=r1[:, 0:n], func=AF.Sqrt), reads=[r_r1], writes=[r_r1])
                    S.op("dve", lambda e, r1=r1, n=n: e.reciprocal(out=r1[:, 0:n], in_=r1[:, 0:n]), reads=[r_r1], writes=[r_r1])
                    S.op("dve", lambda e, r1=r1, t0=t0, n=n: e.tensor_tensor(out=r1[:, 0:n], in0=r1[:, 0:n], in1=oall[:, t0:t0 + n], op=ALU.mult), reads=[r_r1, r_oall], writes=[r_r1])
                    S.op("dve", lambda e, r1=r1, t0=t0, n=n, hd=hd: e.scalar_tensor_tensor(out=aT[:, t0:t0 + n], in0=r1[:, 0:n], scalar=gain[:, hd:hd + 1], in1=sgT[:, t0:t0 + n], op0=ALU.mult, op1=ALU.mult),
                         reads=[r_r1, r_gain, r_sgT], writes=[r_aT])
                S.dma("sp", lambda e, hd=hd: e.dma_start(out=self.AT[hd], in_=aT[:]), reads=[r_aT], writes=[self.r_AT[hd]])
            self.end_phase()

    def phase_attn(self, l, s):
        cfg, nc, S = self.cfg, self.nc, self.S
        T, CTX, NCT, NT = cfg.T, cfg.CTX, cfg.NCT, cfg.NT
        NQ = cfg.SEQ // 128
        ident, r_ident = self.C["ident"]
        mprev, r_mprev = self.C["mneg_prev"]
        mnext, r_mnext = self.C["mneg_next"]
        onesb, r_onesb = self.C["ones"]
        with ExitStack() as ph:
            kc_ = [tile1(ph, nc, "a_k%d" % i, [128, T], BF16) for i in range(2)]
            vtok, r_vtok = tile1(ph, nc, "a_v", [128, NT, 128], BF16)
            qpool = TPool(ph, nc, "a_q", [128, T], BF16, 2)
            opool = TPool(ph, nc, "a_o", [128, T], BF16, 2)
            esb, r_esb = tile1(ph, nc, "a_esb", [128, 16], F32)
            es2, r_es2 = tile1(ph, nc, "a_es2", [128, 8], F32)
            ppool = TPool(ph, nc, "a_p", [128, 5, 128], BF16, 4)
            rpool = TPool(ph, nc, "a_r", [128, 128], F32, 3)
            psS = TPool(ph, nc, "a_psS", [128, 8, 128], F32, 3, psum=True)
            psOD = TPool(ph, nc, "a_psOD", [128, 2, 128], F32, 2, psum=True)
            for i in range(2):
                S.dma("sp", lambda e, i=i: e.dma_start(out=kc_[i][0][:], in_=self.KA[i]), reads=[self.r_KA[i]], writes=[kc_[i][1]])
            S.dma("sp", lambda e: e.dma_start(out=vtok[:], in_=self.VA.rearrange("(c p) v -> p c v", p=128)), reads=[self.r_VA], writes=[r_vtok])
            S.dma("sp", lambda e: e.dma_start(out=esb[:], in_=self.sink[l:l + 1, :].partition_broadcast(128)), writes=[r_esb])
            S.op("act", lambda e: e.activation(out=esb[:], in_=esb[:], func=AF.Exp), reads=[r_esb], writes=[r_esb])
            esb3 = esb[:].rearrange("p (c two) -> p c two", two=2)
            S.op("dve", lambda e: e.tensor_copy(out=es2[0:64, :], in_=esb3[0:64, :, 0]), reads=[r_esb], writes=[r_es2])
            S.op("dve", lambda e: e.tensor_copy(out=es2[64:128, :], in_=esb3[64:128, :, 1]), reads=[r_esb], writes=[r_es2])
            qblocks = []
            if l == 0:
                for m in range(NCT):
                    qblocks.append((m * 128, [(t, None) for t in range(NCT)]))
            for n in range(NQ):
                tiles = [(t, None) for t in range(NCT)]
                if n > 0:
                    tiles.append((NCT + n - 1, "prev"))
                tiles.append((NCT + n, None))
                if n < NQ - 1:
                    tiles.append((NCT + n + 1, "next"))
                qblocks.append((CTX + n * 128, tiles))
            for c in range(8):
                g = c // 4
                self.pump(2)
                qc, r_qc = qpool.next()
                S.dma("sp", lambda e, c=c, qc=qc: e.dma_start(out=qc[:], in_=self.QA[c]), reads=[self.r_QA[c]], writes=[r_qc])
                oc, r_oc = opool.next()
                kops = [(kc_[0] if g == 0 else kc_[1], 0), (kc_[1] if g == 0 else kc_[0], 64)]
                for (q0, tiles) in qblocks:
                    nt = len(tiles)
                    pts = []
                    for hh in range(2):
                        (k_t, k_r), r0 = kops[hh]
                        ps, r_ps = psS.next()
                        for ti, (tile, mkind) in enumerate(tiles):
                            S.op("pe", lambda e, ps=ps, ti=ti, tile=tile, k_t=k_t, r0=r0, q0=q0, qc=qc, mkind=mkind: e.matmul(ps[:, ti, :], lhsT=k_t[r0:r0 + 64, tile * 128:(tile + 1) * 128], rhs=qc[r0:r0 + 64, q0:q0 + 128], start=True, stop=(mkind is None)),
                                 reads=[k_r, r_qc], writes=[r_ps], signal=(mkind is None and ti == nt - 1))
                            if mkind is not None:
                                m_t, m_r = (mprev, r_mprev) if mkind == "prev" else (mnext, r_mnext)
                                S.op("pe", lambda e, ps=ps, ti=ti, m_t=m_t: e.matmul(ps[:, ti, :], lhsT=ident[:], rhs=m_t[:], start=False, stop=True),
                                     reads=[r_ident, m_r], writes=[r_ps], signal=(ti == nt - 1))
                        pt, r_pt = ppool.next()
                        S.op("act", lambda e, pt=pt, ps=ps, nt=nt: e.activation(out=pt[:, 0:nt, :], in_=ps[:, 0:nt, :], func=AF.Exp), reads=[r_ps], writes=[r_pt])
                        pts.append((pt, r_pt))
                    od, r_od = psOD.next()
                    for hh in range(2):
                        pt, r_pt = pts[hh]
                        r0 = 64 * hh
                        tp = None if hh == 0 else (0, 64)
                        for ti, (tile, mkind) in enumerate(tiles):
                            S.op("pe", lambda e, od=od, r0=r0, ti=ti, tile=tile, pt=pt, tp=tp, nt=nt, g=g: e.matmul(od[r0:r0 + 64, 0, :], lhsT=vtok[:, tile, g * 64:(g + 1) * 64], rhs=pt[:, ti, :], start=(ti == 0), stop=(ti == nt - 1), tile_position=tp),
                                 reads=[r_vtok, r_pt], writes=[r_od], signal=False)
                        for ti, (tile, mkind) in enumerate(tiles):
                            S.op("pe", lambda e, od=od, r0=r0, ti=ti, pt=pt, tp=tp, nt=nt: e.matmul(od[r0:r0 + 64, 1, :], lhsT=onesb[:, 0:64], rhs=pt[:, ti, :], start=(ti == 0), stop=(ti == nt - 1), tile_position=tp),
                                 reads=[r_onesb, r_pt], writes=[r_od], signal=(ti == nt - 1))
                    rc, r_rc = rpool.next()
                    S.op("dve", lambda e, rc=rc, od=od, c=c: e.tensor_scalar(out=rc[:], in0=od[:, 1, :], scalar1=es2[:, c:c + 1], scalar2=None, op0=ALU.add), reads=[r_od, r_es2], writes=[r_rc])
                    S.op("dve", lambda e, rc=rc: e.reciprocal(out=rc[:], in_=rc[:]), reads=[r_rc], writes=[r_rc])
                    S.op("dve", lambda e, rc=rc, od=od, oc=oc, q0=q0: e.tensor_tensor(out=oc[:, q0:q0 + 128], in0=od[:, 0, :], in1=rc[:], op=ALU.mult), reads=[r_od, r_rc], writes=[r_oc])
                if l == 0:
                    S.dma("sp", lambda e, c=c, oc=oc: e.dma_start(out=self.OAT[c], in_=oc[:]), reads=[r_oc], writes=[self.r_OAT[c]])
                else:
                    S.dma("sp", lambda e, c=c, oc=oc: e.dma_start(out=self.OAT[c][:, CTX:T], in_=oc[:, CTX:T]), reads=[r_oc], writes=[self.r_OAT[c]])
            self.end_phase()

    def phase_merge(self, l, s):
        cfg, nc, S = self.cfg, self.nc, self.S
        D, KC, T, CTX = cfg.D, cfg.KC, cfg.T, cfg.CTX
        BIG, r_BIG = self.BIG
        blocks = cfg.tblocks if l == 0 else cfg.lat_blocks
        tiles = list(range(cfg.NT)) if l == 0 else list(range(cfg.NCT, cfg.NT))
        with ExitStack() as ph:
            aT, r_aT = tile1(ph, nc, "m_aT", [128, 8, T], BF16)
            oT, r_oT = tile1(ph, nc, "m_oT", [128, 8, T], BF16)
            S.dma("sp", lambda e: e.dma_start(out=aT[:], in_=self.AT.rearrange("c p t -> p c t")), reads=self.r_AT, writes=[r_aT])
            S.dma("sp", lambda e: e.dma_start(out=oT[:], in_=self.OAT.rearrange("c p t -> p c t")), reads=self.r_OAT, writes=[r_oT])
            wap = TPool(ph, nc, "m_wa", [128, 8, 512], BF16, 2)
            wbp = TPool(ph, nc, "m_wb", [128, 8, 512], BF16, 2)
            gap = TPool(ph, nc, "m_ga", [128, T], BF16, 2)
            gbp = TPool(ph, nc, "m_gb", [128, T], BF16, 2)
            tp = TPool(ph, nc, "m_t", [128, 512], F32, 4)
            ps1 = TPool(ph, nc, "m_ps1", [128, 512], F32, 2, psum=True)
            ps2 = TPool(ph, nc, "m_ps2", [128, 512], F32, 2, psum=True)
            for jb in range(4):
                wa, r_wa = self.load_wblock(wap, self.w_a[l], jb * 512, 512)
                wb, r_wb = self.load_wblock(wbp, self.w_b[l], jb * 512, 512)
                for c in range(4):
                    j = jb * 4 + c
                    ga, r_ga = gap.next()
                    gb, r_gb = gbp.next()
                    S.dma("sp", lambda e, ga=ga, j=j: e.dma_start(out=ga[:], in_=self.GA[j]), reads=[self.r_GA[j]], writes=[r_ga])
                    S.dma("sp", lambda e, gb=gb, j=j: e.dma_start(out=gb[:], in_=self.GB[j]), reads=[self.r_GB[j]], writes=[r_gb])
                    for (t0, n) in blocks:
                        p1, r_p1 = ps1.next()
                        p2, r_p2 = ps2.next()
                        for kc in range(8):
                            S.op("pe", lambda e, p1=p1, wa=wa, kc=kc, c=c, t0=t0, n=n: e.matmul(p1[:, 0:n], lhsT=wa[:, kc, c * 128:(c + 1) * 128], rhs=aT[:, kc, t0:t0 + n], start=(kc == 0), stop=(kc == 7)),
                                 reads=[r_wa, r_aT], writes=[r_p1], signal=(kc == 7))
                        for kc in range(8):
                            S.op("pe", lambda e, p2=p2, wb=wb, kc=kc, c=c, t0=t0, n=n: e.matmul(p2[:, 0:n], lhsT=wb[:, kc, c * 128:(c + 1) * 128], rhs=oT[:, kc, t0:t0 + n], start=(kc == 0), stop=(kc == 7)),
                                 reads=[r_wb, r_oT], writes=[r_p2], signal=(kc == 7))
                        t1, r_t1 = tp.next()
                        t2, r_t2 = tp.next()
                        S.op("dve", lambda e, t1=t1, p1=p1, ga=ga, t0=t0, n=n: e.tensor_tensor(out=t1[:, 0:n], in0=p1[:, 0:n], in1=ga[:, t0:t0 + n], op=ALU.mult), reads=[r_p1, r_ga], writes=[r_t1])
                        S.op("dve", lambda e, t2=t2, p2=p2, gb=gb, t0=t0, n=n: e.tensor_tensor(out=t2[:, 0:n], in0=p2[:, 0:n], in1=gb[:, t0:t0 + n], op=ALU.mult), reads=[r_p2, r_gb], writes=[r_t2])
                        S.op("pool", lambda e, t1=t1, t2=t2, j=j, t0=t0, n=n: e.tensor_tensor(out=BIG[:, j, t0:t0 + n], in0=t1[:, 0:n], in1=t2[:, 0:n], op=ALU.add), reads=[r_t1, r_t2], writes=[r_BIG])
            self.end_phase()
        with ExitStack() as ph:
            wop = TPool(ph, nc, "m_wo", [128, KC, 512], BF16, 2)
            g1 = {}
            for kind, row in (("lat", s), ("ctx", cfg.NSEQ)):
                g1[kind] = tile1(ph, nc, "m_g1" + kind, [128, D], F32)
                self.bcast_row(g1[kind][0], g1[kind][1], self.MOD[l, row:row + 1, 2 * D:3 * D], D)
            xp = TPool(ph, nc, "m_x", [128, 512], F32, 3)
            tp = TPool(ph, nc, "m_t2", [128, 512], F32, 2)
            xo = TPool(ph, nc, "m_xo", [128, 512], F32, 3)
            pso = TPool(ph, nc, "m_pso", [128, 512], F32, 3, psum=True)
            first = (l == 0)
            for nb in range(4):
                wo, r_wo = self.load_wblock(wop, self.w_o[l], nb * 512, 512)
                loaded = {}

                def load(i, nb=nb, loaded=loaded):
                    tile = tiles[i]
                    xt, r_xt = xp.next()
                    src = self.xsrc(first, s, tile)[:, nb * 512:(nb + 1) * 512]
                    S.dma("sp", lambda e, xt=xt, src=src: e.dma_start(out=xt[:], in_=src), reads=([] if first else [self.r_XR[s][tile]]), writes=[r_xt])
                    loaded[i] = (xt, r_xt)
                load(0)
                for i, tile in enumerate(tiles):
                    if i + 1 < len(tiles):
                        load(i + 1)
                    xt, r_xt = loaded.pop(i)
                    g_t, g_r = g1["ctx" if tile < cfg.NCT else "lat"]
                    ps, r_ps = pso.next()
                    for kc in range(KC):
                        S.op("pe", lambda e, ps=ps, kc=kc, tile=tile, wo=wo: e.matmul(ps[:], lhsT=BIG[:, kc, tile * 128:(tile + 1) * 128], rhs=wo[:, kc, :], start=(kc == 0), stop=(kc == KC - 1)),
                             reads=[r_BIG, r_wo], writes=[r_ps], signal=(kc == KC - 1))
                    t1, r_t1 = tp.next()
                    S.op("dve", lambda e, t1=t1, ps=ps, g_t=g_t, nb=nb: e.tensor_tensor(out=t1[:], in0=ps[:], in1=g_t[:, nb * 512:(nb + 1) * 512], op=ALU.mult), reads=[r_ps, g_r], writes=[r_t1])
                    xn, r_xn = xo.next()
                    S.op("dve", lambda e, xn=xn, t1=t1, xt=xt: e.tensor_tensor(out=xn[:], in0=t1[:], in1=xt[:], op=ALU.add), reads=[r_t1, r_xt], writes=[r_xn])
                    S.dma("sp", lambda e, xn=xn, tile=tile, nb=nb: e.dma_start(out=self.XR[s, tile * 128:(tile + 1) * 128, nb * 512:(nb + 1) * 512], in_=xn[:]), reads=[r_xn], writes=[self.r_XR[s][tile]])
            self.end_phase()

    def phase_swiglu(self, l, s):
        cfg, nc, S = self.cfg, self.nc, self.S
        D, KC, T, CTX, NCT = cfg.D, cfg.KC, cfg.T, cfg.CTX, cfg.NCT
        BIG, r_BIG = self.BIG
        moe = (l == 1)
        blocks = cfg.lat_blocks if moe else cfg.tblocks
        if moe:
            experts = [(self.moe_wg[0, e], self.moe_wu[0, e], self.moe_wd[0, e], cfg.DEXP, e) for e in range(8)]
        else:
            experts = [(self.ffn_wg[0], self.ffn_wu[0], self.ffn_wd[0], cfg.DFF, None)]
        GF = 256
        with ExitStack() as ph:
            g2 = {}
            for kind, row in (("lat", s), ("ctx", cfg.NSEQ)):
                if moe and kind == "ctx":
                    continue
                g2[kind] = tile1(ph, nc, "f_g2" + kind, [128, D], F32)
                self.bcast_row(g2[kind][0], g2[kind][1], self.MOD[l, row:row + 1, 5 * D:6 * D], D)
            comb = None
            if moe:
                comb = self.routing(ph, s)
            wgp = TPool(ph, nc, "f_wg", [128, KC, GF], BF16, 2)
            wup = TPool(ph, nc, "f_wu", [128, KC, GF], BF16, 2)
            wdp = TPool(ph, nc, "f_wd", [128, GF // 128, D], BF16, 2)
            if moe:
                sblocks = [[b] for b in blocks]
            else:
                sblocks = []
                SB = CTX + 512
                if T % SB == 0 and CTX % 128 == 0 and CTX <= 512:
                    for b0 in range(0, T, SB):
                        sblocks.append([(b0, CTX), (b0 + CTX, 512)] if b0 == 0 else [(b0, 512), (b0 + 512, CTX)])
                else:
                    sblocks = [[blocks[0], blocks[1]]] + [[b] for b in blocks[2:]]
            nacc = max(sum(n for (_, n) in sb) for sb in sblocks) // 128
            acc, r_acc = tile1(ph, nc, "f_acc", [128, nacc, D], F32)
            actp = TPool(ph, nc, "f_act", [128, GF // 128, 512], BF16, 2)
            sgp = TPool(ph, nc, "f_sg", [128, 512], F32, 2 if moe else 1)
            HW_ = D // 4
            xp = TPool(ph, nc, "f_x", [128, HW_], F32, 4)
            psg = TPool(ph, nc, "f_psg", [128, 512], F32, 2, psum=True)
            psu = TPool(ph, nc, "f_psu", [128, 512], F32, 2, psum=True)
            pso = TPool(ph, nc, "f_pso", [128, 512], F32, 2 if moe else 3, psum=True)
            for (wg2d, wu2d, wd2d, dff, eidx) in experts:
                ngr = dff // GF
                for sb in sblocks:
                  for gr in range(ngr):
                    wg, r_wg = self.load_wblock(wgp, wg2d, gr * GF, GF)
                    wu, r_wu = self.load_wblock(wup, wu2d, gr * GF, GF)
                    wd, r_wd = wdp.next()
                    for hlf in range(2):
                        srcd = wd2d[gr * GF:(gr + 1) * GF, hlf * 1024:(hlf + 1) * 1024].rearrange("(c p) n -> p c n", p=128)
                        S.dma("pool", lambda e, wd=wd, srcd=srcd, hlf=hlf: e.dma_start(out=wd[:, :, hlf * 1024:(hlf + 1) * 1024], in_=srcd), writes=[r_wd])
                    tb = 0
                    for (t0, n) in sb:
                        ntl = n // 128
                        act, r_act = actp.next()
                        for c in range(GF // 128):
                            pg, r_pg = psg.next()
                            pu, r_pu = psu.next()
                            for kc in range(KC):
                                S.op("pe", lambda e, pg=pg, wg=wg, kc=kc, c=c, t0=t0, n=n: e.matmul(pg[:, 0:n], lhsT=wg[:, kc, c * 128:(c + 1) * 128], rhs=BIG[:, kc, t0:t0 + n], start=(kc == 0), stop=(kc == KC - 1)),
                                     reads=[r_wg, r_BIG], writes=[r_pg], signal=(kc == KC - 1))
                            for kc in range(KC):
                                S.op("pe", lambda e, pu=pu, wu=wu, kc=kc, c=c, t0=t0, n=n: e.matmul(pu[:, 0:n], lhsT=wu[:, kc, c * 128:(c + 1) * 128], rhs=BIG[:, kc, t0:t0 + n], start=(kc == 0), stop=(kc == KC - 1)),
                                     reads=[r_wu, r_BIG], writes=[r_pu], signal=(kc == KC - 1))
                            sg, r_sg = sgp.next()
                            S.op("act", lambda e, sg=sg, pg=pg, n=n: e.activation(out=sg[:, 0:n], in_=pg[:, 0:n], func=AF.Silu), reads=[r_pg], writes=[r_sg])
                            S.op("dve", lambda e, act=act, sg=sg, pu=pu, c=c, n=n: e.tensor_tensor(out=act[:, c, 0:n], in0=sg[:, 0:n], in1=pu[:, 0:n], op=ALU.mult), reads=[r_sg, r_pu], writes=[r_act])
                        for tl in range(ntl):
                            ai = tb + tl
                            for nb in range(4):
                                po, r_po = pso.next()
                                nch = GF // 128
                                for c in range(nch):
                                    S.op("pe", lambda e, po=po, act=act, wd=wd, c=c, tl=tl, nb=nb, nch=nch: e.matmul(po[:], lhsT=act[:, c, tl * 128:(tl + 1) * 128], rhs=wd[:, c, nb * 512:(nb + 1) * 512], start=(c == 0), stop=(c == nch - 1)),
                                         reads=[r_act, r_wd], writes=[r_po], signal=(c == nch - 1))
                                if gr == 0:
                                    S.op("act", lambda e, po=po, ai=ai, nb=nb: e.copy(out=acc[:, ai, nb * 512:(nb + 1) * 512], in_=po[:]), reads=[r_po], writes=[r_acc])
                                else:
                                    S.op("dve", lambda e, po=po, ai=ai, nb=nb: e.tensor_tensor(out=acc[:, ai, nb * 512:(nb + 1) * 512], in0=po[:], in1=acc[:, ai, nb * 512:(nb + 1) * 512], op=ALU.add), reads=[r_po, r_acc], writes=[r_acc])
                        tb += ntl
                  tb = 0
                  for (t0, n) in sb:
                    ntl = n // 128
                    for tl_ in range(ntl):
                        tl = tb + tl_
                        tile = t0 // 128 + tl_
                        g_t, g_r = g2["ctx" if tile < NCT else "lat"]
                        xq = []
                        for hf in range(4):
                            c0 = hf * HW_
                            xt, r_xt = xp.next()
                            S.dma("sp", lambda e, xt=xt, tile=tile, c0=c0: e.dma_start(out=xt[:], in_=self.XR[s, tile * 128:(tile + 1) * 128, c0:c0 + HW_]), reads=[self.r_XR[s][tile]], writes=[r_xt])
                            xq.append((xt, r_xt))
                        for hf in range(4):
                            c0 = hf * HW_
                            xt, r_xt = xq[hf]
                            S.op("dve", lambda e, tl=tl, g_t=g_t, c0=c0: e.tensor_tensor(out=acc[:, tl, c0:c0 + HW_], in0=acc[:, tl, c0:c0 + HW_], in1=g_t[:, c0:c0 + HW_], op=ALU.mult), reads=[r_acc, g_r], writes=[r_acc])
                            if eidx is None:
                                S.op("dve", lambda e, tl=tl, xt=xt, c0=c0: e.tensor_tensor(out=xt[:], in0=acc[:, tl, c0:c0 + HW_], in1=xt[:], op=ALU.add), reads=[r_acc, r_xt], writes=[r_xt])
                            else:
                                cb, r_cb = comb
                                lt = tile - NCT
                                S.op("dve", lambda e, tl=tl, xt=xt, cb=cb, lt=lt, eidx=eidx, c0=c0: e.scalar_tensor_tensor(out=xt[:], in0=acc[:, tl, c0:c0 + HW_], scalar=cb[:, lt, eidx:eidx + 1], in1=xt[:], op0=ALU.mult, op1=ALU.add),
                                     reads=[r_acc, r_xt, r_cb], writes=[r_xt])
                            S.dma("sp", lambda e, xt=xt, tile=tile, c0=c0: e.dma_start(out=self.XR[s, tile * 128:(tile + 1) * 128, c0:c0 + HW_], in_=xt[:]), reads=[r_xt], writes=[self.r_XR[s][tile]])
                    tb += ntl
            self.end_phase()

    def routing(self, ph, s, want_sel=False):
        cfg, nc, S = self.cfg, self.nc, self.S
        D, KC, NCT = cfg.D, cfg.KC, cfg.NCT
        BIG, r_BIG = self.BIG
        NTl = cfg.SEQ // 128
        rf, r_rf = tile1(ph, nc, "r_rf", [128, KC, 8], F32)
        rh, r_rh = tile1(ph, nc, "r_rh", [128, KC, 8], BF16)
        rl, r_rl = tile1(ph, nc, "r_rl", [128, KC, 8], BF16)
        S.dma("sp", lambda e: e.dma_start(out=rf[:], in_=self.router[0].rearrange("(kc p) e -> p kc e", p=128)), writes=[r_rf])
        S.op("dve", lambda e: e.tensor_copy(out=rh[:], in_=rf[:]), reads=[r_rf], writes=[r_rh])
        S.op("dve", lambda e: e.tensor_tensor(out=rl[:], in0=rf[:], in1=rh[:], op=ALU.subtract), reads=[r_rf, r_rh], writes=[r_rl])
        lg, r_lg = tile1(ph, nc, "r_lg", [128, NTl, 8], F32)
        psl = TPool(ph, nc, "r_ps", [128, 8], F32, 2, psum=True)
        for lt in range(NTl):
            tile = NCT + lt
            ps, r_ps = psl.next()
            for i, (rt, rr) in enumerate(((rh, r_rh), (rl, r_rl))):
                for kc in range(KC):
                    S.op("pe", lambda e, ps=ps, rt=rt, kc=kc, tile=tile, i=i: e.matmul(ps[:], lhsT=BIG[:, kc, tile * 128:(tile + 1) * 128], rhs=rt[:, kc, :], start=(i == 0 and kc == 0), stop=(i == 1 and kc == KC - 1)),
                         reads=[r_BIG, rr], writes=[r_ps], signal=(i == 1 and kc == KC - 1))
            S.op("act", lambda e, ps=ps, lt=lt: e.copy(out=lg[:, lt, :], in_=ps[:]), reads=[r_ps], writes=[r_lg])
        shp = [128, NTl, 8]
        m1, r_m1 = tile1(ph, nc, "r_m1", [128, NTl], F32)
        m2, r_m2 = tile1(ph, nc, "r_m2", [128, NTl], F32)
        t8, r_t8 = tile1(ph, nc, "r_t8", shp, F32)
        l2, r_l2 = tile1(ph, nc, "r_l2", shp, F32)
        sel, r_sel = tile1(ph, nc, "r_sel", shp, F32)
        cb, r_cb = tile1(ph, nc, "r_cb", shp, F32)
        bc = lambda t: t[:].unsqueeze(2).broadcast_to(shp)
        S.op("dve", lambda e: e.tensor_reduce(out=m1[:], in_=lg[:], axis=AX.X, op=ALU.max), reads=[r_lg], writes=[r_m1])
        S.op("dve", lambda e: e.tensor_tensor(out=t8[:], in0=lg[:], in1=bc(m1), op=ALU.is_equal), reads=[r_lg, r_m1], writes=[r_t8])
        S.op("dve", lambda e: e.scalar_tensor_tensor(out=l2[:], in0=t8[:], scalar=-1e30, in1=lg[:], op0=ALU.mult, op1=ALU.add), reads=[r_t8, r_lg], writes=[r_l2])
        S.op("dve", lambda e: e.tensor_reduce(out=m2[:], in_=l2[:], axis=AX.X, op=ALU.max), reads=[r_l2], writes=[r_m2])
        S.op("dve", lambda e: e.tensor_tensor(out=sel[:], in0=lg[:], in1=bc(m2), op=ALU.is_ge), reads=[r_lg, r_m2], writes=[r_sel])
        S.op("dve", lambda e: e.tensor_tensor(out=t8[:], in0=lg[:], in1=bc(m1), op=ALU.subtract), reads=[r_lg, r_m1], writes=[r_t8])
        S.op("act", lambda e: e.activation(out=t8[:], in_=t8[:], func=AF.Exp), reads=[r_t8], writes=[r_t8])
        S.op("dve", lambda e: e.tensor_tensor(out=t8[:], in0=t8[:], in1=sel[:], op=ALU.mult), reads=[r_t8, r_sel], writes=[r_t8])
        S.op("dve", lambda e: e.tensor_tensor(out=m2[:], in0=m2[:], in1=m1[:], op=ALU.subtract), reads=[r_m2, r_m1], writes=[r_m2])
        S.op("act", lambda e: e.activation(out=m2[:], in_=m2[:], func=AF.Exp), reads=[r_m2], writes=[r_m2])
        S.op("dve", lambda e: e.tensor_scalar(out=m2[:], in0=m2[:], scalar1=1.0, scalar2=None, op0=ALU.add), reads=[r_m2], writes=[r_m2])
        S.op("dve", lambda e: e.reciprocal(out=m2[:], in_=m2[:]), reads=[r_m2], writes=[r_m2])
        S.op("dve", lambda e: e.tensor_tensor(out=cb[:], in0=t8[:], in1=bc(m2), op=ALU.mult), reads=[r_t8, r_m2], writes=[r_cb])
        if want_sel:
            return cb, r_cb, sel, r_sel
        return cb, r_cb

    def phase_final(self):
        cfg, nc, S = self.cfg, self.nc, self.S
        D, NCT = cfg.D, cfg.NCT
        with ExitStack() as ph:
            fn, r_fn = tile1(ph, nc, "z_fn", [128, D], F32)
            S.dma("sp", lambda e: e.dma_start(out=fn[:], in_=self.final_norm.unsqueeze(0).partition_broadcast(128)), writes=[r_fn])
            xp = TPool(ph, nc, "z_x", [128, D], F32, 3)
            op_ = TPool(ph, nc, "z_o", [128, D], F32, 2)
            junk, r_junk = tile1(ph, nc, "z_junk", [128, D], BF16)
            stp = TPool(ph, nc, "z_st", [128, 4], F32, 2)
            jobs = [(s, tile) for s in range(cfg.NSEQ) for tile in range(NCT, cfg.NT)]
            loaded = {}

            def load(i):
                s, tile = jobs[i]
                xt, r_xt = xp.next()
                S.dma("sp", lambda e, xt=xt, s=s, tile=tile: e.dma_start(out=xt[:], in_=self.XR[s, tile * 128:(tile + 1) * 128, :]), reads=[self.r_XR[s][tile]], writes=[r_xt])
                loaded[i] = (xt, r_xt)
            load(0)
            for i, (s, tile) in enumerate(jobs):
                if i + 1 < len(jobs):
                    load(i + 1)
                xt, r_xt = loaded.pop(i)
                st, r_st = stp.next()
                S.op("act", lambda e, xt=xt, st=st: e.activation(out=junk[:], in_=xt[:], func=AF.Square, accum_out=st[:, 0:1]), reads=[r_xt], writes=[r_junk, r_st])
                S.op("dve", lambda e, st=st: e.tensor_scalar(out=st[:, 1:2], in0=st[:, 0:1], scalar1=1.0 / D, scalar2=EPS, op0=ALU.mult, op1=ALU.add), reads=[r_st], writes=[r_st])
                S.op("act", lambda e, st=st: e.activation(out=st[:, 2:3], in_=st[:, 1:2], func=AF.Sqrt), reads=[r_st], writes=[r_st])
                S.op("dve", lambda e, st=st: e.reciprocal(out=st[:, 3:4], in_=st[:, 2:3]), reads=[r_st], writes=[r_st])
                ot, r_ot = op_.next()
                S.op("dve", lambda e, ot=ot, xt=xt, st=st: e.scalar_tensor_tensor(out=ot[:], in0=xt[:], scalar=st[:, 3:4], in1=fn[:], op0=ALU.mult, op1=ALU.mult), reads=[r_xt, r_st, r_fn], writes=[r_ot])
                lt = tile - NCT
                S.dma("sp", lambda e, ot=ot, s=s, lt=lt: e.dma_start(out=self.out[s, lt * 128:(lt + 1) * 128, :], in_=ot[:]), reads=[r_ot], writes=[self.r_out])
            self.end_phase()

    def prepass_jobs(self):
        cfg = self.cfg
        NG = cfg.DEXP // 512
        jobs = []
        for e_ in range(8):
            for g in range(NG):
                row0 = (e_ * NG + g) * 128
                for (dst_t, src_t) in ((self.WGB, self.moe_wg), (self.WUB, self.moe_wu)):
                    dst = dst_t[row0:row0 + 128, :].rearrange("p (kc n) -> p kc n", n=512)
                    src = src_t[0, e_, :, g * 512:(g + 1) * 512].rearrange("(kc p) n -> p kc n", p=128)
                    jobs.append((dst, src))
                for hlf in range(2):
                    dst = self.WDB[row0:row0 + 128, :].rearrange("p (c n) -> p c n", n=2048)[:, :, hlf * 1024:(hlf + 1) * 1024]
                    src = self.moe_wd[0, e_, g * 512:(g + 1) * 512, hlf * 1024:(hlf + 1) * 1024].rearrange("(c p) n -> p c n", p=128)
                    jobs.append((dst, src))
        return jobs

    def pump(self, k):
        if self.cfg.MOE != "routed":
            return
        for _ in range(k):
            if not self.pre_jobs:
                return
            dst, src = self.pre_jobs.pop(0)
            self.S.dma("pool", lambda e, dst=dst, src=src: e.dma_start(out=dst, in_=src))

    def route_local(self, s):
        cfg, nc, S = self.cfg, self.nc, self.S
        NTl = cfg.SEQ // 128
        with ExitStack() as ph:
            cb, r_cb, sel, r_sel = self.routing(ph, s, want_sel=True)
            S.dma("sp", lambda e: e.dma_start(out=self.SELD[:, s * NTl:(s + 1) * NTl, :], in_=sel[:]), reads=[r_sel], writes=[self.r_SELD])
            S.dma("sp", lambda e: e.dma_start(out=self.CBD[:, s * NTl:(s + 1) * NTl, :], in_=cb[:]), reads=[r_cb], writes=[self.r_SELD])
            self.end_phase()

    def phase_moe_routed(self):
        cfg, nc, S = self.cfg, self.nc, self.S
        D, KC, NCT = cfg.D, cfg.KC, cfg.NCT
        NTl = cfg.SEQ // 128
        NTg = cfg.NSEQ * NTl
        NSLOT = cfg.NSLOT
        NG = cfg.DEXP // 512
        ident, r_ident = self.C["ident"]
        ustrict, r_us = self.C["ustrict"]
        onesb, r_onesb = self.C["ones"]
        shp = [128, NTg, 8]
        self.pump(100000)
        with ExitStack() as ms:
            posA, r_posA = tile1(ms, nc, "q_posA", [128, NTg], I32)
            posB, r_posB = tile1(ms, nc, "q_posB", [128, NTg], I32)
            wA, r_wA = tile1(ms, nc, "q_wA", [128, NTg], F32)
            wB, r_wB = tile1(ms, nc, "q_wB", [128, NTg], F32)
            idxW, r_idxW = tile1(ms, nc, "q_idxW", [128, NSLOT, NG], I32)
            with ExitStack() as ph:
                sel, r_sel = tile1(ph, nc, "q_sel", shp, F32)
                cb, r_cb = tile1(ph, nc, "q_cb", shp, F32)
                selb, r_selb = tile1(ph, nc, "q_selb", shp, BF16)
                S.dma("sp", lambda e: e.dma_start(out=sel[:], in_=self.SELD), reads=[self.r_SELD], writes=[r_sel])
                S.dma("sp", lambda e: e.dma_start(out=cb[:], in_=self.CBD), reads=[self.r_SELD], writes=[r_cb])
                S.op("dve", lambda e: e.tensor_copy(out=selb[:], in_=sel[:]), reads=[r_sel], writes=[r_selb])
                pR, r_pR = tile1(ph, nc, "q_pR", [128, NTg * 8], F32, psum=True)
                pC, r_pC = tile1(ph, nc, "q_pC", [128, NTg * 8], F32, psum=True)
                flat = lambda t: t[:].rearrange("p g e -> p (g e)")
                S.op("pe", lambda e: e.matmul(pR[:], lhsT=ustrict[:], rhs=flat(selb), start=True, stop=True), reads=[r_us, r_selb], writes=[r_pR])
                S.op("pe", lambda e: e.matmul(pC[:], lhsT=onesb[:], rhs=flat(selb), start=True, stop=True), reads=[r_onesb, r_selb], writes=[r_pC])
                Cs, r_Cs = tile1(ph, nc, "q_Cs", shp, F32)
                incl, r_incl = tile1(ph, nc, "q_incl", shp, F32)
                pos, r_pos = tile1(ph, nc, "q_pos", shp, F32)
                v, r_v = tile1(ph, nc, "q_v", shp, F32)
                t8, r_t8 = tile1(ph, nc, "q_t8", shp, F32)
                o32, r_o32 = tile1(ph, nc, "q_o32", [128, NTg], F32)
                S.op("dve", lambda e: e.memset(o32[:], 1.0), writes=[r_o32])
                S.op("act", lambda e: e.copy(out=flat(Cs), in_=pC[:]), reads=[r_pC], writes=[r_Cs])
                for e_ in range(8):
                    S.op("dve", lambda e, e_=e_: e.tensor_tensor_scan(out=incl[:, :, e_], data0=o32[:], data1=Cs[:, :, e_], initial=0.0, op0=ALU.mult, op1=ALU.add),
                         reads=[r_o32, r_Cs], writes=[r_incl])
                sm = lambda name, w: tile1(ph, nc, name, [128, w], F32)
                n_e, r_n = sm("q_n", 8)
                np_e, r_np = sm("q_np", 8)
                base, r_base = sm("q_base", 8)
                cs, r_cs = sm("q_cs", 8)
                S.op("dve", lambda e: e.tensor_copy(out=n_e[:], in_=incl[:, NTg - 1, :]), reads=[r_incl], writes=[r_n])
                KMAX = max(1, (cfg.NSEQ * cfg.SEQ) // 512)
                kg, r_kg = tile1(ph, nc, "q_kg", [128, 8, KMAX], F32)
                S.dma("sp", lambda e: e.dma_start(out=kg[:], in_=self.cst_d["kgrid"].rearrange("p (e k) -> p e k", k=KMAX)), writes=[r_kg])
                S.op("dve", lambda e: e.tensor_tensor(out=kg[:], in0=n_e[:].unsqueeze(2).broadcast_to([128, 8, KMAX]), in1=kg[:], op=ALU.is_gt), reads=[r_n, r_kg], writes=[r_kg])
                S.op("dve", lambda e: e.tensor_reduce(out=np_e[:], in_=kg[:], axis=AX.X, op=ALU.add), reads=[r_kg], writes=[r_np])
                S.op("dve", lambda e: e.tensor_scalar(out=np_e[:], in0=np_e[:], scalar1=512.0, scalar2=None, op0=ALU.mult), reads=[r_np], writes=[r_np])
                S.op("dve", lambda e: e.memset(base[:], 0.0), writes=[r_base])
                for e_ in range(1, 8):
                    S.op("dve", lambda e, e_=e_: e.tensor_tensor(out=base[:, e_:e_ + 1], in0=base[:, e_ - 1:e_], in1=np_e[:, e_ - 1:e_], op=ALU.add), reads=[r_base, r_np], writes=[r_base])
                S.op("dve", lambda e: e.tensor_tensor(out=pos[:], in0=incl[:], in1=Cs[:], op=ALU.subtract), reads=[r_incl, r_Cs], writes=[r_pos])
                S.op("dve", lambda e: e.tensor_tensor(out=flat(pos), in0=pR[:], in1=flat(pos), op=ALU.add), reads=[r_pR, r_pos], writes=[r_pos])
                S.op("dve", lambda e: e.tensor_tensor(out=pos[:], in0=pos[:], in1=base[:].unsqueeze(1).broadcast_to(shp), op=ALU.add), reads=[r_pos, r_base], writes=[r_pos])
                S.op("dve", lambda e: e.scalar_tensor_tensor(out=v[:], in0=pos[:], scalar=1.0, in1=sel[:], op0=ALU.add, op1=ALU.mult), reads=[r_pos, r_sel], writes=[r_v])
                pA1, r_pA1 = sm("q_pA1", NTg)
                pB1, r_pB1 = sm("q_pB1", NTg)
                bc = lambda t: t[:].unsqueeze(2).broadcast_to(shp)
                S.op("dve", lambda e: e.tensor_reduce(out=pA1[:], in_=v[:], axis=AX.X, op=ALU.max), reads=[r_v], writes=[r_pA1])
                S.op("dve", lambda e: e.tensor_tensor(out=t8[:], in0=v[:], in1=bc(pA1), op=ALU.is_equal), reads=[r_v, r_pA1], writes=[r_t8])
                S.op("dve", lambda e: e.tensor_tensor(out=pos[:], in0=t8[:], in1=cb[:], op=ALU.mult), reads=[r_t8, r_cb], writes=[r_pos])
                S.op("dve", lambda e: e.tensor_reduce(out=wA[:], in_=pos[:], axis=AX.X, op=ALU.add), reads=[r_pos], writes=[r_wA])
                S.op("dve", lambda e: e.tensor_scalar(out=wB[:], in0=wA[:], scalar1=-1.0, scalar2=1.0, op0=ALU.mult, op1=ALU.add), reads=[r_wA], writes=[r_wB])
                S.op("dve", lambda e: e.tensor_tensor(out=t8[:], in0=t8[:], in1=v[:], op=ALU.mult), reads=[r_t8, r_v], writes=[r_t8])
                S.op("dve", lambda e: e.tensor_tensor(out=t8[:], in0=v[:], in1=t8[:], op=ALU.subtract), reads=[r_t8, r_v], writes=[r_t8])
                S.op("dve", lambda e: e.tensor_reduce(out=pB1[:], in_=t8[:], axis=AX.X, op=ALU.max), reads=[r_t8], writes=[r_pB1])
                S.op("dve", lambda e: e.tensor_scalar(out=posA[:], in0=pA1[:], scalar1=-1.0, scalar2=None, op0=ALU.add), reads=[r_pA1], writes=[r_posA])
                S.op("dve", lambda e: e.tensor_scalar(out=posB[:], in0=pB1[:], scalar1=-1.0, scalar2=None, op0=ALU.add), reads=[r_pB1], writes=[r_posB])
                S.op("dve", lambda e: e.tensor_tensor(out=cs[:], in0=base[:], in1=np_e[:], op=ALU.add), reads=[r_base, r_np], writes=[r_cs])
                jg, r_jg = tile1(ph, nc, "q_jg", [128, NSLOT, 8], F32)
                gp, r_gp = tile1(ph, nc, "q_gp", [128, NG], F32)
                S.dma("sp", lambda e: e.dma_start(out=jg[:], in_=self.cst_d["jgrid"].rearrange("p (j e) -> p j e", e=8)), writes=[r_jg])
                S.dma("sp", lambda e: e.dma_start(out=gp[:], in_=self.cst_d["gp"][:, :]), writes=[r_gp])
                S.op("dve", lambda e: e.tensor_tensor(out=jg[:], in0=cs[:].unsqueeze(1).broadcast_to([128, NSLOT, 8]), in1=jg[:], op=ALU.is_le), reads=[r_cs, r_jg], writes=[r_jg])
                eid, r_eid = sm("q_eid", NSLOT)
                S.op("dve", lambda e: e.tensor_reduce(out=eid[:], in_=jg[:], axis=AX.X, op=ALU.add), reads=[r_jg], writes=[r_eid])
                S.op("dve", lambda e: e.tensor_scalar(out=eid[:], in0=eid[:], scalar1=7.0, scalar2=None, op0=ALU.min), reads=[r_eid], writes=[r_eid])
                S.op("dve", lambda e: e.scalar_tensor_tensor(out=idxW[:], in0=eid[:].unsqueeze(2).broadcast_to([128, NSLOT, NG]), scalar=float(NG * 128), in1=gp[:].unsqueeze(1).broadcast_to([128, NSLOT, NG]), op0=ALU.mult, op1=ALU.add),
                     reads=[r_eid, r_gp], writes=[r_idxW])
                if cfg.DEBUG:
                    self.dump("posA", posA, r_posA, [128, NTg], I32)
                    self.dump("posB", posB, r_posB, [128, NTg], I32)
                    self.dump("wA", wA, r_wA, [128, NTg], F32)
                    self.dump("idxW", idxW, r_idxW, [128, NSLOT, NG], I32)
                    self.dump("eid", eid, r_eid, [128, NSLOT], F32)
                    self.dump("cs", cs, r_cs, [128, 8], F32)
                    self.dump("jg", jg, r_jg, [128, NSLOT, 8], F32)
                    self.dump("incl", incl, r_incl, shp, F32)
                    self.dump("Cs", Cs, r_Cs, shp, F32)
                    self.dump("np", np_e, r_np, [128, 8], F32)
                    self.dump("base", base, r_base, [128, 8], F32)
                self.end_phase()
                if cfg.STOP == "route":
                    return
            with ExitStack() as ph:
                hp = TPool(ph, nc, "q_h", [128, D], BF16, 3)
                for g in range(NTg):
                    ht, r_ht = hp.next()
                    S.dma("sp", lambda e, ht=ht, g=g: e.dma_start(out=ht[:], in_=self.H2[g * 128:(g + 1) * 128, :]), reads=[self.r_H2], writes=[r_ht])
                    for (pt, pr) in ((posA, r_posA), (posB, r_posB)):
                        S.dma("pool", lambda e, ht=ht, g=g, pt=pt: e.indirect_dma_start(out=self.HS, out_offset=bass.IndirectOffsetOnAxis(ap=pt[:, g:g + 1], axis=0), in_=ht[:], in_offset=None),
                              reads=[r_ht, pr], writes=[self.r_HS])
                self.end_phase()
                if cfg.STOP == "scatter":
                    return
            with ExitStack() as ph:
                hrp = TPool(ph, nc, "q_hr", [128, D], BF16, 2)
                hTp = TPool(ph, nc, "q_hT", [128, KC, 512], BF16, 2)
                wgp = TPool(ph, nc, "q_wg", [128, KC * 512], BF16, 2)
                wup = TPool(ph, nc, "q_wu", [128, KC * 512], BF16, 2)
                wdp = TPool(ph, nc, "q_wd", [128, 4 * D], BF16, 2)
                acc, r_acc = tile1(ph, nc, "q_acc", [128, 4, D], F32)
                actp = TPool(ph, nc, "q_act", [128, 4, 512], BF16, 2)
                sgp = TPool(ph, nc, "q_sg", [128, 512], F32, 2)
                psT = TPool(ph, nc, "q_psT", [128, KC, 128], BF16, 1, psum=True)
                psg = TPool(ph, nc, "q_psg", [128, 512], F32, 2, psum=True)
                psu = TPool(ph, nc, "q_psu", [128, 512], F32, 2, psum=True)
                pso = TPool(ph, nc, "q_pso", [128, 512], F32, 2, psum=True)
                def prep_slot(j):
                    hT, r_hT = hTp.next()
                    for tl in range(4):
                        hr, r_hr = hrp.next()
                        S.dma("sp", lambda e, hr=hr, j=j, tl=tl: e.dma_start(out=hr[:], in_=self.HS[j * 512 + tl * 128:j * 512 + (tl + 1) * 128, :]), reads=[self.r_HS], writes=[r_hr])
                        ps, r_ps = psT.next()
                        for kc in range(KC):
                            S.op("pe", lambda e, ps=ps, hr=hr, kc=kc: e.transpose(out=ps[:, kc, :], in_=hr[:, kc * 128:(kc + 1) * 128], identity=ident[:]),
                                 reads=[r_hr, r_ident], writes=[r_ps], signal=(kc == KC - 1))
                        S.op("act", lambda e, ps=ps, hT=hT, tl=tl: e.copy(out=hT[:, :, tl * 128:(tl + 1) * 128], in_=ps[:]), reads=[r_ps], writes=[r_hT])
                    return hT, r_hT

                nxt = prep_slot(0)
                for j in range(NSLOT):
                    hT, r_hT = nxt
                    for gr in range(NG):
                        if gr == max(NG - 2, 0) and j + 1 < NSLOT:
                            nxt = prep_slot(j + 1)
                        wts = []
                        for (pool_, src_) in ((wgp, self.WGB), (wup, self.WUB), (wdp, self.WDB)):
                            wt, r_wt = pool_.next()
                            S.dma("pool", lambda e, wt=wt, src_=src_, j=j, gr=gr: e.indirect_dma_start(out=wt[:], out_offset=None, in_=src_, in_offset=bass.IndirectOffsetOnAxis(ap=idxW[:, j, gr:gr + 1], axis=0)),
                                  reads=[r_idxW, self.r_WB], writes=[r_wt])
                            wts.append((wt, r_wt))
                        (wg_, r_wg), (wu_, r_wu), (wd_, r_wd) = wts
                        wg = wg_[:].rearrange("p (kc n) -> p kc n", n=512)
                        wu = wu_[:].rearrange("p (kc n) -> p kc n", n=512)
                        wd = wd_[:].rearrange("p (c n) -> p c n", n=D)
                        act, r_act = actp.next()
                        for c in range(4):
                            pg, r_pg = psg.next()
                            pu, r_pu = psu.next()
                            for kc in range(KC):
                                S.op("pe", lambda e, pg=pg, wg=wg, kc=kc, c=c, hT=hT: e.matmul(pg[:], lhsT=wg[:, kc, c * 128:(c + 1) * 128], rhs=hT[:, kc, :], start=(kc == 0), stop=(kc == KC - 1)),
                                     reads=[r_wg, r_hT], writes=[r_pg], signal=(kc == KC - 1))
                            for kc in range(KC):
                                S.op("pe", lambda e, pu=pu, wu=wu, kc=kc, c=c, hT=hT: e.matmul(pu[:], lhsT=wu[:, kc, c * 128:(c + 1) * 128], rhs=hT[:, kc, :], start=(kc == 0), stop=(kc == KC - 1)),
                                     reads=[r_wu, r_hT], writes=[r_pu], signal=(kc == KC - 1))
                            sg, r_sg = sgp.next()
                            S.op("act", lambda e, sg=sg, pg=pg: e.activation(out=sg[:], in_=pg[:], func=AF.Silu), reads=[r_pg], writes=[r_sg])
                            S.op("dve", lambda e, act=act, sg=sg, pu=pu, c=c: e.tensor_tensor(out=act[:, c, :], in0=sg[:], in1=pu[:], op=ALU.mult), reads=[r_sg, r_pu], writes=[r_act])
                        for tl in range(4):
                            for nb in range(4):
                                po, r_po = pso.next()
                                for c in range(4):
                                    S.op("pe", lambda e, po=po, act=act, wd=wd, c=c, tl=tl, nb=nb: e.matmul(po[:], lhsT=act[:, c, tl * 128:(tl + 1) * 128], rhs=wd[:, c, nb * 512:(nb + 1) * 512], start=(c == 0), stop=(c == 3)),
                                         reads=[r_act, r_wd], writes=[r_po], signal=(c == 3))
                                if gr == 0:
                                    S.op("act", lambda e, po=po, tl=tl, nb=nb: e.copy(out=acc[:, tl, nb * 512:(nb + 1) * 512], in_=po[:]), reads=[r_po], writes=[r_acc])
                                else:
                                    S.op("dve", lambda e, po=po, tl=tl, nb=nb: e.tensor_tensor(out=acc[:, tl, nb * 512:(nb + 1) * 512], in0=po[:], in1=acc[:, tl, nb * 512:(nb + 1) * 512], op=ALU.add), reads=[r_po, r_acc], writes=[r_acc])
                            if gr == NG - 1:
                                S.dma("sp", lambda e, j=j, tl=tl: e.dma_start(out=self.YP[j * 512 + tl * 128:j * 512 + (tl + 1) * 128, :], in_=acc[:, tl, :]), reads=[r_acc], writes=[self.r_YP])
                self.end_phase()
            if cfg.STOP == "slots":
                return
            with ExitStack() as ph:
                fn, r_fn = tile1(ph, nc, "z_fn", [128, D], F32)
                S.dma("sp", lambda e: e.dma_start(out=fn[:], in_=self.final_norm.unsqueeze(0).partition_broadcast(128)), writes=[r_fn])
                g2 = []
                for s in range(cfg.NSEQ):
                    g2.append(tile1(ph, nc, "z_g2%d" % s, [128, D], F32))
                    self.bcast_row(g2[s][0], g2[s][1], self.MOD[1, s:s + 1, 5 * D:6 * D], D)
                xp = TPool(ph, nc, "z_x", [128, D], F32, 2)
                yap = TPool(ph, nc, "z_ya", [128, D], F32, 2)
                ybp = TPool(ph, nc, "z_yb", [128, D], F32, 2)
                op_ = TPool(ph, nc, "z_o", [128, D], F32, 2)
                junk, r_junk = tile1(ph, nc, "z_junk", [128, D], BF16)
                stp = TPool(ph, nc, "z_st", [128, 4], F32, 2)
                for g in range(NTg):
                    s, lt = g // NTl, g % NTl
                    tile = NCT + lt
                    xt, r_xt = xp.next()
                    S.dma("sp", lambda e, xt=xt, s=s, tile=tile: e.dma_start(out=xt[:], in_=self.XR[s, tile * 128:(tile + 1) * 128, :]), reads=[self.r_XR[s][tile]], writes=[r_xt])
                    ya, r_ya = yap.next()
                    yb, r_yb = ybp.next()
                    for (yt, r_yt, pt, pr) in ((ya, r_ya, posA, r_posA), (yb, r_yb, posB, r_posB)):
                        S.dma("pool", lambda e, yt=yt, pt=pt, g=g: e.indirect_dma_start(out=yt[:], out_offset=None, in_=self.YP, in_offset=bass.IndirectOffsetOnAxis(ap=pt[:, g:g + 1], axis=0)),
                              reads=[self.r_YP, pr], writes=[r_yt])
                    S.op("dve", lambda e, ya=ya, g=g: e.tensor_scalar(out=ya[:], in0=ya[:], scalar1=wA[:, g:g + 1], scalar2=None, op0=ALU.mult), reads=[r_ya, r_wA], writes=[r_ya])
                    S.op("dve", lambda e, ya=ya, yb=yb, g=g: e.scalar_tensor_tensor(out=ya[:], in0=yb[:], scalar=wB[:, g:g + 1], in1=ya[:], op0=ALU.mult, op1=ALU.add), reads=[r_ya, r_yb, r_wB], writes=[r_ya])
                    S.op("dve", lambda e, ya=ya, s=s: e.tensor_tensor(out=ya[:], in0=ya[:], in1=g2[s][0][:], op=ALU.mult), reads=[r_ya, g2[s][1]], writes=[r_ya])
                    S.op("dve", lambda e, ya=ya, xt=xt: e.tensor_tensor(out=xt[:], in0=ya[:], in1=xt[:], op=ALU.add), reads=[r_ya, r_xt], writes=[r_xt])
                    st, r_st = stp.next()
                    S.op("act", lambda e, xt=xt, st=st: e.activation(out=junk[:], in_=xt[:], func=AF.Square, accum_out=st[:, 0:1]), reads=[r_xt], writes=[r_junk, r_st])
                    S.op("dve", lambda e, st=st: e.tensor_scalar(out=st[:, 1:2], in0=st[:, 0:1], scalar1=1.0 / D, scalar2=EPS, op0=ALU.mult, op1=ALU.add), reads=[r_st], writes=[r_st])
                    S.op("act", lambda e, st=st: e.activation(out=st[:, 2:3], in_=st[:, 1:2], func=AF.Sqrt), reads=[r_st], writes=[r_st])
                    S.op("dve", lambda e, st=st: e.reciprocal(out=st[:, 3:4], in_=st[:, 2:3]), reads=[r_st], writes=[r_st])
                    ot, r_ot = op_.next()
                    S.op("dve", lambda e, ot=ot, xt=xt, st=st: e.scalar_tensor_tensor(out=ot[:], in0=xt[:], scalar=st[:, 3:4], in1=fn[:], op0=ALU.mult, op1=ALU.mult), reads=[r_xt, r_st, r_fn], writes=[r_ot])
                    S.dma("sp", lambda e, ot=ot, s=s, lt=lt: e.dma_start(out=self.out[s, lt * 128:(lt + 1) * 128, :], in_=ot[:]), reads=[r_ot], writes=[self.r_out])
                self.end_phase()


def make_in_maps(cfg, inputs, ncores):
    consts = host_consts(cfg)
    maps = []
    for core in range(ncores):
        b0 = core * cfg.NSEQ
        m = {}
        m["x_in"] = np.ascontiguousarray(inputs["x"][b0:b0 + cfg.NSEQ])
        m["ctx_in"] = np.ascontiguousarray(inputs["ctx"][b0:b0 + cfg.NSEQ])
        m["cvec"] = np.ascontiguousarray(np.concatenate([inputs["c"][b0:b0 + cfg.NSEQ], inputs["c_ctx"][None, :]], 0))
        for k in ("w_mod", "b_mod", "norm_mix", "norm_ffn", "w_in", "hg_lb_fwd", "hg_lb_bwd", "hg_norm", "attn_sink",
                  "w_branch_a", "w_branch_b", "w_out", "ffn_w_gate", "ffn_w_up", "ffn_w_down", "moe_router",
                  "moe_w_gate", "moe_w_up", "moe_w_down", "final_norm"):
            m[k] = inputs[k]
        for k, v in consts.items():
            m["c_" + k] = v
        maps.append(m)
    return maps


_CACHE = {}


def kernel(**inputs):
    cfg = Cfg()
    inputs = {k: np.asarray(v) for k, v in inputs.items()}
    if "nc" not in _CACHE:
        _CACHE["nc"] = K(cfg).build()
    nc = _CACHE["nc"]
    ncores = 16 // cfg.NSEQ
    maps = make_in_maps(cfg, inputs, ncores)
    res = run_bass_kernel_spmd(nc, maps, core_ids=list(range(ncores)))
    out = np.concatenate([np.asarray(r["out"]) for r in res.results], axis=0)
    return out.astype(np.float32, copy=False)
```

```python
import numpy as np
from contextlib import ExitStack
import concourse.bass as bass
import concourse.mybir as mybir
from concourse.bass_utils import run_bass_kernel_spmd

F32 = mybir.dt.float32
BF16 = mybir.dt.bfloat16
I32 = mybir.dt.int32
AF = mybir.ActivationFunctionType
ALU = mybir.AluOpType
AX = mybir.AxisListType

EPS = 1e-6
NEG = -30000.0


class Res:
    __slots__ = ("name", "last_w", "readers")

    def __init__(self, name=""):
        self.name = name
        self.last_w = None
        self.readers = {}


class Sched:
    COMPUTE = ("pe", "act", "dve", "pool")
    NDMA = 8

    def __init__(self, nc, es):
        self.nc = nc
        self.sem = {}
        for e in self.COMPUTE:
            self.sem[e] = es.enter_context(nc.semaphore("sem_" + e))
        self.count = {e: 0 for e in self.COMPUTE}
        self.engs = ("pe", "act", "dve", "pool", "sp")
        self.seen = {e: {} for e in self.engs}
        self.q = {e: [] for e in self.engs}
        self.dma_uses = {}
        self.dma_i = {}
        for e in ("sp", "act", "pool"):
            for i in range(self.NDMA):
                self.sem[("d", e, i)] = es.enter_context(nc.semaphore("dsem_%s%d" % (e, i)))
            self.dma_uses[e] = [0] * self.NDMA
            self.dma_i[e] = 0
        self.ninstr = 0

    def _deps(self, reads, writes):
        deps = {}
        for r in reads:
            ev = r.last_w
            if ev is not None and deps.get(ev[0], 0) < ev[1]:
                deps[ev[0]] = ev[1]
        for w in writes:
            ev = w.last_w
            if ev is not None and deps.get(ev[0], 0) < ev[1]:
                deps[ev[0]] = ev[1]
            for k, v in w.readers.items():
                if deps.get(k, 0) < v:
                    deps[k] = v
        return deps

    def _waits(self, eng, deps, skip=None):
        waits = []
        seen = self.seen[eng]
        for k, v in deps.items():
            if k == skip or seen.get(k, 0) >= v:
                continue
            seen[k] = v
            waits.append((k, v))
        return waits

    def _commit(self, ev, reads, writes):
        k, v = ev
        for r in reads:
            if r.readers.get(k, 0) < v:
                r.readers[k] = v
        for w in writes:
            w.last_w = ev
            w.readers = {}

    def op(self, eng, fn, reads=(), writes=(), signal=True):
        deps = self._deps(reads, writes)
        waits = self._waits(eng, deps, skip=("pe" if eng == "pe" else None))
        sem = self.sem
        if signal:
            self.count[eng] += 1
            ev = (eng, self.count[eng])
            own = sem[eng]
        else:
            ev = (eng, self.count[eng] + 1)
            own = None

        def run(e, waits=waits, fn=fn, own=own):
            for k, v in waits:
                e.wait_ge(sem[k], v)
            ins = fn(e)
            if own is not None:
                ins.then_inc(own, 1)
        self.q[eng].append(run)
        self._commit(ev, reads, writes)
        self.ninstr += 1
        return ev

    def dma(self, eng, fn, reads=(), writes=()):
        i = self.dma_i[eng] % self.NDMA
        self.dma_i[eng] += 1
        key = ("d", eng, i)
        prev = 16 * self.dma_uses[eng][i]
        self.dma_uses[eng][i] += 1
        ev = (key, prev + 16)
        deps = self._deps(reads, writes)
        if prev > 0 and deps.get(key, 0) < prev:
            deps[key] = prev
        waits = self._waits(eng, deps)
        sem = self.sem
        own = sem[key]

        def run(e, waits=waits, fn=fn, own=own):
            for k, v in waits:
                e.wait_ge(sem[k], v)
            fn(e).then_inc(own, 16)
        self.q[eng].append(run)
        self._commit(ev, reads, writes)
        self.ninstr += 1
        return ev

    def barrier(self):
        targets = {}
        for e in self.COMPUTE:
            if self.count[e] > 0:
                targets[e] = self.count[e]
        for e in ("sp", "act", "pool"):
            for i in range(self.NDMA):
                if self.dma_uses[e][i] > 0:
                    targets[("d", e, i)] = 16 * self.dma_uses[e][i]
        sem = self.sem
        for eng in self.engs:
            waits = self._waits(eng, targets, skip=(eng if eng in self.COMPUTE else None))
            if waits:
                def run(e, waits=waits):
                    for k, v in waits:
                        e.wait_ge(sem[k], v)
                self.q[eng].append(run)

    def flush(self):
        nc = self.nc
        q = self.q
        with nc.Block() as block:
            if q["pe"]:
                @block.tensor
                def _(e):
                    for f in q["pe"]:
                        f(e)
            if q["act"]:
                @block.scalar
                def _(e):
                    for f in q["act"]:
                        f(e)
            if q["dve"]:
                @block.vector
                def _(e):
                    for f in q["dve"]:
                        f(e)
            if q["pool"]:
                @block.gpsimd
                def _(e):
                    for f in q["pool"]:
                        f(e)
            if q["sp"]:
                @block.sync
                def _(e):
                    for f in q["sp"]:
                        f(e)
        self.q = {e: [] for e in self.engs}


_uid = [0]


class TPool:
    def __init__(self, es, nc, name, shape, dtype, n, psum=False):
        self.tiles = []
        for i in range(n):
            _uid[0] += 1
            nm = "%s_%d" % (name, _uid[0])
            mk = nc.psum_tensor if psum else nc.sbuf_tensor
            t = es.enter_context(mk(nm, list(shape), dtype))
            self.tiles.append((t, Res(nm)))
        self.i = 0

    def next(self):
        t = self.tiles[self.i % len(self.tiles)]
        self.i += 1
        return t


def tile1(es, nc, name, shape, dtype, psum=False):
    return TPool(es, nc, name, shape, dtype, 1, psum).tiles[0]


class Cfg:
    def __init__(self, NSEQ=2, SEQ=2048, CTX=256, DFF=5632, DEXP=7168, MOE="routed", DEBUG=False, STOP=None):
        self.DEBUG = DEBUG
        self.STOP = STOP
        self.D = 2048
        self.KC = 16
        self.NSEQ = NSEQ
        self.NR = NSEQ + 1
        self.SEQ = SEQ
        self.CTX = CTX
        self.T = CTX + SEQ
        self.NT = self.T // 128
        self.NCT = CTX // 128
        self.DFF = DFF
        self.DEXP = DEXP
        self.NE = 8
        self.NIN = 10496
        self.NCH = self.T // 64
        self.NCC = CTX // 64
        self.MOE = MOE
        self.PUMP = 7
        self.NSLOT = (2 * NSEQ * SEQ) // 512 + 7
        self.tblocks = [(0, CTX)] + [(CTX + i * 512, 512) for i in range(SEQ // 512)]
        self.lat_blocks = self.tblocks[1:]
        self.groups = [list(range(0, self.NCC))] + [list(range(self.NCC + 8 * i, self.NCC + 8 * i + 8))
                                                    for i in range((SEQ // 64) // 8)]


COL = dict(hq=0, ff=1024, fb=2048, hv=3072, hg=4096, aq=5120, ak=6144, av=6272, ga=6400, gb=8448)


def host_consts(cfg):
    c = {}
    c["ident"] = np.eye(128, dtype=np.float32)
    i = np.arange(64)
    sb_, tb_ = i[:, None] // 16, i[None, :] // 16
    c["maskD_f"] = ((i[:, None] <= i[None, :]) & (sb_ == tb_)).astype(np.float32)
    c["maskO_f"] = (sb_ < tb_).astype(np.float32)
    c["maskD_b"] = ((i[:, None] >= i[None, :]) & (sb_ == tb_)).astype(np.float32)
    c["maskO_b"] = (sb_ > tb_).astype(np.float32)
    j = np.arange(128)
    c["mneg_prev"] = np.where(j[None, :] <= j[:, None], 0.0, NEG).astype(np.float32)
    c["mneg_next"] = np.where(j[:, None] <= j[None, :], 0.0, NEG).astype(np.float32)
    c["ustrict"] = (j[:, None] < j[None, :]).astype(np.float32)
    t = np.arange(cfg.SEQ)
    row = (t // 64).astype(np.float32)
    col = (t % 64).astype(np.float32)
    inv = (10000.0 ** (-np.arange(16, dtype=np.float32) / 16)).astype(np.float32)
    d = np.arange(64)
    axis = d // 32
    freq = d % 16
    second = (d % 32) >= 16
    pos = np.where(axis[:, None] == 0, row[None, :], col[None, :]).astype(np.float32)
    ang = (pos * inv[freq][:, None]).astype(np.float32)
    cos = np.cos(ang).astype(np.float32)
    sin = np.sin(ang).astype(np.float32)
    sin_s = np.where(second[:, None], sin, -sin).astype(np.float32)
    c["cosT"] = np.concatenate([cos, cos], 0)
    c["sinT"] = np.concatenate([sin_s, sin_s], 0)
    pm = np.zeros((128, 128), np.float32)
    for m in range(128):
        dd = m % 64
        partner = dd - 16 if (dd % 32) >= 16 else dd + 16
        pm[(m // 64) * 64 + partner, m] = 1.0
    c["pm"] = pm
    c["ones"] = np.ones((128, 128), np.float32)
    NG = cfg.DEXP // 512
    c["jgrid"] = np.broadcast_to(np.repeat(512.0 * np.arange(cfg.NSLOT, dtype=np.float32), 8)[None, :], (128, cfg.NSLOT * 8)).copy()
    KMAX = max(1, (cfg.NSEQ * cfg.SEQ) // 512)
    c["kgrid"] = np.broadcast_to(np.tile(512.0 * np.arange(KMAX, dtype=np.float32), 8)[None, :], (128, 8 * KMAX)).copy()
    c["gp"] = (np.arange(NG, dtype=np.float32)[None, :] * 128 + np.arange(128, dtype=np.float32)[:, None]).astype(np.float32)
    return c


CONST_SHAPES = lambda cfg: dict(ident=[128, 128], maskD_f=[64, 64], maskO_f=[64, 64], maskD_b=[64, 64], maskO_b=[64, 64], mneg_prev=[128, 128],
                                mneg_next=[128, 128], ustrict=[128, 128], cosT=[128, cfg.SEQ],
                                sinT=[128, cfg.SEQ], pm=[128, 128], ones=[128, 128], jgrid=[128, cfg.NSLOT * 8], gp=[128, cfg.DEXP // 512], kgrid=[128, 8 * max(1, (cfg.NSEQ * cfg.SEQ) // 512)])


class K:
    def __init__(self, cfg):
        self.cfg = cfg
        nc = self.nc = bass.Bass("TRN2", target_bir_lowering=False)
        D = cfg.D
        T = cfg.T
        def din(name, shape, dt=F32):
            return nc.dram_tensor(name, list(shape), dt, kind="ExternalInput").ap()
        def scr(name, shape, dt):
            return nc.dram_tensor(name, list(shape), dt, kind=("ExternalOutput" if cfg.DEBUG else "Internal")).ap()
        self.x_in = din("x_in", [cfg.NSEQ, cfg.SEQ, D])
        self.ctx_in = din("ctx_in", [cfg.NSEQ, cfg.CTX, D])
        self.cvec = din("cvec", [cfg.NR, D])
        self.w_mod = din("w_mod", [2, D, 6 * D])
        self.b_mod = din("b_mod", [2, 6 * D])
        self.norm_mix = din("norm_mix", [2, D])
        self.norm_ffn = din("norm_ffn", [2, D])
        self.w_in = din("w_in", [2, D, cfg.NIN])
        self.lb_f = din("hg_lb_fwd", [2, 1024])
        self.lb_b = din("hg_lb_bwd", [2, 1024])
        self.hg_norm = din("hg_norm", [2, 1024])
        self.sink = din("attn_sink", [2, 16])
        self.w_a = din("w_branch_a", [2, 1024, D])
        self.w_b = din("w_branch_b", [2, 1024, D])
        self.w_o = din("w_out", [2, D, D])
        self.ffn_wg = din("ffn_w_gate", [1, D, cfg.DFF])
        self.ffn_wu = din("ffn_w_up", [1, D, cfg.DFF])
        self.ffn_wd = din("ffn_w_down", [1, cfg.DFF, D])
        self.router = din("moe_router", [1, D, 8])
        self.moe_wg = din("moe_w_gate", [1, 8, D, cfg.DEXP])
        self.moe_wu = din("moe_w_up", [1, 8, D, cfg.DEXP])
        self.moe_wd = din("moe_w_down", [1, 8, cfg.DEXP, D])
        self.final_norm = din("final_norm", [D])
        self.cst_d = {k: din("c_" + k, s) for k, s in CONST_SHAPES(cfg).items()}
        self.out = nc.dram_tensor("out", [cfg.NSEQ, cfg.SEQ, D], F32, kind="ExternalOutput").ap()
        self.MOD = scr("MOD", [2, cfg.NR, 6 * D], F32)
        self.XR = scr("XR", [cfg.NSEQ, T, D], F32)
        self.QT = scr("QT", [8, 128, T], BF16)
        self.GT = scr("GT", [2, 8, 128, T], F32)
        self.KT = scr("KT", [2, 8, 128, T], BF16)
        self.SGT = scr("SGT", [8, 128, T], BF16)
        self.V = scr("V", [T, 1024], BF16)
        self.QA = scr("QA", [8, 128, T], BF16)
        self.KA = scr("KA", [2, 128, T], BF16)
        self.VA = scr("VA", [T, 128], BF16)
        self.GA = scr("GA", [16, 128, T], BF16)
        self.GB = scr("GB", [16, 128, T], BF16)
        self.AT = scr("AT", [8, 128, T], BF16)
        self.OAT = scr("OAT", [8, 128, T], BF16)
        NG = cfg.DEXP // 512
        NTg = cfg.NSEQ * cfg.SEQ // 128
        if cfg.MOE == "routed":
            self.H2 = scr("H2", [cfg.NSEQ * cfg.SEQ, D], BF16)
            self.HS = scr("HS", [cfg.NSLOT * 512, D], BF16)
            self.YP = scr("YP", [cfg.NSLOT * 512, D], F32)
            self.SELD = scr("SELD", [128, NTg, 8], F32)
            self.CBD = scr("CBD", [128, NTg, 8], F32)
            self.WGB = scr("WGB", [8 * NG * 128, 16 * 512], BF16)
            self.WUB = scr("WUB", [8 * NG * 128, 16 * 512], BF16)
            self.WDB = scr("WDB", [8 * NG * 128, 4 * D], BF16)
        self.r_H2 = Res(); self.r_HS = Res(); self.r_YP = Res(); self.r_SELD = Res(); self.r_WB = Res()
        self.pre_jobs = self.prepass_jobs() if cfg.MOE == "routed" else []
        self.pump_n = 0
        self.dbg = {}
        self.r_MOD = Res('MOD')
        self.r_out = Res('out')
        self.r_QT = [Res() for _ in range(8)]
        self.r_SGT = [Res() for _ in range(8)]
        self.r_GA = [Res() for _ in range(16)]
        self.r_GB = [Res() for _ in range(16)]
        self.r_GT = [[Res() for _ in range(8)] for _ in range(2)]
        self.r_KT = [[Res() for _ in range(8)] for _ in range(2)]
        self.r_QA = [Res() for _ in range(8)]
        self.r_KA = [Res() for _ in range(2)]
        self.r_V = Res()
        self.r_VA = Res()
        self.r_AT = [Res() for _ in range(8)]
        self.r_OAT = [Res() for _ in range(8)]
        self.r_XR = [[Res('XR') for _ in range(cfg.NT)] for _ in range(cfg.NSEQ)]

    def xsrc(self, first, s, tile):
        cfg = self.cfg
        if first:
            if tile < cfg.NCT:
                return self.ctx_in[s, tile * 128:(tile + 1) * 128, :]
            tt = tile - cfg.NCT
            return self.x_in[s, tt * 128:(tt + 1) * 128, :]
        return self.XR[s, tile * 128:(tile + 1) * 128, :]

    def load_wblock(self, pool, w2d, n0, nn, nk=None, k0=0, eng="pool"):
        S = self.S
        nk = nk if nk is not None else w2d.shape[0] // 128
        t, r = pool.next()
        src = w2d[k0 * 128:(k0 + nk) * 128, n0:n0 + nn].rearrange("(kc p) n -> p kc n", p=128)
        S.dma(eng, lambda e: e.dma_start(out=t[:, 0:nk, 0:nn], in_=src), writes=[r])
        if self.pump_n:
            self.pump(self.pump_n)
        return t, r

    def bcast_row(self, t, r, row_ap, n, eng="sp"):
        self.S.dma(eng, lambda e: e.dma_start(out=t[:, 0:n], in_=row_ap.partition_broadcast(128)), writes=[r])

    def build(self):
        cfg = self.cfg
        nc = self.nc
        with ExitStack() as es:
            self.S = S = Sched(nc, es)
            es.enter_context(nc.allow_non_contiguous_dma(reason='small strided layout loads'))
            self.C = {}
            for k in ("ident", "mneg_prev", "mneg_next", "ustrict", "pm", "ones"):
                shp = CONST_SHAPES(cfg)[k]
                t, r = tile1(es, nc, "c_" + k, shp, BF16)
                S.dma("pool", lambda e, t=t, k=k: e.dma_start(out=t[:], in_=self.cst_d[k][:, :]), writes=[r])
                self.C[k] = (t, r)
            stop = cfg.STOP
            self.phase_mod()
            if stop == "mod":
                return nc
            for l in range(2):
                for s in range(cfg.NSEQ):
                    with ExitStack() as bs:
                        self.BIG = tile1(bs, nc, "BIG", [128, cfg.KC, cfg.T], BF16)
                        self.phase_norm(l, s, 1)
                        if stop == "norm":
                            self.dump("BIG", self.BIG[0], self.BIG[1], [128, cfg.KC, cfg.T], BF16)
                            self.end_phase()
                            return nc
                        self.pump_n = 3
                        self.phase_inproj(l, s)
                        self.pump_n = 0
                    if stop == "inproj":
                        return nc
                    self.phase_scan(l, s)
                    if stop in ("scan", "scanprep"):
                        return nc
                    self.phase_attn(l, s)
                    if stop == "attn":
                        return nc
                    with ExitStack() as bs:
                        self.BIG = tile1(bs, nc, "BIG", [128, cfg.KC, cfg.T], BF16)
                        self.phase_merge(l, s)
                        if stop == "merge":
                            return nc
                        self.phase_norm(l, s, 2)
                        if l == 1 and cfg.MOE == "routed":
                            self.route_local(s)
                        else:
                            self.phase_swiglu(l, s)
                    if stop == "ffn":
                        return nc
                if stop == "layer0":
                    return nc
            if cfg.MOE == "routed":
                self.phase_moe_routed()
            else:
                self.phase_final()
        return nc

    def dump(self, name, tile, r, shape, dt):
        if not self.cfg.DEBUG:
            return
        d = self.nc.dram_tensor("dbg_" + name, list(shape), dt, kind="ExternalOutput").ap()
        self.S.dma("sp", lambda e: e.dma_start(out=d, in_=tile[:]), reads=[r])

    def end_phase(self):
        self.S.barrier()
        self.S.flush()

    def phase_mod(self):
        cfg, nc, S = self.cfg, self.nc, self.S
        D, KC, NR = cfg.D, cfg.KC, cfg.NR
        with ExitStack() as ph:
            if cfg.MOE == "routed":
                zt, r_zt = tile1(ph, nc, "q_z", [128, 4, D], BF16)
                S.op("dve", lambda e: e.memset(zt[:], 0.0), writes=[r_zt])
                for j in range(cfg.NSLOT):
                    S.dma("sp", lambda e, j=j: e.dma_start(out=self.HS[j * 512:(j + 1) * 512, :].rearrange("(a p) d -> p a d", p=128), in_=zt[:]), reads=[r_zt])
            cT, r_cT = tile1(ph, nc, "cT", [128, KC, NR], F32)
            cTb, r_cTb = tile1(ph, nc, "cTb", [128, KC, NR], BF16)
            for r in range(NR):
                src = self.cvec[r:r + 1, :].rearrange("o (kc p) -> p kc o", p=128)
                S.dma("sp", lambda e, src=src, r=r: e.dma_start(out=cT[:, :, r:r + 1], in_=src), writes=[r_cT])
            S.op("act", lambda e: e.activation(out=cTb[:], in_=cT[:], func=AF.Silu), reads=[r_cT], writes=[r_cTb])
            wpool = TPool(ph, nc, "wmod", [128, KC, 512], BF16, 3)
            pspool = TPool(ph, nc, "psmod", [NR, 512], F32, 2, psum=True)
            bias, r_bias = tile1(ph, nc, "bmod", [NR, 6 * D], F32)
            res, r_res = tile1(ph, nc, "resmod", [NR, 6 * D], F32)
            for l in range(2):
                S.dma("sp", lambda e, l=l: e.dma_start(out=bias[:], in_=self.b_mod[l:l + 1, :].partition_broadcast(NR)), writes=[r_bias])
                nb = 6 * D // 512
                for b in range(nb):
                    w, r_w = self.load_wblock(wpool, self.w_mod[l], b * 512, 512)
                    ps, r_ps = pspool.next()
                    for kc in range(KC):
                        S.op("pe", lambda e, ps=ps, w=w, kc=kc: e.matmul(ps[:], lhsT=cTb[:, kc, :], rhs=w[:, kc, :], start=(kc == 0), stop=(kc == KC - 1)),
                             reads=[r_cTb, r_w], writes=[r_ps], signal=(kc == KC - 1))
                    S.op("dve", lambda e, ps=ps, b=b: e.tensor_tensor(out=res[:, b * 512:(b + 1) * 512], in0=ps[:], in1=bias[:, b * 512:(b + 1) * 512], op=ALU.add),
                         reads=[r_ps, r_bias], writes=[r_res])
                S.dma("sp", lambda e, l=l: e.dma_start(out=self.MOD[l], in_=res[:]), reads=[r_res], writes=[self.r_MOD])
            self.end_phase()

    def phase_norm(self, l, s, which):
        cfg, nc, S = self.cfg, self.nc, self.S
        D, KC = cfg.D, cfg.KC
        first = (l == 0 and which == 1)
        nw = self.norm_mix if which == 1 else self.norm_ffn
        base = 0 if which == 1 else 3
        BIG, r_BIG = self.BIG
        ident, r_ident = self.C["ident"]
        tiles = list(range(cfg.NT))
        if which == 2 and l == 1:
            tiles = list(range(cfg.NCT, cfg.NT))
        with ExitStack() as ph:
            gm = {}
            sh = {}
            for kind, row in (("lat", s), ("ctx", cfg.NSEQ)):
                if kind == "ctx" and which == 2 and l == 1:
                    continue
                g_t, g_r = tile1(ph, nc, "gm" + kind, [128, D], F32)
                s_t, s_r = tile1(ph, nc, "sh" + kind, [128, D], F32)
                n_t, n_r = tile1(ph, nc, "nw" + kind, [128, D], F32)
                self.S.dma("sp", lambda e, n_t=n_t: e.dma_start(out=n_t[:], in_=nw[l:l + 1, :].partition_broadcast(128)), writes=[n_r])
                self.S.dma("sp", lambda e, g_t=g_t, row=row: e.dma_start(out=g_t[:], in_=self.MOD[l, row:row + 1, (base + 1) * D:(base + 2) * D].partition_broadcast(128)), reads=[self.r_MOD], writes=[g_r])
                self.S.dma("sp", lambda e, s_t=s_t, row=row: e.dma_start(out=s_t[:], in_=self.MOD[l, row:row + 1, base * D:(base + 1) * D].partition_broadcast(128)), reads=[self.r_MOD], writes=[s_r])
                S.op("dve", lambda e, g_t=g_t, n_t=n_t: e.scalar_tensor_tensor(out=g_t[:], in0=g_t[:], scalar=1.0, in1=n_t[:], op0=ALU.add, op1=ALU.mult),
                     reads=[g_r, n_r], writes=[g_r])
                gm[kind] = (g_t, g_r)
                sh[kind] = (s_t, s_r)
            xpool = TPool(ph, nc, "xt", [128, D], F32, 2)
            junk, r_junk = tile1(ph, nc, "junk", [128, D], BF16)
            t1pool = TPool(ph, nc, "t1", [128, D], F32, 1)
            hpool = TPool(ph, nc, "hb", [128, D], BF16, 2)
            stpool = TPool(ph, nc, "st", [128, 4], F32, 2)
            pspool = TPool(ph, nc, "pT", [128, KC, 128], BF16, 2, psum=True)
            loaded = {}

            def load(i):
                tile = tiles[i]
                xt, r_xt = xpool.next()
                src = self.xsrc(first, s, tile)
                S.dma("sp", lambda e, xt=xt, src=src: e.dma_start(out=xt[:], in_=src), reads=[self.r_XR[s][tile]], writes=[r_xt])
                loaded[i] = (xt, r_xt)

            load(0)
            pend = None
            for i, tile in enumerate(tiles):
                if i + 1 < len(tiles):
                    load(i + 1)
                xt, r_xt = loaded.pop(i)
                kind = "ctx" if tile < cfg.NCT else "lat"
                g_t, g_r = gm[kind]
                s_t, s_r = sh[kind]
                st, r_st = stpool.next()
                S.op("act", lambda e, xt=xt, st=st: e.activation(out=junk[:], in_=xt[:], func=AF.Square, accum_out=st[:, 0:1]),
                     reads=[r_xt], writes=[r_junk, r_st])
                t1, r_t1 = t1pool.next()
                S.op("dve", lambda e, t1=t1, xt=xt, g_t=g_t: e.tensor_tensor(out=t1[:], in0=xt[:], in1=g_t[:], op=ALU.mult),
                     reads=[r_xt, g_r], writes=[r_t1])
                S.op("dve", lambda e, st=st: e.tensor_scalar(out=st[:, 1:2], in0=st[:, 0:1], scalar1=1.0 / D, scalar2=EPS, op0=ALU.mult, op1=ALU.add),
                     reads=[r_st], writes=[r_st])
                S.op("act", lambda e, st=st: e.activation(out=st[:, 2:3], in_=st[:, 1:2], func=AF.Sqrt), reads=[r_st], writes=[r_st])
                S.op("dve", lambda e, st=st: e.reciprocal(out=st[:, 3:4], in_=st[:, 2:3]), reads=[r_st], writes=[r_st])
                hb, r_hb = hpool.next()
                S.op("dve", lambda e, hb=hb, t1=t1, st=st, s_t=s_t: e.scalar_tensor_tensor(out=hb[:], in0=t1[:], scalar=st[:, 3:4], in1=s_t[:], op0=ALU.mult, op1=ALU.add),
                     reads=[r_t1, r_st, s_r], writes=[r_hb])
                if which == 2 and l == 1 and cfg.MOE == "routed":
                    g_ = s * (cfg.SEQ // 128) + (tile - cfg.NCT)
                    S.dma("sp", lambda e, hb=hb, g_=g_: e.dma_start(out=self.H2[g_ * 128:(g_ + 1) * 128, :], in_=hb[:]), reads=[r_hb], writes=[self.r_H2])
                def back(hb=hb, r_hb=r_hb, tile=tile):
                    ps, r_ps = pspool.next()
                    for kc in range(KC):
                        S.op("pe", lambda e, ps=ps, hb=hb, kc=kc: e.transpose(out=ps[:, kc, :], in_=hb[:, kc * 128:(kc + 1) * 128], identity=ident[:]),
                             reads=[r_hb, r_ident], writes=[r_ps], signal=(kc == KC - 1))
                    S.op("act", lambda e, ps=ps, tile=tile: e.copy(out=BIG[:, :, tile * 128:(tile + 1) * 128], in_=ps[:]),
                         reads=[r_ps], writes=[r_BIG])
                if pend is not None:
                    pend()
                pend = back
            if pend is not None:
                pend()
            self.end_phase()

    def phase_inproj(self, l, s):
        cfg, nc, S = self.cfg, self.nc, self.S
        D, KC, T = cfg.D, cfg.KC, cfg.T
        BIG, r_BIG = self.BIG
        CTX, SEQ = cfg.CTX, cfg.SEQ
        pm, r_pm = self.C["pm"]
        w_in = self.w_in[l]
        with ExitStack() as ph:
            wpool = TPool(ph, nc, "win", [128, KC, 512], BF16, 3)
            pspool = TPool(ph, nc, "psin", [128, 512], F32, 4, psum=True)
            psrot = TPool(ph, nc, "psrot", [128, 512], F32, 2, psum=True)
            stb = TPool(ph, nc, "stb", [128, T], BF16, 4)
            stf = TPool(ph, nc, "stf", [128, T], F32, 2)
            tmpf = TPool(ph, nc, "tmpf", [128, 512], F32, 6)
            cosT, r_cos = tile1(ph, nc, "cosT", [128, SEQ], F32)
            sinT, r_sin = tile1(ph, nc, "sinT", [128, SEQ], F32)
            S.dma("sp", lambda e: e.dma_start(out=cosT[:], in_=self.cst_d["cosT"][:, :]), writes=[r_cos])
            S.dma("sp", lambda e: e.dma_start(out=sinT[:], in_=self.cst_d["sinT"][:, :]), writes=[r_sin])
            lbv, oml = [], []
            for di, lbsrc in enumerate((self.lb_f, self.lb_b)):
                lb_t, lb_r = tile1(ph, nc, "lb%d" % di, [128, 8], F32)
                om_t, om_r = tile1(ph, nc, "oml%d" % di, [128, 8], F32)
                if l == 0:
                    S.op("dve", lambda e, lb_t=lb_t: e.memset(lb_t[:], 0.0), writes=[lb_r])
                else:
                    r0_t, r0_r = tile1(ph, nc, "lr0%d" % di, [128, 8], F32)
                    r1_t, r1_r = tile1(ph, nc, "lr1%d" % di, [128, 8], F32)
                    S.dma("sp", lambda e, r0_t=r0_t, lbsrc=lbsrc: e.dma_start(out=r0_t[:], in_=lbsrc[0:1, :].rearrange("o (h p) -> p (o h)", p=128)), writes=[r0_r])
                    S.dma("sp", lambda e, r1_t=r1_t, lbsrc=lbsrc: e.dma_start(out=r1_t[:], in_=lbsrc[1:2, :].rearrange("o (h p) -> p (o h)", p=128)), writes=[r1_r])
                    S.op("dve", lambda e, r0_t=r0_t, r1_t=r1_t: e.tensor_tensor(out=r0_t[:], in0=r0_t[:], in1=r1_t[:], op=ALU.subtract), reads=[r0_r, r1_r], writes=[r0_r])
                    S.op("act", lambda e, r0_t=r0_t: e.activation(out=r0_t[:], in_=r0_t[:], func=AF.Exp), reads=[r0_r], writes=[r0_r])
                    S.op("dve", lambda e, r0_t=r0_t: e.tensor_scalar(out=r0_t[:], in0=r0_t[:], scalar1=1.0, scalar2=None, op0=ALU.add), reads=[r0_r], writes=[r0_r])
                    S.op("dve", lambda e, r0_t=r0_t, lb_t=lb_t: e.reciprocal(out=lb_t[:], in_=r0_t[:]), reads=[r0_r], writes=[lb_r])
                S.op("dve", lambda e, lb_t=lb_t, om_t=om_t: e.tensor_scalar(out=om_t[:], in0=lb_t[:], scalar1=-1.0, scalar2=1.0, op0=ALU.mult, op1=ALU.add), reads=[lb_r], writes=[om_r])
                lbv.append((lb_t, lb_r))
                oml.append((om_t, om_r))

            def proj_chunk(w, r_w, c, epi):
                for (t0, n) in cfg.tblocks:
                    ps, r_ps = pspool.next()
                    for kc in range(KC):
                        S.op("pe", lambda e, ps=ps, kc=kc, t0=t0, n=n: e.matmul(ps[:, 0:n], lhsT=w[:, kc, c * 128:(c + 1) * 128], rhs=BIG[:, kc, t0:t0 + n], start=(kc == 0), stop=(kc == KC - 1)),
                             reads=[r_w, r_BIG], writes=[r_ps], signal=(kc == KC - 1))
                    epi(ps, r_ps, t0, n)

            def store(dst, st, r_st, r_dst):
                S.dma("sp", lambda e: e.dma_start(out=dst, in_=st[:]), reads=[r_st], writes=[r_dst])

            def simple_job(col0, nchunks, func, scale, dst, r_dst):
                for b in range(0, nchunks, 4):
                    w, r_w = self.load_wblock(wpool, w_in, col0 + b * 128, 512)
                    for c in range(4):
                        st, r_st = stb.next()
                        def epi(ps, r_ps, t0, n, st=st, r_st=r_st):
                            S.op("act", lambda e: e.activation(out=st[:, t0:t0 + n], in_=ps[:, 0:n], func=func, scale=scale), reads=[r_ps], writes=[r_st])
                        proj_chunk(w, r_w, c, epi)
                        store(dst[b + c], st, r_st, r_dst[b + c])

            simple_job(COL["hq"], 8, AF.Copy, 128.0 ** -0.5, self.QT, self.r_QT)
            simple_job(COL["hg"], 8, AF.Silu, 1.0, self.SGT, self.r_SGT)
            simple_job(COL["ga"], 16, AF.Sigmoid, 1.0, self.GA, self.r_GA)
            simple_job(COL["gb"], 16, AF.Sigmoid, 1.0, self.GB, self.r_GB)
            for di, col0 in enumerate((COL["ff"], COL["fb"])):
                lb_t, lb_r = lbv[di]
                om_t, om_r = oml[di]
                for b in range(0, 8, 4):
                    w, r_w = self.load_wblock(wpool, w_in, col0 + b * 128, 512)
                    for c in range(4):
                        hd = b + c
                        sg_, r_sg = stf.next()
                        sk_, r_sk = stb.next()
                        def epi(ps, r_ps, t0, n, sg_=sg_, r_sg=r_sg, sk_=sk_, r_sk=r_sk, hd=hd, om_t=om_t, om_r=om_r):
                            e_t, r_e = tmpf.next()
                            t_t, r_t = tmpf.next()
                            k_t, r_k = tmpf.next()
                            S.op("act", lambda e: e.activation(out=e_t[:, 0:n], in_=ps[:, 0:n], func=AF.Exp, scale=-1.0), reads=[r_ps], writes=[r_e])
                            S.op("dve", lambda e: e.tensor_scalar(out=t_t[:, 0:n], in0=e_t[:, 0:n], scalar1=1.0, scalar2=None, op0=ALU.add), reads=[r_e], writes=[r_t])
                            S.op("dve", lambda e: e.reciprocal(out=t_t[:, 0:n], in_=t_t[:, 0:n]), reads=[r_t], writes=[r_t])
                            S.op("dve", lambda e: e.scalar_tensor_tensor(out=k_t[:, 0:n], in0=e_t[:, 0:n], scalar=om_t[:, hd:hd + 1], in1=t_t[:, 0:n], op0=ALU.mult, op1=ALU.mult),
                                 reads=[r_e, r_t, om_r], writes=[r_k])
                            S.op("act", lambda e: e.activation(out=sg_[:, t0:t0 + n], in_=k_t[:, 0:n], func=AF.Ln, scale=-1.0, bias=1.0), reads=[r_k], writes=[r_sg])
                            S.op("act", lambda e: e.copy(out=sk_[:, t0:t0 + n], in_=k_t[:, 0:n]), reads=[r_k], writes=[r_sk])
                        proj_chunk(w, r_w, c, epi)
                        store(self.GT[di, hd], sg_, r_sg, self.r_GT[di][hd])
                        store(self.KT[di, hd], sk_, r_sk, self.r_KT[di][hd])

            def rope_job(w, r_w, c, scale, dst, r_dst):
                raw, r_raw = stb.next()
                outt, r_out = stb.next()
                def epi(ps, r_ps, t0, n):
                    S.op("act", lambda e: e.activation(out=raw[:, t0:t0 + n], in_=ps[:, 0:n], func=AF.Copy, scale=scale), reads=[r_ps], writes=[r_raw])
                    if t0 < CTX:
                        S.op("act", lambda e: e.copy(out=outt[:, t0:t0 + n], in_=raw[:, t0:t0 + n]), reads=[r_raw], writes=[r_out])
                        return
                    p0 = t0 - CTX
                    pr, r_pr = psrot.next()
                    S.op("pe", lambda e: e.matmul(pr[:, 0:n], lhsT=pm[:], rhs=raw[:, t0:t0 + n], start=True, stop=True), reads=[r_pm, r_raw], writes=[r_pr])
                    a_t, r_a = tmpf.next()
                    b_t, r_b = tmpf.next()
                    S.op("dve", lambda e: e.tensor_tensor(out=a_t[:, 0:n], in0=raw[:, t0:t0 + n], in1=cosT[:, p0:p0 + n], op=ALU.mult), reads=[r_raw, r_cos], writes=[r_a])
                    S.op("dve", lambda e: e.tensor_tensor(out=b_t[:, 0:n], in0=pr[:, 0:n], in1=sinT[:, p0:p0 + n], op=ALU.mult), reads=[r_pr, r_sin], writes=[r_b])
                    S.op("dve", lambda e: e.tensor_tensor(out=outt[:, t0:t0 + n], in0=a_t[:, 0:n], in1=b_t[:, 0:n], op=ALU.add), reads=[r_a, r_b], writes=[r_out])
                proj_chunk(w, r_w, c, epi)
                store(dst, outt, r_out, r_dst)

            for b in range(0, 8, 4):
                w, r_w = self.load_wblock(wpool, w_in, COL["aq"] + b * 128, 512)
                for c in range(4):
                    rope_job(w, r_w, c, 64.0 ** -0.5, self.QA[b + c], self.r_QA[b + c])
            w, r_w = wpool.next()
            for (o, c0, nn) in ((0, COL["ak"], 128), (128, COL["ak"] + 64, 64), (192, COL["ak"], 64)):
                src = w_in[:, c0:c0 + nn].rearrange("(kc p) n -> p kc n", p=128)
                S.dma("pool", lambda e, o=o, nn=nn, src=src: e.dma_start(out=w[:, :, o:o + nn], in_=src), writes=[r_w])
            for c in range(2):
                rope_job(w, r_w, c, 1.0, self.KA[c], self.r_KA[c])

            vst = TPool(ph, nc, "vst", [128, 512], BF16, 3)
            for (col0, ncols, dst, r_dst) in ((COL["hv"], 512, self.V[:, 0:512], self.r_V), (COL["hv"] + 512, 512, self.V[:, 512:1024], self.r_V), (COL["av"], 128, self.VA, self.r_VA)):
                w, r_w = self.load_wblock(wpool, w_in, col0, ncols)
                for tile in range(cfg.NT):
                    ps, r_ps = pspool.next()
                    for kc in range(KC):
                        S.op("pe", lambda e, ps=ps, kc=kc, tile=tile, ncols=ncols, w=w: e.matmul(ps[:, 0:ncols], lhsT=BIG[:, kc, tile * 128:(tile + 1) * 128], rhs=w[:, kc, 0:ncols], start=(kc == 0), stop=(kc == KC - 1)),
                             reads=[r_w, r_BIG], writes=[r_ps], signal=(kc == KC - 1))
                    vt, r_vt = vst.next()
                    S.op("act", lambda e, vt=vt, ps=ps, ncols=ncols: e.copy(out=vt[:, 0:ncols], in_=ps[:, 0:ncols]), reads=[r_ps], writes=[r_vt])
                    S.dma("sp", lambda e, vt=vt, tile=tile, ncols=ncols, dst=dst: e.dma_start(out=dst[tile * 128:(tile + 1) * 128, :], in_=vt[:, 0:ncols]), reads=[r_vt], writes=[r_dst])
            self.end_phase()

    def phase_scan(self, l, s):
        cfg, nc, S = self.cfg, self.nc, self.S
        T, NCH = cfg.T, cfg.NCH
        NB16 = T // 16
        ident, r_ident = self.C["ident"]
        groups = cfg.groups
        ng = len(groups)
        order = [[list(g) for g in groups],
                 [list(reversed(groups[0]))] + [list(reversed(g)) for g in reversed(groups[1:])]]
        VN = ("qd", "kd", "qb", "qin", "kout", "k1", "k2", "k3")
        with ExitStack() as ph:
            qT, r_qT = tile1(ph, nc, "s_qT", [128, T], BF16)
            sgT, r_sgT = tile1(ph, nc, "s_sgT", [128, T], BF16)
            vt, r_vt = tile1(ph, nc, "s_v", [64, NCH, 128], BF16)
            gT = [tile1(ph, nc, "s_gT%d" % d, [128, T], F32) for d in range(2)]
            kT = [tile1(ph, nc, "s_kT%d" % d, [128, T], BF16) for d in range(2)]
            Zps = [tile1(ph, nc, "s_Zp%d" % d, [128, T + 1], F32) for d in range(2)]
            tmp = TPool(ph, nc, "s_tmp", [128, T], F32, 3)
            var = [{n: tile1(ph, nc, "s_%s%d" % (n, d), [128, T], BF16) for n in VN} for d in range(2)]
            tr = [tile1(ph, nc, "s_tr%d" % d, [128, NCH], F32) for d in range(2)]
            oall, r_oall = tile1(ph, nc, "s_oall", [128, T], F32)
            aT, r_aT = tile1(ph, nc, "s_aT", [128, T], BF16)
            ones, r_ones = tile1(ph, nc, "s_ones", [128, T], F32)
            onesf, r_onesf = tile1(ph, nc, "s_onesf", [128, 128], F32)
            gain, r_gain = tile1(ph, nc, "s_gain", [128, 8], F32)
            mk = {}
            for n in ("maskD_f", "maskO_f", "maskD_b", "maskO_b"):
                mk[n] = tile1(ph, nc, "s_" + n, [64, 64], BF16)
                S.dma("pool", lambda e, n=n: e.dma_start(out=mk[n][0][:], in_=self.cst_d[n][:, :]), writes=[mk[n][1]])
            Sf = [tile1(ph, nc, "s_Sf%d" % d, [128, 128], F32) for d in range(2)]
            Sb = [tile1(ph, nc, "s_Sb%d" % d, [128, 128], BF16) for d in range(2)]
            At = TPool(ph, nc, "s_At", [64, 8, 64], BF16, 4)
            At2 = TPool(ph, nc, "s_At2", [64, 8, 64], BF16, 2)
            ktok = TPool(ph, nc, "s_ktok", [64, 8, 128], BF16, 4)
            rs = TPool(ph, nc, "s_rs", [128, 512], F32, 2)
            psA = TPool(ph, nc, "s_psA", [64, 8, 64], F32, 1, psum=True)
            psA2 = [tile1(ph, nc, "s_psA2%d" % d, [64, 8, 64], F32, psum=True) for d in range(2)]
            psK = TPool(ph, nc, "s_psK", [64, 8, 128], BF16, 1, psum=True)
            psO = TPool(ph, nc, "s_psO", [128, 8, 64], F32, 2, psum=True)
            psD = TPool(ph, nc, "s_psD", [128, 128], F32, 1, psum=True)
            psS = TPool(ph, nc, "s_psS", [128, 512], F32, 1, psum=True)
            S.op("dve", lambda e: e.memset(ones[:], 1.0), writes=[r_ones])
            S.op("dve", lambda e: e.memset(onesf[:], 1.0), writes=[r_onesf])
            for d in range(2):
                S.op("dve", lambda e, d=d: e.memset(psA2[d][0][:], 0.0), writes=[psA2[d][1]])
                S.op("dve", lambda e, d=d: e.memset(Zps[d][0][:], 0.0), writes=[Zps[d][1]])
            S.dma("sp", lambda e: e.dma_start(out=gain[:], in_=self.hg_norm[l:l + 1, :].rearrange("o (h p) -> p (o h)", p=128)), writes=[r_gain])

            def v3(ap, j):
                return ap.rearrange("p (c j) -> p c j", j=j)

            def load_pre(hd):
                for d in range(2):
                    S.dma("sp", lambda e, hd=hd, d=d: e.dma_start(out=gT[d][0][:], in_=self.GT[d, hd]), reads=[self.r_GT[d][hd]], writes=[gT[d][1]])
                S.dma("sp", lambda e, hd=hd: e.dma_start(out=qT[:], in_=self.QT[hd]), reads=[self.r_QT[hd]], writes=[r_qT])
                for d in range(2):
                    S.dma("sp", lambda e, hd=hd, d=d: e.dma_start(out=kT[d][0][:], in_=self.KT[d, hd]), reads=[self.r_KT[d][hd]], writes=[kT[d][1]])

            load_pre(0)
            for hd in range(8):
                self.pump(4)
                S.dma("sp", lambda e, hd=hd: e.dma_start(out=vt[:], in_=self.V[:, hd * 128:(hd + 1) * 128].rearrange("(c p) v -> p c v", p=64)), reads=[self.r_V], writes=[r_vt])
                S.dma("sp", lambda e, hd=hd: e.dma_start(out=sgT[:], in_=self.SGT[hd]), reads=[self.r_SGT[hd]], writes=[r_sgT])
                mjobs = []
                for d in range(2):
                    g_t, g_r = gT[d]
                    k_t, k_r = kT[d]
                    Zp, r_Zp = Zps[d]
                    sg = 1.0 if d == 0 else -1.0
                    S.op("dve", lambda e, Zp=Zp, g_t=g_t: e.tensor_tensor_scan(out=Zp[:, 1:T + 1], data0=ones[:], data1=g_t[:], initial=0.0, op0=ALU.mult, op1=ALU.add),
                         reads=[r_ones, g_r], writes=[r_Zp])
                    X = Zp[:, 1:T + 1] if d == 0 else Zp[:, 0:T]
                    lo16 = v3(Zp[:, 0:T], 16)
                    hi16 = v3(Zp[:, 1:T + 1], 16)
                    lo64 = v3(Zp[:, 0:T], 64)
                    hi64 = v3(Zp[:, 1:T + 1], 64)
                    ref_mid = lo16[:, :, 8:9]
                    ref_qb = lo16[:, :, 0:1] if d == 0 else hi16[:, :, 15:16]
                    ref_qin = lo64[:, :, 0:1] if d == 0 else hi64[:, :, 63:64]
                    ref_kout = hi64[:, :, 63:64] if d == 0 else lo64[:, :, 0:1]
                    ref_k = [lo64[:, :, 16 * i:16 * i + 1] for i in (1, 2, 3)]

                    def make(name, src_t, src_r, ref, blk, scale, clamp=None, X=X, r_Zp=r_Zp, d=d):
                        nb = T // blk
                        st_ = {}

                        def s1():
                            D, r_D = tmp.next()
                            st_["D"] = (D, r_D)
                            S.op("dve", lambda e: e.tensor_tensor(out=v3(D[:], blk), in0=v3(X, blk), in1=ref.broadcast_to([128, nb, blk]), op=ALU.subtract),
                                 reads=[r_Zp], writes=[r_D])
                            S.op("act", lambda e: e.activation(out=D[:], in_=D[:], func=AF.Exp, scale=scale), reads=[r_D], writes=[r_D])

                        def s2():
                            D, r_D = st_["D"]
                            o_t, o_r = var[d][name]
                            if clamp is None:
                                S.op("dve", lambda e: e.tensor_tensor(out=o_t[:], in0=src_t[:], in1=D[:], op=ALU.mult), reads=[src_r, r_D], writes=[o_r])
                            else:
                                S.op("dve", lambda e: e.scalar_tensor_tensor(out=o_t[:], in0=D[:], scalar=1.0, in1=src_t[:], op0=ALU.min, op1=ALU.mult), reads=[src_r, r_D], writes=[o_r])
                        mjobs.append((s1, s2))

                    make("qd", qT, r_qT, ref_mid, 16, sg)
                    make("kd", k_t, k_r, ref_mid, 16, -sg)
                    make("qb", qT, r_qT, ref_qb, 16, sg)
                    make("qin", qT, r_qT, ref_qin, 64, sg)
                    make("kout", k_t, k_r, ref_kout, 64, -sg)
                    for i in range(3):
                        make("k%d" % (i + 1), k_t, k_r, ref_k[i], 64, -sg, clamp=(ALU.max if d == 0 else ALU.min))
                    tr_t, tr_r = tr[d]
                    S.op("dve", lambda e, tr_t=tr_t, hi64=hi64, lo64=lo64: e.tensor_tensor(out=tr_t[:].unsqueeze(2), in0=hi64[:, :, 63:64], in1=lo64[:, :, 0:1], op=ALU.subtract), reads=[r_Zp], writes=[tr_r])
                    S.op("act", lambda e, tr_t=tr_t: e.activation(out=tr_t[:], in_=tr_t[:], func=AF.Exp), reads=[tr_r], writes=[tr_r])
                    S.op("dve", lambda e, d=d: e.memset(Sf[d][0][:], 0.0), writes=[Sf[d][1]])
                    S.op("dve", lambda e, d=d: e.memset(Sb[d][0][:], 0.0), writes=[Sb[d][1]])
                for i in range(len(mjobs) + 1):
                    if i < len(mjobs):
                        mjobs[i][0]()
                    if i >= 1:
                        mjobs[i - 1][1]()
                if hd + 1 < 8:
                    load_pre(hd + 1)
                touched = set()
                for gi in range(ng):
                    ctxs = []
                    for d in range(2):
                        cl = order[d][gi]
                        g0 = min(cl)
                        n = len(cl)
                        V = var[d]
                        pa, r_pa = psA.next()
                        for c in cl:
                            S.op("pe", lambda e, pa=pa, c=c, g0=g0, V=V: e.matmul(pa[:, c - g0, :], lhsT=V["kd"][0][:, c * 64:(c + 1) * 64], rhs=V["qd"][0][:, c * 64:(c + 1) * 64], start=True, stop=True),
                                 reads=[V["kd"][1], V["qd"][1]], writes=[r_pa], signal=(c == cl[-1]))
                        pa2, r_pa2 = psA2[d]
                        subs = (1, 2, 3) if d == 0 else (0, 1, 2)
                        for c in cl:
                            for i in subs:
                                kv = V["k%d" % (i if d == 0 else i + 1)]
                                S.op("pe", lambda e, pa2=pa2, c=c, g0=g0, i=i, kv=kv, V=V: e.matmul(pa2[:, c - g0, 16 * i:16 * i + 16], lhsT=kv[0][:, c * 64:(c + 1) * 64], rhs=V["qb"][0][:, c * 64 + 16 * i:c * 64 + 16 * i + 16], start=True, stop=True),
                                     reads=[kv[1], V["qb"][1]], writes=[r_pa2], signal=(c == cl[-1] and i == subs[-1]))
                        at, r_at = At.next()
                        at2, r_at2 = At2.next()
                        mD, r_mD = mk["maskD_f" if d == 0 else "maskD_b"]
                        mO, r_mO = mk["maskO_f" if d == 0 else "maskO_b"]
                        S.op("dve", lambda e, at=at, pa=pa, n=n, mD=mD: e.tensor_tensor(out=at[:, 0:n, :], in0=pa[:, 0:n, :], in1=mD[:].unsqueeze(1).broadcast_to([64, n, 64]), op=ALU.mult),
                             reads=[r_pa, r_mD], writes=[r_at])
                        S.op("dve", lambda e, at2=at2, pa2=pa2, n=n, mO=mO: e.tensor_tensor(out=at2[:, 0:n, :], in0=pa2[:, 0:n, :], in1=mO[:].unsqueeze(1).broadcast_to([64, n, 64]), op=ALU.mult),
                             reads=[r_pa2, r_mO], writes=[r_at2])
                        S.op("pool", lambda e, at=at, at2=at2, n=n: e.tensor_tensor(out=at[:, 0:n, :], in0=at[:, 0:n, :], in1=at2[:, 0:n, :], op=ALU.add), reads=[r_at, r_at2], writes=[r_at])
                        pk, r_pk = psK.next()
                        for c in cl:
                            S.op("pe", lambda e, pk=pk, c=c, g0=g0, V=V: e.transpose(out=pk[:, c - g0, :], in_=V["kout"][0][:, c * 64:(c + 1) * 64], identity=ident[:]),
                                 reads=[V["kout"][1], r_ident], writes=[r_pk], signal=(c == cl[-1]))
                        kk, r_kk = ktok.next()
                        S.op("act", lambda e, kk=kk, pk=pk, n=n: e.copy(out=kk[:, 0:n, :], in_=pk[:, 0:n, :]), reads=[r_pk], writes=[r_kk])
                        po, r_po = psO.next()
                        ctxs.append((d, cl, g0, at, r_at, kk, r_kk, po, r_po))
                    nsteps = max(len(c[1]) for c in ctxs)
                    for j in range(nsteps):
                        for (d, cl, g0, at, r_at, kk, r_kk, po, r_po) in ctxs:
                            if j >= len(cl):
                                continue
                            c = cl[j]
                            pos = c - g0
                            V = var[d]
                            S.op("pe", lambda e, po=po, pos=pos, c=c, at=at: e.matmul(po[:, pos, :], lhsT=vt[:, c, :], rhs=at[:, pos, :], start=True, stop=False),
                                 reads=[r_vt, r_at], writes=[r_po], signal=False)
                            S.op("pe", lambda e, po=po, pos=pos, c=c, d=d, V=V: e.matmul(po[:, pos, :], lhsT=Sb[d][0][:], rhs=V["qin"][0][:, c * 64:(c + 1) * 64], start=False, stop=True),
                                 reads=[Sb[d][1], V["qin"][1]], writes=[r_po], signal=(j == len(cl) - 1))
                            last = (gi == ng - 1 and j == len(cl) - 1)
                            if not last:
                                pd, r_pd = psD.next()
                                S.op("pe", lambda e, pd=pd, kk=kk, pos=pos, c=c: e.matmul(pd[:], lhsT=kk[:, pos, :], rhs=vt[:, c, :], start=True, stop=True),
                                     reads=[r_kk, r_vt], writes=[r_pd])
                                S.op("dve", lambda e, pd=pd, d=d, c=c: e.scalar_tensor_tensor(out=Sf[d][0][:], in0=Sf[d][0][:], scalar=tr[d][0][:, c:c + 1], in1=pd[:], op0=ALU.mult, op1=ALU.add),
                                     reads=[r_pd, Sf[d][1], tr[d][1]], writes=[Sf[d][1]])
                                S.op("act", lambda e, d=d: e.copy(out=Sb[d][0][:], in_=Sf[d][0][:]), reads=[Sf[d][1]], writes=[Sb[d][1]])
                    for (d, cl, g0, at, r_at, kk, r_kk, po, r_po) in ctxs:
                        n = len(cl)
                        dst = v3(oall[:, g0 * 64:(g0 + n) * 64], 64)
                        if g0 not in touched:
                            touched.add(g0)
                            S.op("act", lambda e, dst=dst, po=po, n=n: e.copy(out=dst, in_=po[:, 0:n, :]), reads=[r_po], writes=[r_oall])
                        else:
                            S.op("dve", lambda e, dst=dst, po=po, n=n: e.tensor_tensor(out=dst, in0=po[:, 0:n, :], in1=dst, op=ALU.add), reads=[r_po, r_oall], writes=[r_oall])
                sq, r_sq = tmp.next()
                S.op("act", lambda e, sq=sq: e.activation(out=sq[:], in_=oall[:], func=AF.Square), reads=[r_oall], writes=[r_sq])
                for (t0, n) in cfg.tblocks:
                    pss, r_pss = psS.next()
                    S.op("pe", lambda e, pss=pss, sq=sq, t0=t0, n=n: e.matmul(pss[:, 0:n], lhsT=onesf[:], rhs=sq[:, t0:t0 + n], start=True, stop=True), reads=[r_onesf, r_sq], writes=[r_pss])
                    r1, r_r1 = rs.next()
                    S.op("dve", lambda e, r1=r1, pss=pss, n=n: e.tensor_scalar(out=r1[:, 0:n], in0=pss[:, 0:n], scalar1=1.0 / 128, scalar2=EPS, op0=ALU.mult, op1=ALU.add), reads=[r_pss], writes=[r_r1])
                    S.op("act", lambda e, r1=r1, n=n: e.activation(out=r1[:, 0:n], in_=r1[:, 0:n], func=AF.Sqrt), reads=[r_r1], writes=[r_r1])
                    S.op("dve", lambda e, r1=r1, n=n: e.reciprocal(out=r1[:, 0:n], in_=r1[:, 0:n]), reads=[r_r1], writes=[r_r1])
                    S.op("dve", lambda e, r1=r1, t0=t0, n=n: e.tensor_tensor(out=r1[:, 0:n], in0=r1[:, 0:n], in1=oall[:, t0:t0 + n], op=ALU.mult), reads=[r_r1, r_oall], writes=[r_r1])
                    S.op("dve", lambda e, r1=r1, t0=t0, n=n, hd=hd: e.scalar_tensor_tensor(out=aT[:, t0:t0 + n], in0=r1[:, 0:n], scalar=gain[:, hd:hd + 1], in1=sgT[:, t0:t0 + n], op0=ALU.mult, op1=ALU.mult),
                         reads=[r_r1, r_gain, r_sgT], writes=[r_aT])
                S.dma("sp", lambda e, hd=hd: e.dma_start(out=self.AT[hd], in_=aT[:]), reads=[r_aT], writes=[self.r_AT[hd]])
            self.end_phase()

    def phase_attn(self, l, s):
        cfg, nc, S = self.cfg, self.nc, self.S
        T, CTX, NCT, NT = cfg.T, cfg.CTX, cfg.NCT, cfg.NT
        NQ = cfg.SEQ // 128
        ident, r_ident = self.C["ident"]
        mprev, r_mprev = self.C["mneg_prev"]
        mnext, r_mnext = self.C["mneg_next"]
        onesb, r_onesb = self.C["ones"]
        with ExitStack() as ph:
            kc_ = [tile1(ph, nc, "a_k%d" % i, [128, T], BF16) for i in range(2)]
            vtok, r_vtok = tile1(ph, nc, "a_v", [128, NT, 128], BF16)
            qpool = TPool(ph, nc, "a_q", [128, T], BF16, 2)
            opool = TPool(ph, nc, "a_o", [128, T], BF16, 2)
            esb, r_esb = tile1(ph, nc, "a_esb", [128, 16], F32)
            es2, r_es2 = tile1(ph, nc, "a_es2", [128, 8], F32)
            ppool = TPool(ph, nc, "a_p", [128, 5, 128], BF16, 4)
            rpool = TPool(ph, nc, "a_r", [128, 128], F32, 3)
            psS = TPool(ph, nc, "a_psS", [128, 8, 128], F32, 3, psum=True)
            psOD = TPool(ph, nc, "a_psOD", [128, 2, 128], F32, 2, psum=True)
            for i in range(2):
                S.dma("sp", lambda e, i=i: e.dma_start(out=kc_[i][0][:], in_=self.KA[i]), reads=[self.r_KA[i]], writes=[kc_[i][1]])
            S.dma("sp", lambda e: e.dma_start(out=vtok[:], in_=self.VA.rearrange("(c p) v -> p c v", p=128)), reads=[self.r_VA], writes=[r_vtok])
            S.dma("sp", lambda e: e.dma_start(out=esb[:], in_=self.sink[l:l + 1, :].partition_broadcast(128)), writes=[r_esb])
            S.op("act", lambda e: e.activation(out=esb[:], in_=esb[:], func=AF.Exp), reads=[r_esb], writes=[r_esb])
            esb3 = esb[:].rearrange("p (c two) -> p c two", two=2)
            S.op("dve", lambda e: e.tensor_copy(out=es2[0:64, :], in_=esb3[0:64, :, 0]), reads=[r_esb], writes=[r_es2])
            S.op("dve", lambda e: e.tensor_copy(out=es2[64:128, :], in_=esb3[64:128, :, 1]), reads=[r_esb], writes=[r_es2])
            qblocks = []
            if l == 0:
                for m in range(NCT):
                    qblocks.append((m * 128, [(t, None) for t in range(NCT)]))
            for n in range(NQ):
                tiles = [(t, None) for t in range(NCT)]
                if n > 0:
                    tiles.append((NCT + n - 1, "prev"))
                tiles.append((NCT + n, None))
                if n < NQ - 1:
                    tiles.append((NCT + n + 1, "next"))
                qblocks.append((CTX + n * 128, tiles))
            for c in range(8):
                g = c // 4
                self.pump(2)
                qc, r_qc = qpool.next()
                S.dma("sp", lambda e, c=c, qc=qc: e.dma_start(out=qc[:], in_=self.QA[c]), reads=[self.r_QA[c]], writes=[r_qc])
                oc, r_oc = opool.next()
                kops = [(kc_[0] if g == 0 else kc_[1], 0), (kc_[1] if g == 0 else kc_[0], 64)]
                for (q0, tiles) in qblocks:
                    nt = len(tiles)
                    pts = []
                    for hh in range(2):
                        (k_t, k_r), r0 = kops[hh]
                        ps, r_ps = psS.next()
                        for ti, (tile, mkind) in enumerate(tiles):
                            S.op("pe", lambda e, ps=ps, ti=ti, tile=tile, k_t=k_t, r0=r0, q0=q0, qc=qc, mkind=mkind: e.matmul(ps[:, ti, :], lhsT=k_t[r0:r0 + 64, tile * 128:(tile + 1) * 128], rhs=qc[r0:r0 + 64, q0:q0 + 128], start=True, stop=(mkind is None)),
                                 reads=[k_r, r_qc], writes=[r_ps], signal=(mkind is None and ti == nt - 1))
                            if mkind is not None:
                                m_t, m_r = (mprev, r_mprev) if mkind == "prev" else (mnext, r_mnext)
                                S.op("pe", lambda e, ps=ps, ti=ti, m_t=m_t: e.matmul(ps[:, ti, :], lhsT=ident[:], rhs=m_t[:], start=False, stop=True),
                                     reads=[r_ident, m_r], writes=[r_ps], signal=(ti == nt - 1))
                        pt, r_pt = ppool.next()
                        S.op("act", lambda e, pt=pt, ps=ps, nt=nt: e.activation(out=pt[:, 0:nt, :], in_=ps[:, 0:nt, :], func=AF.Exp), reads=[r_ps], writes=[r_pt])
                        pts.append((pt, r_pt))
                    od, r_od = psOD.next()
                    for hh in range(2):
                        pt, r_pt = pts[hh]
                        r0 = 64 * hh
                        tp = None if hh == 0 else (0, 64)
                        for ti, (tile, mkind) in enumerate(tiles):
                            S.op("pe", lambda e, od=od, r0=r0, ti=ti, tile=tile, pt=pt, tp=tp, nt=nt, g=g: e.matmul(od[r0:r0 + 64, 0, :], lhsT=vtok[:, tile, g * 64:(g + 1) * 64], rhs=pt[:, ti, :], start=(ti == 0), stop=(ti == nt - 1), tile_position=tp),
                                 reads=[r_vtok, r_pt], writes=[r_od], signal=False)
                        for ti, (tile, mkind) in enumerate(tiles):
                            S.op("pe", lambda e, od=od, r0=r0, ti=ti, pt=pt, tp=tp, nt=nt: e.matmul(od[r0:r0 + 64, 1, :], lhsT=onesb[:, 0:64], rhs=pt[:, ti, :], start=(ti == 0), stop=(ti == nt - 1), tile_position=tp),
                                 reads=[r_onesb, r_pt], writes=[r_od], signal=(ti == nt - 1))
                    rc, r_rc = rpool.next()
                    S.op("dve", lambda e, rc=rc, od=od, c=c: e.tensor_scalar(out=rc[:], in0=od[:, 1, :], scalar1=es2[:, c:c + 1], scalar2=None, op0=ALU.add), reads=[r_od, r_es2], writes=[r_rc])
                    S.op("dve", lambda e, rc=rc: e.reciprocal(out=rc[:], in_=rc[:]), reads=[r_rc], writes=[r_rc])
                    S.op("dve", lambda e, rc=rc, od=od, oc=oc, q0=q0: e.tensor_tensor(out=oc[:, q0:q0 + 128], in0=od[:, 0, :], in1=rc[:], op=ALU.mult), reads=[r_od, r_rc], writes=[r_oc])
                if l == 0:
                    S.dma("sp", lambda e, c=c, oc=oc: e.dma_start(out=self.OAT[c], in_=oc[:]), reads=[r_oc], writes=[self.r_OAT[c]])
                else:
                    S.dma("sp", lambda e, c=c, oc=oc: e.dma_start(out=self.OAT[c][:, CTX:T], in_=oc[:, CTX:T]), reads=[r_oc], writes=[self.r_OAT[c]])
            self.end_phase()

    def phase_merge(self, l, s):
        cfg, nc, S = self.cfg, self.nc, self.S
        D, KC, T, CTX = cfg.D, cfg.KC, cfg.T, cfg.CTX
        BIG, r_BIG = self.BIG
        blocks = cfg.tblocks if l == 0 else cfg.lat_blocks
        tiles = list(range(cfg.NT)) if l == 0 else list(range(cfg.NCT, cfg.NT))
        with ExitStack() as ph:
            aT, r_aT = tile1(ph, nc, "m_aT", [128, 8, T], BF16)
            oT, r_oT = tile1(ph, nc, "m_oT", [128, 8, T], BF16)
            S.dma("sp", lambda e: e.dma_start(out=aT[:], in_=self.AT.rearrange("c p t -> p c t")), reads=self.r_AT, writes=[r_aT])
            S.dma("sp", lambda e: e.dma_start(out=oT[:], in_=self.OAT.rearrange("c p t -> p c t")), reads=self.r_OAT, writes=[r_oT])
            wap = TPool(ph, nc, "m_wa", [128, 8, 512], BF16, 2)
            wbp = TPool(ph, nc, "m_wb", [128, 8, 512], BF16, 2)
            gap = TPool(ph, nc, "m_ga", [128, T], BF16, 2)
            gbp = TPool(ph, nc, "m_gb", [128, T], BF16, 2)
            tp = TPool(ph, nc, "m_t", [128, 512], F32, 4)
            ps1 = TPool(ph, nc, "m_ps1", [128, 512], F32, 2, psum=True)
            ps2 = TPool(ph, nc, "m_ps2", [128, 512], F32, 2, psum=True)
            for jb in range(4):
                wa, r_wa = self.load_wblock(wap, self.w_a[l], jb * 512, 512)
                wb, r_wb = self.load_wblock(wbp, self.w_b[l], jb * 512, 512)
                for c in range(4):
                    j = jb * 4 + c
                    ga, r_ga = gap.next()
                    gb, r_gb = gbp.next()
                    S.dma("sp", lambda e, ga=ga, j=j: e.dma_start(out=ga[:], in_=self.GA[j]), reads=[self.r_GA[j]], writes=[r_ga])
                    S.dma("sp", lambda e, gb=gb, j=j: e.dma_start(out=gb[:], in_=self.GB[j]), reads=[self.r_GB[j]], writes=[r_gb])
                    for (t0, n) in blocks:
                        p1, r_p1 = ps1.next()
                        p2, r_p2 = ps2.next()
                        for kc in range(8):
                            S.op("pe", lambda e, p1=p1, wa=wa, kc=kc, c=c, t0=t0, n=n: e.matmul(p1[:, 0:n], lhsT=wa[:, kc, c * 128:(c + 1) * 128], rhs=aT[:, kc, t0:t0 + n], start=(kc == 0), stop=(kc == 7)),
                                 reads=[r_wa, r_aT], writes=[r_p1], signal=(kc == 7))
                        for kc in range(8):
                            S.op("pe", lambda e, p2=p2, wb=wb, kc=kc, c=c, t0=t0, n=n: e.matmul(p2[:, 0:n], lhsT=wb[:, kc, c * 128:(c + 1) * 128], rhs=oT[:, kc, t0:t0 + n], start=(kc == 0), stop=(kc == 7)),
                                 reads=[r_wb, r_oT], writes=[r_p2], signal=(kc == 7))
                        t1, r_t1 = tp.next()
                        t2, r_t2 = tp.next()
                        S.op("dve", lambda e, t1=t1, p1=p1, ga=ga, t0=t0, n=n: e.tensor_tensor(out=t1[:, 0:n], in0=p1[:, 0:n], in1=ga[:, t0:t0 + n], op=ALU.mult), reads=[r_p1, r_ga], writes=[r_t1])
                        S.op("dve", lambda e, t2=t2, p2=p2, gb=gb, t0=t0, n=n: e.tensor_tensor(out=t2[:, 0:n], in0=p2[:, 0:n], in1=gb[:, t0:t0 + n], op=ALU.mult), reads=[r_p2, r_gb], writes=[r_t2])
                        S.op("pool", lambda e, t1=t1, t2=t2, j=j, t0=t0, n=n: e.tensor_tensor(out=BIG[:, j, t0:t0 + n], in0=t1[:, 0:n], in1=t2[:, 0:n], op=ALU.add), reads=[r_t1, r_t2], writes=[r_BIG])
            self.end_phase()
        with ExitStack() as ph:
            wop = TPool(ph, nc, "m_wo", [128, KC, 512], BF16, 2)
            g1 = {}
            for kind, row in (("lat", s), ("ctx", cfg.NSEQ)):
                g1[kind] = tile1(ph, nc, "m_g1" + kind, [128, D], F32)
                self.bcast_row(g1[kind][0], g1[kind][1], self.MOD[l, row:row + 1, 2 * D:3 * D], D)
            xp = TPool(ph, nc, "m_x", [128, 512], F32, 3)
            tp = TPool(ph, nc, "m_t2", [128, 512], F32, 2)
            xo = TPool(ph, nc, "m_xo", [128, 512], F32, 3)
            pso = TPool(ph, nc, "m_pso", [128, 512], F32, 3, psum=True)
            first = (l == 0)
            for nb in range(4):
                wo, r_wo = self.load_wblock(wop, self.w_o[l], nb * 512, 512)
                loaded = {}

                def load(i, nb=nb, loaded=loaded):
                    tile = tiles[i]
                    xt, r_xt = xp.next()
                    src = self.xsrc(first, s, tile)[:, nb * 512:(nb + 1) * 512]
                    S.dma("sp", lambda e, xt=xt, src=src: e.dma_start(out=xt[:], in_=src), reads=([] if first else [self.r_XR[s][tile]]), writes=[r_xt])
                    loaded[i] = (xt, r_xt)
                load(0)
                for i, tile in enumerate(tiles):
                    if i + 1 < len(tiles):
                        load(i + 1)
                    xt, r_xt = loaded.pop(i)
                    g_t, g_r = g1["ctx" if tile < cfg.NCT else "lat"]
                    ps, r_ps = pso.next()
                    for kc in range(KC):
                        S.op("pe", lambda e, ps=ps, kc=kc, tile=tile, wo=wo: e.matmul(ps[:], lhsT=BIG[:, kc, tile * 128:(tile + 1) * 128], rhs=wo[:, kc, :], start=(kc == 0), stop=(kc == KC - 1)),
                             reads=[r_BIG, r_wo], writes=[r_ps], signal=(kc == KC - 1))
                    t1, r_t1 = tp.next()
                    S.op("dve", lambda e, t1=t1, ps=ps, g_t=g_t, nb=nb: e.tensor_tensor(out=t1[:], in0=ps[:], in1=g_t[:, nb * 512:(nb + 1) * 512], op=ALU.mult), reads=[r_ps, g_r], writes=[r_t1])
                    xn, r_xn = xo.next()
                    S.op("dve", lambda e, xn=xn, t1=t1, xt=xt: e.tensor_tensor(out=xn[:], in0=t1[:], in1=xt[:], op=ALU.add), reads=[r_t1, r_xt], writes=[r_xn])
                    S.dma("sp", lambda e, xn=xn, tile=tile, nb=nb: e.dma_start(out=self.XR[s, tile * 128:(tile + 1) * 128, nb * 512:(nb + 1) * 512], in_=xn[:]), reads=[r_xn], writes=[self.r_XR[s][tile]])
            self.end_phase()

    def phase_swiglu(self, l, s):
        cfg, nc, S = self.cfg, self.nc, self.S
        D, KC, T, CTX, NCT = cfg.D, cfg.KC, cfg.T, cfg.CTX, cfg.NCT
        BIG, r_BIG = self.BIG
        moe = (l == 1)
        blocks = cfg.lat_blocks if moe else cfg.tblocks
        if moe:
            experts = [(self.moe_wg[0, e], self.moe_wu[0, e], self.moe_wd[0, e], cfg.DEXP, e) for e in range(8)]
        else:
            experts = [(self.ffn_wg[0], self.ffn_wu[0], self.ffn_wd[0], cfg.DFF, None)]
        GF = 256
        with ExitStack() as ph:
            g2 = {}
            for kind, row in (("lat", s), ("ctx", cfg.NSEQ)):
                if moe and kind == "ctx":
                    continue
                g2[kind] = tile1(ph, nc, "f_g2" + kind, [128, D], F32)
                self.bcast_row(g2[kind][0], g2[kind][1], self.MOD[l, row:row + 1, 5 * D:6 * D], D)
            comb = None
            if moe:
                comb = self.routing(ph, s)
            wgp = TPool(ph, nc, "f_wg", [128, KC, GF], BF16, 2)
            wup = TPool(ph, nc, "f_wu", [128, KC, GF], BF16, 2)
            wdp = TPool(ph, nc, "f_wd", [128, GF // 128, D], BF16, 2)
            if moe:
                sblocks = [[b] for b in blocks]
            else:
                sblocks = []
                SB = CTX + 512
                if T % SB == 0 and CTX % 128 == 0 and CTX <= 512:
                    for b0 in range(0, T, SB):
                        sblocks.append([(b0, CTX), (b0 + CTX, 512)] if b0 == 0 else [(b0, 512), (b0 + 512, CTX)])
                else:
                    sblocks = [[blocks[0], blocks[1]]] + [[b] for b in blocks[2:]]
            nacc = max(sum(n for (_, n) in sb) for sb in sblocks) // 128
            acc, r_acc = tile1(ph, nc, "f_acc", [128, nacc, D], F32)
            actp = TPool(ph, nc, "f_act", [128, GF // 128, 512], BF16, 2)
            sgp = TPool(ph, nc, "f_sg", [128, 512], F32, 2 if moe else 1)
            HW_ = D // 4
            xp = TPool(ph, nc, "f_x", [128, HW_], F32, 4)
            psg = TPool(ph, nc, "f_psg", [128, 512], F32, 2, psum=True)
            psu = TPool(ph, nc, "f_psu", [128, 512], F32, 2, psum=True)
            pso = TPool(ph, nc, "f_pso", [128, 512], F32, 2 if moe else 3, psum=True)
            for (wg2d, wu2d, wd2d, dff, eidx) in experts:
                ngr = dff // GF
                for sb in sblocks:
                  for gr in range(ngr):
                    wg, r_wg = self.load_wblock(wgp, wg2d, gr * GF, GF)
                    wu, r_wu = self.load_wblock(wup, wu2d, gr * GF, GF)
                    wd, r_wd = wdp.next()
                    for hlf in range(2):
                        srcd = wd2d[gr * GF:(gr + 1) * GF, hlf * 1024:(hlf + 1) * 1024].rearrange("(c p) n -> p c n", p=128)
                        S.dma("pool", lambda e, wd=wd, srcd=srcd, hlf=hlf: e.dma_start(out=wd[:, :, hlf * 1024:(hlf + 1) * 1024], in_=srcd), writes=[r_wd])
                    tb = 0
                    for (t0, n) in sb:
                        ntl = n // 128
                        act, r_act = actp.next()
                        for c in range(GF // 128):
                            pg, r_pg = psg.next()
                            pu, r_pu = psu.next()
                            for kc in range(KC):
                                S.op("pe", lambda e, pg=pg, wg=wg, kc=kc, c=c, t0=t0, n=n: e.matmul(pg[:, 0:n], lhsT=wg[:, kc, c * 128:(c + 1) * 128], rhs=BIG[:, kc, t0:t0 + n], start=(kc == 0), stop=(kc == KC - 1)),
                                     reads=[r_wg, r_BIG], writes=[r_pg], signal=(kc == KC - 1))
                            for kc in range(KC):
                                S.op("pe", lambda e, pu=pu, wu=wu, kc=kc, c=c, t0=t0, n=n: e.matmul(pu[:, 0:n], lhsT=wu[:, kc, c * 128:(c + 1) * 128], rhs=BIG[:, kc, t0:t0 + n], start=(kc == 0), stop=(kc == KC - 1)),
                                     reads=[r_wu, r_BIG], writes=[r_pu], signal=(kc == KC - 1))
                            sg, r_sg = sgp.next()
                            S.op("act", lambda e, sg=sg, pg=pg, n=n: e.activation(out=sg[:, 0:n], in_=pg[:, 0:n], func=AF.Silu), reads=[r_pg], writes=[r_sg])
                            S.op("dve", lambda e, act=act, sg=sg, pu=pu, c=c, n=n: e.tensor_tensor(out=act[:, c, 0:n], in0=sg[:, 0:n], in1=pu[:, 0:n], op=ALU.mult), reads=[r_sg, r_pu], writes=[r_act])
                        for tl in range(ntl):
                            ai = tb + tl
                            for nb in range(4):
                                po, r_po = pso.next()
                                nch = GF // 128
                                for c in range(nch):
                                    S.op("pe", lambda e, po=po, act=act, wd=wd, c=c, tl=tl, nb=nb, nch=nch: e.matmul(po[:], lhsT=act[:, c, tl * 128:(tl + 1) * 128], rhs=wd[:, c, nb * 512:(nb + 1) * 512], start=(c == 0), stop=(c == nch - 1)),
                                         reads=[r_act, r_wd], writes=[r_po], signal=(c == nch - 1))
                                if gr == 0:
                                    S.op("act", lambda e, po=po, ai=ai, nb=nb: e.copy(out=acc[:, ai, nb * 512:(nb + 1) * 512], in_=po[:]), reads=[r_po], writes=[r_acc])
                                else:
                                    S.op("dve", lambda e, po=po, ai=ai, nb=nb: e.tensor_tensor(out=acc[:, ai, nb * 512:(nb + 1) * 512], in0=po[:], in1=acc[:, ai, nb * 512:(nb + 1) * 512], op=ALU.add), reads=[r_po, r_acc], writes=[r_acc])
                        tb += ntl
                  tb = 0
                  for (t0, n) in sb:
                    ntl = n // 128
                    for tl_ in range(ntl):
                        tl = tb + tl_
                        tile = t0 // 128 + tl_
                        g_t, g_r = g2["ctx" if tile < NCT else "lat"]
                        xq = []
                        for hf in range(4):
                            c0 = hf * HW_
                            xt, r_xt = xp.next()
                            S.dma("sp", lambda e, xt=xt, tile=tile, c0=c0: e.dma_start(out=xt[:], in_=self.XR[s, tile * 128:(tile + 1) * 128, c0:c0 + HW_]), reads=[self.r_XR[s][tile]], writes=[r_xt])
                            xq.append((xt, r_xt))
                        for hf in range(4):
                            c0 = hf * HW_
                            xt, r_xt = xq[hf]
                            S.op("dve", lambda e, tl=tl, g_t=g_t, c0=c0: e.tensor_tensor(out=acc[:, tl, c0:c0 + HW_], in0=acc[:, tl, c0:c0 + HW_], in1=g_t[:, c0:c0 + HW_], op=ALU.mult), reads=[r_acc, g_r], writes=[r_acc])
                            if eidx is None:
                                S.op("dve", lambda e, tl=tl, xt=xt, c0=c0: e.tensor_tensor(out=xt[:], in0=acc[:, tl, c0:c0 + HW_], in1=xt[:], op=ALU.add), reads=[r_acc, r_xt], writes=[r_xt])
                            else:
                                cb, r_cb = comb
                                lt = tile - NCT
                                S.op("dve", lambda e, tl=tl, xt=xt, cb=cb, lt=lt, eidx=eidx, c0=c0: e.scalar_tensor_tensor(out=xt[:], in0=acc[:, tl, c0:c0 + HW_], scalar=cb[:, lt, eidx:eidx + 1], in1=xt[:], op0=ALU.mult, op1=ALU.add),
                                     reads=[r_acc, r_xt, r_cb], writes=[r_xt])
                            S.dma("sp", lambda e, xt=xt, tile=tile, c0=c0: e.dma_start(out=self.XR[s, tile * 128:(tile + 1) * 128, c0:c0 + HW_], in_=xt[:]), reads=[r_xt], writes=[self.r_XR[s][tile]])
                    tb += ntl
            self.end_phase()

    def routing(self, ph, s, want_sel=False):
        cfg, nc, S = self.cfg, self.nc, self.S
        D, KC, NCT = cfg.D, cfg.KC, cfg.NCT
        BIG, r_BIG = self.BIG
        NTl = cfg.SEQ // 128
        rf, r_rf = tile1(ph, nc, "r_rf", [128, KC, 8], F32)
        rh, r_rh = tile1(ph, nc, "r_rh", [128, KC, 8], BF16)
        rl, r_rl = tile1(ph, nc, "r_rl", [128, KC, 8], BF16)
        S.dma("sp", lambda e: e.dma_start(out=rf[:], in_=self.router[0].rearrange("(kc p) e -> p kc e", p=128)), writes=[r_rf])
        S.op("dve", lambda e: e.tensor_copy(out=rh[:], in_=rf[:]), reads=[r_rf], writes=[r_rh])
        S.op("dve", lambda e: e.tensor_tensor(out=rl[:], in0=rf[:], in1=rh[:], op=ALU.subtract), reads=[r_rf, r_rh], writes=[r_rl])
        lg, r_lg = tile1(ph, nc, "r_lg", [128, NTl, 8], F32)
        psl = TPool(ph, nc, "r_ps", [128, 8], F32, 2, psum=True)
        for lt in range(NTl):
            tile = NCT + lt
            ps, r_ps = psl.next()
            for i, (rt, rr) in enumerate(((rh, r_rh), (rl, r_rl))):
                for kc in range(KC):
                    S.op("pe", lambda e, ps=ps, rt=rt, kc=kc, tile=tile, i=i: e.matmul(ps[:], lhsT=BIG[:, kc, tile * 128:(tile + 1) * 128], rhs=rt[:, kc, :], start=(i == 0 and kc == 0), stop=(i == 1 and kc == KC - 1)),
                         reads=[r_BIG, rr], writes=[r_ps], signal=(i == 1 and kc == KC - 1))
            S.op("act", lambda e, ps=ps, lt=lt: e.copy(out=lg[:, lt, :], in_=ps[:]), reads=[r_ps], writes=[r_lg])
        shp = [128, NTl, 8]
        m1, r_m1 = tile1(ph, nc, "r_m1", [128, NTl], F32)
        m2, r_m2 = tile1(ph, nc, "r_m2", [128, NTl], F32)
        t8, r_t8 = tile1(ph, nc, "r_t8", shp, F32)
        l2, r_l2 = tile1(ph, nc, "r_l2", shp, F32)
        sel, r_sel = tile1(ph, nc, "r_sel", shp, F32)
        cb, r_cb = tile1(ph, nc, "r_cb", shp, F32)
        bc = lambda t: t[:].unsqueeze(2).broadcast_to(shp)
        S.op("dve", lambda e: e.tensor_reduce(out=m1[:], in_=lg[:], axis=AX.X, op=ALU.max), reads=[r_lg], writes=[r_m1])
        S.op("dve", lambda e: e.tensor_tensor(out=t8[:], in0=lg[:], in1=bc(m1), op=ALU.is_equal), reads=[r_lg, r_m1], writes=[r_t8])
        S.op("dve", lambda e: e.scalar_tensor_tensor(out=l2[:], in0=t8[:], scalar=-1e30, in1=lg[:], op0=ALU.mult, op1=ALU.add), reads=[r_t8, r_lg], writes=[r_l2])
        S.op("dve", lambda e: e.tensor_reduce(out=m2[:], in_=l2[:], axis=AX.X, op=ALU.max), reads=[r_l2], writes=[r_m2])
        S.op("dve", lambda e: e.tensor_tensor(out=sel[:], in0=lg[:], in1=bc(m2), op=ALU.is_ge), reads=[r_lg, r_m2], writes=[r_sel])
        S.op("dve", lambda e: e.tensor_tensor(out=t8[:], in0=lg[:], in1=bc(m1), op=ALU.subtract), reads=[r_lg, r_m1], writes=[r_t8])
        S.op("act", lambda e: e.activation(out=t8[:], in_=t8[:], func=AF.Exp), reads=[r_t8], writes=[r_t8])
        S.op("dve", lambda e: e.tensor_tensor(out=t8[:], in0=t8[:], in1=sel[:], op=ALU.mult), reads=[r_t8, r_sel], writes=[r_t8])
        S.op("dve", lambda e: e.tensor_tensor(out=m2[:], in0=m2[:], in1=m1[:], op=ALU.subtract), reads=[r_m2, r_m1], writes=[r_m2])
        S.op("act", lambda e: e.activation(out=m2[:], in_=m2[:], func=AF.Exp), reads=[r_m2], writes=[r_m2])
        S.op("dve", lambda e: e.tensor_scalar(out=m2[:], in0=m2[:], scalar1=1.0, scalar2=None, op0=ALU.add), reads=[r_m2], writes=[r_m2])
        S.op("dve", lambda e: e.reciprocal(out=m2[:], in_=m2[:]), reads=[r_m2], writes=[r_m2])
        S.op("dve", lambda e: e.tensor_tensor(out=cb[:], in0=t8[:], in1=bc(m2), op=ALU.mult), reads=[r_t8, r_m2], writes=[r_cb])
        if want_sel:
            return cb, r_cb, sel, r_sel
        return cb, r_cb

    def phase_final(self):
        cfg, nc, S = self.cfg, self.nc, self.S
        D, NCT = cfg.D, cfg.NCT
        with ExitStack() as ph:
            fn, r_fn = tile1(ph, nc, "z_fn", [128, D], F32)
            S.dma("sp", lambda e: e.dma_start(out=fn[:], in_=self.final_norm.unsqueeze(0).partition_broadcast(128)), writes=[r_fn])
            xp = TPool(ph, nc, "z_x", [128, D], F32, 3)
            op_ = TPool(ph, nc, "z_o", [128, D], F32, 2)
            junk, r_junk = tile1(ph, nc, "z_junk", [128, D], BF16)
            stp = TPool(ph, nc, "z_st", [128, 4], F32, 2)
            jobs = [(s, tile) for s in range(cfg.NSEQ) for tile in range(NCT, cfg.NT)]
            loaded = {}

            def load(i):
                s, tile = jobs[i]
                xt, r_xt = xp.next()
                S.dma("sp", lambda e, xt=xt, s=s, tile=tile: e.dma_start(out=xt[:], in_=self.XR[s, tile * 128:(tile + 1) * 128, :]), reads=[self.r_XR[s][tile]], writes=[r_xt])
                loaded[i] = (xt, r_xt)
            load(0)
            for i, (s, tile) in enumerate(jobs):
                if i + 1 < len(jobs):
                    load(i + 1)
                xt, r_xt = loaded.pop(i)
                st, r_st = stp.next()
                S.op("act", lambda e, xt=xt, st=st: e.activation(out=junk[:], in_=xt[:], func=AF.Square, accum_out=st[:, 0:1]), reads=[r_xt], writes=[r_junk, r_st])
                S.op("dve", lambda e, st=st: e.tensor_scalar(out=st[:, 1:2], in0=st[:, 0:1], scalar1=1.0 / D, scalar2=EPS, op0=ALU.mult, op1=ALU.add), reads=[r_st], writes=[r_st])
                S.op("act", lambda e, st=st: e.activation(out=st[:, 2:3], in_=st[:, 1:2], func=AF.Sqrt), reads=[r_st], writes=[r_st])
                S.op("dve", lambda e, st=st: e.reciprocal(out=st[:, 3:4], in_=st[:, 2:3]), reads=[r_st], writes=[r_st])
                ot, r_ot = op_.next()
                S.op("dve", lambda e, ot=ot, xt=xt, st=st: e.scalar_tensor_tensor(out=ot[:], in0=xt[:], scalar=st[:, 3:4], in1=fn[:], op0=ALU.mult, op1=ALU.mult), reads=[r_xt, r_st, r_fn], writes=[r_ot])
                lt = tile - NCT
                S.dma("sp", lambda e, ot=ot, s=s, lt=lt: e.dma_start(out=self.out[s, lt * 128:(lt + 1) * 128, :], in_=ot[:]), reads=[r_ot], writes=[self.r_out])
            self.end_phase()

    def prepass_jobs(self):
        cfg = self.cfg
        NG = cfg.DEXP // 512
        jobs = []
        for e_ in range(8):
            for g in range(NG):
                row0 = (e_ * NG + g) * 128
                for (dst_t, src_t) in ((self.WGB, self.moe_wg), (self.WUB, self.moe_wu)):
                    dst = dst_t[row0:row0 + 128, :].rearrange("p (kc n) -> p kc n", n=512)
                    src = src_t[0, e_, :, g * 512:(g + 1) * 512].rearrange("(kc p) n -> p kc n", p=128)
                    jobs.append((dst, src))
                for hlf in range(2):
                    dst = self.WDB[row0:row0 + 128, :].rearrange("p (c n) -> p c n", n=2048)[:, :, hlf * 1024:(hlf + 1) * 1024]
                    src = self.moe_wd[0, e_, g * 512:(g + 1) * 512, hlf * 1024:(hlf + 1) * 1024].rearrange("(c p) n -> p c n", p=128)
                    jobs.append((dst, src))
        return jobs

    def pump(self, k):
        if self.cfg.MOE != "routed":
            return
        for _ in range(k):
            if not self.pre_jobs:
                return
            dst, src = self.pre_jobs.pop(0)
            self.S.dma("pool", lambda e, dst=dst, src=src: e.dma_start(out=dst, in_=src))

    def route_local(self, s):
        cfg, nc, S = self.cfg, self.nc, self.S
        NTl = cfg.SEQ // 128
        with ExitStack() as ph:
            cb, r_cb, sel, r_sel = self.routing(ph, s, want_sel=True)
            S.dma("sp", lambda e: e.dma_start(out=self.SELD[:, s * NTl:(s + 1) * NTl, :], in_=sel[:]), reads=[r_sel], writes=[self.r_SELD])
            S.dma("sp", lambda e: e.dma_start(out=self.CBD[:, s * NTl:(s + 1) * NTl, :], in_=cb[:]), reads=[r_cb], writes=[self.r_SELD])
            self.end_phase()

    def phase_moe_routed(self):
        cfg, nc, S = self.cfg, self.nc, self.S
        D, KC, NCT = cfg.D, cfg.KC, cfg.NCT
        NTl = cfg.SEQ // 128
        NTg = cfg.NSEQ * NTl
        NSLOT = cfg.NSLOT
        NG = cfg.DEXP // 512
        ident, r_ident = self.C["ident"]
        ustrict, r_us = self.C["ustrict"]
        onesb, r_onesb = self.C["ones"]
        shp = [128, NTg, 8]
        self.pump(100000)
        with ExitStack() as ms:
            posA, r_posA = tile1(ms, nc, "q_posA", [128, NTg], I32)
            posB, r_posB = tile1(ms, nc, "q_posB", [128, NTg], I32)
            wA, r_wA = tile1(ms, nc, "q_wA", [128, NTg], F32)
            wB, r_wB = tile1(ms, nc, "q_wB", [128, NTg], F32)
            idxW, r_idxW = tile1(ms, nc, "q_idxW", [128, NSLOT, NG], I32)
            with ExitStack() as ph:
                sel, r_sel = tile1(ph, nc, "q_sel", shp, F32)
                cb, r_cb = tile1(ph, nc, "q_cb", shp, F32)
                selb, r_selb = tile1(ph, nc, "q_selb", shp, BF16)
                S.dma("sp", lambda e: e.dma_start(out=sel[:], in_=self.SELD), reads=[self.r_SELD], writes=[r_sel])
                S.dma("sp", lambda e: e.dma_start(out=cb[:], in_=self.CBD), reads=[self.r_SELD], writes=[r_cb])
                S.op("dve", lambda e: e.tensor_copy(out=selb[:], in_=sel[:]), reads=[r_sel], writes=[r_selb])
                pR, r_pR = tile1(ph, nc, "q_pR", [128, NTg * 8], F32, psum=True)
                pC, r_pC = tile1(ph, nc, "q_pC", [128, NTg * 8], F32, psum=True)
                flat = lambda t: t[:].rearrange("p g e -> p (g e)")
                S.op("pe", lambda e: e.matmul(pR[:], lhsT=ustrict[:], rhs=flat(selb), start=True, stop=True), reads=[r_us, r_selb], writes=[r_pR])
                S.op("pe", lambda e: e.matmul(pC[:], lhsT=onesb[:], rhs=flat(selb), start=True, stop=True), reads=[r_onesb, r_selb], writes=[r_pC])
                Cs, r_Cs = tile1(ph, nc, "q_Cs", shp, F32)
                incl, r_incl = tile1(ph, nc, "q_incl", shp, F32)
                pos, r_pos = tile1(ph, nc, "q_pos", shp, F32)
                v, r_v = tile1(ph, nc, "q_v", shp, F32)
                t8, r_t8 = tile1(ph, nc, "q_t8", shp, F32)
                o32, r_o32 = tile1(ph, nc, "q_o32", [128, NTg], F32)
                S.op("dve", lambda e: e.memset(o32[:], 1.0), writes=[r_o32])
                S.op("act", lambda e: e.copy(out=flat(Cs), in_=pC[:]), reads=[r_pC], writes=[r_Cs])
                for e_ in range(8):
                    S.op("dve", lambda e, e_=e_: e.tensor_tensor_scan(out=incl[:, :, e_], data0=o32[:], data1=Cs[:, :, e_], initial=0.0, op0=ALU.mult, op1=ALU.add),
                         reads=[r_o32, r_Cs], writes=[r_incl])
                sm = lambda name, w: tile1(ph, nc, name, [128, w], F32)
                n_e, r_n = sm("q_n", 8)
                np_e, r_np = sm("q_np", 8)
                base, r_base = sm("q_base", 8)
                cs, r_cs = sm("q_cs", 8)
                S.op("dve", lambda e: e.tensor_copy(out=n_e[:], in_=incl[:, NTg - 1, :]), reads=[r_incl], writes=[r_n])
                KMAX = max(1, (cfg.NSEQ * cfg.SEQ) // 512)
                kg, r_kg = tile1(ph, nc, "q_kg", [128, 8, KMAX], F32)
                S.dma("sp", lambda e: e.dma_start(out=kg[:], in_=self.cst_d["kgrid"].rearrange("p (e k) -> p e k", k=KMAX)), writes=[r_kg])
                S.op("dve", lambda e: e.tensor_tensor(out=kg[:], in0=n_e[:].unsqueeze(2).broadcast_to([128, 8, KMAX]), in1=kg[:], op=ALU.is_gt), reads=[r_n, r_kg], writes=[r_kg])
                S.op("dve", lambda e: e.tensor_reduce(out=np_e[:], in_=kg[:], axis=AX.X, op=ALU.add), reads=[r_kg], writes=[r_np])
                S.op("dve", lambda e: e.tensor_scalar(out=np_e[:], in0=np_e[:], scalar1=512.0, scalar2=None, op0=ALU.mult), reads=[r_np], writes=[r_np])
                S.op("dve", lambda e: e.memset(base[:], 0.0), writes=[r_base])
                for e_ in range(1, 8):
                    S.op("dve", lambda e, e_=e_: e.tensor_tensor(out=base[:, e_:e_ + 1], in0=base[:, e_ - 1:e_], in1=np_e[:, e_ - 1:e_], op=ALU.add), reads=[r_base, r_np], writes=[r_base])
                S.op("dve", lambda e: e.tensor_tensor(out=pos[:], in0=incl[:], in1=Cs[:], op=ALU.subtract), reads=[r_incl, r_Cs], writes=[r_pos])
                S.op("dve", lambda e: e.tensor_tensor(out=flat(pos), in0=pR[:], in1=flat(pos), op=ALU.add), reads=[r_pR, r_pos], writes=[r_pos])
                S.op("dve", lambda e: e.tensor_tensor(out=pos[:], in0=pos[:], in1=base[:].unsqueeze(1).broadcast_to(shp), op=ALU.add), reads=[r_pos, r_base], writes=[r_pos])
                S.op("dve", lambda e: e.scalar_tensor_tensor(out=v[:], in0=pos[:], scalar=1.0, in1=sel[:], op0=ALU.add, op1=ALU.mult), reads=[r_pos, r_sel], writes=[r_v])
                pA1, r_pA1 = sm("q_pA1", NTg)
                pB1, r_pB1 = sm("q_pB1", NTg)
                bc = lambda t: t[:].unsqueeze(2).broadcast_to(shp)
                S.op("dve", lambda e: e.tensor_reduce(out=pA1[:], in_=v[:], axis=AX.X, op=ALU.max), reads=[r_v], writes=[r_pA1])
                S.op("dve", lambda e: e.tensor_tensor(out=t8[:], in0=v[:], in1=bc(pA1), op=ALU.is_equal), reads=[r_v, r_pA1], writes=[r_t8])
                S.op("dve", lambda e: e.tensor_tensor(out=pos[:], in0=t8[:], in1=cb[:], op=ALU.mult), reads=[r_t8, r_cb], writes=[r_pos])
                S.op("dve", lambda e: e.tensor_reduce(out=wA[:], in_=pos[:], axis=AX.X, op=ALU.add), reads=[r_pos], writes=[r_wA])
                S.op("dve", lambda e: e.tensor_scalar(out=wB[:], in0=wA[:], scalar1=-1.0, scalar2=1.0, op0=ALU.mult, op1=ALU.add), reads=[r_wA], writes=[r_wB])
                S.op("dve", lambda e: e.tensor_tensor(out=t8[:], in0=t8[:], in1=v[:], op=ALU.mult), reads=[r_t8, r_v], writes=[r_t8])
                S.op("dve", lambda e: e.tensor_tensor(out=t8[:], in0=v[:], in1=t8[:], op=ALU.subtract), reads=[r_t8, r_v], writes=[r_t8])
                S.op("dve", lambda e: e.tensor_reduce(out=pB1[:], in_=t8[:], axis=AX.X, op=ALU.max), reads=[r_t8], writes=[r_pB1])
                S.op("dve", lambda e: e.tensor_scalar(out=posA[:], in0=pA1[:], scalar1=-1.0, scalar2=None, op0=ALU.add), reads=[r_pA1], writes=[r_posA])
                S.op("dve", lambda e: e.tensor_scalar(out=posB[:], in0=pB1[:], scalar1=-1.0, scalar2=None, op0=ALU.add), reads=[r_pB1], writes=[r_posB])
                S.op("dve", lambda e: e.tensor_tensor(out=cs[:], in0=base[:], in1=np_e[:], op=ALU.add), reads=[r_base, r_np], writes=[r_cs])
                jg, r_jg = tile1(ph, nc, "q_jg", [128, NSLOT, 8], F32)
                gp, r_gp = tile1(ph, nc, "q_gp", [128, NG], F32)
                S.dma("sp", lambda e: e.dma_start(out=jg[:], in_=self.cst_d["jgrid"].rearrange("p (j e) -> p j e", e=8)), writes=[r_jg])
                S.dma("sp", lambda e: e.dma_start(out=gp[:], in_=self.cst_d["gp"][:, :]), writes=[r_gp])
                S.op("dve", lambda e: e.tensor_tensor(out=jg[:], in0=cs[:].unsqueeze(1).broadcast_to([128, NSLOT, 8]), in1=jg[:], op=ALU.is_le), reads=[r_cs, r_jg], writes=[r_jg])
                eid, r_eid = sm("q_eid", NSLOT)
                S.op("dve", lambda e: e.tensor_reduce(out=eid[:], in_=jg[:], axis=AX.X, op=ALU.add), reads=[r_jg], writes=[r_eid])
                S.op("dve", lambda e: e.tensor_scalar(out=eid[:], in0=eid[:], scalar1=7.0, scalar2=None, op0=ALU.min), reads=[r_eid], writes=[r_eid])
                S.op("dve", lambda e: e.scalar_tensor_tensor(out=idxW[:], in0=eid[:].unsqueeze(2).broadcast_to([128, NSLOT, NG]), scalar=float(NG * 128), in1=gp[:].unsqueeze(1).broadcast_to([128, NSLOT, NG]), op0=ALU.mult, op1=ALU.add),
                     reads=[r_eid, r_gp], writes=[r_idxW])
                if cfg.DEBUG:
                    self.dump("posA", posA, r_posA, [128, NTg], I32)
                    self.dump("posB", posB, r_posB, [128, NTg], I32)
                    self.dump("wA", wA, r_wA, [128, NTg], F32)
                    self.dump("idxW", idxW, r_idxW, [128, NSLOT, NG], I32)
                    self.dump("eid", eid, r_eid, [128, NSLOT], F32)
                    self.dump("cs", cs, r_cs, [128, 8], F32)
                    self.dump("jg", jg, r_jg, [128, NSLOT, 8], F32)
                    self.dump("incl", incl, r_incl, shp, F32)
                    self.dump("Cs", Cs, r_Cs, shp, F32)
                    self.dump("np", np_e, r_np, [128, 8], F32)
                    self.dump("base", base, r_base, [128, 8], F32)
                self.end_phase()
                if cfg.STOP == "route":
                    return
            with ExitStack() as ph:
                hp = TPool(ph, nc, "q_h", [128, D], BF16, 3)
                for g in range(NTg):
                    ht, r_ht = hp.next()
                    S.dma("sp", lambda e, ht=ht, g=g: e.dma_start(out=ht[:], in_=self.H2[g * 128:(g + 1) * 128, :]), reads=[self.r_H2], writes=[r_ht])
                    for (pt, pr) in ((posA, r_posA), (posB, r_posB)):
                        S.dma("pool", lambda e, ht=ht, g=g, pt=pt: e.indirect_dma_start(out=self.HS, out_offset=bass.IndirectOffsetOnAxis(ap=pt[:, g:g + 1], axis=0), in_=ht[:], in_offset=None),
                              reads=[r_ht, pr], writes=[self.r_HS])
                self.end_phase()
                if cfg.STOP == "scatter":
                    return
            with ExitStack() as ph:
                hrp = TPool(ph, nc, "q_hr", [128, D], BF16, 2)
                hTp = TPool(ph, nc, "q_hT", [128, KC, 512], BF16, 2)
                wgp = TPool(ph, nc, "q_wg", [128, KC * 512], BF16, 2)
                wup = TPool(ph, nc, "q_wu", [128, KC * 512], BF16, 2)
                wdp = TPool(ph, nc, "q_wd", [128, 4 * D], BF16, 2)
                acc, r_acc = tile1(ph, nc, "q_acc", [128, 4, D], F32)
                actp = TPool(ph, nc, "q_act", [128, 4, 512], BF16, 2)
                sgp = TPool(ph, nc, "q_sg", [128, 512], F32, 2)
                psT = TPool(ph, nc, "q_psT", [128, KC, 128], BF16, 1, psum=True)
                psg = TPool(ph, nc, "q_psg", [128, 512], F32, 2, psum=True)
                psu = TPool(ph, nc, "q_psu", [128, 512], F32, 2, psum=True)
                pso = TPool(ph, nc, "q_pso", [128, 512], F32, 2, psum=True)
                def prep_slot(j):
                    hT, r_hT = hTp.next()
                    for tl in range(4):
                        hr, r_hr = hrp.next()
                        S.dma("sp", lambda e, hr=hr, j=j, tl=tl: e.dma_start(out=hr[:], in_=self.HS[j * 512 + tl * 128:j * 512 + (tl + 1) * 128, :]), reads=[self.r_HS], writes=[r_hr])
                        ps, r_ps = psT.next()
                        for kc in range(KC):
                            S.op("pe", lambda e, ps=ps, hr=hr, kc=kc: e.transpose(out=ps[:, kc, :], in_=hr[:, kc * 128:(kc + 1) * 128], identity=ident[:]),
                                 reads=[r_hr, r_ident], writes=[r_ps], signal=(kc == KC - 1))
                        S.op("act", lambda e, ps=ps, hT=hT, tl=tl: e.copy(out=hT[:, :, tl * 128:(tl + 1) * 128], in_=ps[:]), reads=[r_ps], writes=[r_hT])
                    return hT, r_hT

                nxt = prep_slot(0)
                pend_down = None
                for j in range(NSLOT):
                    hT, r_hT = nxt
                    for gr in range(NG):
                        if gr == max(NG - 2, 0) and j + 1 < NSLOT:
                            nxt = prep_slot(j + 1)
                        wts = []
                        for (pool_, src_) in ((wgp, self.WGB), (wup, self.WUB), (wdp, self.WDB)):
                            wt, r_wt = pool_.next()
                            S.dma("pool", lambda e, wt=wt, src_=src_, j=j, gr=gr: e.indirect_dma_start(out=wt[:], out_offset=None, in_=src_, in_offset=bass.IndirectOffsetOnAxis(ap=idxW[:, j, gr:gr + 1], axis=0)),
                                  reads=[r_idxW, self.r_WB], writes=[r_wt])
                            wts.append((wt, r_wt))
                        (wg_, r_wg), (wu_, r_wu), (wd_, r_wd) = wts
                        wg = wg_[:].rearrange("p (kc n) -> p kc n", n=512)
                        wu = wu_[:].rearrange("p (kc n) -> p kc n", n=512)
                        wd = wd_[:].rearrange("p (c n) -> p c n", n=D)
                        act, r_act = actp.next()
                        for c in range(4):
                            pg, r_pg = psg.next()
                            pu, r_pu = psu.next()
                            for kc in range(KC):
                                S.op("pe", lambda e, pg=pg, wg=wg, kc=kc, c=c, hT=hT: e.matmul(pg[:], lhsT=wg[:, kc, c * 128:(c + 1) * 128], rhs=hT[:, kc, :], start=(kc == 0), stop=(kc == KC - 1)),
                                     reads=[r_wg, r_hT], writes=[r_pg], signal=(kc == KC - 1))
                            for kc in range(KC):
                                S.op("pe", lambda e, pu=pu, wu=wu, kc=kc, c=c, hT=hT: e.matmul(pu[:], lhsT=wu[:, kc, c * 128:(c + 1) * 128], rhs=hT[:, kc, :], start=(kc == 0), stop=(kc == KC - 1)),
                                     reads=[r_wu, r_hT], writes=[r_pu], signal=(kc == KC - 1))
                            sg, r_sg = sgp.next()
                            S.op("act", lambda e, sg=sg, pg=pg: e.activation(out=sg[:], in_=pg[:], func=AF.Silu), reads=[r_pg], writes=[r_sg])
                            S.op("dve", lambda e, act=act, sg=sg, pu=pu, c=c: e.tensor_tensor(out=act[:, c, :], in0=sg[:], in1=pu[:], op=ALU.mult), reads=[r_sg, r_pu], writes=[r_act])
                        def down(j=j, gr=gr, act=act, r_act=r_act, wd=wd, r_wd=r_wd):
                            for tl in range(4):
                                for nb in range(4):
                                    po, r_po = pso.next()
                                    for c in range(4):
                                        S.op("pe", lambda e, po=po, act=act, wd=wd, c=c, tl=tl, nb=nb: e.matmul(po[:], lhsT=act[:, c, tl * 128:(tl + 1) * 128], rhs=wd[:, c, nb * 512:(nb + 1) * 512], start=(c == 0), stop=(c == 3)),
                                             reads=[r_act, r_wd], writes=[r_po], signal=(c == 3))
                                    if gr == 0:
                                        S.op("act", lambda e, po=po, tl=tl, nb=nb: e.copy(out=acc[:, tl, nb * 512:(nb + 1) * 512], in_=po[:]), reads=[r_po], writes=[r_acc])
                                    else:
                                        S.op("dve", lambda e, po=po, tl=tl, nb=nb: e.tensor_tensor(out=acc[:, tl, nb * 512:(nb + 1) * 512], in0=po[:], in1=acc[:, tl, nb * 512:(nb + 1) * 512], op=ALU.add), reads=[r_po, r_acc], writes=[r_acc])
                                if gr == NG - 1:
                                    S.dma("sp", lambda e, j=j, tl=tl: e.dma_start(out=self.YP[j * 512 + tl * 128:j * 512 + (tl + 1) * 128, :], in_=acc[:, tl, :]), reads=[r_acc], writes=[self.r_YP])
                        if pend_down is not None:
                            pend_down()
                        pend_down = down
                if pend_down is not None:
                    pend_down()
                self.end_phase()
            if cfg.STOP == "slots":
                return
            with ExitStack() as ph:
                fn, r_fn = tile1(ph, nc, "z_fn", [128, D], F32)
                S.dma("sp", lambda e: e.dma_start(out=fn[:], in_=self.final_norm.unsqueeze(0).partition_broadcast(128)), writes=[r_fn])
                g2 = []
                for s in range(cfg.NSEQ):
                    g2.append(tile1(ph, nc, "z_g2%d" % s, [128, D], F32))
                    self.bcast_row(g2[s][0], g2[s][1], self.MOD[1, s:s + 1, 5 * D:6 * D], D)
                xp = TPool(ph, nc, "z_x", [128, D], F32, 2)
                yap = TPool(ph, nc, "z_ya", [128, D], F32, 2)
                ybp = TPool(ph, nc, "z_yb", [128, D], F32, 2)
                op_ = TPool(ph, nc, "z_o", [128, D], F32, 2)
                junk, r_junk = tile1(ph, nc, "z_junk", [128, D], BF16)
                stp = TPool(ph, nc, "z_st", [128, 4], F32, 2)
                for g in range(NTg):
                    s, lt = g // NTl, g % NTl
                    tile = NCT + lt
                    xt, r_xt = xp.next()
                    S.dma("sp", lambda e, xt=xt, s=s, tile=tile: e.dma_start(out=xt[:], in_=self.XR[s, tile * 128:(tile + 1) * 128, :]), reads=[self.r_XR[s][tile]], writes=[r_xt])
                    ya, r_ya = yap.next()
                    yb, r_yb = ybp.next()
                    for (yt, r_yt, pt, pr) in ((ya, r_ya, posA, r_posA), (yb, r_yb, posB, r_posB)):
                        S.dma("pool", lambda e, yt=yt, pt=pt, g=g: e.indirect_dma_start(out=yt[:], out_offset=None, in_=self.YP, in_offset=bass.IndirectOffsetOnAxis(ap=pt[:, g:g + 1], axis=0)),
                              reads=[self.r_YP, pr], writes=[r_yt])
                    S.op("dve", lambda e, ya=ya, g=g: e.tensor_scalar(out=ya[:], in0=ya[:], scalar1=wA[:, g:g + 1], scalar2=None, op0=ALU.mult), reads=[r_ya, r_wA], writes=[r_ya])
                    S.op("dve", lambda e, ya=ya, yb=yb, g=g: e.scalar_tensor_tensor(out=ya[:], in0=yb[:], scalar=wB[:, g:g + 1], in1=ya[:], op0=ALU.mult, op1=ALU.add), reads=[r_ya, r_yb, r_wB], writes=[r_ya])
                    S.op("dve", lambda e, ya=ya, s=s: e.tensor_tensor(out=ya[:], in0=ya[:], in1=g2[s][0][:], op=ALU.mult), reads=[r_ya, g2[s][1]], writes=[r_ya])
                    S.op("dve", lambda e, ya=ya, xt=xt: e.tensor_tensor(out=xt[:], in0=ya[:], in1=xt[:], op=ALU.add), reads=[r_ya, r_xt], writes=[r_xt])
                    st, r_st = stp.next()
                    S.op("act", lambda e, xt=xt, st=st: e.activation(out=junk[:], in_=xt[:], func=AF.Square, accum_out=st[:, 0:1]), reads=[r_xt], writes=[r_junk, r_st])
                    S.op("dve", lambda e, st=st: e.tensor_scalar(out=st[:, 1:2], in0=st[:, 0:1], scalar1=1.0 / D, scalar2=EPS, op0=ALU.mult, op1=ALU.add), reads=[r_st], writes=[r_st])
                    S.op("act", lambda e, st=st: e.activation(out=st[:, 2:3], in_=st[:, 1:2], func=AF.Sqrt), reads=[r_st], writes=[r_st])
                    S.op("dve", lambda e, st=st: e.reciprocal(out=st[:, 3:4], in_=st[:, 2:3]), reads=[r_st], writes=[r_st])
                    ot, r_ot = op_.next()
                    S.op("dve", lambda e, ot=ot, xt=xt, st=st: e.scalar_tensor_tensor(out=ot[:], in0=xt[:], scalar=st[:, 3:4], in1=fn[:], op0=ALU.mult, op1=ALU.mult), reads=[r_xt, r_st, r_fn], writes=[r_ot])
                    S.dma("sp", lambda e, ot=ot, s=s, lt=lt: e.dma_start(out=self.out[s, lt * 128:(lt + 1) * 128, :], in_=ot[:]), reads=[r_ot], writes=[self.r_out])
                self.end_phase()


def make_in_maps(cfg, inputs, ncores):
    consts = host_consts(cfg)
    maps = []
    for core in range(ncores):
        b0 = core * cfg.NSEQ
        m = {}
        m["x_in"] = np.ascontiguousarray(inputs["x"][b0:b0 + cfg.NSEQ])
        m["ctx_in"] = np.ascontiguousarray(inputs["ctx"][b0:b0 + cfg.NSEQ])
        m["cvec"] = np.ascontiguousarray(np.concatenate([inputs["c"][b0:b0 + cfg.NSEQ], inputs["c_ctx"][None, :]], 0))
        for k in ("w_mod", "b_mod", "norm_mix", "norm_ffn", "w_in", "hg_lb_fwd", "hg_lb_bwd", "hg_norm", "attn_sink",
                  "w_branch_a", "w_branch_b", "w_out", "ffn_w_gate", "ffn_w_up", "ffn_w_down", "moe_router",
                  "moe_w_gate", "moe_w_up", "moe_w_down", "final_norm"):
            m[k] = inputs[k]
        for k, v in consts.items():
            m["c_" + k] = v
        maps.append(m)
    return maps


_CACHE = {}


def kernel(**inputs):
    cfg = Cfg()
    inputs = {k: np.asarray(v) for k, v in inputs.items()}
    if "nc" not in _CACHE:
        _CACHE["nc"] = K(cfg).build()
    nc = _CACHE["nc"]
    ncores = 16 // cfg.NSEQ
    maps = make_in_maps(cfg, inputs, ncores)
    res = run_bass_kernel_spmd(nc, maps, core_ids=list(range(ncores)))
    out = np.concatenate([np.asarray(r["out"]) for r in res.results], axis=0)
    return out.astype(np.float32, copy=False)
```
